# Optimizing a Trainium2 kernel written in Bass

```python
import jax
import jax.numpy as jnp
from jax import lax
import numpy as np

D_MODEL = 1024
BATCH = 2
SEQ = 16384
DEPTH = 2

N_MIXERS = 2
N_META = 16
BLOCK = 128
EPS = 1e-6
NEG_INF = -1e30

FOX_HEADS = 16
FOX_HEAD_DIM = D_MODEL // FOX_HEADS
FOX_IN = 3 * D_MODEL + FOX_HEADS
FOX_FORGET_BIAS = 2.0

HG_HEADS = 8
HG_KEY_DIM = 128
HG_VAL_DIM = D_MODEL // HG_HEADS
HG_KEY_TOTAL = HG_HEADS * HG_KEY_DIM
HG_IN = 2 * HG_KEY_TOTAL + 2 * D_MODEL

N_GROUPS = 4
EXPERTS_PER_GROUP = 8
N_EXPERTS = N_GROUPS * EXPERTS_PER_GROUP
TOP_K = 2
D_EXPERT = D_MODEL // 2
EXPERT_ROW_BLOCK = 128

kernel_name = 'hybrid_fox_hgrn2_hmoe'


def rms_norm(x, gain):
    xf = x.astype(jnp.float32)
    y = xf * lax.rsqrt(jnp.mean(xf * xf, axis=-1, keepdims=True) + EPS)
    return (y * gain.astype(jnp.float32)).astype(x.dtype)


def front_pad(t, pad):
    widths = [(0, 0)] * t.ndim
    widths[1] = (pad, 0)
    return jnp.pad(t, widths)


def fox_attention(h, w_in, b_f, g_q, g_k, w_out):
    bsz, length, _ = h.shape
    q, k, v, f_logit = jnp.split(h @ w_in, [D_MODEL, 2 * D_MODEL, 3 * D_MODEL], axis=-1)
    q = rms_norm(q.reshape(bsz, length, FOX_HEADS, FOX_HEAD_DIM), g_q)
    k = rms_norm(k.reshape(bsz, length, FOX_HEADS, FOX_HEAD_DIM), g_k)
    v = v.reshape(bsz, length, FOX_HEADS, FOX_HEAD_DIM)
    log_f = jax.nn.log_sigmoid(f_logit.astype(jnp.float32) + b_f.astype(jnp.float32))
    pad = (-length) % BLOCK
    q, k, v, log_f = (front_pad(t, pad) for t in (q, k, v, log_f))
    lp = length + pad
    n_blk = lp // BLOCK
    cum = jnp.cumsum(log_f, axis=1).transpose(0, 2, 1)
    kh = k.transpose(0, 2, 1, 3)
    vh = v.transpose(0, 2, 1, 3)
    qb = q.reshape(bsz, n_blk, BLOCK, FOX_HEADS, FOX_HEAD_DIM).transpose(1, 0, 3, 2, 4)
    cb = cum.reshape(bsz, FOX_HEADS, n_blk, BLOCK).transpose(2, 0, 1, 3)
    key_pos = jnp.arange(lp)
    key_ok = key_pos >= pad
    scale = FOX_HEAD_DIM ** -0.5

    def attend(args):
        qi, ci, bi = args
        s = jnp.einsum('bhqd,bhkd->bhqk', qi, kh).astype(jnp.float32) * scale
        s = s + ci[..., :, None] - cum[:, :, None, :]
        q_pos = bi * BLOCK + jnp.arange(BLOCK)
        mask = (key_pos[None, :] <= q_pos[:, None]) & key_ok[None, :]
        p = jax.nn.softmax(jnp.where(mask, s, NEG_INF), axis=-1)
        return jnp.einsum('bhqk,bhkd->bhqd', p.astype(vh.dtype), vh)

    o = lax.map(attend, (qb, cb, jnp.arange(n_blk)))
    o = o.transpose(1, 0, 3, 2, 4).reshape(bsz, lp, D_MODEL)[:, pad:]
    return o @ w_out


def hgrn_lower_bounds(lb_logits):
    p = jax.nn.softmax(lb_logits.astype(jnp.float32), axis=0)
    cum = jnp.cumsum(p, axis=0)
    return cum - cum[:1]


def hgrn2_recurrence(h, w_in, b_f, lower_bound, g_o, w_out):
    bsz, length, _ = h.shape
    q, f_logit, v, g = jnp.split(
        h @ w_in, [HG_KEY_TOTAL, 2 * HG_KEY_TOTAL, 2 * HG_KEY_TOTAL + D_MODEL], axis=-1)
    f = lower_bound + (1.0 - lower_bound) * jax.nn.sigmoid(
        f_logit.astype(jnp.float32) + b_f.astype(jnp.float32))
    log_f = jnp.log(f)
    k = 1.0 - f
    q = jax.nn.silu(q.astype(jnp.float32))
    v = v.astype(jnp.float32)
    pad = (-length) % BLOCK
    lp = length + pad
    n_chk = lp // BLOCK

    def to_chunks(t, d):
        t = front_pad(t, pad).reshape(bsz, n_chk, BLOCK, HG_HEADS, d)
        return t.transpose(1, 0, 3, 2, 4)

    qc = to_chunks(q, HG_KEY_DIM)
    kc = to_chunks(k, HG_KEY_DIM)
    vc = to_chunks(v, HG_VAL_DIM)
    lc = to_chunks(log_f, HG_KEY_DIM)
    causal = jnp.tril(jnp.ones((BLOCK, BLOCK), dtype=bool))

    def step(state, args):
        qi, ki, vi, li = args
        b = jnp.cumsum(li, axis=2)
        o_inter = jnp.einsum('bhtk,bhkv->bhtv', qi * jnp.exp(b), state)
        diff = b[:, :, :, None, :] - b[:, :, None, :, :]
        decay = jnp.exp(jnp.where(causal[:, :, None], diff, -jnp.inf))
        scores = jnp.einsum('bhtk,bhsk,bhtsk->bhts', qi, ki, decay)
        o_intra = jnp.einsum('bhts,bhsv->bhtv', scores, vi)
        b_end = b[:, :, -1:, :]
        state = (jnp.exp(b_end[:, :, 0, :])[..., None] * state
                 + jnp.einsum('bhsk,bhsv->bhkv', ki * jnp.exp(b_end - b), vi))
        return state, o_inter + o_intra

    s0 = jnp.zeros((bsz, HG_HEADS, HG_KEY_DIM, HG_VAL_DIM), jnp.float32)
    _, o = lax.scan(step, s0, (qc, kc, vc, lc))
    o = o.transpose(1, 0, 3, 2, 4).reshape(bsz, lp, HG_HEADS, HG_VAL_DIM)[:, pad:]
    o = rms_norm(o, g_o).reshape(bsz, length, D_MODEL) * jax.nn.silu(g.astype(jnp.float32))
    return o.astype(h.dtype) @ w_out


def hier_moe(h, w_grp, b_grp, w_rt, b_rt, w_up, w_down):
    bsz, length, dm = h.shape
    n_tok = bsz * length
    n_asg = n_tok * TOP_K
    xt = h.reshape(n_tok, dm)
    g_prob = jax.nn.softmax((xt @ w_grp).astype(jnp.float32) + b_grp.astype(jnp.float32), axis=-1)
    g_gate, g_idx = lax.top_k(g_prob, 1)
    e_logit = ((xt @ w_rt).astype(jnp.float32) + b_rt.astype(jnp.float32)).reshape(
        n_tok, N_GROUPS, EXPERTS_PER_GROUP)
    e_logit = jnp.take_along_axis(e_logit, g_idx[:, :, None], axis=1)[:, 0]
    e_val, e_idx = lax.top_k(e_logit, TOP_K)
    gate = jax.nn.softmax(e_val, axis=-1) * g_gate
    expert = (g_idx * EXPERTS_PER_GROUP + e_idx).reshape(-1)
    order = jnp.argsort(expert)
    se = expert[order]
    tok = order // TOP_K
    w_sorted = gate.reshape(-1)[order]
    counts = jnp.bincount(expert, length=N_EXPERTS)
    padded = (counts + EXPERT_ROW_BLOCK - 1) // EXPERT_ROW_BLOCK * EXPERT_ROW_BLOCK
    start = jnp.cumsum(counts) - counts
    pend = jnp.cumsum(padded)
    pstart = pend - padded
    dest = pstart[se] + jnp.arange(n_asg) - start[se]
    n_blocks = (n_asg + N_EXPERTS * (EXPERT_ROW_BLOCK - 1) + EXPERT_ROW_BLOCK - 1) // EXPERT_ROW_BLOCK
    rows = n_blocks * EXPERT_ROW_BLOCK
    x_pad = jnp.zeros((rows, dm), h.dtype).at[dest].set(xt[tok])
    block_expert = jnp.minimum(
        jnp.searchsorted(pend, jnp.arange(n_blocks) * EXPERT_ROW_BLOCK, side='right'), N_EXPERTS - 1)

    def expert_block(args):
        xb, e = args
        gate_up = xb @ w_up[e]
        a, u = jnp.split(gate_up, 2, axis=-1)
        return (jax.nn.silu(a) * u) @ w_down[e]

    y_pad = lax.map(expert_block, (x_pad.reshape(n_blocks, EXPERT_ROW_BLOCK, dm), block_expert))
    y = y_pad.reshape(rows, dm)[dest] * w_sorted[:, None].astype(h.dtype)
    return jnp.zeros_like(xt).at[tok].add(y).reshape(bsz, length, dm)


def setup_inputs(seed: int = 0) -> dict:
    key = jax.random.key(seed)
    ks = jax.random.split(key, 24)
    n_fox = (DEPTH + N_MIXERS - 1) // N_MIXERS
    n_hg = DEPTH // N_MIXERS
    out_scale = (2 * DEPTH) ** -0.5

    def nrm(k, shape, scale):
        return jax.random.normal(k, shape, jnp.float32) * scale

    def gain(k, shape):
        return 1.0 + nrm(k, shape, 0.02)

    return {
        'x': nrm(ks[0], (BATCH, SEQ, D_MODEL), 1.0),
        'meta_tokens': nrm(ks[1], (N_META, D_MODEL), 1.0),
        'fox_norm': gain(ks[2], (n_fox, D_MODEL)),
        'fox_w_in': nrm(ks[3], (n_fox, D_MODEL, FOX_IN), D_MODEL ** -0.5),
        'fox_b_f': FOX_FORGET_BIAS + nrm(ks[4], (n_fox, FOX_HEADS), 0.1),
        'fox_q_norm': gain(ks[5], (n_fox, FOX_HEAD_DIM)),
        'fox_k_norm': gain(ks[6], (n_fox, FOX_HEAD_DIM)),
        'fox_w_out': nrm(ks[7], (n_fox, D_MODEL, D_MODEL), D_MODEL ** -0.5 * out_scale),
        'hg_norm': gain(ks[8], (n_hg, D_MODEL)),
        'hg_w_in': nrm(ks[9], (n_hg, D_MODEL, HG_IN), D_MODEL ** -0.5),
        'hg_b_f': nrm(ks[10], (n_hg, HG_KEY_TOTAL), 0.02),
        'hg_lb_logits': nrm(ks[11], (DEPTH, HG_KEY_TOTAL), 0.5),
        'hg_o_norm': gain(ks[12], (n_hg, HG_VAL_DIM)),
        'hg_w_out': nrm(ks[13], (n_hg, D_MODEL, D_MODEL), D_MODEL ** -0.5 * out_scale),
        'moe_norm': gain(ks[14], (DEPTH, D_MODEL)),
        'moe_w_grp': nrm(ks[15], (DEPTH, D_MODEL, N_GROUPS), D_MODEL ** -0.5),
        'moe_b_grp': nrm(ks[16], (DEPTH, N_GROUPS), 0.01),
        'moe_w_rt': nrm(ks[17], (DEPTH, D_MODEL, N_EXPERTS), D_MODEL ** -0.5),
        'moe_b_rt': nrm(ks[18], (DEPTH, N_EXPERTS), 0.01),
        'moe_w_up': nrm(ks[19], (DEPTH, N_EXPERTS, D_MODEL, 2 * D_EXPERT), D_MODEL ** -0.5),
        'moe_w_down': nrm(ks[20], (DEPTH, N_EXPERTS, D_EXPERT, D_MODEL), D_EXPERT ** -0.5 * out_scale),
    }


def reference(x, meta_tokens, fox_norm, fox_w_in, fox_b_f, fox_q_norm, fox_k_norm, fox_w_out,
              hg_norm, hg_w_in, hg_b_f, hg_lb_logits, hg_o_norm, hg_w_out,
              moe_norm, moe_w_grp, moe_b_grp, moe_w_rt, moe_b_rt, moe_w_up, moe_w_down):
    bsz = x.shape[0]
    meta = jnp.broadcast_to(meta_tokens[None].astype(x.dtype), (bsz, N_META, D_MODEL))
    h = jnp.concatenate([meta, x], axis=1)
    lower_bounds = hgrn_lower_bounds(hg_lb_logits)
    for i in range(DEPTH):
        j = i // N_MIXERS
        if i % N_MIXERS == 0:
            h = h + fox_attention(rms_norm(h, fox_norm[j]), fox_w_in[j], fox_b_f[j],
                                  fox_q_norm[j], fox_k_norm[j], fox_w_out[j])
        else:
            h = h + hgrn2_recurrence(rms_norm(h, hg_norm[j]), hg_w_in[j], hg_b_f[j],
                                     lower_bounds[i], hg_o_norm[j], hg_w_out[j])
        h = h + hier_moe(rms_norm(h, moe_norm[i]), moe_w_grp[i], moe_b_grp[i],
                         moe_w_rt[i], moe_b_rt[i], moe_w_up[i], moe_w_down[i])
    return h[:, N_META:]
```

```python
import contextlib
import numpy as np
import ml_dtypes
import concourse.bass as bass
import concourse.mybir as mybir
from concourse.bass_utils import run_bass_kernel_spmd

F32 = mybir.dt.float32
BF16 = mybir.dt.bfloat16
I32 = mybir.dt.int32
U32 = mybir.dt.uint32
AF = mybir.ActivationFunctionType
ALU = mybir.AluOpType
AX = mybir.AxisListType
NPBF = ml_dtypes.bfloat16

D = 1024
NCORES = 8
SEQ = 16384
NMETA = 16
BLK = 128
EPS = 1e-6
FH = 16
FD = 64
HH = 8
NE = 32
CAP = 384
DE = 512
NTX = 32
NT = NTX + 1
LP = SEQ + BLK
NB = LP // BLK


class Dep:
    __slots__ = ("w", "r", "name", "ro")

    def __init__(self, name=""):
        self.w = None
        self.r = []
        self.name = name
        self.ro = False


class _E:
    def __init__(self, name, eng, sem):
        self.name = name
        self.eng = eng
        self.sem = sem
        self.n = 0
        self.waited = {}
        self.pool = []
        self.pool_i = 0


class KB:
    def __init__(self, nc, dma_pool=(("sp", 16), ("pool", 16), ("act", 4)), same_eng_sync=True):
        self.nc = nc
        self.stacks = [contextlib.ExitStack()]
        self.same = same_eng_sync
        self.e = {}
        for name, eng in (("pe", nc.tensor), ("dve", nc.vector), ("act", nc.scalar),
                          ("pool", nc.gpsimd), ("sp", nc.sync)):
            sem = self.stacks[0].enter_context(nc.semaphore("c_" + name))
            self.e[name] = _E(name, eng, sem)
        for q, n in dma_pool:
            for i in range(n):
                sem = self.stacks[0].enter_context(nc.semaphore("d_%s%d" % (q, i)))
                self.e[q].pool.append([sem, 0])
        self.ninst = 0
        self.uid = 0

    def sb(self, name, shape, dt):
        self.uid += 1
        return self.stacks[-1].enter_context(self.nc.sbuf_tensor("%s_%d" % (name, self.uid), list(shape), dt))

    def ps(self, name, shape, dt):
        self.uid += 1
        return self.stacks[-1].enter_context(self.nc.psum_tensor("%s_%d" % (name, self.uid), list(shape), dt))

    def ring(self, name, shape, dt, n, psum=False):
        out = []
        for i in range(n):
            t = (self.ps if psum else self.sb)("%s%d" % (name, i), shape, dt)
            out.append((t, Dep("%s%d" % (name, i))))
        return out

    @contextlib.contextmanager
    def scope(self):
        self.stacks.append(contextlib.ExitStack())
        try:
            yield
        finally:
            self.barrier()
            self.stacks.pop().close()

    def _wait(self, E, ev, own_ok=False):
        if ev is None:
            return
        sem, val = ev
        if sem is E.sem and not own_ok:
            return
        k = id(sem)
        if E.waited.get(k, 0) >= val:
            return
        E.eng.wait_ge(sem, val)
        E.waited[k] = val

    def _deps(self, E, R, W):
        same = self.same and E.name != "pe"
        for d in R:
            self._wait(E, d.w, own_ok=same)
        for d in W:
            self._wait(E, d.w, own_ok=same)
            for ev in d.r:
                self._wait(E, ev, own_ok=False)

    def _record(self, ev, R, W):
        for d in R:
            if not d.ro:
                for i, (sem, val) in enumerate(d.r):
                    if sem is ev[0]:
                        if ev[1] > val:
                            d.r[i] = ev
                        break
                else:
                    d.r.append(ev)
        for d in W:
            d.w = ev
            d.r = []

    def op(self, en, fn, R=(), W=()):
        E = self.e[en]
        self._deps(E, R, W)
        ins = fn(E.eng)
        E.n += 1
        ins.then_inc(E.sem, 1)
        ev = (E.sem, E.n)
        self._record(ev, R, W)
        self.ninst += 1
        return ev

    def _dma_issue(self, E, issue, R, W):
        self._deps(E, R, W)
        slot = E.pool[E.pool_i % len(E.pool)]
        E.pool_i += 1
        sem, cur = slot
        if cur:
            self._wait(E, (sem, cur))
        ins = issue(E.eng)
        ins.then_inc(sem, 16)
        slot[1] = cur + 16
        ev = (sem, cur + 16)
        self._record(ev, R, W)
        self.ninst += 1
        return ev

    def dma(self, q, out, in_, R=(), W=(), **kw):
        return self._dma_issue(self.e[q], lambda e: e.dma_start(out=out, in_=in_, **kw), R, W)

    def idma(self, out, out_off, in_, in_off, R=(), W=(), **kw):
        return self._dma_issue(
            self.e["pool"],
            lambda e: e.indirect_dma_start(out=out, out_offset=out_off, in_=in_, in_offset=in_off, **kw),
            R, W)

    def cc(self, in_ap, out_ap, groups, R=(), W=()):
        E = self.e["pool"]
        self._deps(E, R, W)
        if not hasattr(self, "ccsem"):
            self.ccsem = self.stacks[0].enter_context(self.nc.semaphore("ccsem"))
            self.ccn = 0
        ins = E.eng.collective_compute("AllGather", ALU.bypass, replica_groups=groups, ins=[in_ap], outs=[out_ap], dma_qos="P2")
        ins.then_inc(self.ccsem, 1)
        self.ccn += 1
        ev = (self.ccsem, self.ccn)
        self._record(ev, R, W)
        self.ninst += 1
        return ev

    def barrier(self):
        evs = [(E.sem, E.n) for E in self.e.values() if E.n]
        for q in ("sp", "pool", "act"):
            for sem, cur in self.e[q].pool:
                if cur:
                    evs.append((sem, cur))
        if getattr(self, "ccn", 0):
            evs.append((self.ccsem, self.ccn))
        for E in self.e.values():
            for ev in evs:
                self._wait(E, ev, own_ok=False)

    def finish(self):
        self.barrier()
        self.e["sp"].eng.nop()
        while self.stacks:
            self.stacks.pop().close()


def make_ident(kb, n=128):
    identf = kb.sb("identf", [128, 128], F32)
    ident = kb.sb("ident", [128, 128], BF16)
    d1, d2 = Dep("identf"), Dep("ident")
    kb.op("pool", lambda e: e.memset(identf[:], 0.0), W=[d1])
    kb.op("pool", lambda e: e.affine_select(out=identf[:], in_=identf[:], pattern=[[-1, 128]],
                                            compare_op=ALU.not_equal, fill=1.0, base=0,
                                            channel_multiplier=1), R=[d1], W=[d1])
    kb.op("dve", lambda e: e.tensor_copy(out=ident[:], in_=identf[:]), R=[d1], W=[d2])
    d1.ro = True
    d2.ro = True
    return identf, d1, ident, d2


def load_bc(kb, name, src_ap, n, q="sp"):
    t = kb.sb(name, [128, n], F32)
    d = Dep(name)
    kb.dma(q, t[:], src_ap.to_broadcast([128, n]), W=[d])
    d.ro = True
    return t, d


def load_w(kb, name, w_ap, kin, nout, dt=BF16):
    kc = kin // 128
    t = kb.sb(name, [128, kc, nout], dt)
    d = Dep(name)
    wv = w_ap.rearrange("(c p) n -> p c n", p=128)
    for c in range(kc):
        kb.dma("pool" if dt != F32 else "sp", t[:, c, :], wv[:, c, :], W=[d])
    return t, d


def rms_rstd(kb, src, d_src, n, scr, d_scr, ss, d_ss):
    kb.op("act", lambda e: e.activation(out=scr, in_=src, func=AF.Square, accum_out=ss),
          R=[d_src], W=[d_scr, d_ss])
    kb.op("act", lambda e: e.activation(out=ss, in_=ss, func=AF.Ln, scale=1.0 / n, bias=EPS),
          R=[d_ss], W=[d_ss])
    kb.op("act", lambda e: e.activation(out=ss, in_=ss, func=AF.Exp, scale=-0.5), R=[d_ss], W=[d_ss])


def transpose_chunks(kb, src, d_src, nchunk, pst, d_pst, dst, d_dst, ident, d_id, copy_eng="dve"):
    for c in range(nchunk):
        kb.op("pe", lambda e: e.transpose(out=pst[:, c, :], in_=src[:, c * 128:(c + 1) * 128], identity=ident[:]),
              R=[d_src, d_id], W=[d_pst])
    if copy_eng == "act":
        kb.op("act", lambda e: e.copy(out=dst[:, 0:nchunk, :], in_=pst[:, 0:nchunk, :]), R=[d_pst], W=[d_dst])
    else:
        kb.op("dve", lambda e: e.tensor_copy(out=dst[:, 0:nchunk, :], in_=pst[:, 0:nchunk, :]), R=[d_pst], W=[d_dst])


def mm_acc(kb, ps_ap, d_ps, xT, d_xT, w, d_w, c0, c1, kc=8):
    for c in range(kc):
        kb.op("pe", lambda e: e.matmul(ps_ap, lhsT=xT[:, c, :], rhs=w[:, c, c0:c1], start=(c == 0), stop=(c == kc - 1)),
              R=[d_xT, d_w], W=[d_ps])


def build_A(nt):
    nc = bass.Bass("TRN2", target_bir_lowering=False)
    rows = nt * 128
    h_in = nc.dram_tensor("h", [rows, D], F32, kind="ExternalInput").ap()
    gain = nc.dram_tensor("gain", [1, D], F32, kind="ExternalInput").ap()
    w_in = nc.dram_tensor("w_in", [D, 3088], F32, kind="ExternalInput").ap()
    gq = nc.dram_tensor("gq", [1, D], F32, kind="ExternalInput").ap()
    gk = nc.dram_tensor("gk", [1, D], F32, kind="ExternalInput").ap()
    bfv = nc.dram_tensor("bf", [1, FH], F32, kind="ExternalInput").ap()
    qo = nc.dram_tensor("qo", [rows, D], BF16, kind="ExternalOutput").ap()
    ko = nc.dram_tensor("ko", [rows, D], BF16, kind="ExternalOutput").ap()
    vo = nc.dram_tensor("vo", [rows, D], BF16, kind="ExternalOutput").ap()
    lfo = nc.dram_tensor("lfo", [rows, FH], F32, kind="ExternalOutput").ap()
    d_out = Dep("out")
    kb = KB(nc)
    identf, d_idf, ident, d_id = make_ident(kb)
    g_t, d_g = load_bc(kb, "gain", gain, D)
    gq_t, d_gq = load_bc(kb, "gq", gq, D)
    gk_t, d_gk = load_bc(kb, "gk", gk, D)
    bf_t, d_bf = load_bc(kb, "bf", bfv, FH)
    w_t, d_w = load_w(kb, "w_in", w_in, D, 3088)
    d_w.ro = True
    xin = kb.ring("xin", [128, D], F32, 2)
    scr = kb.ring("scr", [128, D], F32, 2)
    ssr = kb.ring("ss", [128, 1], F32, 2)
    xnr = kb.ring("xn", [128, D], BF16, 2)
    xTr = kb.ring("xT", [128, 8, 128], BF16, 2)
    pstr = kb.ring("pst", [128, 8, 128], BF16, 2, psum=True)
    pmm = kb.ring("pmm", [128, 512], F32, 6, psum=True)
    qf = kb.ring("qf", [128, D], F32, 2)
    s16 = kb.ring("s16", [128, FH], F32, 2)
    qn = kb.ring("qn", [128, D], BF16, 2)
    kn = kb.ring("kn", [128, D], BF16, 2)
    vb = kb.ring("vb", [128, D], BF16, 2)
    lft = kb.ring("lft", [128, FH], F32, 2)
    pi = 0
    for t in range(nt):
        b = t % 2
        x_t, d_x = xin[b]
        sc_t, d_sc = scr[b]
        ss_t, d_ss = ssr[b]
        xn_t, d_xn = xnr[b]
        xT_t, d_xT = xTr[b]
        ps_t, d_pst = pstr[b]
        kb.dma("sp", x_t[:], h_in[t * 128:(t + 1) * 128, :], W=[d_x])
        rms_rstd(kb, x_t[:], d_x, D, sc_t[:], d_sc, ss_t[:], d_ss)
        kb.op("dve", lambda e: e.scalar_tensor_tensor(out=xn_t[:], in0=x_t[:], scalar=ss_t[:, 0:1], in1=g_t[:],
                                                       op0=ALU.mult, op1=ALU.mult),
              R=[d_x, d_ss, d_g], W=[d_xn])
        transpose_chunks(kb, xn_t, d_xn, 8, ps_t, d_pst, xT_t, d_xT, ident, d_id)
        for which, (g2_t, d_g2, o_ring, o_dram, scl) in enumerate(
                ((gq_t, d_gq, qn, qo, FD ** -0.5), (gk_t, d_gk, kn, ko, 1.0))):
            qf_t, d_qf = qf[which]
            for half in range(2):
                p_t, d_p = pmm[pi % 6]
                pi += 1
                c0 = which * 1024 + half * 512
                mm_acc(kb, p_t[:], d_p, xT_t, d_xT, w_t, d_w, c0, c0 + 512)
                kb.op("act", lambda e: e.copy(out=qf_t[:, half * 512:(half + 1) * 512], in_=p_t[:]),
                      R=[d_p], W=[d_qf])
            s_t, d_s = s16[which]
            kb.op("pool", lambda e: e.tensor_tensor(out=sc_t[:], in0=qf_t[:], in1=qf_t[:], op=ALU.mult),
                  R=[d_qf], W=[d_sc])
            kb.op("dve", lambda e: e.tensor_reduce(out=s_t[:], in_=sc_t[:].rearrange("p (h d) -> p h d", d=FD),
                                                    axis=AX.X, op=ALU.add), R=[d_sc], W=[d_s])
            kb.op("act", lambda e: e.activation(out=s_t[:], in_=s_t[:], func=AF.Sqrt, scale=1.0 / FD, bias=EPS),
                  R=[d_s], W=[d_s])
            kb.op("dve", lambda e: e.reciprocal(out=s_t[:], in_=s_t[:]), R=[d_s], W=[d_s])
            kb.op("dve", lambda e: e.tensor_tensor(
                out=sc_t[:].rearrange("p (h d) -> p h d", d=FD), in0=qf_t[:].rearrange("p (h d) -> p h d", d=FD),
                in1=s_t[:].unsqueeze(2).to_broadcast([128, FH, FD]), op=ALU.mult), R=[d_qf, d_s], W=[d_sc])
            o_t, d_o = o_ring[b]
            kb.op("dve", lambda e: e.scalar_tensor_tensor(out=o_t[:], in0=sc_t[:], scalar=float(scl), in1=g2_t[:],
                                                           op0=ALU.mult, op1=ALU.mult),
                  R=[d_sc, d_g2], W=[d_o])
            kb.dma("sp", o_dram[t * 128:(t + 1) * 128, :], o_t[:], R=[d_o])
        v_t, d_v = vb[b]
        for half in range(2):
            p_t, d_p = pmm[pi % 6]
            pi += 1
            c0 = 2048 + half * 512
            mm_acc(kb, p_t[:], d_p, xT_t, d_xT, w_t, d_w, c0, c0 + 512)
            kb.op("act", lambda e: e.copy(out=v_t[:, half * 512:(half + 1) * 512], in_=p_t[:]), R=[d_p], W=[d_v])
        kb.dma("sp", vo[t * 128:(t + 1) * 128, :], v_t[:], R=[d_v])
        p_t, d_p = pmm[pi % 6]
        pi += 1
        mm_acc(kb, p_t[:, 0:FH], d_p, xT_t, d_xT, w_t, d_w, 3072, 3088)
        l_t, d_l = lft[b]
        kb.op("dve", lambda e: e.tensor_tensor(out=l_t[:], in0=p_t[:, 0:FH], in1=bf_t[:], op=ALU.add),
              R=[d_p, d_bf], W=[d_l])
        kb.op("act", lambda e: e.activation(out=l_t[:], in_=l_t[:], func=AF.Exp, scale=-1.0), R=[d_l], W=[d_l])
        kb.op("act", lambda e: e.activation(out=l_t[:], in_=l_t[:], func=AF.Ln, bias=1.0), R=[d_l], W=[d_l])
        kb.op("dve", lambda e: e.tensor_scalar(out=l_t[:], in0=l_t[:], scalar1=-1.0, scalar2=None, op0=ALU.mult),
              R=[d_l], W=[d_l])
        kb.dma("sp", lfo[t * 128:(t + 1) * 128, :], l_t[:], R=[d_l])
    kb.finish()
    return nc


def build_B(nb, nbh):
    assert (nb - 1) % 4 == 0
    L = nb * 128
    nI = (nb - 1) // 4 + 1
    nc = bass.Bass("TRN2", target_bir_lowering=False)
    qT = nc.dram_tensor("qT", [nbh, FD, L], BF16, kind="ExternalInput").ap()
    kT = nc.dram_tensor("kT", [nbh, FD, L], BF16, kind="ExternalInput").ap()
    vv = nc.dram_tensor("v", [nbh, L, FD], BF16, kind="ExternalInput").ap()
    lfr = nc.dram_tensor("lfr", [nbh, L], F32, kind="ExternalInput").ap()
    lfT = nc.dram_tensor("lfT", [nbh, 128, nb], F32, kind="ExternalInput").ap()
    oT = nc.dram_tensor("oT", [nbh, FD + 1, L], F32, kind="ExternalOutput").ap()
    kb = KB(nc)
    trif = kb.sb("trif", [128, 128], F32); d_tri = Dep("trif")
    onesf = kb.sb("onesf", [128, 128], F32); d_ones = Dep("onesf")
    sel0 = kb.sb("sel0", [128, 128], F32); d_sel = Dep("sel0")
    maskb = kb.sb("maskb", [128, 128], BF16); d_mask = Dep("maskb")
    onerow = kb.sb("onerow", [128, nb], F32); d_or = Dep("onerow")
    kb.op("pool", lambda e: e.memset(onesf[:], 1.0), W=[d_ones])
    kb.op("pool", lambda e: e.memset(onerow[:], 1.0), W=[d_or])
    kb.op("pool", lambda e: e.affine_select(out=trif[:], in_=onesf[:], pattern=[[1, 128]], compare_op=ALU.is_ge,
                                            fill=0.0, base=0, channel_multiplier=-1), R=[d_ones], W=[d_tri])
    kb.op("pool", lambda e: e.affine_select(out=sel0[:], in_=onesf[:], pattern=[[0, 128]], compare_op=ALU.is_ge,
                                            fill=0.0, base=0, channel_multiplier=-1), R=[d_ones], W=[d_sel])
    kb.op("dve", lambda e: e.tensor_copy(out=maskb[:], in_=trif[:]), R=[d_tri], W=[d_mask])
    for d in (d_tri, d_ones, d_sel, d_mask, d_or):
        d.ro = True
    crow = kb.sb("crow", [nbh, L], F32); d_crow = Dep("crow")
    drow = kb.sb("drow", [nbh, L], BF16); d_drow = Dep("drow")
    kb.dma("sp", crow[:], lfr, W=[d_crow])
    kb.op("dve", lambda e: e.tensor_tensor_scan(out=crow[:], data0=onerow[0:nbh, 0:1].to_broadcast([nbh, L]),
                                                 data1=crow[:], initial=0.0, op0=ALU.mult, op1=ALU.add),
          R=[d_crow, d_or], W=[d_crow])
    kb.op("dve", lambda e: e.tensor_scalar(out=drow[:, 0:128], in0=crow[:, 0:128], scalar1=crow[:, 0:1], scalar2=None,
                                            op0=ALU.subtract), R=[d_crow], W=[d_drow])
    if nI > 1:
        kb.op("dve", lambda e: e.tensor_tensor(
            out=drow[:, 128:L].rearrange("p (i c) -> p i c", c=512),
            in0=crow[:, 128:L].rearrange("p (i c) -> p i c", c=512),
            in1=crow[:, 128:L].rearrange("p (i c) -> p i c", c=512)[:, :, 0:1].to_broadcast([nbh, nI - 1, 512]),
            op=ALU.subtract), R=[d_crow], W=[d_drow])
    QA = kb.sb("QA", [FD + 1, L], BF16); d_QA = Dep("QA")
    KA = kb.sb("KA", [FD + 1, L], BF16); d_KA = Dep("KA")
    VA = kb.sb("VA", [128, nb, FD + 1], BF16); d_VA = Dep("VA")
    lft = kb.sb("lft", [128, nb], F32); d_lft = Dep("lft")
    ct = kb.sb("ct", [128, nb], F32); d_ct = Dep("ct")
    tot = kb.sb("tot", [128, nb], F32); d_tot = Dep("tot")
    rall = kb.sb("rall", [128, nb], F32); d_rall = Dep("rall")
    biasr = kb.ring("bias", [128, nb], F32, 2)
    pr = kb.ring("P", [128, 512], BF16, 4)
    osb = kb.ring("osb", [FD + 1, 512], F32, 2)
    psS = kb.ring("psS", [128, 512], F32, 4, psum=True)
    psO = kb.ring("psO", [128, 512], F32, 2, psum=True)
    psM = kb.ring("psM", [128, 512], F32, 2, psum=True)
    si = 0
    oi = 0
    for bh in range(nbh):
        kb.dma("sp", QA[0:FD, :], qT[bh], W=[d_QA])
        kb.dma("sp", QA[FD:FD + 1, :], drow[bh:bh + 1, :], R=[d_drow], W=[d_QA])
        kb.dma("sp", KA[0:FD, :], kT[bh], W=[d_KA])
        kb.op("pool", lambda e: e.memset(KA[FD:FD + 1, :], 1.0), W=[d_KA])
        kb.dma("sp", VA[:, :, 0:FD], vv[bh].rearrange("(j p) d -> p j d", p=128), W=[d_VA])
        kb.op("pool", lambda e: e.memset(VA[:, :, FD:FD + 1], 1.0), W=[d_VA])
        kb.op("pool", lambda e: e.memset(VA[0:112, 0, :], 0.0), W=[d_VA])
        kb.dma("sp", lft[:], lfT[bh], W=[d_lft])
        pm0, d_pm0 = psM[0]
        pm1, d_pm1 = psM[1]
        kb.op("pe", lambda e: e.matmul(pm0[:, 0:nb], lhsT=trif[:], rhs=lft[:], start=True, stop=True),
              R=[d_tri, d_lft], W=[d_pm0])
        kb.op("pe", lambda e: e.matmul(pm1[:, 0:nb], lhsT=onesf[:], rhs=lft[:], start=True, stop=True),
              R=[d_ones, d_lft], W=[d_pm1])
        kb.op("dve", lambda e: e.tensor_copy(out=tot[:], in_=pm1[:, 0:nb]), R=[d_pm1], W=[d_tot])
        kb.op("dve", lambda e: e.tensor_tensor_scan(out=ct[:], data0=onerow[:], data1=tot[:], initial=0.0,
                                                     op0=ALU.mult, op1=ALU.add), R=[d_tot, d_or], W=[d_ct])
        kb.op("dve", lambda e: e.tensor_tensor(out=ct[:], in0=ct[:], in1=tot[:], op=ALU.subtract),
              R=[d_ct, d_tot], W=[d_ct])
        kb.op("dve", lambda e: e.tensor_tensor(out=ct[:], in0=ct[:], in1=pm0[:, 0:nb], op=ALU.add),
              R=[d_ct, d_pm0], W=[d_ct])
        kb.op("pe", lambda e: e.matmul(pm1[:, 0:nb], lhsT=sel0[:], rhs=ct[:], start=True, stop=True),
              R=[d_sel, d_ct], W=[d_pm1])
        kb.op("dve", lambda e: e.tensor_copy(out=rall[:], in_=pm1[:, 0:nb]), R=[d_pm1], W=[d_rall])
        steps = []
        for I in range(nI):
            j0 = 0 if I == 0 else 4 * I - 3
            nblk = 1 if I == 0 else 4
            nJ = j0 + nblk
            for J in range(nJ):
                steps.append((I, J, j0, nblk, nJ))
        LA = 2
        cur = {}
        for idx in range(len(steps) + LA):
            if idx < len(steps):
                I, J, j0, nblk, nJ = steps[idx]
                q0 = j0 * 128
                bias_t, d_bias = biasr[I % 2]
                if J == 0:
                    kb.op("dve", lambda e: e.tensor_scalar(out=bias_t[:, 0:nJ], in0=ct[:, 0:nJ], scalar1=-1.0,
                                                            scalar2=rall[:, j0:j0 + 1], op0=ALU.mult, op1=ALU.add),
                          R=[d_ct, d_rall], W=[d_bias])
                m = max(0, J - j0)
                c0 = m * 128
                c1 = nblk * 128
                ps, d_ps = psS[idx % 4]
                p_t, d_p = pr[idx % 4]
                kb.op("pe", lambda e: e.matmul(ps[:, c0:c1], lhsT=KA[:, J * 128:(J + 1) * 128],
                                               rhs=QA[:, q0 + c0:q0 + c1], start=True, stop=True),
                      R=[d_KA, d_QA], W=[d_ps])
                kb.op("act", lambda e: e.activation(out=p_t[:, c0:c1], in_=ps[:, c0:c1], func=AF.Exp,
                                                    bias=bias_t[:, J:J + 1], scale=1.0),
                      R=[d_ps, d_bias], W=[d_p])
                if J >= j0:
                    kb.op("dve", lambda e: e.tensor_tensor(out=p_t[:, c0:c0 + 128], in0=p_t[:, c0:c0 + 128],
                                                            in1=maskb[:], op=ALU.mult), R=[d_p, d_mask], W=[d_p])
            if idx >= LA:
                I, J, j0, nblk, nJ = steps[idx - LA]
                q0 = j0 * 128
                m = max(0, J - j0)
                c0 = m * 128
                c1 = nblk * 128
                p_t, d_p = pr[(idx - LA) % 4]
                po, d_po = psO[I % 2]
                kb.op("pe", lambda e: e.matmul(po[0:FD + 1, c0:c1], lhsT=VA[:, J, :], rhs=p_t[:, c0:c1],
                                               start=(J == 0), stop=(J == nJ - 1)),
                      R=[d_VA, d_p], W=[d_po])
                if J == nJ - 1:
                    o_t, d_o = osb[I % 2]
                    ncol = nblk * 128
                    kb.op("dve", lambda e: e.tensor_copy(out=o_t[:, 0:ncol], in_=po[0:FD + 1, 0:ncol]),
                          R=[d_po], W=[d_o])
                    kb.dma("sp", oT[bh, :, q0:q0 + ncol], o_t[:, 0:ncol], R=[d_o])
    kb.finish()
    return nc


BIGIDX = 1.0e6


def build_CE(stage, nt, has_meta, cap=CAP):
    nc = bass.Bass("TRN2", target_bir_lowering=False)
    rows = nt * 128
    nslot = NE * cap
    nst = cap // 128
    a_in = nc.dram_tensor("a_in", [rows, D], F32, kind="ExternalInput").ap()
    hp_in = nc.dram_tensor("hp_in", [rows, D], F32, kind="ExternalInput").ap()
    if stage == "C":
        den_in = nc.dram_tensor("den_in", [rows, FH], F32, kind="ExternalInput").ap()
    else:
        gs_in = nc.dram_tensor("gs_in", [rows, D], BF16, kind="ExternalInput").ap()
    valid_in = nc.dram_tensor("valid_in", [rows, 1], F32, kind="ExternalInput").ap()
    w_out = nc.dram_tensor("w_out", [D, D], F32, kind="ExternalInput").ap()
    mnorm = nc.dram_tensor("mnorm", [1, D], F32, kind="ExternalInput").ap()
    w_r = nc.dram_tensor("w_r", [D, 36], F32, kind="ExternalInput").ap()
    b_r = nc.dram_tensor("b_r", [1, 36], F32, kind="ExternalInput").ap()
    w_up = nc.dram_tensor("w_up", [NE, D, 2 * DE], F32, kind="ExternalInput").ap()
    w_dn = nc.dram_tensor("w_dn", [NE, DE, D], F32, kind="ExternalInput").ap()
    h2_out = nc.dram_tensor("h2_out", [rows, D], F32, kind="ExternalOutput").ap()
    if stage == "C":
        hnorm = nc.dram_tensor("hnorm", [1, D], F32, kind="ExternalInput").ap()
        hw_in = nc.dram_tensor("hw_in", [D, 4 * D], F32, kind="ExternalInput").ap()
        hbf = nc.dram_tensor("hbf", [1, D], F32, kind="ExternalInput").ap()
        lbl = nc.dram_tensor("lbl", [2, D], F32, kind="ExternalInput").ap()
        q1_out = nc.dram_tensor("q1_out", [rows, D], BF16, kind="ExternalOutput").ap()
        lf1_out = nc.dram_tensor("lf1_out", [rows, D], F32, kind="ExternalOutput").ap()
        v1_out = nc.dram_tensor("v1_out", [rows, D], BF16, kind="ExternalOutput").ap()
        gs_out = nc.dram_tensor("gs_out", [rows, D], BF16, kind="ExternalOutput").ap()
    xpad = nc.dram_tensor("xpad", [nslot, D], BF16, kind="Internal").ap()
    ypad = nc.dram_tensor("ypad", [nslot, D], BF16, kind="Internal").ap()
    h1s = nc.dram_tensor("h1s", [rows, D], F32, kind="Internal").ap()
    d_xpad, d_ypad, d_h1s = Dep("xpad"), Dep("ypad"), Dep("h1s")

    kb = KB(nc)
    identf, d_idf, ident, d_id = make_ident(kb)
    breg = nc.gpsimd.to_reg(nslot - 1)
    dest_i = kb.sb("dest_i", [128, nt, 2], I32); d_dest = Dep("dest_i")
    gw = kb.sb("gw", [128, nt, 2], F32); d_gw = Dep("gw")
    cntb = kb.sb("cntb", [128, NE], F32); d_cntb = Dep("cntb")
    usb = kb.sb("usb", [128, 128], BF16); d_us = Dep("usb")
    onesb = kb.sb("onesb", [128, 128], BF16); d_onesb = Dep("onesb")
    onesf = kb.sb("onesf", [128, 128], F32); d_onesf = Dep("onesf")
    tmpf = kb.sb("tmpf", [128, 128], F32); d_tmpf = Dep("tmpf")
    kb.op("pool", lambda e: e.memset(onesf[:], 1.0), W=[d_onesf])
    kb.op("pool", lambda e: e.affine_select(out=tmpf[:], in_=onesf[:], pattern=[[1, 128]], compare_op=ALU.is_gt,
                                            fill=0.0, base=0, channel_multiplier=-1), R=[d_onesf], W=[d_tmpf])
    kb.op("dve", lambda e: e.tensor_copy(out=usb[:], in_=tmpf[:]), R=[d_tmpf], W=[d_us])
    kb.op("dve", lambda e: e.tensor_copy(out=onesb[:], in_=onesf[:]), R=[d_onesf], W=[d_onesb])
    kb.op("pool", lambda e: e.iota(cntb[:], pattern=[[cap, NE]], base=0, channel_multiplier=0,
                                   allow_small_or_imprecise_dtypes=True), W=[d_cntb])
    for d in (d_us, d_onesb, d_onesf):
        d.ro = True
    mn_t, d_mn = load_bc(kb, "mnorm", mnorm, D)
    br_t, d_br = load_bc(kb, "b_r", b_r, 36)
    wr_t, d_wr = load_w(kb, "w_r", w_r, D, 36, dt=F32)
    d_wr.ro = True

    with kb.scope():
        wo_t, d_wo = load_w(kb, "w_out", w_out, D, D)
        d_wo.ro = True
        a_r = kb.ring("a", [128, D], F32, 2)
        hp_r = kb.ring("hp", [128, D], F32, 2)
        den_r = kb.ring("den", [128, FH], F32, 2)
        gs_r = kb.ring("gs", [128, D], BF16, 2)
        ob_r = kb.ring("ob", [128, D], BF16, 2)
        oT_r = kb.ring("oT", [128, 8, 128], BF16, 2)
        h1_r = kb.ring("h1", [128, D], F32, 2)
        scr_r = kb.ring("scr", [128, D], F32, 2)
        ss_r = kb.ring("ss", [128, 1], F32, 2)
        hmf_r = kb.ring("hmf", [128, D], F32, 2)
        hmb_r = kb.ring("hmb", [128, D], BF16, 2)
        hmT_r = kb.ring("hmT", [128, 8, 128], F32, 2)
        sm_r = kb.ring("sm", [128, 256], F32, 2)
        ab_r = kb.ring("ab", [128, NE], BF16, 2)
        val_r = kb.ring("val", [128, 2], F32, 2)
        pst_r = kb.ring("pst", [128, 8, 128], BF16, 2, psum=True)
        pstf_r = kb.ring("pstf", [128, 8, 128], F32, 1, psum=True)
        pmm_r = kb.ring("pmm", [128, 512], F32, 2, psum=True)
        prt_r = kb.ring("prt", [128, 512], F32, 2, psum=True)
        for t in range(nt):
            b = t % 2
            r0, r1 = t * 128, (t + 1) * 128
            a_t, d_a = a_r[b]
            hp_t, d_hp = hp_r[b]
            ob_t, d_ob = ob_r[b]
            kb.dma("sp", a_t[:], a_in[r0:r1, :], W=[d_a])
            kb.dma("sp", hp_t[:], hp_in[r0:r1, :], W=[d_hp])
            if stage == "C":
                den_t, d_den = den_r[b]
                kb.dma("sp", den_t[:], den_in[r0:r1, :], W=[d_den])
                kb.op("dve", lambda e: e.tensor_scalar(out=den_t[:], in0=den_t[:], scalar1=1e-30, scalar2=None,
                                                        op0=ALU.max), R=[d_den], W=[d_den])
                kb.op("dve", lambda e: e.reciprocal(out=den_t[:], in_=den_t[:]), R=[d_den], W=[d_den])
                kb.op("dve", lambda e: e.tensor_tensor(
                    out=ob_t[:].rearrange("p (h d) -> p h d", d=FD), in0=a_t[:].rearrange("p (h d) -> p h d", d=FD),
                    in1=den_t[:].unsqueeze(2).to_broadcast([128, FH, FD]), op=ALU.mult), R=[d_a, d_den], W=[d_ob])
            else:
                gs_t, d_gs = gs_r[b]
                kb.dma("sp", gs_t[:], gs_in[r0:r1, :], W=[d_gs])
                kb.op("dve", lambda e: e.tensor_tensor(out=ob_t[:], in0=a_t[:], in1=gs_t[:], op=ALU.mult),
                      R=[d_a, d_gs], W=[d_ob])
            oT_t, d_oT = oT_r[b]
            ps_t, d_pst = pst_r[b]
            transpose_chunks(kb, ob_t, d_ob, 8, ps_t, d_pst, oT_t, d_oT, ident, d_id, copy_eng="act")
            h1_t, d_h1 = h1_r[b]
            for half in range(2):
                p_t, d_p = pmm_r[half]
                mm_acc(kb, p_t[:], d_p, oT_t, d_oT, wo_t, d_wo, half * 512, half * 512 + 512)
                kb.op("dve", lambda e: e.tensor_tensor(out=h1_t[:, half * 512:(half + 1) * 512], in0=p_t[:],
                                                        in1=hp_t[:, half * 512:(half + 1) * 512], op=ALU.add),
                      R=[d_p, d_hp], W=[d_h1])
            kb.dma("sp", h1s[r0:r1, :], h1_t[:], R=[d_h1], W=[d_h1s])
            sc_t, d_sc = scr_r[b]
            ss_t, d_ss = ss_r[b]
            hmf_t, d_hmf = hmf_r[b]
            hmb_t, d_hmb = hmb_r[b]
            rms_rstd(kb, h1_t[:], d_h1, D, sc_t[:], d_sc, ss_t[:], d_ss)
            kb.op("dve", lambda e: e.scalar_tensor_tensor(out=hmf_t[:], in0=h1_t[:], scalar=ss_t[:, 0:1], in1=mn_t[:],
                                                           op0=ALU.mult, op1=ALU.mult),
                  R=[d_h1, d_ss, d_mn], W=[d_hmf])
            kb.op("pool", lambda e: e.tensor_copy(out=hmb_t[:], in_=hmf_t[:]), R=[d_hmf], W=[d_hmb])
            pf_t, d_pf = pstf_r[0]
            hmT_t, d_hmT = hmT_r[b]
            for c in range(8):
                kb.op("pe", lambda e: e.transpose(out=pf_t[:, c, :], in_=hmf_t[:, c * 128:(c + 1) * 128],
                                                  identity=identf[:]), R=[d_hmf, d_idf], W=[d_pf])
            kb.op("act", lambda e: e.copy(out=hmT_t[:], in_=pf_t[:]), R=[d_pf], W=[d_hmT])
            pr_t, d_pr = prt_r[b]
            for c in range(8):
                kb.op("pe", lambda e: e.matmul(pr_t[:, 0:36], lhsT=hmT_t[:, c, :], rhs=wr_t[:, c, :],
                                               start=(c == 0), stop=(c == 7)), R=[d_hmT, d_wr], W=[d_pr])
            sm, d_sm = sm_r[b]
            lg = sm[:, 0:36]
            gmax = sm[:, 36:37]
            ngmax = sm[:, 37:38]
            gsum = sm[:, 38:39]
            ggate = sm[:, 39:40]
            eg = sm[:, 40:44]
            ohg = sm[:, 44:48]
            esel = sm[:, 48:56]
            top8 = sm[:, 56:64]
            oh0 = sm[:, 64:72]
            oh1 = sm[:, 72:80]
            A0 = sm[:, 80:112]
            A1 = sm[:, 112:144]
            sbt = sm[:, 144:176]
            tmp = sm[:, 176:208]
            destf = sm[:, 208:210]
            diff = sm[:, 210:211]
            sg = sm[:, 211:212]
            S = [d_sm]
            dv = lambda fn, R=(), W=(): kb.op("dve", fn, R=list(R) + S, W=list(W) + S)
            dv(lambda e: e.tensor_tensor(out=lg, in0=pr_t[:, 0:36], in1=br_t[:], op=ALU.add), R=[d_pr, d_br])
            dv(lambda e: e.tensor_reduce(out=gmax, in_=lg[:, 0:4], axis=AX.X, op=ALU.max))
            dv(lambda e: e.tensor_scalar(out=ngmax, in0=gmax, scalar1=-1.0, scalar2=None, op0=ALU.mult))
            kb.op("act", lambda e: e.activation(out=eg, in_=lg[:, 0:4], func=AF.Exp, bias=ngmax, scale=1.0,
                                                accum_out=gsum), R=S, W=S)
            dv(lambda e: e.reciprocal(out=ggate, in_=gsum))
            dv(lambda e: e.tensor_scalar(out=ohg, in0=lg[:, 0:4], scalar1=gmax, scalar2=None, op0=ALU.is_equal))
            dv(lambda e: e.tensor_scalar(out=esel, in0=lg[:, 4:12], scalar1=ohg[:, 0:1], scalar2=None, op0=ALU.mult))
            for g in range(1, 4):
                dv(lambda e: e.scalar_tensor_tensor(out=esel, in0=lg[:, 4 + 8 * g:12 + 8 * g], scalar=ohg[:, g:g + 1],
                                                    in1=esel, op0=ALU.mult, op1=ALU.add))
            dv(lambda e: e.max(out=top8, in_=esel))
            dv(lambda e: e.tensor_scalar(out=oh0, in0=esel, scalar1=top8[:, 0:1], scalar2=None, op0=ALU.is_equal))
            dv(lambda e: e.tensor_scalar(out=oh1, in0=esel, scalar1=top8[:, 1:2], scalar2=None, op0=ALU.is_equal))
            for (Ak, ohk) in ((A0, oh0), (A1, oh1)):
                dv(lambda e: e.tensor_tensor(out=Ak.rearrange("p (g j) -> p g j", j=8),
                                             in0=ohg.unsqueeze(2).to_broadcast([128, 4, 8]),
                                             in1=ohk.unsqueeze(1).to_broadcast([128, 4, 8]), op=ALU.mult))
            use_valid = has_meta and t == 0
            if use_valid:
                val_t, d_val = val_r[b]
                kb.dma("sp", val_t[:, 0:1], valid_in[r0:r1, :], W=[d_val])
                kb.op("dve", lambda e: e.tensor_scalar(out=val_t[:, 1:2], in0=val_t[:, 0:1], scalar1=-BIGIDX,
                                                        scalar2=BIGIDX, op0=ALU.mult, op1=ALU.add),
                      R=[d_val], W=[d_val])
                for Ak in (A0, A1):
                    dv(lambda e: e.tensor_scalar(out=Ak, in0=Ak, scalar1=val_t[:, 0:1], scalar2=None, op0=ALU.mult),
                       R=[d_val])
            ab_t, d_ab = ab_r[b]
            kb.op("dve", lambda e: e.tensor_tensor(out=ab_t[:], in0=A0, in1=A1, op=ALU.add), R=S, W=[d_ab])
            kb.op("pe", lambda e: e.matmul(pr_t[:, 64:96], lhsT=usb[:], rhs=ab_t[:], start=True, stop=True),
                  R=[d_us, d_ab], W=[d_pr])
            kb.op("pe", lambda e: e.matmul(pr_t[:, 96:128], lhsT=onesb[:], rhs=ab_t[:], start=True, stop=True),
                  R=[d_onesb, d_ab], W=[d_pr])
            dv(lambda e: e.tensor_tensor(out=sbt, in0=pr_t[:, 64:96], in1=cntb[:], op=ALU.add), R=[d_pr, d_cntb])
            kb.op("dve", lambda e: e.tensor_tensor(out=cntb[:], in0=cntb[:], in1=pr_t[:, 96:128], op=ALU.add),
                  R=[d_pr, d_cntb] + S, W=[d_cntb])
            for k, Ak in enumerate((A0, A1)):
                dv(lambda e: e.tensor_tensor(out=tmp, in0=Ak, in1=sbt, op=ALU.mult))
                dv(lambda e: e.tensor_reduce(out=destf[:, k:k + 1], in_=tmp, axis=AX.X, op=ALU.add))
            if use_valid:
                dv(lambda e: e.tensor_scalar(out=destf, in0=destf, scalar1=val_t[:, 0:1], scalar2=val_t[:, 1:2],
                                             op0=ALU.mult, op1=ALU.add), R=[d_val])
            kb.op("dve", lambda e: e.tensor_copy(out=dest_i[:, t, :], in_=destf), R=S, W=[d_dest])
            dv(lambda e: e.tensor_tensor(out=diff, in0=top8[:, 0:1], in1=top8[:, 1:2], op=ALU.subtract))
            kb.op("act", lambda e: e.activation(out=sg, in_=diff, func=AF.Sigmoid), R=S, W=S)
            kb.op("dve", lambda e: e.tensor_tensor(out=gw[:, t, 0:1], in0=sg, in1=ggate, op=ALU.mult), R=S, W=[d_gw])
            kb.op("dve", lambda e: e.tensor_tensor(out=gw[:, t, 1:2], in0=ggate, in1=gw[:, t, 0:1], op=ALU.subtract),
                  R=S + [d_gw], W=[d_gw])
            for k in range(2):
                kb.idma(out=xpad[:, :], out_off=bass.IndirectOffsetOnAxis(ap=dest_i[:, t, k:k + 1], axis=0),
                        in_=hmb_t[:], in_off=None, R=[d_hmb, d_dest], W=[d_xpad],
                        bounds_check=breg, oob_is_err=False)

    with kb.scope():
        wup_r = kb.ring("wup", [128, 8, 2 * DE], BF16, 2)
        wdn_r = kb.ring("wdn", [128, 4, D], BF16, 2)
        xs_r = kb.ring("xs", [128, D], BF16, 3)
        xT_r = kb.ring("xT", [128, 8, cap], BF16, 2)
        sa_r = kb.ring("sa", [128, cap], F32, 2)
        aT_r = kb.ring("aT", [128, 4, cap], BF16, 2)
        yb_r = kb.ring("yb", [128, D], BF16, 2)
        pst_r = kb.ring("pst", [128, 8, 128], BF16, 2, psum=True)
        pau_r = kb.ring("pau", [128, 512], F32, 4, psum=True)
        py_r = kb.ring("py", [128, 512], F32, 2, psum=True)
        xi = 0
        yi = 0
        for ex in range(NE):
            b = ex % 2
            wup_t, d_wup = wup_r[b]
            wdn_t, d_wdn = wdn_r[b]
            wuv = w_up[ex].rearrange("(c p) n -> p c n", p=128)
            wdv = w_dn[ex].rearrange("(c p) n -> p c n", p=128)
            for c in range(8):
                kb.dma("pool", wup_t[:, c, :], wuv[:, c, :], W=[d_wup])
            for c in range(4):
                kb.dma("pool", wdn_t[:, c, :], wdv[:, c, :], W=[d_wdn])
            xT_t, d_xT = xT_r[b]
            for st in range(nst):
                xs_t, d_xs = xs_r[xi % 3]
                ps_t, d_pst = pst_r[xi % 2]
                xi += 1
                s0 = ex * cap + st * 128
                kb.dma("sp", xs_t[:], xpad[s0:s0 + 128, :], R=[d_xpad], W=[d_xs])
                for c in range(8):
                    kb.op("pe", lambda e: e.transpose(out=ps_t[:, c, :], in_=xs_t[:, c * 128:(c + 1) * 128],
                                                      identity=ident[:]), R=[d_xs, d_id], W=[d_pst])
                kb.op("act" if st % 2 else "dve",
                      (lambda e: e.copy(out=xT_t[:, :, st * 128:(st + 1) * 128], in_=ps_t[:])) if st % 2 else
                      (lambda e: e.tensor_copy(out=xT_t[:, :, st * 128:(st + 1) * 128], in_=ps_t[:])),
                      R=[d_pst], W=[d_xT])
            aT_t, d_aT = aT_r[b]
            for fc in range(4):
                pa, d_pa = pau_r[(2 * fc) % 4]
                pu, d_pu = pau_r[(2 * fc + 1) % 4]
                for c in range(8):
                    kb.op("pe", lambda e: e.matmul(pa[:, 0:cap], lhsT=wup_t[:, c, fc * 128:(fc + 1) * 128],
                                                   rhs=xT_t[:, c, :], start=(c == 0), stop=(c == 7)),
                          R=[d_wup, d_xT], W=[d_pa])
                for c in range(8):
                    kb.op("pe", lambda e: e.matmul(pu[:, 0:cap], lhsT=wup_t[:, c, DE + fc * 128:DE + (fc + 1) * 128],
                                                   rhs=xT_t[:, c, :], start=(c == 0), stop=(c == 7)),
                          R=[d_wup, d_xT], W=[d_pu])
                sa_t, d_sa = sa_r[fc % 2]
                kb.op("act", lambda e: e.activation(out=sa_t[:], in_=pa[:, 0:cap], func=AF.Silu), R=[d_pa], W=[d_sa])
                kb.op("dve", lambda e: e.tensor_tensor(out=aT_t[:, fc, :], in0=sa_t[:], in1=pu[:, 0:cap], op=ALU.mult),
                      R=[d_sa, d_pu], W=[d_aT])
            for st in range(nst):
                yb_t, d_yb = yb_r[yi % 2]
                yi += 1
                for half in range(2):
                    py, d_py = py_r[half]
                    for fc in range(4):
                        kb.op("pe", lambda e: e.matmul(py[:], lhsT=aT_t[:, fc, st * 128:(st + 1) * 128],
                                                       rhs=wdn_t[:, fc, half * 512:(half + 1) * 512],
                                                       start=(fc == 0), stop=(fc == 3)),
                              R=[d_aT, d_wdn], W=[d_py])
                    if half == 0:
                        kb.op("act", lambda e: e.copy(out=yb_t[:, 0:512], in_=py[:]), R=[d_py], W=[d_yb])
                    else:
                        kb.op("dve", lambda e: e.tensor_copy(out=yb_t[:, 512:1024], in_=py[:]), R=[d_py], W=[d_yb])
                s0 = ex * cap + st * 128
                kb.dma("sp", ypad[s0:s0 + 128, :], yb_t[:], R=[d_yb], W=[d_ypad])

    with kb.scope():
        h1_r = kb.ring("h1", [128, D], F32, 2)
        y_r = kb.ring("y", [128, D], BF16, 4)
        h2_r = kb.ring("h2", [128, D], F32, 2)
        for (y_t, d_y) in y_r:
            kb.op("pool", lambda e: e.memset(y_t[:], 0.0), W=[d_y])
        if stage == "C":
            hw_t, d_hw = load_w(kb, "hw_in", hw_in, D, 4 * D)
            d_hw.ro = True
            hn_t, d_hn = load_bc(kb, "hnorm", hnorm, D)
            hbf_t, d_hbf = load_bc(kb, "hbf", hbf, D)
            l0_t, d_l0 = load_bc(kb, "l0", lbl[0:1, :], D)
            l1_t, d_l1 = load_bc(kb, "l1", lbl[1:2, :], D)
            lb_t = kb.sb("lb", [128, D], F32); d_lb = Dep("lb")
            oml_t = kb.sb("oml", [128, D], F32); d_oml = Dep("oml")
            kb.op("dve", lambda e: e.tensor_tensor(out=lb_t[:], in0=l1_t[:], in1=l0_t[:], op=ALU.subtract),
                  R=[d_l0, d_l1], W=[d_lb])
            kb.op("act", lambda e: e.activation(out=oml_t[:], in_=lb_t[:], func=AF.Sigmoid, scale=-1.0),
                  R=[d_lb], W=[d_oml])
            kb.op("act", lambda e: e.activation(out=lb_t[:], in_=lb_t[:], func=AF.Sigmoid), R=[d_lb], W=[d_lb])
            d_lb.ro = True
            d_oml.ro = True
            scr_r = kb.ring("scr", [128, D], F32, 2)
            ss_r = kb.ring("ss", [128, 1], F32, 2)
            xn_r = kb.ring("xn", [128, D], BF16, 2)
            xT_r = kb.ring("xT", [128, 8, 128], BF16, 2)
            pst_r = kb.ring("pst", [128, 8, 128], BF16, 2, psum=True)
            pmm_r = kb.ring("pmm", [128, 512], F32, 4, psum=True)
            ob_r = kb.ring("obf", [128, D], BF16, 4)
            of_r = kb.ring("off", [128, D], F32, 2)
            zf_r = kb.ring("zf", [128, 512], F32, 2)
        pi = 0
        obi = 0
        for t in range(nt):
            b = t % 2
            r0, r1 = t * 128, (t + 1) * 128
            h1_t, d_h1 = h1_r[b]
            h2_t, d_h2 = h2_r[b]
            y0_t, d_y0 = y_r[2 * b]
            y1_t, d_y1 = y_r[2 * b + 1]
            kb.dma("sp", h1_t[:], h1s[r0:r1, :], R=[d_h1s], W=[d_h1])
            for k, (y_t, d_y) in enumerate(((y0_t, d_y0), (y1_t, d_y1))):
                kb.idma(out=y_t[:], out_off=None, in_=ypad[:, :],
                        in_off=bass.IndirectOffsetOnAxis(ap=dest_i[:, t, k:k + 1], axis=0),
                        R=[d_ypad, d_dest], W=[d_y], bounds_check=breg, oob_is_err=False)
            kb.op("dve", lambda e: e.scalar_tensor_tensor(out=h2_t[:], in0=y0_t[:], scalar=gw[:, t, 0:1], in1=h1_t[:],
                                                           op0=ALU.mult, op1=ALU.add), R=[d_y0, d_gw, d_h1], W=[d_h2])
            kb.op("dve", lambda e: e.scalar_tensor_tensor(out=h2_t[:], in0=y1_t[:], scalar=gw[:, t, 1:2], in1=h2_t[:],
                                                           op0=ALU.mult, op1=ALU.add), R=[d_y1, d_gw, d_h2], W=[d_h2])
            kb.dma("sp", h2_out[r0:r1, :], h2_t[:], R=[d_h2])
            if stage != "C":
                continue
            sc_t, d_sc = scr_r[b]
            ss_t, d_ss = ss_r[b]
            xn_t, d_xn = xn_r[b]
            xT_t, d_xT = xT_r[b]
            ps_t, d_pst = pst_r[b]
            rms_rstd(kb, h2_t[:], d_h2, D, sc_t[:], d_sc, ss_t[:], d_ss)
            kb.op("dve", lambda e: e.scalar_tensor_tensor(out=xn_t[:], in0=h2_t[:], scalar=ss_t[:, 0:1], in1=hn_t[:],
                                                           op0=ALU.mult, op1=ALU.mult), R=[d_h2, d_ss, d_hn], W=[d_xn])
            transpose_chunks(kb, xn_t, d_xn, 8, ps_t, d_pst, xT_t, d_xT, ident, d_id)
            for grp, (dram_o, kind) in enumerate(((q1_out, "silu"), (lf1_out, "f"), (v1_out, "copy"), (gs_out, "silu"))):
                if kind == "f":
                    o_t, d_o = of_r[b]
                else:
                    o_t, d_o = ob_r[obi % 4]
                    obi += 1
                for half in range(2):
                    p_t, d_p = pmm_r[pi % 4]
                    pi += 1
                    c0 = grp * 1024 + half * 512
                    hs = slice(half * 512, (half + 1) * 512)
                    mm_acc(kb, p_t[:], d_p, xT_t, d_xT, hw_t, d_hw, c0, c0 + 512)
                    if kind == "silu":
                        kb.op("act", lambda e: e.activation(out=o_t[:, hs], in_=p_t[:], func=AF.Silu), R=[d_p], W=[d_o])
                    elif kind == "copy":
                        kb.op("act", lambda e: e.copy(out=o_t[:, hs], in_=p_t[:]), R=[d_p], W=[d_o])
                    else:
                        z_t, d_z = zf_r[half]
                        kb.op("dve", lambda e: e.tensor_tensor(out=z_t[:], in0=p_t[:], in1=hbf_t[:, hs], op=ALU.add),
                              R=[d_p, d_hbf], W=[d_z])
                        kb.op("act", lambda e: e.activation(out=z_t[:], in_=z_t[:], func=AF.Sigmoid), R=[d_z], W=[d_z])
                        kb.op("dve", lambda e: e.tensor_tensor(out=z_t[:], in0=z_t[:], in1=oml_t[:, hs], op=ALU.mult),
                              R=[d_z, d_oml], W=[d_z])
                        kb.op("dve", lambda e: e.tensor_tensor(out=z_t[:], in0=z_t[:], in1=lb_t[:, hs], op=ALU.add),
                              R=[d_z, d_lb], W=[d_z])
                        kb.op("act", lambda e: e.activation(out=o_t[:, hs], in_=z_t[:], func=AF.Ln), R=[d_z], W=[d_o])
                kb.dma("sp", dram_o[r0:r1, :], o_t[:], R=[d_o])
    kb.finish()
    return nc


def build_D(nb, nbh):
    L = nb * 128
    nc = bass.Bass("TRN2", target_bir_lowering=False)
    qT = nc.dram_tensor("qT", [nbh, 128, L], BF16, kind="ExternalInput").ap()
    lfT = nc.dram_tensor("lfT", [nbh, 128, L], F32, kind="ExternalInput").ap()
    vv = nc.dram_tensor("v", [nbh, L, 128], BF16, kind="ExternalInput").ap()
    go = nc.dram_tensor("go", [1, 128], F32, kind="ExternalInput").ap()
    on = nc.dram_tensor("on", [nbh, L, 128], F32, kind="ExternalOutput").ap()
    kb = KB(nc)
    identf, d_idf, ident, d_id = make_ident(kb)
    go_t, d_go = load_bc(kb, "go", go, 128)
    onesf = kb.sb("onesf", [128, 128], F32); d_onesf = Dep("onesf")
    mf = kb.sb("mf", [128, 128], F32); d_mf = Dep("mf")
    mku = kb.sb("mku", [128, 128], U32); d_mk = Dep("mku")
    kb.op("pool", lambda e: e.memset(onesf[:], 1.0), W=[d_onesf])
    kb.op("pool", lambda e: e.affine_select(out=mf[:], in_=onesf[:], pattern=[[1, 128]], compare_op=ALU.is_ge,
                                            fill=0.0, base=0, channel_multiplier=-1), R=[d_onesf], W=[d_mf])
    kb.op("dve", lambda e: e.tensor_copy(out=mku[:], in_=mf[:]), R=[d_mf], W=[d_mk])
    d_mk.ro = True
    d_onesf.ro = True
    q_t = kb.sb("q", [128, L], BF16); d_q = Dep("q")
    lf_t = kb.sb("lf", [128, L], F32); d_lf = Dep("lf")
    v_t = kb.sb("v", [128, nb, 128], BF16); d_v = Dep("v")
    S_t = kb.sb("S", [128, 128], F32); d_S = Dep("S")
    Sb_r = kb.ring("Sb", [128, 128], BF16, 2)
    b_r = kb.ring("b", [128, 128], F32, 2)
    col_r = kb.ring("col", [128, 8], F32, 2)
    e1_r = kb.ring("e1", [128, 128], F32, 2)
    e2_r = kb.ring("e2", [128, 128], F32, 2)
    kk_r = kb.ring("kk", [128, 128], F32, 2)
    qd_r = kb.ring("qd", [128, 128], BF16, 2)
    kd_r = kb.ring("kd", [128, 128], BF16, 2)
    qb_r = kb.ring("qb", [128, 128], BF16, 2)
    ke_r = kb.ring("ke", [128, 128], BF16, 2)
    keT_r = kb.ring("keT", [128, 128], BF16, 2)
    scm_r = kb.ring("scm", [128, 128], BF16, 2)
    osq_r = kb.ring("osq", [128, 128], F32, 2)
    oss_r = kb.ring("oss", [128, 2], F32, 2)
    on_r = kb.ring("on", [128, 128], F32, 2)
    psc_r = kb.ring("psc", [128, 512], F32, 2, psum=True)
    po_r = kb.ring("po", [128, 512], F32, 2, psum=True)
    pt_r = kb.ring("pt", [128, 8, 128], BF16, 2, psum=True)
    pu_r = kb.ring("pu", [128, 512], F32, 2, psum=True)
    for (t_, d_) in scm_r:
        kb.op("pool", lambda e: e.memset(t_[:], 0.0), W=[d_])
    for bh in range(nbh):
        kb.dma("sp", q_t[:], qT[bh], W=[d_q])
        for hlf in range(4):
            c0 = (L // 4) * hlf
            kb.dma("sp", lf_t[:, c0:c0 + L // 4], lfT[bh][:, c0:c0 + L // 4], W=[d_lf])
        kb.dma("sp", v_t[:], vv[bh].rearrange("(j p) d -> p j d", p=128), W=[d_v])
        kb.op("pool", lambda e: e.memset(S_t[:], 0.0), W=[d_S])
        kb.op("pool", lambda e: e.memset(Sb_r[0][0][:], 0.0), W=[Sb_r[0][1]])
        for c in range(nb):
            r = c % 2
            cs = slice(c * 128, (c + 1) * 128)
            b_t, d_b = b_r[r]
            col, d_col = col_r[r]
            e1, d_e1 = e1_r[r]
            e2, d_e2 = e2_r[r]
            kk, d_kk = kk_r[r]
            qd, d_qd = qd_r[r]
            kd, d_kd = kd_r[r]
            qb, d_qb = qb_r[r]
            ke, d_ke = ke_r[r]
            keT, d_keT = keT_r[r]
            scm, d_scm = scm_r[r]
            kb.op("dve", lambda e: e.tensor_tensor_scan(out=b_t[:], data0=onesf[:], data1=lf_t[:, cs], initial=0.0,
                                                         op0=ALU.mult, op1=ALU.add), R=[d_lf, d_onesf], W=[d_b])
            kb.op("dve", lambda e: e.tensor_copy(out=col[:, 0:1], in_=b_t[:, 63:64]), R=[d_b], W=[d_col])
            kb.op("dve", lambda e: e.tensor_tensor(out=col[:, 1:2], in0=b_t[:, 127:128], in1=b_t[:, 63:64],
                                                    op=ALU.subtract), R=[d_b], W=[d_col])
            kb.op("dve", lambda e: e.tensor_copy(out=col[:, 2:3], in_=b_t[:, 127:128]), R=[d_b], W=[d_col])
            kb.op("dve", lambda e: e.tensor_scalar(out=col[:, 3:4], in0=b_t[:, 63:64], scalar1=-1.0, scalar2=None,
                                                    op0=ALU.mult), R=[d_b], W=[d_col])
            kb.op("act", lambda e: e.activation(out=col[:, 4:7], in_=col[:, 0:3], func=AF.Exp), R=[d_col], W=[d_col])
            kb.op("act", lambda e: e.activation(out=e1[:], in_=b_t[:], func=AF.Exp, bias=col[:, 3:4], scale=1.0),
                  R=[d_b, d_col], W=[d_e1])
            kb.op("act", lambda e: e.activation(out=e2[:], in_=b_t[:], func=AF.Exp, bias=col[:, 0:1], scale=-1.0),
                  R=[d_b, d_col], W=[d_e2])
            kb.op("act", lambda e: e.activation(out=kk[:], in_=lf_t[:, cs], func=AF.Exp), R=[d_lf], W=[d_kk])
            kb.op("pool", lambda e: e.tensor_scalar(out=kk[:], in0=kk[:], scalar1=-1.0, scalar2=1.0, op0=ALU.mult,
                                                     op1=ALU.add), R=[d_kk], W=[d_kk])
            kb.op("dve", lambda e: e.tensor_tensor(out=qd[:], in0=q_t[:, cs], in1=e1[:], op=ALU.mult),
                  R=[d_q, d_e1], W=[d_qd])
            kb.op("dve", lambda e: e.tensor_tensor(out=kd[:], in0=kk[:], in1=e2[:], op=ALU.mult),
                  R=[d_kk, d_e2], W=[d_kd])
            kb.op("dve", lambda e: e.scalar_tensor_tensor(out=qb[:], in0=q_t[:, cs], scalar=col[:, 4:5], in1=e1[:],
                                                           op0=ALU.mult, op1=ALU.mult), R=[d_q, d_col, d_e1], W=[d_qb])
            kb.op("dve", lambda e: e.scalar_tensor_tensor(out=ke[:], in0=kk[:], scalar=col[:, 5:6], in1=e2[:],
                                                           op0=ALU.mult, op1=ALU.mult), R=[d_kk, d_col, d_e2], W=[d_ke])
            psc, d_psc = psc_r[r]
            kb.op("pe", lambda e: e.matmul(psc[:, 0:128], lhsT=kd[:], rhs=qd[:], start=True, stop=True),
                  R=[d_kd, d_qd], W=[d_psc])
            kb.op("dve", lambda e: e.copy_predicated(out=scm[:], mask=mku[:], data=psc[:, 0:128]),
                  R=[d_psc, d_mk], W=[d_scm])
            po, d_po = po_r[r]
            Sb, d_Sb = Sb_r[c % 2]
            kb.op("pe", lambda e: e.matmul(po[:, 0:128], lhsT=scm[:], rhs=v_t[:, c, :], start=True, stop=False),
                  R=[d_scm, d_v], W=[d_po])
            kb.op("pe", lambda e: e.matmul(po[:, 0:128], lhsT=qb[:], rhs=Sb[:], start=False, stop=True),
                  R=[d_qb, d_Sb], W=[d_po])
            pt, d_pt = pt_r[r]
            kb.op("pe", lambda e: e.transpose(out=pt[:, 0, :], in_=ke[:], identity=ident[:]), R=[d_ke, d_id], W=[d_pt])
            kb.op("act", lambda e: e.copy(out=keT[:], in_=pt[:, 0, :]), R=[d_pt], W=[d_keT])
            pu, d_pu = pu_r[r]
            kb.op("pe", lambda e: e.matmul(pu[:, 0:128], lhsT=keT[:], rhs=v_t[:, c, :], start=True, stop=True),
                  R=[d_keT, d_v], W=[d_pu])
            kb.op("dve", lambda e: e.scalar_tensor_tensor(out=S_t[:], in0=S_t[:], scalar=col[:, 6:7], in1=pu[:, 0:128],
                                                           op0=ALU.mult, op1=ALU.add), R=[d_S, d_col, d_pu], W=[d_S])
            Sb2, d_Sb2 = Sb_r[(c + 1) % 2]
            kb.op("pool", lambda e: e.tensor_copy(out=Sb2[:], in_=S_t[:]), R=[d_S], W=[d_Sb2])
            osq, d_osq = osq_r[r]
            oss, d_oss = oss_r[r]
            on_t, d_on = on_r[r]
            kb.op("act", lambda e: e.activation(out=osq[:], in_=po[:, 0:128], func=AF.Square, accum_out=oss[:, 0:1]),
                  R=[d_po], W=[d_osq, d_oss])
            kb.op("act", lambda e: e.activation(out=oss[:, 1:2], in_=oss[:, 0:1], func=AF.Ln, scale=1.0 / 128, bias=EPS),
                  R=[d_oss], W=[d_oss])
            kb.op("act", lambda e: e.activation(out=oss[:, 1:2], in_=oss[:, 1:2], func=AF.Exp, scale=-0.5),
                  R=[d_oss], W=[d_oss])
            kb.op("dve", lambda e: e.scalar_tensor_tensor(out=on_t[:], in0=po[:, 0:128], scalar=oss[:, 1:2], in1=go_t[:],
                                                           op0=ALU.mult, op1=ALU.mult), R=[d_po, d_oss, d_go], W=[d_on])
            kb.dma("sp", on[bh, c * 128:(c + 1) * 128, :], on_t[:], R=[d_on])
    kb.finish()
    return nc


_CACHE = {}


def _prog(key, fn):
    if key not in _CACHE:
        _CACHE[key] = fn()
    return _CACHE[key]


def _run(nc, in_maps):
    res = run_bass_kernel_spmd(nc, in_maps, core_ids=list(range(NCORES)))
    return res.results


def kernel_unfused(**inp):
    inp = {k: np.asarray(v) for k, v in inp.items()}
    x = inp["x"]
    meta = inp["meta_tokens"]
    f32 = np.float32
    metatile = np.zeros((128, D), f32)
    metatile[128 - NMETA:] = meta
    seg = SEQ // 4

    def core_rows(full_b, c, with_meta=True):
        s = 128 + (c % 4) * seg
        body = full_b[s:s + seg]
        return np.concatenate([full_b[0:128], body], 0) if with_meta else body

    def assemble(per_core):
        out = []
        for b in range(2):
            parts = [per_core[4 * b][0:128]] + [per_core[4 * b + j][128:] for j in range(4)]
            out.append(np.concatenate(parts, 0))
        return out

    hA = [np.concatenate([metatile, x[c // 4, (c % 4) * seg:(c % 4 + 1) * seg]], 0) for c in range(NCORES)]
    valid = np.ones((NT * 128, 1), f32)
    valid[0:128 - NMETA] = 0.0

    ncA = _prog("A", lambda: build_A(NT))
    cA = {"gain": inp["fox_norm"][0][None], "w_in": inp["fox_w_in"][0],
          "gq": np.tile(inp["fox_q_norm"][0], FH)[None], "gk": np.tile(inp["fox_k_norm"][0], FH)[None],
          "bf": inp["fox_b_f"][0][None]}
    rA = _run(ncA, [dict(cA, h=hA[c]) for c in range(NCORES)])
    qf = assemble([np.asarray(r["qo"]) for r in rA])
    kf = assemble([np.asarray(r["ko"]) for r in rA])
    vf = assemble([np.asarray(r["vo"]) for r in rA])
    lff = assemble([np.asarray(r["lfo"]) for r in rA])

    ncB = _prog("B", lambda: build_B(NB, 4))
    imB = []
    for c in range(NCORES):
        b = c // 4
        hs = [(4 * c + i) % FH for i in range(4)]
        imB.append({
            "qT": np.ascontiguousarray(np.stack([qf[b][:, h * FD:(h + 1) * FD].T for h in hs])),
            "kT": np.ascontiguousarray(np.stack([kf[b][:, h * FD:(h + 1) * FD].T for h in hs])),
            "v": np.ascontiguousarray(np.stack([vf[b][:, h * FD:(h + 1) * FD] for h in hs])),
            "lfr": np.ascontiguousarray(np.stack([lff[b][:, h] for h in hs])),
            "lfT": np.ascontiguousarray(np.stack([lff[b][:, h].reshape(NB, 128).T for h in hs])),
        })
    rB = _run(ncB, imB)
    oun = [np.zeros((LP, D), f32) for _ in range(2)]
    den = [np.zeros((LP, FH), f32) for _ in range(2)]
    for c in range(NCORES):
        b = c // 4
        o = np.asarray(rB[c]["oT"])
        for i in range(4):
            h = (4 * c + i) % FH
            oun[b][:, h * FD:(h + 1) * FD] = o[i, 0:FD].T
            den[b][:, h] = o[i, FD]

    ncC = _prog("C", lambda: build_CE("C", NT, True))
    cC = {"valid_in": valid, "w_out": inp["fox_w_out"][0], "mnorm": inp["moe_norm"][0][None],
          "w_r": np.ascontiguousarray(np.concatenate([inp["moe_w_grp"][0], inp["moe_w_rt"][0]], 1)),
          "b_r": np.concatenate([inp["moe_b_grp"][0], inp["moe_b_rt"][0]])[None],
          "w_up": inp["moe_w_up"][0], "w_dn": inp["moe_w_down"][0],
          "hnorm": inp["hg_norm"][0][None], "hw_in": inp["hg_w_in"][0], "hbf": inp["hg_b_f"][0][None],
          "lbl": inp["hg_lb_logits"]}
    rC = _run(ncC, [dict(cC, a_in=core_rows(oun[c // 4], c), den_in=core_rows(den[c // 4], c), hp_in=hA[c])
                    for c in range(NCORES)])
    h2 = [np.asarray(r["h2_out"]) for r in rC]
    q1 = assemble([np.asarray(r["q1_out"]) for r in rC])
    lf1 = assemble([np.asarray(r["lf1_out"]) for r in rC])
    v1 = assemble([np.asarray(r["v1_out"]) for r in rC])
    gs = assemble([np.asarray(r["gs_out"]) for r in rC])
    npad = 128 - NMETA
    for b in range(2):
        q1[b][0:npad] = 0
        lf1[b][0:npad] = 0
        v1[b][0:npad] = 0

    ncD = _prog("D", lambda: build_D(NB, 2))
    imD = []
    for c in range(NCORES):
        prs = [2 * c, 2 * c + 1]
        imD.append({
            "qT": np.ascontiguousarray(np.stack([q1[p // HH][:, (p % HH) * 128:(p % HH + 1) * 128].T for p in prs])),
            "lfT": np.ascontiguousarray(np.stack([lf1[p // HH][:, (p % HH) * 128:(p % HH + 1) * 128].T for p in prs])),
            "v": np.ascontiguousarray(np.stack([v1[p // HH][:, (p % HH) * 128:(p % HH + 1) * 128] for p in prs])),
            "go": inp["hg_o_norm"][0][None],
        })
    rD = _run(ncD, imD)
    onf = [np.zeros((LP, D), f32) for _ in range(2)]
    for c in range(NCORES):
        o = np.asarray(rD[c]["on"])
        for i in range(2):
            p = 2 * c + i
            onf[p // HH][:, (p % HH) * 128:(p % HH + 1) * 128] = o[i]

    ncE = _prog("E", lambda: build_CE("E", NTX, False))
    cE = {"valid_in": np.ones((NTX * 128, 1), f32), "w_out": inp["hg_w_out"][0], "mnorm": inp["moe_norm"][1][None],
          "w_r": np.ascontiguousarray(np.concatenate([inp["moe_w_grp"][1], inp["moe_w_rt"][1]], 1)),
          "b_r": np.concatenate([inp["moe_b_grp"][1], inp["moe_b_rt"][1]])[None],
          "w_up": inp["moe_w_up"][1], "w_dn": inp["moe_w_down"][1]}
    rE = _run(ncE, [dict(cE, a_in=core_rows(onf[c // 4], c, False), gs_in=core_rows(gs[c // 4], c, False),
                         hp_in=h2[c][128:]) for c in range(NCORES)])
    out = np.zeros((2, SEQ, D), f32)
    for c in range(NCORES):
        out[c // 4, (c % 4) * seg:(c % 4 + 1) * seg] = np.asarray(rE[c]["h2_out"])
    return out


GROUPS = [[0, 1, 2, 3], [4, 5, 6, 7]]


def build_fused(ntx=NTX, cap=CAP, stop=None):
    assert ntx % 4 == 0
    nt = ntx + 1
    nb = 4 * ntx + 1
    L = nb * 128
    nch = ntx // 4
    TX = ntx * 128
    nI = ntx + 1
    nslot = NE * cap
    nst = cap // 128
    nc = bass.Bass("TRN2", target_bir_lowering=False)

    def din(name, shape, dt=F32):
        return nc.dram_tensor(name, list(shape), dt, kind="ExternalInput").ap()

    def dint(name, shape, dt=BF16):
        return nc.dram_tensor(name, list(shape), dt).ap()

    x_in = din("x_in", [nt * 128, D])
    valid_in = din("valid_in", [nt * 128, 1])
    fnorm = din("fnorm", [1, D])
    wq_d, wk_d, wv_d = din("wq", [D, 256]), din("wk", [D, 256]), din("wv", [D, 256])
    wf_d = din("wf", [D, 4])
    gqc_d, gkc_d = din("gqc", [128, 1]), din("gkc", [128, 1])
    bf4r_d, bf4c_d = din("bf4r", [1, 4]), din("bf4c", [4, 1])
    wo1_d = din("wo1", [D, D])
    idx2_d = din("idx2", [128, 8], I32)
    idx3_d = din("idx3", [128, 8], I32)
    moe_d = []
    for l in range(2):
        if stop in ("T1", "H1p", "H1a") or (stop in ("T2", "H2") and l == 1):
            moe_d.append(None)
            continue
        moe_d.append(dict(mnorm=din("mnorm%d" % l, [1, D]), w_r=din("w_r%d" % l, [D, 36]), b_r=din("b_r%d" % l, [1, 36]),
                          w_up=din("w_up%d" % l, [NE, D, 2 * DE]), w_dn=din("w_dn%d" % l, [NE, DE, D])))
    hnorm = din("hnorm", [1, D])
    hwq_d, hwf_d, hwv_d = din("hwq", [2, D, 128]), din("hwf", [2, D, 128]), din("hwv", [2, D, 128])
    hwg_d = din("hwg", [D, D])
    hbfc_d, l0c_d, l1c_d = din("hbfc", [2, 128, 1]), din("l0c", [2, 128, 1]), din("l1c", [2, 128, 1])
    go_d = din("go", [1, 128])
    wo2_d = din("wo2", [D, D])
    out_d = nc.dram_tensor("out", [TX, D], F32, kind="ExternalOutput").ap()

    MA, MC = dint("MA", [128, 8 * 128]), dint("MC", [128, 8 * 128])
    SA, RA = dint("SA", [nch * 128, 8 * 512]), dint("RA", [nch * 512, 8 * 512])
    SC, RC = dint("SC", [nch * 128, 8 * 512]), dint("RC", [nch * 512, 8 * 512])
    qTs, kTs, vs = dint("qTs", [4, FD + 1, L]), dint("kTs", [4, FD, L]), dint("vs", [L, 256])
    SBb, RBb = dint("SBb", [8 * 128, TX]), dint("RBb", [8 * 512, TX])
    SBm, RBm = dint("SBm", [256, 128]), dint("RBm", [1024, 128])
    h1s, h2s = dint("h1s", [nt * 128, D], F32), dint("h2s", [nt * 128, D], F32)
    xpad, ypad = dint("xpad", [nslot, D]), dint("ypad", [nslot, D])
    GS = dint("GS", [nch * 128, 8 * 512])
    SD, RD = dint("SD", [8 * 128, TX]), dint("RD", [8 * 512, TX])
    d_MA, d_MC = (Dep(n) for n in ("MA", "MC"))
    d_SA = (Dep("SAw"), [Dep("SA%d" % j) for j in range(nch)])
    d_SC = (Dep("SCw"), [Dep("SC%d" % j) for j in range(nch)])
    d_SBc = [Dep("SBb%d" % j) for j in range(8)]
    d_SDc = [Dep("SD%d" % j) for j in range(8)]
    d_RA = [Dep("RA%d" % j) for j in range(nch)]
    d_RC = [Dep("RC%d" % j) for j in range(nch)]
    d_qTs, d_kTs, d_vs, d_SBb, d_RBb, d_SBm, d_RBm = (Dep(n) for n in ("qTs", "kTs", "vs", "SBb", "RBb", "SBm", "RBm"))
    d_h1s, d_h2s, d_xpad, d_ypad, d_GS, d_SD, d_RD = (Dep(n) for n in ("h1s", "h2s", "xpad", "ypad", "GS", "SD", "RD"))

    kb = KB(nc)
    identf, d_idf, ident, d_id = make_ident(kb)
    breg = nc.gpsimd.to_reg(nslot - 1)
    breg2 = nc.gpsimd.to_reg(8 * 512 - 1)

    def dump_and_finish(items):
        for name, ap, dep in items:
            o = nc.dram_tensor("dbg_" + name, list(ap.shape), ap.dtype, kind="ExternalOutput").ap()
            kb.dma("sp", o, ap, R=[dep])
        kb.finish()
        return nc

    def emit_hnT(src_t, d_src, gain_t, d_gain, scr, ssr, xnr, pstr, hcr, t, Mloc, d_Mloc, Ssend, d_S, Rrecv, d_R, hc_hook=None):
        b = t % 2
        sc_t, d_sc = scr[b]
        ss_t, d_ss = ssr[b]
        xn_t, d_xn = xnr[b]
        ps_t, d_pst = pstr[b]
        rms_rstd(kb, src_t[:], d_src, D, sc_t[:], d_sc, ss_t[:], d_ss)
        kb.op("dve", lambda e: e.scalar_tensor_tensor(out=xn_t[:], in0=src_t[:], scalar=ss_t[:, 0:1], in1=gain_t[:],
                                                       op0=ALU.mult, op1=ALU.mult), R=[d_src, d_ss, d_gain], W=[d_xn])
        for c in range(8):
            kb.op("pe", lambda e: e.transpose(out=ps_t[:, c, :], in_=xn_t[:, c * 128:(c + 1) * 128], identity=ident[:]),
                  R=[d_xn, d_id], W=[d_pst])
        if t == 0:
            hc_t, d_hc = hcr[0]
            kb.op("dve", lambda e: e.tensor_copy(out=hc_t[:, :, 0:128], in_=ps_t[:]), R=[d_pst], W=[d_hc])
            kb.dma("sp", Mloc.rearrange("p (c t) -> p c t", c=8), hc_t[:, :, 0:128], R=[d_hc], W=[d_Mloc])
            return
        j, s = (t - 1) // 4, (t - 1) % 4
        hc_t, d_hc = hcr[(j + 1) % 2]
        kb.op("dve", lambda e: e.tensor_copy(out=hc_t[:, :, s * 128:(s + 1) * 128], in_=ps_t[:]), R=[d_pst], W=[d_hc])
        if s == 3:
            kb.dma("sp", Ssend[j * 128:(j + 1) * 128, :], hc_t[:].rearrange("p c t -> p (c t)"), R=[d_hc],
                   W=[d_S[0], d_S[1][j]])
            kb.cc(Ssend[j * 128:(j + 1) * 128, :], Rrecv[j * 512:(j + 1) * 512, :], GROUPS, R=[d_S[1][j]], W=[d_R[j]])
            if hc_hook is not None:
                hc_hook(j, hc_t, d_hc)

    lfall = kb.sb("lfall", [128, nb, 4], F32)
    d_lfall = Dep("lfall")
    with kb.scope():
        g_t, d_g = load_bc(kb, "fnorm", fnorm, D)
        wq_t, d_wq = load_w(kb, "wq", wq_d, D, 256)
        wk_t, d_wk = load_w(kb, "wk", wk_d, D, 256)
        wv_t, d_wv = load_w(kb, "wv", wv_d, D, 256)
        wf_t, d_wf = load_w(kb, "wf", wf_d, D, 4)
        for d in (d_wq, d_wk, d_wv, d_wf):
            d.ro = True
        gqc = kb.sb("gqc", [128, 1], F32); d_gqc = Dep("gqc")
        gkc = kb.sb("gkc", [128, 1], F32); d_gkc = Dep("gkc")
        bf4c = kb.sb("bf4c", [4, 1], F32); d_bf4c = Dep("bf4c")
        kb.dma("sp", gqc[:], gqc_d, W=[d_gqc])
        kb.dma("sp", gkc[:], gkc_d, W=[d_gkc])
        kb.dma("sp", bf4c[:], bf4c_d, W=[d_bf4c])
        kb.op("dve", lambda e: e.tensor_scalar(out=bf4c[:], in0=bf4c[:], scalar1=-1.0, scalar2=None, op0=ALU.mult),
              R=[d_bf4c], W=[d_bf4c])
        kb.op("dve", lambda e: e.tensor_scalar(out=gqc[:], in0=gqc[:], scalar1=float(FD ** -0.5), scalar2=None,
                                                op0=ALU.mult), R=[d_gqc], W=[d_gqc])
        bf4r, d_bf4r = load_bc(kb, "bf4r", bf4r_d, 4)
        blkf = kb.sb("blkf", [128, 128], F32); d_blkf = Dep("blkf")
        blk = kb.sb("blk", [128, 128], BF16); d_blk = Dep("blk")
        ones1 = kb.sb("ones1", [128, 512], F32); d_ones1 = Dep("ones1")
        kb.op("pool", lambda e: e.memset(blkf[:], 0.0), W=[d_blkf])
        kb.op("pool", lambda e: e.memset(blkf[0:64, 0:64], 1.0), W=[d_blkf])
        kb.op("pool", lambda e: e.memset(blkf[64:128, 64:128], 1.0), W=[d_blkf])
        kb.op("dve", lambda e: e.tensor_copy(out=blk[:], in_=blkf[:]), R=[d_blkf], W=[d_blk])
        kb.op("pool", lambda e: e.memset(ones1[:], 1.0), W=[d_ones1])
        d_blk.ro = True
        d_ones1.ro = True
        xin = kb.ring("xin", [128, D], F32, 4)
        scr = kb.ring("scr", [128, D], F32, 2)
        ssr = kb.ring("ss", [128, 1], F32, 2)
        xnr = kb.ring("xn", [128, D], BF16, 2)
        hcr = kb.ring("hc", [128, 8, 512], BF16, 2)
        pstr = kb.ring("pst", [128, 8, 128], BF16, 2, psum=True)
        def t1_load(t):
            x_t, d_x = xin[t % 4]
            kb.dma("sp", x_t[:], x_in[t * 128:(t + 1) * 128, :], W=[d_x])
        for t in range(min(3, nt)):
            t1_load(t)
        for t in range(nt):
            if t + 3 < nt:
                t1_load(t + 3)
            x_t, d_x = xin[t % 4]
            emit_hnT(x_t, d_x, g_t, d_g, scr, ssr, xnr, pstr, hcr, t, MA, d_MA, SA, d_SA, RA, d_RA)

        if stop == "T1":
            kb.barrier()
            return dump_and_finish([("RA", RA, d_RA[nch - 1]), ("MA", MA, d_MA)])
        hgr = kb.ring("hg", [128, 8, 512], BF16, 2)
        sqr = kb.ring("sq", [128, 512], BF16, 2)
        rsr = kb.ring("rs", [128, 512], F32, 2)
        qnr = kb.ring("qn", [128, 512], BF16, 2)
        vsr = kb.ring("vsb", [128, 256], BF16, 2)
        lzr = kb.ring("lz", [128, 4], F32, 2)
        lrr = kb.ring("lr", [4, 512], F32, 2)
        drr = kb.ring("dr", [4, 512], BF16, 2)
        pqr = kb.ring("pq", [128, 512], F32, 2, psum=True)
        pssr = kb.ring("pss", [128, 512], F32, 1, psum=True)
        pvr = kb.ring("pv", [128, 512], F32, 2, psum=True)
        pfr = kb.ring("pf", [128, 512], F32, 1, psum=True)
        qi = 0
        vi = 0
        def h1_load(G):
            hg, d_hg = hgr[G % 2]
            if G == 0:
                kb.dma("sp", hg[:, :, 0:128], MA.rearrange("p (c t) -> p c t", c=8), R=[d_MA], W=[d_hg])
            else:
                j, rank = (G - 1) // 4, (G - 1) % 4
                r0 = j * 512 + rank * 128
                kb.dma("sp", hg[:].rearrange("p c t -> p (c t)"), RA[r0:r0 + 128, :], R=[d_RA[j]], W=[d_hg])
        h1_load(0)
        for G in range(1 + 4 * nch):
            hg, d_hg = hgr[G % 2]
            if G + 1 < 1 + 4 * nch:
                h1_load(G + 1)
            if G == 0:
                n, tok0 = 128, 0
            else:
                n, tok0 = 512, 128 + ((G - 1) % 4) * TX + ((G - 1) // 4) * 512
            for (w_t, d_w, gc, d_gc, dst, d_dst) in ((wq_t, d_wq, gqc, d_gqc, qTs, d_qTs), (wk_t, d_wk, gkc, d_gkc, kTs, d_kTs)):
                for pr in range(2):
                    pq, d_pq = pqr[qi % 2]
                    pss, d_pss = pssr[0]
                    sq, d_sq = sqr[qi % 2]
                    rs, d_rs = rsr[qi % 2]
                    qn, d_qn = qnr[qi % 2]
                    qi += 1
                    for c in range(8):
                        kb.op("pe", lambda e: e.matmul(pq[:, 0:n], lhsT=w_t[:, c, pr * 128:(pr + 1) * 128], rhs=hg[:, c, 0:n],
                                                       start=(c == 0), stop=(c == 7)), R=[d_w, d_hg], W=[d_pq])
                    kb.op("act", lambda e: e.activation(out=sq[:, 0:n], in_=pq[:, 0:n], func=AF.Square), R=[d_pq], W=[d_sq])
                    kb.op("pe", lambda e: e.matmul(pss[:, 0:n], lhsT=blk[:], rhs=sq[:, 0:n], start=True, stop=True),
                          R=[d_blk, d_sq], W=[d_pss])
                    kb.op("act", lambda e: e.activation(out=rs[:, 0:n], in_=pss[:, 0:n], func=AF.Ln, scale=1.0 / FD, bias=EPS),
                          R=[d_pss], W=[d_rs])
                    kb.op("act", lambda e: e.activation(out=rs[:, 0:n], in_=rs[:, 0:n], func=AF.Exp, scale=-0.5), R=[d_rs], W=[d_rs])
                    kb.op("dve", lambda e: e.scalar_tensor_tensor(out=qn[:, 0:n], in0=pq[:, 0:n], scalar=gc[:, 0:1], in1=rs[:, 0:n],
                                                                   op0=ALU.mult, op1=ALU.mult), R=[d_pq, d_gc, d_rs], W=[d_qn])
                    for hh in range(2):
                        kb.dma("sp", dst[2 * pr + hh, 0:FD, tok0:tok0 + n], qn[hh * 64:(hh + 1) * 64, 0:n], R=[d_qn], W=[d_dst])
            for s in range(n // 128):
                pv, d_pv = pvr[vi % 2]
                vsb, d_vsb = vsr[vi % 2]
                lz, d_lz = lzr[vi % 2]
                vi += 1
                for c in range(8):
                    kb.op("pe", lambda e: e.matmul(pv[:, 0:256], lhsT=hg[:, c, s * 128:(s + 1) * 128], rhs=wv_t[:, c, :],
                                                   start=(c == 0), stop=(c == 7)), R=[d_wv, d_hg], W=[d_pv])
                for c in range(8):
                    kb.op("pe", lambda e: e.matmul(pv[:, 256:260], lhsT=hg[:, c, s * 128:(s + 1) * 128], rhs=wf_t[:, c, :],
                                                   start=(c == 0), stop=(c == 7)), R=[d_wf, d_hg], W=[d_pv])
                kb.op("act", lambda e: e.copy(out=vsb[:], in_=pv[:, 0:256]), R=[d_pv], W=[d_vsb])
                kb.dma("sp", vs[tok0 + s * 128:tok0 + (s + 1) * 128, :], vsb[:], R=[d_vsb], W=[d_vs])
                blk_i = tok0 // 128 + s
                kb.op("dve", lambda e: e.tensor_tensor(out=lz[:], in0=pv[:, 256:260], in1=bf4r[:], op=ALU.add),
                      R=[d_pv, d_bf4r], W=[d_lz])
                kb.op("act", lambda e: e.activation(out=lz[:], in_=lz[:], func=AF.Exp, scale=-1.0), R=[d_lz], W=[d_lz])
                kb.op("act", lambda e: e.activation(out=lz[:], in_=lz[:], func=AF.Ln, bias=1.0), R=[d_lz], W=[d_lz])
                kb.op("dve", lambda e: e.tensor_scalar(out=lfall[:, blk_i, :], in0=lz[:], scalar1=-1.0, scalar2=None,
                                                        op0=ALU.mult), R=[d_lz], W=[d_lfall])
            pf, d_pf = pfr[0]
            lr, d_lr = lrr[G % 2]
            dr, d_dr = drr[G % 2]
            for c in range(8):
                kb.op("pe", lambda e: e.matmul(pf[0:4, 0:n], lhsT=wf_t[:, c, :], rhs=hg[:, c, 0:n], start=(c == 0), stop=(c == 7)),
                      R=[d_wf, d_hg], W=[d_pf])
            kb.op("act", lambda e: e.activation(out=lr[:, 0:n], in_=pf[0:4, 0:n], func=AF.Exp, scale=-1.0, bias=bf4c[:, 0:1]),
                  R=[d_pf, d_bf4c], W=[d_lr])
            kb.op("act", lambda e: e.activation(out=lr[:, 0:n], in_=lr[:, 0:n], func=AF.Ln, bias=1.0), R=[d_lr], W=[d_lr])
            kb.op("dve", lambda e: e.tensor_tensor_scan(out=lr[:, 0:n], data0=ones1[0:4, 0:n], data1=lr[:, 0:n], initial=0.0,
                                                         op0=ALU.mult, op1=ALU.subtract), R=[d_lr, d_ones1], W=[d_lr])
            kb.op("dve", lambda e: e.tensor_copy(out=dr[:, 0:n], in_=lr[:, 0:n]), R=[d_lr], W=[d_dr])
            kb.dma("sp", qTs[:, FD, tok0:tok0 + n], dr[:, 0:n], R=[d_dr], W=[d_qTs])

    if stop == "H1p":
        return dump_and_finish([("qTs", qTs, d_qTs), ("kTs", kTs, d_kTs), ("vs", vs, d_vs)])
    with kb.scope():
        trif = kb.sb("trif", [128, 128], F32); d_tri = Dep("trif")
        onesf = kb.sb("onesf", [128, 128], F32); d_ones = Dep("onesf")
        sel0 = kb.sb("sel0", [128, 128], F32); d_sel = Dep("sel0")
        maskb = kb.sb("maskb", [128, 128], BF16); d_mask = Dep("maskb")
        onerow = kb.sb("onerow", [128, nb], F32); d_or = Dep("onerow")
        kb.op("pool", lambda e: e.memset(onesf[:], 1.0), W=[d_ones])
        kb.op("pool", lambda e: e.memset(onerow[:], 1.0), W=[d_or])
        kb.op("pool", lambda e: e.affine_select(out=trif[:], in_=onesf[:], pattern=[[1, 128]], compare_op=ALU.is_ge,
                                                fill=0.0, base=0, channel_multiplier=-1), R=[d_ones], W=[d_tri])
        kb.op("pool", lambda e: e.affine_select(out=sel0[:], in_=onesf[:], pattern=[[0, 128]], compare_op=ALU.is_ge,
                                                fill=0.0, base=0, channel_multiplier=-1), R=[d_ones], W=[d_sel])
        kb.op("dve", lambda e: e.tensor_copy(out=maskb[:], in_=trif[:]), R=[d_tri], W=[d_mask])
        for d in (d_tri, d_ones, d_sel, d_mask, d_or):
            d.ro = True
        QA = kb.sb("QA", [FD + 1, L], BF16); d_QA = Dep("QA")
        KAr = kb.ring("KA", [FD + 1, L], BF16, 2)
        VAr = kb.ring("VA", [128, nb, 128], BF16, 2)
        lft = kb.sb("lft", [128, nb], F32); d_lft = Dep("lft")
        ct = kb.sb("ct", [128, nb], F32); d_ct = Dep("ct")
        tot = kb.sb("tot", [128, nb], F32); d_tot = Dep("tot")
        rall = kb.sb("rall", [128, nb], F32); d_rall = Dep("rall")
        biasr = kb.ring("bias", [128, nb], F32, 2)
        pr_ = kb.ring("P", [128, 512], BF16, 4)
        denr = kb.ring("den", [128, 512], F32, 2)
        rdr = kb.ring("rden", [64, 512], F32, 2)
        onr = kb.ring("onb", [64, 512], BF16, 2)
        psS = kb.ring("psS", [128, 512], F32, 4, psum=True)
        psO = kb.ring("psO", [128, 512], F32, 2, psum=True)
        psM = kb.ring("psM", [128, 512], F32, 2, psum=True)
        for (KA_, d_KA_), (VA_, d_VA_) in zip(KAr, VAr):
            kb.op("pool", lambda e: e.memset(KA_[FD:FD + 1, :], 1.0), W=[d_KA_])
            kb.op("pool", lambda e: e.memset(VA_[:, :, FD:128], 1.0), W=[d_VA_])

        def kv_load(hx):
            KA_, d_KA_ = KAr[hx % 2]
            VA_, d_VA_ = VAr[hx % 2]
            kb.dma("sp", KA_[0:FD, :], kTs[hx], R=[d_kTs], W=[d_KA_])
            kb.dma("sp", VA_[:, :, 0:FD], vs[:, hx * FD:(hx + 1) * FD].rearrange("(j p) d -> p j d", p=128), R=[d_vs], W=[d_VA_])
            kb.op("pool", lambda e: e.memset(VA_[0:112, 0, :], 0.0), W=[d_VA_])

        kv_load(0)
        for h4 in range(4):
            pr4, hh4 = h4 // 2, h4 % 2
            KA, d_KA = KAr[h4 % 2]
            VA, d_VA = VAr[h4 % 2]
            kb.dma("sp", QA[:, :], qTs[h4], R=[d_qTs], W=[d_QA])
            if h4 + 1 < 4:
                kv_load(h4 + 1)
            kb.op("dve", lambda e: e.tensor_copy(out=lft[:], in_=lfall[:, :, h4]), R=[d_lfall], W=[d_lft])
            pm0, d_pm0 = psM[0]
            pm1, d_pm1 = psM[1]
            kb.op("pe", lambda e: e.matmul(pm0[:, 0:nb], lhsT=trif[:], rhs=lft[:], start=True, stop=True),
                  R=[d_tri, d_lft], W=[d_pm0])
            kb.op("pe", lambda e: e.matmul(pm1[:, 0:nb], lhsT=onesf[:], rhs=lft[:], start=True, stop=True),
                  R=[d_ones, d_lft], W=[d_pm1])
            kb.op("dve", lambda e: e.tensor_copy(out=tot[:], in_=pm1[:, 0:nb]), R=[d_pm1], W=[d_tot])
            kb.op("dve", lambda e: e.tensor_tensor_scan(out=ct[:], data0=onerow[:], data1=tot[:], initial=0.0,
                                                         op0=ALU.mult, op1=ALU.add), R=[d_tot, d_or], W=[d_ct])
            kb.op("dve", lambda e: e.tensor_tensor(out=ct[:], in0=ct[:], in1=tot[:], op=ALU.subtract),
                  R=[d_ct, d_tot], W=[d_ct])
            kb.op("dve", lambda e: e.tensor_tensor(out=ct[:], in0=ct[:], in1=pm0[:, 0:nb], op=ALU.add),
                  R=[d_ct, d_pm0], W=[d_ct])
            kb.op("pe", lambda e: e.matmul(pm1[:, 0:nb], lhsT=sel0[:], rhs=ct[:], start=True, stop=True),
                  R=[d_sel, d_ct], W=[d_pm1])
            kb.op("dve", lambda e: e.tensor_copy(out=rall[:], in_=pm1[:, 0:nb]), R=[d_pm1], W=[d_rall])
            steps = []
            for I in range(nI):
                j0 = 0 if I == 0 else 4 * I - 3
                nblk = 1 if I == 0 else 4
                nJ = j0 + nblk
                for J in range(nJ):
                    steps.append((I, J, j0, nblk, nJ))
            LA = 2
            for idx in range(len(steps) + LA):
                if idx < len(steps):
                    I, J, j0, nblk, nJ = steps[idx]
                    q0 = j0 * 128
                    bias_t, d_bias = biasr[I % 2]
                    if J == 0:
                        kb.op("dve", lambda e: e.tensor_scalar(out=bias_t[:, 0:nJ], in0=ct[:, 0:nJ], scalar1=-1.0,
                                                                scalar2=rall[:, j0:j0 + 1], op0=ALU.mult, op1=ALU.add),
                              R=[d_ct, d_rall], W=[d_bias])
                    m = max(0, J - j0)
                    c0 = m * 128
                    c1 = nblk * 128
                    ps, d_ps = psS[idx % 4]
                    p_t, d_p = pr_[idx % 4]
                    kb.op("pe", lambda e: e.matmul(ps[:, c0:c1], lhsT=KA[:, J * 128:(J + 1) * 128],
                                                   rhs=QA[:, q0 + c0:q0 + c1], start=True, stop=True),
                          R=[d_KA, d_QA], W=[d_ps])
                    kb.op("act", lambda e: e.activation(out=p_t[:, c0:c1], in_=ps[:, c0:c1], func=AF.Exp,
                                                        bias=bias_t[:, J:J + 1], scale=1.0),
                          R=[d_ps, d_bias], W=[d_p])
                    if J >= j0:
                        kb.op("dve", lambda e: e.tensor_tensor(out=p_t[:, c0:c0 + 128], in0=p_t[:, c0:c0 + 128],
                                                                in1=maskb[:], op=ALU.mult), R=[d_p, d_mask], W=[d_p])
                if idx >= LA:
                    I, J, j0, nblk, nJ = steps[idx - LA]
                    m = max(0, J - j0)
                    c0 = m * 128
                    c1 = nblk * 128
                    p_t, d_p = pr_[(idx - LA) % 4]
                    po, d_po = psO[I % 2]
                    kb.op("pe", lambda e: e.matmul(po[:, c0:c1], lhsT=VA[:, J, :], rhs=p_t[:, c0:c1],
                                                   start=(J == 0), stop=(J == nJ - 1)), R=[d_VA, d_p], W=[d_po])
                    if J == nJ - 1:
                        ncol = nblk * 128
                        den, d_den = denr[I % 2]
                        rd, d_rd = rdr[I % 2]
                        onb, d_onb = onr[I % 2]
                        kb.op("dve", lambda e: e.tensor_scalar(out=den[64:128, 0:ncol], in0=po[64:128, 0:ncol], scalar1=1e-30,
                                                                scalar2=None, op0=ALU.max), R=[d_po], W=[d_den])
                        kb.dma("sp", rd[:, 0:ncol], den[64:128, 0:ncol], R=[d_den], W=[d_rd])
                        kb.op("dve", lambda e: e.reciprocal(out=rd[:, 0:ncol], in_=rd[:, 0:ncol]), R=[d_rd], W=[d_rd])
                        kb.op("dve", lambda e: e.tensor_tensor(out=onb[:, 0:ncol], in0=po[0:64, 0:ncol], in1=rd[:, 0:ncol],
                                                                op=ALU.mult), R=[d_po, d_rd], W=[d_onb])
                        if I == 0:
                            kb.dma("sp", SBm[h4 * 64:(h4 + 1) * 64, :], onb[:, 0:128], R=[d_onb], W=[d_SBm])
                        else:
                            off = (I - 1) * 512
                            qq, col = off // TX, off % TX
                            r0 = (pr4 * 4 + qq) * 128 + hh4 * 64
                            cidx = pr4 * 4 + qq
                            kb.dma("sp", SBb[r0:r0 + 64, col:col + 512], onb[:, 0:512], R=[d_onb], W=[d_SBb, d_SBc[cidx]])
                            if hh4 == 1 and (off + 512) % TX == 0:
                                kb.cc(SBb[cidx * 128:(cidx + 1) * 128, :], RBb[cidx * 512:(cidx + 1) * 512, :], GROUPS,
                                      R=[d_SBc[cidx]], W=[d_RBb])
        kb.cc(SBm[:, :], RBm[:, :], GROUPS, R=[d_SBm], W=[d_RBm])

    if stop == "H1a":
        return dump_and_finish([("RBb", RBb, d_RBb), ("RBm", RBm, d_RBm)])
    usb = kb.sb("usb", [128, 128], BF16); d_us = Dep("usb")
    onesb = kb.sb("onesb", [128, 128], BF16); d_onesb = Dep("onesb")
    onesf2 = kb.sb("onesf2", [128, 128], F32); d_onesf2 = Dep("onesf2")
    tmpf = kb.sb("tmpf", [128, 128], F32); d_tmpf = Dep("tmpf")
    kb.op("pool", lambda e: e.memset(onesf2[:], 1.0), W=[d_onesf2])
    kb.op("pool", lambda e: e.affine_select(out=tmpf[:], in_=onesf2[:], pattern=[[1, 128]], compare_op=ALU.is_gt,
                                            fill=0.0, base=0, channel_multiplier=-1), R=[d_onesf2], W=[d_tmpf])
    kb.op("dve", lambda e: e.tensor_copy(out=usb[:], in_=tmpf[:]), R=[d_tmpf], W=[d_us])
    kb.op("dve", lambda e: e.tensor_copy(out=onesb[:], in_=onesf2[:]), R=[d_onesf2], W=[d_onesb])
    for d in (d_us, d_onesb, d_onesf2):
        d.ro = True
    dest_i = kb.sb("dest_i", [128, nt, 2], I32); d_dest = Dep("dest_i")
    gw = kb.sb("gw", [128, nt, 2], F32); d_gw = Dep("gw")
    cntb = kb.sb("cntb", [128, NE], F32); d_cntb = Dep("cntb")

    def tok_stage(l, tiles, has_meta, setup_lhsT, hp_ap, wo_d, pass3_setup, pass3_tile):
        md = moe_d[l]
        kb.op("pool", lambda e: e.iota(cntb[:], pattern=[[cap, NE]], base=0, channel_multiplier=0,
                                       allow_small_or_imprecise_dtypes=True), W=[d_cntb])
        with kb.scope():
            mn_t, d_mn = load_bc(kb, "mnorm", md["mnorm"], D)
            br_t, d_br = load_bc(kb, "b_r", md["b_r"], 36)
            wr_t, d_wr = load_w(kb, "w_r", md["w_r"], D, 36, dt=F32)
            d_wr.ro = True
            with kb.scope():
                wo_t, d_wo = load_w(kb, "w_out", wo_d, D, D)
                d_wo.ro = True
                get_lhsT = setup_lhsT()
                hp_r = kb.ring("hp", [128, D], F32, 4)
                h1_r = kb.ring("h1", [128, D], F32, 2)
                scr_r = kb.ring("scr", [128, D], F32, 2)
                ss_r = kb.ring("ss", [128, 1], F32, 2)
                hmf_r = kb.ring("hmf", [128, D], F32, 2)
                hmb_r = kb.ring("hmb", [128, D], BF16, 6)
                hmT_r = kb.ring("hmT", [128, 8, 128], F32, 2)
                sm_r = kb.ring("sm", [128, 256], F32, 4)
                ab_r = kb.ring("ab", [128, NE], BF16, 4)
                val_r = kb.ring("val", [128, 2], F32, 4)
                pstf_r = kb.ring("pstf", [128, 8, 128], F32, 1, psum=True)
                pmm_r = kb.ring("pmm", [128, 512], F32, 2, psum=True)
                prt_r = kb.ring("prt", [128, 512], F32, 4, psum=True)
                def p1_load(ti):
                    hp_t, d_hp = hp_r[ti % 4]
                    kb.dma("sp", hp_t[:], hp_ap(tiles[ti]), R=[d_h2s], W=[d_hp])
                    if hasattr(get_lhsT, "prefetch"):
                        get_lhsT.prefetch(tiles[ti])

                def front(ti):
                    t = tiles[ti]
                    b = ti % 2
                    r0, r1 = t * 128, (t + 1) * 128
                    hp_t, d_hp = hp_r[ti % 4]
                    if ti + 3 < len(tiles):
                        p1_load(ti + 3)
                    lhsT, d_lhsT = get_lhsT(t)
                    h1_t, d_h1 = h1_r[b]
                    for half in range(2):
                        p_t, d_p = pmm_r[half]
                        for c in range(8):
                            kb.op("pe", lambda e: e.matmul(p_t[:], lhsT=lhsT(c), rhs=wo_t[:, c, half * 512:(half + 1) * 512],
                                                           start=(c == 0), stop=(c == 7)), R=d_lhsT + [d_wo], W=[d_p])
                        kb.op("dve", lambda e: e.tensor_tensor(out=h1_t[:, half * 512:(half + 1) * 512], in0=p_t[:],
                                                                in1=hp_t[:, half * 512:(half + 1) * 512], op=ALU.add),
                              R=[d_p, d_hp], W=[d_h1])
                    kb.dma("sp", h1s[r0:r1, :], h1_t[:], R=[d_h1], W=[d_h1s])
                    sc_t, d_sc = scr_r[b]
                    ss_t, d_ss = ss_r[b]
                    hmf_t, d_hmf = hmf_r[b]
                    hmb_t, d_hmb = hmb_r[ti % 6]
                    rms_rstd(kb, h1_t[:], d_h1, D, sc_t[:], d_sc, ss_t[:], d_ss)
                    kb.op("dve", lambda e: e.scalar_tensor_tensor(out=hmf_t[:], in0=h1_t[:], scalar=ss_t[:, 0:1], in1=mn_t[:],
                                                                   op0=ALU.mult, op1=ALU.mult),
                          R=[d_h1, d_ss, d_mn], W=[d_hmf])
                    kb.op("pool", lambda e: e.tensor_copy(out=hmb_t[:], in_=hmf_t[:]), R=[d_hmf], W=[d_hmb])

                def front2(ti):
                    t = tiles[ti]
                    b = ti % 2
                    r0, r1 = t * 128, (t + 1) * 128
                    hmf_t, d_hmf = hmf_r[b]
                    hmb_t, d_hmb = hmb_r[ti % 6]
                    pf_t, d_pf = pstf_r[0]
                    hmT_t, d_hmT = hmT_r[b]
                    for c in range(8):
                        kb.op("pe", lambda e: e.transpose(out=pf_t[:, c, :], in_=hmf_t[:, c * 128:(c + 1) * 128],
                                                          identity=identf[:]), R=[d_hmf, d_idf], W=[d_pf])
                    kb.op("act", lambda e: e.copy(out=hmT_t[:], in_=pf_t[:]), R=[d_pf], W=[d_hmT])
                    pr_t, d_pr = prt_r[ti % 4]
                    for c in range(8):
                        kb.op("pe", lambda e: e.matmul(pr_t[:, 0:36], lhsT=hmT_t[:, c, :], rhs=wr_t[:, c, :],
                                                       start=(c == 0), stop=(c == 7)), R=[d_hmT, d_wr], W=[d_pr])
                    return dict(t=t, b=ti % 4, r0=r0, r1=r1, hmb_t=hmb_t, d_hmb=d_hmb, pr_t=pr_t, d_pr=d_pr)

                def route_ops(cx):
                    t, b, r0, r1 = cx["t"], cx["b"], cx["r0"], cx["r1"]
                    hmb_t, d_hmb, pr_t, d_pr = cx["hmb_t"], cx["d_hmb"], cx["pr_t"], cx["d_pr"]
                    ops = []
                    add = ops.append
                    sm, d_sm = sm_r[b]
                    lg = sm[:, 0:36]
                    gmax = sm[:, 36:37]
                    ngmax = sm[:, 37:38]
                    gsum = sm[:, 38:39]
                    ggate = sm[:, 39:40]
                    eg = sm[:, 40:44]
                    ohg = sm[:, 44:48]
                    esel = sm[:, 48:56]
                    top8 = sm[:, 56:64]
                    oh0 = sm[:, 64:72]
                    oh1 = sm[:, 72:80]
                    A0 = sm[:, 80:112]
                    A1 = sm[:, 112:144]
                    sbt = sm[:, 144:176]
                    tmp = sm[:, 176:208]
                    destf = sm[:, 208:210]
                    diff = sm[:, 210:211]
                    sg = sm[:, 211:212]
                    S = [d_sm]
                    dv = lambda fn, R=(), W=(): kb.op("dve", fn, R=list(R) + S, W=list(W) + S)
                    add(lambda: dv(lambda e: e.tensor_tensor(out=lg, in0=pr_t[:, 0:36], in1=br_t[:], op=ALU.add), R=[d_pr, d_br]))
                    add(lambda: dv(lambda e: e.tensor_reduce(out=gmax, in_=lg[:, 0:4], axis=AX.X, op=ALU.max)))
                    add(lambda: dv(lambda e: e.tensor_scalar(out=ngmax, in0=gmax, scalar1=-1.0, scalar2=None, op0=ALU.mult)))
                    add(lambda: kb.op("act", lambda e: e.activation(out=eg, in_=lg[:, 0:4], func=AF.Exp, bias=ngmax, scale=1.0,
                                                                    accum_out=gsum), R=S, W=S))
                    add(lambda: dv(lambda e: e.reciprocal(out=ggate, in_=gsum)))
                    add(lambda: dv(lambda e: e.tensor_scalar(out=ohg, in0=lg[:, 0:4], scalar1=gmax, scalar2=None, op0=ALU.is_equal)))
                    add(lambda: dv(lambda e: e.tensor_scalar(out=esel, in0=lg[:, 4:12], scalar1=ohg[:, 0:1], scalar2=None, op0=ALU.mult)))
                    for g in range(1, 4):
                        add(lambda g=g: dv(lambda e: e.scalar_tensor_tensor(out=esel, in0=lg[:, 4 + 8 * g:12 + 8 * g],
                                                                            scalar=ohg[:, g:g + 1], in1=esel, op0=ALU.mult, op1=ALU.add)))
                    add(lambda: dv(lambda e: e.max(out=top8, in_=esel)))
                    add(lambda: dv(lambda e: e.tensor_scalar(out=oh0, in0=esel, scalar1=top8[:, 0:1], scalar2=None, op0=ALU.is_equal)))
                    add(lambda: dv(lambda e: e.tensor_scalar(out=oh1, in0=esel, scalar1=top8[:, 1:2], scalar2=None, op0=ALU.is_equal)))
                    for (Ak, ohk) in ((A0, oh0), (A1, oh1)):
                        add(lambda Ak=Ak, ohk=ohk: dv(lambda e: e.tensor_tensor(
                            out=Ak.rearrange("p (g j) -> p g j", j=8), in0=ohg.unsqueeze(2).to_broadcast([128, 4, 8]),
                            in1=ohk.unsqueeze(1).to_broadcast([128, 4, 8]), op=ALU.mult)))
                    use_valid = has_meta and t == 0
                    val_t, d_val = val_r[b]
                    if use_valid:
                        add(lambda: kb.dma("sp", val_t[:, 0:1], valid_in[r0:r1, :], W=[d_val]))
                        add(lambda: kb.op("dve", lambda e: e.tensor_scalar(out=val_t[:, 1:2], in0=val_t[:, 0:1], scalar1=-BIGIDX,
                                                                            scalar2=BIGIDX, op0=ALU.mult, op1=ALU.add),
                                          R=[d_val], W=[d_val]))
                        for Ak in (A0, A1):
                            add(lambda Ak=Ak: dv(lambda e: e.tensor_scalar(out=Ak, in0=Ak, scalar1=val_t[:, 0:1], scalar2=None,
                                                                           op0=ALU.mult), R=[d_val]))
                    ab_t, d_ab = ab_r[b]
                    add(lambda: kb.op("dve", lambda e: e.tensor_tensor(out=ab_t[:], in0=A0, in1=A1, op=ALU.add), R=S, W=[d_ab]))
                    add(lambda: kb.op("pe", lambda e: e.matmul(pr_t[:, 64:96], lhsT=usb[:], rhs=ab_t[:], start=True, stop=True),
                                      R=[d_us, d_ab], W=[d_pr]))
                    add(lambda: kb.op("pe", lambda e: e.matmul(pr_t[:, 96:128], lhsT=onesb[:], rhs=ab_t[:], start=True, stop=True),
                                      R=[d_onesb, d_ab], W=[d_pr]))

                    def cnt_ops():
                        dv(lambda e: e.tensor_tensor(out=sbt, in0=pr_t[:, 64:96], in1=cntb[:], op=ALU.add), R=[d_pr, d_cntb])
                        kb.op("dve", lambda e: e.tensor_tensor(out=cntb[:], in0=cntb[:], in1=pr_t[:, 96:128], op=ALU.add),
                              R=[d_pr, d_cntb] + S, W=[d_cntb])
                    add(cnt_ops)
                    for k, Ak in enumerate((A0, A1)):
                        add(lambda Ak=Ak: dv(lambda e: e.tensor_tensor(out=tmp, in0=Ak, in1=sbt, op=ALU.mult)))
                        add(lambda k=k: dv(lambda e: e.tensor_reduce(out=destf[:, k:k + 1], in_=tmp, axis=AX.X, op=ALU.add)))
                    if use_valid:
                        add(lambda: dv(lambda e: e.tensor_scalar(out=destf, in0=destf, scalar1=val_t[:, 0:1], scalar2=val_t[:, 1:2],
                                                                 op0=ALU.mult, op1=ALU.add), R=[d_val]))
                    add(lambda: kb.op("dve", lambda e: e.tensor_copy(out=dest_i[:, t, :], in_=destf), R=S, W=[d_dest]))
                    add(lambda: dv(lambda e: e.tensor_tensor(out=diff, in0=top8[:, 0:1], in1=top8[:, 1:2], op=ALU.subtract)))
                    add(lambda: kb.op("act", lambda e: e.activation(out=sg, in_=diff, func=AF.Sigmoid), R=S, W=S))
                    add(lambda: kb.op("dve", lambda e: e.tensor_tensor(out=gw[:, t, 0:1], in0=sg, in1=ggate, op=ALU.mult), R=S, W=[d_gw]))
                    add(lambda: kb.op("dve", lambda e: e.tensor_tensor(out=gw[:, t, 1:2], in0=ggate, in1=gw[:, t, 0:1], op=ALU.subtract),
                                      R=S + [d_gw], W=[d_gw]))
                    for k in range(2):
                        add(lambda k=k: kb.idma(out=xpad[:, :], out_off=bass.IndirectOffsetOnAxis(ap=dest_i[:, t, k:k + 1], axis=0),
                                                in_=hmb_t[:], in_off=None, R=[d_hmb, d_dest], W=[d_xpad],
                                                bounds_check=breg, oob_is_err=False))
                    return ops

                p1_load(0)

                def emit_routes(pair):
                    lists = [route_ops(cxs[i]) for i in pair]
                    for k in range(max(len(l) for l in lists)):
                        for l in lists:
                            if k < len(l):
                                l[k]()

                cxs = {}
                ntl = len(tiles)
                for ti in range(1, min(3, ntl)):
                    p1_load(ti)
                front(0)
                pending = None
                for ti in range(ntl):
                    if ti + 1 < ntl:
                        front(ti + 1)
                    cxs[ti] = front2(ti)
                    if ti % 2 == 1 or ti == ntl - 1:
                        pair = (ti - 1, ti) if ti % 2 == 1 else (ti,)
                        if pending is not None:
                            emit_routes(pending)
                        pending = pair
                emit_routes(pending)
            with kb.scope():
                wup_r = kb.ring("wup", [128, 8, 2 * DE], BF16, 2)
                wdn_r = kb.ring("wdn", [128, 4, D], BF16, 2)
                wst_r = kb.ring("wst", [128, D], F32, 12)
                wdeps = [[Dep("w%d_%d" % (bb, cc)) for cc in range(12)] for bb in range(2)]
                xs_r = kb.ring("xs", [128, D], BF16, 2 * nst)
                xT_r = kb.ring("xT", [128, 8, cap], BF16, 2)
                sa_r = kb.ring("sa", [128, cap], F32, 2)
                aT_r = kb.ring("aT", [128, 4, cap], BF16, 2)
                yb_r = kb.ring("yb", [128, D], BF16, 2)
                pst_r = kb.ring("pst", [128, 8, 128], BF16, 2, psum=True)
                pau_r = kb.ring("pau", [128, 512], F32, 4, psum=True)
                py_r = kb.ring("py", [128, 512], F32, 2, psum=True)
                CE = ("act", "dve", "act", "dve", "act", "dve", "act", "dve", "act", "dve", "act", "dve")

                def w_dma(ex):
                    wuv = md["w_up"][ex].rearrange("(c p) n -> p c n", p=128)
                    wdv = md["w_dn"][ex].rearrange("(c p) n -> p c n", p=128)
                    for c in range(12):
                        stg, d_stg = wst_r[c]
                        kb.dma("sp", stg[:], wuv[:, c, :] if c < 8 else wdv[:, c - 8, :], W=[d_stg])

                def w_cast(ex, cs):
                    bb = ex % 2
                    for c in cs:
                        stg, d_stg = wst_r[c]
                        dst_ap = wup_r[bb][0][:, c, :] if c < 8 else wdn_r[bb][0][:, c - 8, :]
                        if CE[c] == "act":
                            kb.op("act", lambda e: e.copy(out=dst_ap, in_=stg[:]), R=[d_stg], W=[wdeps[bb][c]])
                        else:
                            kb.op("dve", lambda e: e.tensor_copy(out=dst_ap, in_=stg[:]), R=[d_stg], W=[wdeps[bb][c]])

                def x_dma(ex):
                    for st in range(nst):
                        xs_t, d_xs = xs_r[(ex % 2) * nst + st]
                        s0 = ex * cap + st * 128
                        kb.dma("sp", xs_t[:], xpad[s0:s0 + 128, :], R=[d_xpad], W=[d_xs])

                w_dma(0)
                x_dma(0)
                w_cast(0, range(12))
                yi = 0
                for ex in range(NE):
                    b = ex % 2
                    wup_t = wup_r[b][0]
                    wdn_t = wdn_r[b][0]
                    if ex + 1 < NE:
                        w_dma(ex + 1)
                        x_dma(ex + 1)
                    xT_t, d_xT = xT_r[b]
                    for st in range(nst):
                        xs_t, d_xs = xs_r[b * nst + st]
                        ps_t, d_pst = pst_r[st % 2]
                        for c in range(8):
                            kb.op("pe", lambda e: e.transpose(out=ps_t[:, c, :], in_=xs_t[:, c * 128:(c + 1) * 128],
                                                              identity=ident[:]), R=[d_xs, d_id], W=[d_pst])
                        kb.op("act" if st % 2 else "dve",
                              (lambda e: e.copy(out=xT_t[:, :, st * 128:(st + 1) * 128], in_=ps_t[:])) if st % 2 else
                              (lambda e: e.tensor_copy(out=xT_t[:, :, st * 128:(st + 1) * 128], in_=ps_t[:])),
                              R=[d_pst], W=[d_xT])
                    aT_t, d_aT = aT_r[b]
                    for fc in range(4):
                        pa, d_pa = pau_r[(2 * fc) % 4]
                        pu, d_pu = pau_r[(2 * fc + 1) % 4]
                        for c in range(8):
                            kb.op("pe", lambda e: e.matmul(pa[:, 0:cap], lhsT=wup_t[:, c, fc * 128:(fc + 1) * 128],
                                                           rhs=xT_t[:, c, :], start=(c == 0), stop=(c == 7)),
                                  R=[wdeps[b][c], d_xT], W=[d_pa])
                        for c in range(8):
                            kb.op("pe", lambda e: e.matmul(pu[:, 0:cap], lhsT=wup_t[:, c, DE + fc * 128:DE + (fc + 1) * 128],
                                                           rhs=xT_t[:, c, :], start=(c == 0), stop=(c == 7)),
                                  R=[wdeps[b][c], d_xT], W=[d_pu])
                        sa_t, d_sa = sa_r[fc % 2]
                        kb.op("act", lambda e: e.activation(out=sa_t[:], in_=pa[:, 0:cap], func=AF.Silu), R=[d_pa], W=[d_sa])
                        kb.op("dve", lambda e: e.tensor_tensor(out=aT_t[:, fc, :], in0=sa_t[:], in1=pu[:, 0:cap], op=ALU.mult),
                              R=[d_sa, d_pu], W=[d_aT])
                    if ex + 1 < NE:
                        w_cast(ex + 1, range(0, 8))
                    for st in range(nst):
                        yb_t, d_yb = yb_r[yi % 2]
                        yi += 1
                        for half in range(2):
                            py, d_py = py_r[half]
                            for fc in range(4):
                                kb.op("pe", lambda e: e.matmul(py[:], lhsT=aT_t[:, fc, st * 128:(st + 1) * 128],
                                                               rhs=wdn_t[:, fc, half * 512:(half + 1) * 512],
                                                               start=(fc == 0), stop=(fc == 3)),
                                      R=[d_aT, wdeps[b][8 + fc]], W=[d_py])
                            if half == 0:
                                kb.op("act", lambda e: e.copy(out=yb_t[:, 0:512], in_=py[:]), R=[d_py], W=[d_yb])
                            else:
                                kb.op("dve", lambda e: e.tensor_copy(out=yb_t[:, 512:1024], in_=py[:]), R=[d_py], W=[d_yb])
                        s0 = ex * cap + st * 128
                        kb.dma("sp", ypad[s0:s0 + 128, :], yb_t[:], R=[d_yb], W=[d_ypad])
                    if ex + 1 < NE:
                        w_cast(ex + 1, range(8, 12))
            with kb.scope():
                h1_r = kb.ring("h1", [128, D], F32, 4)
                y_r = kb.ring("y", [128, D], BF16, 8)
                h2_r = kb.ring("h2", [128, D], F32, 2)
                for (y_t, d_y) in y_r:
                    kb.op("pool", lambda e: e.memset(y_t[:], 0.0), W=[d_y])
                p3 = pass3_setup()
                def p3_load(ti):
                    tt = tiles[ti]
                    bb = ti % 4
                    h1_t, d_h1 = h1_r[bb]
                    kb.dma("sp", h1_t[:], h1s[tt * 128:(tt + 1) * 128, :], R=[d_h1s], W=[d_h1])
                    for k in range(2):
                        y_t, d_y = y_r[2 * bb + k]
                        kb.idma(out=y_t[:], out_off=None, in_=ypad[:, :],
                                in_off=bass.IndirectOffsetOnAxis(ap=dest_i[:, tt, k:k + 1], axis=0),
                                R=[d_ypad, d_dest], W=[d_y], bounds_check=breg, oob_is_err=False)
                for ti in range(min(3, len(tiles))):
                    p3_load(ti)
                for ti, t in enumerate(tiles):
                    b = ti % 2
                    r0, r1 = t * 128, (t + 1) * 128
                    h1_t, d_h1 = h1_r[ti % 4]
                    h2_t, d_h2 = h2_r[b]
                    y0_t, d_y0 = y_r[2 * (ti % 4)]
                    y1_t, d_y1 = y_r[2 * (ti % 4) + 1]
                    if ti + 3 < len(tiles):
                        p3_load(ti + 3)
                    kb.op("dve", lambda e: e.scalar_tensor_tensor(out=h2_t[:], in0=y0_t[:], scalar=gw[:, t, 0:1], in1=h1_t[:],
                                                                   op0=ALU.mult, op1=ALU.add), R=[d_y0, d_gw, d_h1], W=[d_h2])
                    kb.op("dve", lambda e: e.scalar_tensor_tensor(out=h2_t[:], in0=y1_t[:], scalar=gw[:, t, 1:2], in1=h2_t[:],
                                                                   op0=ALU.mult, op1=ALU.add), R=[d_y1, d_gw, d_h2], W=[d_h2])
                    pass3_tile(p3, t, h2_t, d_h2)

    def t2_setup_lhsT():
        idx2 = kb.sb("idx2", [128, 8], I32); d_idx2 = Dep("idx2")
        kb.dma("sp", idx2[:], idx2_d, W=[d_idx2])
        oTall = kb.sb("oTall", [128, 8, TX], BF16); d_oTall = Dep("oTall")
        oTm = kb.sb("oTm", [128, 8, 128], BF16); d_oTm = Dep("oTm")
        for c8 in range(8):
            kb.idma(out=oTall[:, c8, :], out_off=None, in_=RBb[:, :],
                    in_off=bass.IndirectOffsetOnAxis(ap=idx2[:, c8:c8 + 1], axis=0),
                    R=[d_RBb, d_idx2], W=[d_oTall], bounds_check=breg2, oob_is_err=False)
        kb.dma("sp", oTm[:], RBm.rearrange("(c p) t -> p c t", p=128), R=[d_RBm], W=[d_oTm])

        def get(t):
            if t == 0:
                return (lambda c: oTm[:, c, :]), [d_oTm]
            return (lambda c: oTall[:, c, (t - 1) * 128:t * 128]), [d_oTall]
        return get

    def t2_pass3_setup():
        p = {}
        p["wg"], p["d_wg"] = load_w(kb, "hwg", hwg_d, D, D)
        p["d_wg"].ro = True
        p["hn"], p["d_hn"] = load_bc(kb, "hnorm", hnorm, D)
        p["scr"] = kb.ring("scr", [128, D], F32, 2)
        p["ss"] = kb.ring("ss", [128, 1], F32, 2)
        p["xn"] = kb.ring("xn", [128, D], BF16, 2)
        p["hc"] = kb.ring("hc", [128, 8, 512], BF16, 2)
        p["gsb"] = kb.ring("gsb", [128, 8, 512], BF16, 2)
        p["pst"] = kb.ring("pst", [128, 8, 128], BF16, 2, psum=True)
        p["pg"] = kb.ring("pg", [128, 512], F32, 2, psum=True)
        return p

    def t2_pass3_tile(p, t, h2_t, d_h2):
        kb.dma("sp", h2s[t * 128:(t + 1) * 128, :], h2_t[:], R=[d_h2], W=[d_h2s])

        def hook(j, hc_t, d_hc):
            gsb, d_gsb = p["gsb"][j % 2]
            for g in range(8):
                pg, d_pg = p["pg"][g % 2]
                for c in range(8):
                    kb.op("pe", lambda e: e.matmul(pg[:], lhsT=p["wg"][:, c, g * 128:(g + 1) * 128], rhs=hc_t[:, c, :],
                                                   start=(c == 0), stop=(c == 7)), R=[p["d_wg"], d_hc], W=[d_pg])
                kb.op("act", lambda e: e.activation(out=gsb[:, g, :], in_=pg[:], func=AF.Silu), R=[d_pg], W=[d_gsb])
            kb.dma("sp", GS[j * 128:(j + 1) * 128, :], gsb[:].rearrange("p g t -> p (g t)"), R=[d_gsb], W=[d_GS])
        emit_hnT(h2_t, d_h2, p["hn"], p["d_hn"], p["scr"], p["ss"], p["xn"], p["pst"], p["hc"], t,
                 MC, d_MC, SC, d_SC, RC, d_RC, hc_hook=hook)

    tok_stage(0, list(range(nt)), True, t2_setup_lhsT, lambda t: x_in[t * 128:(t + 1) * 128, :], wo1_d,
              t2_pass3_setup, t2_pass3_tile)

    if stop == "T2":
        return dump_and_finish([("h2s", h2s, d_h2s), ("RC", RC, d_RC[nch - 1]), ("GS", GS, d_GS)])
    for i2 in range(2):
        with kb.scope():
            q_t = kb.sb("q", [128, L], BF16); d_q = Dep("q")
            lf_t = kb.sb("lf", [128, L], F32); d_lf = Dep("lf")
            v_t = kb.sb("v", [128, nb, 128], BF16); d_v = Dep("v")
            go_t, d_go = load_bc(kb, "go", go_d, 128)
            with kb.scope():
                hwq_t, d_hwq = load_w(kb, "hwq", hwq_d[i2], D, 128)
                hwf_t, d_hwf = load_w(kb, "hwf", hwf_d[i2], D, 128)
                hwv_t, d_hwv = load_w(kb, "hwv", hwv_d[i2], D, 128)
                for d in (d_hwq, d_hwf, d_hwv):
                    d.ro = True
                cols = kb.sb("cols", [128, 8], F32); d_cols = Dep("cols")
                kb.dma("sp", cols[:, 0:1], hbfc_d[i2], W=[d_cols])
                kb.dma("sp", cols[:, 1:2], l0c_d[i2], W=[d_cols])
                kb.dma("sp", cols[:, 2:3], l1c_d[i2], W=[d_cols])
                kb.op("dve", lambda e: e.tensor_tensor(out=cols[:, 3:4], in0=cols[:, 2:3], in1=cols[:, 1:2], op=ALU.subtract),
                      R=[d_cols], W=[d_cols])
                kb.op("act", lambda e: e.activation(out=cols[:, 4:5], in_=cols[:, 3:4], func=AF.Sigmoid), R=[d_cols], W=[d_cols])
                kb.op("act", lambda e: e.activation(out=cols[:, 5:6], in_=cols[:, 3:4], func=AF.Sigmoid, scale=-1.0),
                      R=[d_cols], W=[d_cols])
                d_cols.ro = True
                hgr = kb.ring("hg", [128, 8, 512], BF16, 2)
                sgr = kb.ring("sg", [128, 512], F32, 2)
                pqr = kb.ring("pq", [128, 512], F32, 2, psum=True)
                pfr = kb.ring("pf", [128, 512], F32, 2, psum=True)
                pvr = kb.ring("pv", [128, 512], F32, 2, psum=True)
                vi = 0
                def h2_load(G):
                    hg, d_hg = hgr[G % 2]
                    if G == 0:
                        kb.dma("sp", hg[:, :, 0:128], MC.rearrange("p (c t) -> p c t", c=8), R=[d_MC], W=[d_hg])
                    else:
                        rank, j = (G - 1) // nch, (G - 1) % nch
                        r0 = j * 512 + rank * 128
                        kb.dma("sp", hg[:].rearrange("p c t -> p (c t)"), RC[r0:r0 + 128, :], R=[d_RC[j]], W=[d_hg])
                h2_load(0)
                for G in range(1 + 4 * nch):
                    hg, d_hg = hgr[G % 2]
                    if G + 1 < 1 + 4 * nch:
                        h2_load(G + 1)
                    if G == 0:
                        n, tok0 = 128, 0
                    else:
                        n, tok0 = 512, 128 + (G - 1) * 512
                    pq, d_pq = pqr[G % 2]
                    pf, d_pf = pfr[G % 2]
                    sg, d_sg = sgr[G % 2]
                    for c in range(8):
                        kb.op("pe", lambda e: e.matmul(pq[:, 0:n], lhsT=hwq_t[:, c, :], rhs=hg[:, c, 0:n], start=(c == 0), stop=(c == 7)),
                              R=[d_hwq, d_hg], W=[d_pq])
                    kb.op("act", lambda e: e.activation(out=q_t[:, tok0:tok0 + n], in_=pq[:, 0:n], func=AF.Silu), R=[d_pq], W=[d_q])
                    for c in range(8):
                        kb.op("pe", lambda e: e.matmul(pf[:, 0:n], lhsT=hwf_t[:, c, :], rhs=hg[:, c, 0:n], start=(c == 0), stop=(c == 7)),
                              R=[d_hwf, d_hg], W=[d_pf])
                    kb.op("act", lambda e: e.activation(out=sg[:, 0:n], in_=pf[:, 0:n], func=AF.Sigmoid, bias=cols[:, 0:1], scale=1.0),
                          R=[d_pf, d_cols], W=[d_sg])
                    kb.op("dve", lambda e: e.tensor_scalar(out=sg[:, 0:n], in0=sg[:, 0:n], scalar1=cols[:, 5:6], scalar2=cols[:, 4:5],
                                                            op0=ALU.mult, op1=ALU.add), R=[d_sg, d_cols], W=[d_sg])
                    kb.op("act", lambda e: e.activation(out=lf_t[:, tok0:tok0 + n], in_=sg[:, 0:n], func=AF.Ln), R=[d_sg], W=[d_lf])
                    for s_ in range(n // 128):
                        pv, d_pv = pvr[vi % 2]
                        vi += 1
                        for c in range(8):
                            kb.op("pe", lambda e: e.matmul(pv[:, 0:128], lhsT=hg[:, c, s_ * 128:(s_ + 1) * 128], rhs=hwv_t[:, c, :],
                                                           start=(c == 0), stop=(c == 7)), R=[d_hwv, d_hg], W=[d_pv])
                        kb.op("dve", lambda e: e.tensor_copy(out=v_t[:, tok0 // 128 + s_, :], in_=pv[:, 0:128]), R=[d_pv], W=[d_v])
                kb.op("pool", lambda e: e.memset(lf_t[:, 0:128 - NMETA], 0.0), W=[d_lf])
            with kb.scope():
                onesf3 = kb.sb("onesf3", [128, 128], F32); d_onesf3 = Dep("onesf3")
                mf = kb.sb("mf", [128, 128], F32); d_mf = Dep("mf")
                mku = kb.sb("mku", [128, 128], U32); d_mk = Dep("mku")
                kb.op("pool", lambda e: e.memset(onesf3[:], 1.0), W=[d_onesf3])
                kb.op("pool", lambda e: e.affine_select(out=mf[:], in_=onesf3[:], pattern=[[1, 128]], compare_op=ALU.is_ge,
                                                        fill=0.0, base=0, channel_multiplier=-1), R=[d_onesf3], W=[d_mf])
                kb.op("dve", lambda e: e.tensor_copy(out=mku[:], in_=mf[:]), R=[d_mf], W=[d_mk])
                d_mk.ro = True
                d_onesf3.ro = True
                S_t = kb.sb("S", [128, 128], F32); d_S = Dep("S")
                Sb_r = kb.ring("Sb", [128, 128], BF16, 2)
                b_r = kb.ring("b", [128, 128], F32, 3)
                col_r = kb.ring("col", [128, 8], F32, 3)
                e1_r = kb.ring("e1", [128, 128], F32, 3)
                e2_r = kb.ring("e2", [128, 128], F32, 3)
                kk_r = kb.ring("kk", [128, 128], F32, 3)
                qd_r = kb.ring("qd", [128, 128], BF16, 3)
                kd_r = kb.ring("kd", [128, 128], BF16, 3)
                qb_r = kb.ring("qb", [128, 128], BF16, 3)
                ke_r = kb.ring("ke", [128, 128], BF16, 3)
                keT_r = kb.ring("keT", [128, 128], BF16, 2)
                scm_r = kb.ring("scm", [128, 128], BF16, 3)
                osq_r = kb.ring("osq", [128, 128], F32, 2)
                oss_r = kb.ring("oss", [128, 2], F32, 3)
                on_r = kb.ring("on", [128, 128], BF16, 3)
                stg_r = kb.ring("stg", [128, TX], BF16, 2)
                psc_b = [kb.ps("psc%d" % k, [128, 512], F32) for k in range(2)]
                po_b = [kb.ps("po%d" % k, [128, 512], F32) for k in range(2)]
                pu_b = [kb.ps("pu%d" % k, [128, 512], F32) for k in range(2)]
                pt_b = [kb.ps("pt%d" % k, [128, 8, 128], BF16) for k in range(2)]
                psc_r = [(psc_b[k % 2][:, 0:128], Dep("psc%d" % k)) for k in range(2)] * 2
                po_r = [(po_b[k % 2][:, 0:128], Dep("po%d" % k)) for k in range(2)] * 2
                pu_r = [(pu_b[k % 2][:, 0:128], Dep("pu%d" % k)) for k in range(2)] * 2
                ptk_r = [(pt_b[k % 2][:, 0, :], Dep("ptk%d" % k)) for k in range(2)] * 2
                pto_r = [(pt_b[k % 2][:, 1, :], Dep("pto%d" % k)) for k in range(2)] * 2
                for (t_, d_) in scm_r:
                    kb.op("pool", lambda e: e.memset(t_[:], 0.0), W=[d_])
                kb.op("pool", lambda e: e.memset(S_t[:], 0.0), W=[d_S])
                kb.op("pool", lambda e: e.memset(Sb_r[0][0][:], 0.0), W=[Sb_r[0][1]])

                def st_a(c):
                    cs = slice(c * 128, (c + 1) * 128)
                    b_t, d_b = b_r[c % 3]
                    col, d_col = col_r[c % 3]
                    e1, d_e1 = e1_r[c % 3]
                    e2, d_e2 = e2_r[c % 3]
                    kk, d_kk = kk_r[c % 3]
                    qd, d_qd = qd_r[c % 3]
                    kd, d_kd = kd_r[c % 3]
                    qb, d_qb = qb_r[c % 3]
                    ke, d_ke = ke_r[c % 3]
                    kb.op("dve", lambda e: e.tensor_tensor_scan(out=b_t[:], data0=onesf3[:], data1=lf_t[:, cs], initial=0.0,
                                                                 op0=ALU.mult, op1=ALU.add), R=[d_lf, d_onesf3], W=[d_b])
                    kb.op("dve", lambda e: e.tensor_copy(out=col[:, 0:1], in_=b_t[:, 63:64]), R=[d_b], W=[d_col])
                    kb.op("dve", lambda e: e.tensor_tensor(out=col[:, 1:2], in0=b_t[:, 127:128], in1=b_t[:, 63:64],
                                                            op=ALU.subtract), R=[d_b], W=[d_col])
                    kb.op("dve", lambda e: e.tensor_copy(out=col[:, 2:3], in_=b_t[:, 127:128]), R=[d_b], W=[d_col])
                    kb.op("dve", lambda e: e.tensor_scalar(out=col[:, 3:4], in0=b_t[:, 63:64], scalar1=-1.0, scalar2=None,
                                                            op0=ALU.mult), R=[d_b], W=[d_col])
                    kb.op("act", lambda e: e.activation(out=col[:, 4:7], in_=col[:, 0:3], func=AF.Exp), R=[d_col], W=[d_col])
                    kb.op("act", lambda e: e.activation(out=e1[:], in_=b_t[:], func=AF.Exp, bias=col[:, 3:4], scale=1.0),
                          R=[d_b, d_col], W=[d_e1])
                    kb.op("act", lambda e: e.activation(out=e2[:], in_=b_t[:], func=AF.Exp, bias=col[:, 0:1], scale=-1.0),
                          R=[d_b, d_col], W=[d_e2])
                    kb.op("act", lambda e: e.activation(out=kk[:], in_=lf_t[:, cs], func=AF.Exp), R=[d_lf], W=[d_kk])
                    kb.op("pool", lambda e: e.tensor_scalar(out=kk[:], in0=kk[:], scalar1=-1.0, scalar2=1.0, op0=ALU.mult,
                                                             op1=ALU.add), R=[d_kk], W=[d_kk])
                    kb.op("dve", lambda e: e.tensor_tensor(out=qd[:], in0=q_t[:, cs], in1=e1[:], op=ALU.mult),
                          R=[d_q, d_e1], W=[d_qd])
                    kb.op("dve", lambda e: e.tensor_tensor(out=kd[:], in0=kk[:], in1=e2[:], op=ALU.mult),
                          R=[d_kk, d_e2], W=[d_kd])
                    kb.op("dve", lambda e: e.scalar_tensor_tensor(out=qb[:], in0=q_t[:, cs], scalar=col[:, 4:5], in1=e1[:],
                                                                   op0=ALU.mult, op1=ALU.mult), R=[d_q, d_col, d_e1], W=[d_qb])
                    kb.op("dve", lambda e: e.scalar_tensor_tensor(out=ke[:], in0=kk[:], scalar=col[:, 5:6], in1=e2[:],
                                                                   op0=ALU.mult, op1=ALU.mult), R=[d_kk, d_col, d_e2], W=[d_ke])

                def st_f(c):
                    qd, d_qd = qd_r[c % 3]
                    kd, d_kd = kd_r[c % 3]
                    ke, d_ke = ke_r[c % 3]
                    keT, d_keT = keT_r[c % 2]
                    scm, d_scm = scm_r[c % 3]
                    psc, d_psc = psc_r[c % 4]
                    ptk, d_ptk = ptk_r[c % 4]
                    pu, d_pu = pu_r[c % 4]
                    kb.op("pe", lambda e: e.matmul(psc, lhsT=kd[:], rhs=qd[:], start=True, stop=True), R=[d_kd, d_qd], W=[d_psc])
                    kb.op("dve", lambda e: e.copy_predicated(out=scm[:], mask=mku[:], data=psc), R=[d_psc, d_mk], W=[d_scm])
                    kb.op("pe", lambda e: e.transpose(out=ptk, in_=ke[:], identity=ident[:]), R=[d_ke, d_id], W=[d_ptk])
                    kb.op("act", lambda e: e.copy(out=keT[:], in_=ptk), R=[d_ptk], W=[d_keT])
                    kb.op("pe", lambda e: e.matmul(pu, lhsT=keT[:], rhs=v_t[:, c, :], start=True, stop=True), R=[d_keT, d_v], W=[d_pu])

                def st_k(c):
                    col, d_col = col_r[c % 3]
                    qb, d_qb = qb_r[c % 3]
                    scm, d_scm = scm_r[c % 3]
                    po, d_po = po_r[c % 4]
                    pu, d_pu = pu_r[c % 4]
                    Sb, d_Sb = Sb_r[c % 2]
                    kb.op("pe", lambda e: e.matmul(po, lhsT=scm[:], rhs=v_t[:, c, :], start=True, stop=False), R=[d_scm, d_v], W=[d_po])
                    kb.op("pe", lambda e: e.matmul(po, lhsT=qb[:], rhs=Sb[:], start=False, stop=True), R=[d_qb, d_Sb], W=[d_po])
                    kb.op("dve", lambda e: e.scalar_tensor_tensor(out=S_t[:], in0=S_t[:], scalar=col[:, 6:7], in1=pu,
                                                                   op0=ALU.mult, op1=ALU.add), R=[d_S, d_col, d_pu], W=[d_S])
                    Sb2, d_Sb2 = Sb_r[(c + 1) % 2]
                    kb.op("pool", lambda e: e.tensor_copy(out=Sb2[:], in_=S_t[:]), R=[d_S], W=[d_Sb2])

                def st_n(c):
                    po, d_po = po_r[c % 4]
                    osq, d_osq = osq_r[c % 2]
                    oss, d_oss = oss_r[c % 3]
                    on_t, d_on = on_r[c % 3]
                    kb.op("act", lambda e: e.activation(out=osq[:], in_=po, func=AF.Square, accum_out=oss[:, 0:1]),
                          R=[d_po], W=[d_osq, d_oss])
                    kb.op("act", lambda e: e.activation(out=oss[:, 1:2], in_=oss[:, 0:1], func=AF.Ln, scale=1.0 / 128, bias=EPS),
                          R=[d_oss], W=[d_oss])
                    kb.op("act", lambda e: e.activation(out=oss[:, 1:2], in_=oss[:, 1:2], func=AF.Exp, scale=-0.5),
                          R=[d_oss], W=[d_oss])
                    kb.op("dve", lambda e: e.scalar_tensor_tensor(out=on_t[:], in0=po, scalar=oss[:, 1:2], in1=go_t[:],
                                                                   op0=ALU.mult, op1=ALU.mult), R=[d_po, d_oss, d_go], W=[d_on])

                def st_t(c):
                    on_t, d_on = on_r[c % 3]
                    pto, d_pto = pto_r[c % 4]
                    xb = c - 1
                    qq, tb = xb // ntx, xb % ntx
                    stg, d_stg = stg_r[qq % 2]
                    kb.op("pe", lambda e: e.transpose(out=pto, in_=on_t[:], identity=ident[:]), R=[d_on, d_id], W=[d_pto])
                    kb.op("act", lambda e: e.copy(out=stg[:, tb * 128:(tb + 1) * 128], in_=pto), R=[d_pto], W=[d_stg])
                    if tb == ntx - 1:
                        cidx = i2 * 4 + qq
                        kb.dma("sp", SD[cidx * 128:(cidx + 1) * 128, :], stg[:], R=[d_stg], W=[d_SD, d_SDc[cidx]])
                        kb.cc(SD[cidx * 128:(cidx + 1) * 128, :], RD[cidx * 512:(cidx + 1) * 512, :], GROUPS, R=[d_SDc[cidx]], W=[d_RD])

                st_a(0)
                if nb > 1:
                    st_a(1)
                st_f(0)
                for i in range(nb + 2):
                    if i + 2 < nb:
                        st_a(i + 2)
                    if i + 1 < nb:
                        st_f(i + 1)
                    if i < nb:
                        st_k(i)
                    if 1 <= i - 1 < nb:
                        st_n(i - 1)
                    if 1 <= i - 2 < nb:
                        st_t(i - 2)

    if stop == "H2":
        return dump_and_finish([("RD", RD, d_RD)])
    def t3_setup_lhsT():
        idx3 = kb.sb("idx3", [128, 8], I32); d_idx3 = Dep("idx3")
        kb.dma("sp", idx3[:], idx3_d, W=[d_idx3])
        onall = kb.sb("onall", [128, 8, TX], BF16); d_onall = Dep("onall")
        for c8 in range(8):
            kb.idma(out=onall[:, c8, :], out_off=None, in_=RD[:, :],
                    in_off=bass.IndirectOffsetOnAxis(ap=idx3[:, c8:c8 + 1], axis=0),
                    R=[d_RD, d_idx3], W=[d_onall], bounds_check=breg2, oob_is_err=False)
        gsr = kb.ring("gsT", [128, 8, 512], BF16, 2)
        ogr = kb.ring("og", [128, 8, 128], BF16, 2)

        def prefetch(t):
            if (t - 1) % 4 == 0:
                jj = (t - 1) // 4
                gs_t, d_gs = gsr[jj % 2]
                kb.dma("sp", gs_t[:].rearrange("p g t -> p (g t)"), GS[jj * 128:(jj + 1) * 128, :], R=[d_GS], W=[d_gs])

        def get(t):
            jj, ss_ = (t - 1) // 4, (t - 1) % 4
            gs_t, d_gs = gsr[jj % 2]
            og_t, d_og = ogr[t % 2]
            kb.op("dve", lambda e: e.tensor_tensor(out=og_t[:], in0=onall[:, :, (t - 1) * 128:t * 128],
                                                    in1=gs_t[:, :, ss_ * 128:(ss_ + 1) * 128], op=ALU.mult),
                  R=[d_onall, d_gs], W=[d_og])
            return (lambda c: og_t[:, c, :]), [d_og]
        get.prefetch = prefetch
        return get

    def t3_pass3_tile(p, t, h2_t, d_h2):
        kb.dma("sp", out_d[(t - 1) * 128:t * 128, :], h2_t[:], R=[d_h2])

    tok_stage(1, list(range(1, nt)), False, t3_setup_lhsT, lambda t: h2s[t * 128:(t + 1) * 128, :], wo2_d,
              lambda: None, t3_pass3_tile)
    kb.finish()
    return nc


def fused_in_maps(inp, ntx):
    f32 = np.float32
    TX = ntx * 128
    nt = ntx + 1
    x = inp["x"]
    metatile = np.zeros((128, D), f32)
    metatile[128 - NMETA:] = inp["meta_tokens"]
    valid = np.ones((nt * 128, 1), f32)
    valid[0:128 - NMETA] = 0.0
    fw = inp["fox_w_in"][0]
    hw = inp["hg_w_in"][0]
    common = {
        "valid_in": valid, "fnorm": inp["fox_norm"][0][None],
        "gqc": np.tile(inp["fox_q_norm"][0], 2)[:, None].astype(f32), "gkc": np.tile(inp["fox_k_norm"][0], 2)[:, None].astype(f32),
        "wo1": inp["fox_w_out"][0], "wo2": inp["hg_w_out"][0], "hnorm": inp["hg_norm"][0][None],
        "hwg": np.ascontiguousarray(hw[:, 3 * D:4 * D]), "go": inp["hg_o_norm"][0][None],
    }
    for l in range(2):
        common["mnorm%d" % l] = inp["moe_norm"][l][None]
        common["w_r%d" % l] = np.ascontiguousarray(np.concatenate([inp["moe_w_grp"][l], inp["moe_w_rt"][l]], 1))
        common["b_r%d" % l] = np.concatenate([inp["moe_b_grp"][l], inp["moe_b_rt"][l]])[None]
        common["w_up%d" % l] = inp["moe_w_up"][l]
        common["w_dn%d" % l] = inp["moe_w_down"][l]
    maps = []
    p = np.arange(128)
    for c in range(NCORES):
        b, r = c // 4, c % 4
        m = dict(common)
        m["x_in"] = np.concatenate([metatile, x[b, r * TX:(r + 1) * TX]], 0)
        hc = slice(4 * r * FD, (4 * r + 4) * FD)
        m["wq"] = np.ascontiguousarray(fw[:, 0:D][:, hc])
        m["wk"] = np.ascontiguousarray(fw[:, D:2 * D][:, hc])
        m["wv"] = np.ascontiguousarray(fw[:, 2 * D:3 * D][:, hc])
        m["wf"] = np.ascontiguousarray(fw[:, 3 * D + 4 * r:3 * D + 4 * r + 4])
        bf4 = inp["fox_b_f"][0][4 * r:4 * r + 4]
        m["bf4r"] = bf4[None].astype(f32)
        m["bf4c"] = bf4[:, None].astype(f32)
        idx2 = np.zeros((128, 8), np.int32)
        idx3 = np.zeros((128, 8), np.int32)
        for c8 in range(8):
            rank, sub = c8 // 2, c8 % 2
            idx2[:, c8] = (sub * 4 + r) * 512 + rank * 128 + p
            idx3[:, c8] = (sub * 4 + r) * 512 + rank * 128 + p
        m["idx2"] = idx2
        m["idx3"] = idx3
        hs = [slice((2 * r + i) * 128, (2 * r + i + 1) * 128) for i in range(2)]
        m["hwq"] = np.ascontiguousarray(np.stack([hw[:, 0:D][:, s] for s in hs]))
        m["hwf"] = np.ascontiguousarray(np.stack([hw[:, D:2 * D][:, s] for s in hs]))
        m["hwv"] = np.ascontiguousarray(np.stack([hw[:, 2 * D:3 * D][:, s] for s in hs]))
        m["hbfc"] = np.stack([inp["hg_b_f"][0][s][:, None] for s in hs]).astype(f32)
        m["l0c"] = np.stack([inp["hg_lb_logits"][0][s][:, None] for s in hs]).astype(f32)
        m["l1c"] = np.stack([inp["hg_lb_logits"][1][s][:, None] for s in hs]).astype(f32)
        maps.append(m)
    return maps


def kernel_fused(inp, ntx=NTX, cap=CAP, stop=None):
    inp = {k: np.asarray(v) for k, v in inp.items()}
    nc = _prog(("F", ntx, cap, stop), lambda: build_fused(ntx, cap, stop))
    maps = fused_in_maps(inp, ntx)
    if stop is not None:
        drop = []
        for l in range(2):
            if stop in ("T1", "H1p", "H1a") or (stop in ("T2", "H2") and l == 1):
                drop += [k + str(l) for k in ("mnorm", "w_r", "b_r", "w_up", "w_dn")]
        maps = [{k: v for k, v in m.items() if k not in drop} for m in maps]
    res = _run(nc, maps)
    if stop is not None:
        return res
    TX = ntx * 128
    out = np.zeros((2, 4 * TX, D), np.float32)
    for c in range(NCORES):
        out[c // 4, (c % 4) * TX:(c % 4 + 1) * TX] = np.asarray(res[c]["out"])
    return out


def kernel(**inp):
    return kernel_fused(inp, NTX, CAP)
```

```python
import contextlib
import numpy as np
import ml_dtypes
import concourse.bass as bass
import concourse.mybir as mybir
from concourse.bass_utils import run_bass_kernel_spmd

F32 = mybir.dt.float32
BF16 = mybir.dt.bfloat16
I32 = mybir.dt.int32
U32 = mybir.dt.uint32
AF = mybir.ActivationFunctionType
ALU = mybir.AluOpType
AX = mybir.AxisListType
NPBF = ml_dtypes.bfloat16

D = 1024
NCORES = 8
SEQ = 16384
NMETA = 16
BLK = 128
EPS = 1e-6
FH = 16
FD = 64
HH = 8
NE = 32
CAP = 384
DE = 512
NTX = 32
NT = NTX + 1
LP = SEQ + BLK
NB = LP // BLK


class Dep:
    __slots__ = ("w", "r", "name", "ro")

    def __init__(self, name=""):
        self.w = None
        self.r = []
        self.name = name
        self.ro = False


class _E:
    def __init__(self, name, eng, sem):
        self.name = name
        self.eng = eng
        self.sem = sem
        self.n = 0
        self.waited = {}
        self.pool = []
        self.pool_i = 0


class KB:
    def __init__(self, nc, dma_pool=(("sp", 16), ("pool", 16), ("act", 4)), same_eng_sync=True):
        self.nc = nc
        self.stacks = [contextlib.ExitStack()]
        self.same = same_eng_sync
        self.e = {}
        for name, eng in (("pe", nc.tensor), ("dve", nc.vector), ("act", nc.scalar),
                          ("pool", nc.gpsimd), ("sp", nc.sync)):
            sem = self.stacks[0].enter_context(nc.semaphore("c_" + name))
            self.e[name] = _E(name, eng, sem)
        for q, n in dma_pool:
            for i in range(n):
                sem = self.stacks[0].enter_context(nc.semaphore("d_%s%d" % (q, i)))
                self.e[q].pool.append([sem, 0])
        self.ninst = 0
        self.uid = 0

    def sb(self, name, shape, dt):
        self.uid += 1
        return self.stacks[-1].enter_context(self.nc.sbuf_tensor("%s_%d" % (name, self.uid), list(shape), dt))

    def ps(self, name, shape, dt):
        self.uid += 1
        return self.stacks[-1].enter_context(self.nc.psum_tensor("%s_%d" % (name, self.uid), list(shape), dt))

    def ring(self, name, shape, dt, n, psum=False):
        out = []
        for i in range(n):
            t = (self.ps if psum else self.sb)("%s%d" % (name, i), shape, dt)
            out.append((t, Dep("%s%d" % (name, i))))
        return out

    @contextlib.contextmanager
    def scope(self):
        self.stacks.append(contextlib.ExitStack())
        try:
            yield
        finally:
            self.barrier()
            self.stacks.pop().close()

    def _wait(self, E, ev, own_ok=False):
        if ev is None:
            return
        sem, val = ev
        if sem is E.sem and not own_ok:
            return
        k = id(sem)
        if E.waited.get(k, 0) >= val:
            return
        E.eng.wait_ge(sem, val)
        E.waited[k] = val

    def _deps(self, E, R, W):
        same = self.same and E.name != "pe"
        for d in R:
            self._wait(E, d.w, own_ok=same)
        for d in W:
            self._wait(E, d.w, own_ok=same)
            for ev in d.r:
                self._wait(E, ev, own_ok=False)

    def _record(self, ev, R, W):
        for d in R:
            if not d.ro:
                for i, (sem, val) in enumerate(d.r):
                    if sem is ev[0]:
                        if ev[1] > val:
                            d.r[i] = ev
                        break
                else:
                    d.r.append(ev)
        for d in W:
            d.w = ev
            d.r = []

    def op(self, en, fn, R=(), W=()):
        E = self.e[en]
        self._deps(E, R, W)
        ins = fn(E.eng)
        E.n += 1
        ins.then_inc(E.sem, 1)
        ev = (E.sem, E.n)
        self._record(ev, R, W)
        self.ninst += 1
        return ev

    def _dma_issue(self, E, issue, R, W):
        self._deps(E, R, W)
        slot = E.pool[E.pool_i % len(E.pool)]
        E.pool_i += 1
        sem, cur = slot
        if cur:
            self._wait(E, (sem, cur))
        ins = issue(E.eng)
        ins.then_inc(sem, 16)
        slot[1] = cur + 16
        ev = (sem, cur + 16)
        self._record(ev, R, W)
        self.ninst += 1
        return ev

    def dma(self, q, out, in_, R=(), W=(), **kw):
        return self._dma_issue(self.e[q], lambda e: e.dma_start(out=out, in_=in_, **kw), R, W)

    def idma(self, out, out_off, in_, in_off, R=(), W=(), **kw):
        return self._dma_issue(
            self.e["pool"],
            lambda e: e.indirect_dma_start(out=out, out_offset=out_off, in_=in_, in_offset=in_off, **kw),
            R, W)

    def cc(self, in_ap, out_ap, groups, R=(), W=()):
        E = self.e["pool"]
        self._deps(E, R, W)
        if not hasattr(self, "ccsem"):
            self.ccsem = self.stacks[0].enter_context(self.nc.semaphore("ccsem"))
            self.ccn = 0
        ins = E.eng.collective_compute("AllGather", ALU.bypass, replica_groups=groups, ins=[in_ap], outs=[out_ap], dma_qos="P2")
        ins.then_inc(self.ccsem, 1)
        self.ccn += 1
        ev = (self.ccsem, self.ccn)
        self._record(ev, R, W)
        self.ninst += 1
        return ev

    def barrier(self):
        evs = [(E.sem, E.n) for E in self.e.values() if E.n]
        for q in ("sp", "pool", "act"):
            for sem, cur in self.e[q].pool:
                if cur:
                    evs.append((sem, cur))
        if getattr(self, "ccn", 0):
            evs.append((self.ccsem, self.ccn))
        for E in self.e.values():
            for ev in evs:
                self._wait(E, ev, own_ok=False)

    def finish(self):
        self.barrier()
        self.e["sp"].eng.nop()
        while self.stacks:
            self.stacks.pop().close()


def make_ident(kb, n=128):
    identf = kb.sb("identf", [128, 128], F32)
    ident = kb.sb("ident", [128, 128], BF16)
    d1, d2 = Dep("identf"), Dep("ident")
    kb.op("pool", lambda e: e.memset(identf[:], 0.0), W=[d1])
    kb.op("pool", lambda e: e.affine_select(out=identf[:], in_=identf[:], pattern=[[-1, 128]],
                                            compare_op=ALU.not_equal, fill=1.0, base=0,
                                            channel_multiplier=1), R=[d1], W=[d1])
    kb.op("dve", lambda e: e.tensor_copy(out=ident[:], in_=identf[:]), R=[d1], W=[d2])
    d1.ro = True
    d2.ro = True
    return identf, d1, ident, d2


def load_bc(kb, name, src_ap, n, q="sp"):
    t = kb.sb(name, [128, n], F32)
    d = Dep(name)
    kb.dma(q, t[:], src_ap.to_broadcast([128, n]), W=[d])
    d.ro = True
    return t, d


def load_w(kb, name, w_ap, kin, nout, dt=BF16):
    kc = kin // 128
    t = kb.sb(name, [128, kc, nout], dt)
    d = Dep(name)
    wv = w_ap.rearrange("(c p) n -> p c n", p=128)
    for c in range(kc):
        kb.dma("pool" if dt != F32 else "sp", t[:, c, :], wv[:, c, :], W=[d])
    return t, d


def rms_rstd(kb, src, d_src, n, scr, d_scr, ss, d_ss):
    kb.op("act", lambda e: e.activation(out=scr, in_=src, func=AF.Square, accum_out=ss),
          R=[d_src], W=[d_scr, d_ss])
    kb.op("act", lambda e: e.activation(out=ss, in_=ss, func=AF.Ln, scale=1.0 / n, bias=EPS),
          R=[d_ss], W=[d_ss])
    kb.op("act", lambda e: e.activation(out=ss, in_=ss, func=AF.Exp, scale=-0.5), R=[d_ss], W=[d_ss])


def transpose_chunks(kb, src, d_src, nchunk, pst, d_pst, dst, d_dst, ident, d_id, copy_eng="dve"):
    for c in range(nchunk):
        kb.op("pe", lambda e: e.transpose(out=pst[:, c, :], in_=src[:, c * 128:(c + 1) * 128], identity=ident[:]),
              R=[d_src, d_id], W=[d_pst])
    if copy_eng == "act":
        kb.op("act", lambda e: e.copy(out=dst[:, 0:nchunk, :], in_=pst[:, 0:nchunk, :]), R=[d_pst], W=[d_dst])
    else:
        kb.op("dve", lambda e: e.tensor_copy(out=dst[:, 0:nchunk, :], in_=pst[:, 0:nchunk, :]), R=[d_pst], W=[d_dst])


def mm_acc(kb, ps_ap, d_ps, xT, d_xT, w, d_w, c0, c1, kc=8):
    for c in range(kc):
        kb.op("pe", lambda e: e.matmul(ps_ap, lhsT=xT[:, c, :], rhs=w[:, c, c0:c1], start=(c == 0), stop=(c == kc - 1)),
              R=[d_xT, d_w], W=[d_ps])


def build_A(nt):
    nc = bass.Bass("TRN2", target_bir_lowering=False)
    rows = nt * 128
    h_in = nc.dram_tensor("h", [rows, D], F32, kind="ExternalInput").ap()
    gain = nc.dram_tensor("gain", [1, D], F32, kind="ExternalInput").ap()
    w_in = nc.dram_tensor("w_in", [D, 3088], F32, kind="ExternalInput").ap()
    gq = nc.dram_tensor("gq", [1, D], F32, kind="ExternalInput").ap()
    gk = nc.dram_tensor("gk", [1, D], F32, kind="ExternalInput").ap()
    bfv = nc.dram_tensor("bf", [1, FH], F32, kind="ExternalInput").ap()
    qo = nc.dram_tensor("qo", [rows, D], BF16, kind="ExternalOutput").ap()
    ko = nc.dram_tensor("ko", [rows, D], BF16, kind="ExternalOutput").ap()
    vo = nc.dram_tensor("vo", [rows, D], BF16, kind="ExternalOutput").ap()
    lfo = nc.dram_tensor("lfo", [rows, FH], F32, kind="ExternalOutput").ap()
    d_out = Dep("out")
    kb = KB(nc)
    identf, d_idf, ident, d_id = make_ident(kb)
    g_t, d_g = load_bc(kb, "gain", gain, D)
    gq_t, d_gq = load_bc(kb, "gq", gq, D)
    gk_t, d_gk = load_bc(kb, "gk", gk, D)
    bf_t, d_bf = load_bc(kb, "bf", bfv, FH)
    w_t, d_w = load_w(kb, "w_in", w_in, D, 3088)
    d_w.ro = True
    xin = kb.ring("xin", [128, D], F32, 2)
    scr = kb.ring("scr", [128, D], F32, 2)
    ssr = kb.ring("ss", [128, 1], F32, 2)
    xnr = kb.ring("xn", [128, D], BF16, 2)
    xTr = kb.ring("xT", [128, 8, 128], BF16, 2)
    pstr = kb.ring("pst", [128, 8, 128], BF16, 2, psum=True)
    pmm = kb.ring("pmm", [128, 512], F32, 6, psum=True)
    qf = kb.ring("qf", [128, D], F32, 2)
    s16 = kb.ring("s16", [128, FH], F32, 2)
    qn = kb.ring("qn", [128, D], BF16, 2)
    kn = kb.ring("kn", [128, D], BF16, 2)
    vb = kb.ring("vb", [128, D], BF16, 2)
    lft = kb.ring("lft", [128, FH], F32, 2)
    pi = 0
    for t in range(nt):
        b = t % 2
        x_t, d_x = xin[b]
        sc_t, d_sc = scr[b]
        ss_t, d_ss = ssr[b]
        xn_t, d_xn = xnr[b]
        xT_t, d_xT = xTr[b]
        ps_t, d_pst = pstr[b]
        kb.dma("sp", x_t[:], h_in[t * 128:(t + 1) * 128, :], W=[d_x])
        rms_rstd(kb, x_t[:], d_x, D, sc_t[:], d_sc, ss_t[:], d_ss)
        kb.op("dve", lambda e: e.scalar_tensor_tensor(out=xn_t[:], in0=x_t[:], scalar=ss_t[:, 0:1], in1=g_t[:],
                                                       op0=ALU.mult, op1=ALU.mult),
              R=[d_x, d_ss, d_g], W=[d_xn])
        transpose_chunks(kb, xn_t, d_xn, 8, ps_t, d_pst, xT_t, d_xT, ident, d_id)
        for which, (g2_t, d_g2, o_ring, o_dram, scl) in enumerate(
                ((gq_t, d_gq, qn, qo, FD ** -0.5), (gk_t, d_gk, kn, ko, 1.0))):
            qf_t, d_qf = qf[which]
            for half in range(2):
                p_t, d_p = pmm[pi % 6]
                pi += 1
                c0 = which * 1024 + half * 512
                mm_acc(kb, p_t[:], d_p, xT_t, d_xT, w_t, d_w, c0, c0 + 512)
                kb.op("act", lambda e: e.copy(out=qf_t[:, half * 512:(half + 1) * 512], in_=p_t[:]),
                      R=[d_p], W=[d_qf])
            s_t, d_s = s16[which]
            kb.op("pool", lambda e: e.tensor_tensor(out=sc_t[:], in0=qf_t[:], in1=qf_t[:], op=ALU.mult),
                  R=[d_qf], W=[d_sc])
            kb.op("dve", lambda e: e.tensor_reduce(out=s_t[:], in_=sc_t[:].rearrange("p (h d) -> p h d", d=FD),
                                                    axis=AX.X, op=ALU.add), R=[d_sc], W=[d_s])
            kb.op("act", lambda e: e.activation(out=s_t[:], in_=s_t[:], func=AF.Sqrt, scale=1.0 / FD, bias=EPS),
                  R=[d_s], W=[d_s])
            kb.op("dve", lambda e: e.reciprocal(out=s_t[:], in_=s_t[:]), R=[d_s], W=[d_s])
            kb.op("dve", lambda e: e.tensor_tensor(
                out=sc_t[:].rearrange("p (h d) -> p h d", d=FD), in0=qf_t[:].rearrange("p (h d) -> p h d", d=FD),
                in1=s_t[:].unsqueeze(2).to_broadcast([128, FH, FD]), op=ALU.mult), R=[d_qf, d_s], W=[d_sc])
            o_t, d_o = o_ring[b]
            kb.op("dve", lambda e: e.scalar_tensor_tensor(out=o_t[:], in0=sc_t[:], scalar=float(scl), in1=g2_t[:],
                                                           op0=ALU.mult, op1=ALU.mult),
                  R=[d_sc, d_g2], W=[d_o])
            kb.dma("sp", o_dram[t * 128:(t + 1) * 128, :], o_t[:], R=[d_o])
        v_t, d_v = vb[b]
        for half in range(2):
            p_t, d_p = pmm[pi % 6]
            pi += 1
            c0 = 2048 + half * 512
            mm_acc(kb, p_t[:], d_p, xT_t, d_xT, w_t, d_w, c0, c0 + 512)
            kb.op("act", lambda e: e.copy(out=v_t[:, half * 512:(half + 1) * 512], in_=p_t[:]), R=[d_p], W=[d_v])
        kb.dma("sp", vo[t * 128:(t + 1) * 128, :], v_t[:], R=[d_v])
        p_t, d_p = pmm[pi % 6]
        pi += 1
        mm_acc(kb, p_t[:, 0:FH], d_p, xT_t, d_xT, w_t, d_w, 3072, 3088)
        l_t, d_l = lft[b]
        kb.op("dve", lambda e: e.tensor_tensor(out=l_t[:], in0=p_t[:, 0:FH], in1=bf_t[:], op=ALU.add),
              R=[d_p, d_bf], W=[d_l])
        kb.op("act", lambda e: e.activation(out=l_t[:], in_=l_t[:], func=AF.Exp, scale=-1.0), R=[d_l], W=[d_l])
        kb.op("act", lambda e: e.activation(out=l_t[:], in_=l_t[:], func=AF.Ln, bias=1.0), R=[d_l], W=[d_l])
        kb.op("dve", lambda e: e.tensor_scalar(out=l_t[:], in0=l_t[:], scalar1=-1.0, scalar2=None, op0=ALU.mult),
              R=[d_l], W=[d_l])
        kb.dma("sp", lfo[t * 128:(t + 1) * 128, :], l_t[:], R=[d_l])
    kb.finish()
    return nc


def build_B(nb, nbh):
    assert (nb - 1) % 4 == 0
    L = nb * 128
    nI = (nb - 1) // 4 + 1
    nc = bass.Bass("TRN2", target_bir_lowering=False)
    qT = nc.dram_tensor("qT", [nbh, FD, L], BF16, kind="ExternalInput").ap()
    kT = nc.dram_tensor("kT", [nbh, FD, L], BF16, kind="ExternalInput").ap()
    vv = nc.dram_tensor("v", [nbh, L, FD], BF16, kind="ExternalInput").ap()
    lfr = nc.dram_tensor("lfr", [nbh, L], F32, kind="ExternalInput").ap()
    lfT = nc.dram_tensor("lfT", [nbh, 128, nb], F32, kind="ExternalInput").ap()
    oT = nc.dram_tensor("oT", [nbh, FD + 1, L], F32, kind="ExternalOutput").ap()
    kb = KB(nc)
    trif = kb.sb("trif", [128, 128], F32); d_tri = Dep("trif")
    onesf = kb.sb("onesf", [128, 128], F32); d_ones = Dep("onesf")
    sel0 = kb.sb("sel0", [128, 128], F32); d_sel = Dep("sel0")
    maskb = kb.sb("maskb", [128, 128], BF16); d_mask = Dep("maskb")
    onerow = kb.sb("onerow", [128, nb], F32); d_or = Dep("onerow")
    kb.op("pool", lambda e: e.memset(onesf[:], 1.0), W=[d_ones])
    kb.op("pool", lambda e: e.memset(onerow[:], 1.0), W=[d_or])
    kb.op("pool", lambda e: e.affine_select(out=trif[:], in_=onesf[:], pattern=[[1, 128]], compare_op=ALU.is_ge,
                                            fill=0.0, base=0, channel_multiplier=-1), R=[d_ones], W=[d_tri])
    kb.op("pool", lambda e: e.affine_select(out=sel0[:], in_=onesf[:], pattern=[[0, 128]], compare_op=ALU.is_ge,
                                            fill=0.0, base=0, channel_multiplier=-1), R=[d_ones], W=[d_sel])
    kb.op("dve", lambda e: e.tensor_copy(out=maskb[:], in_=trif[:]), R=[d_tri], W=[d_mask])
    for d in (d_tri, d_ones, d_sel, d_mask, d_or):
        d.ro = True
    crow = kb.sb("crow", [nbh, L], F32); d_crow = Dep("crow")
    drow = kb.sb("drow", [nbh, L], BF16); d_drow = Dep("drow")
    kb.dma("sp", crow[:], lfr, W=[d_crow])
    kb.op("dve", lambda e: e.tensor_tensor_scan(out=crow[:], data0=onerow[0:nbh, 0:1].to_broadcast([nbh, L]),
                                                 data1=crow[:], initial=0.0, op0=ALU.mult, op1=ALU.add),
          R=[d_crow, d_or], W=[d_crow])
    kb.op("dve", lambda e: e.tensor_scalar(out=drow[:, 0:128], in0=crow[:, 0:128], scalar1=crow[:, 0:1], scalar2=None,
                                            op0=ALU.subtract), R=[d_crow], W=[d_drow])
    if nI > 1:
        kb.op("dve", lambda e: e.tensor_tensor(
            out=drow[:, 128:L].rearrange("p (i c) -> p i c", c=512),
            in0=crow[:, 128:L].rearrange("p (i c) -> p i c", c=512),
            in1=crow[:, 128:L].rearrange("p (i c) -> p i c", c=512)[:, :, 0:1].to_broadcast([nbh, nI - 1, 512]),
            op=ALU.subtract), R=[d_crow], W=[d_drow])
    QA = kb.sb("QA", [FD + 1, L], BF16); d_QA = Dep("QA")
    KA = kb.sb("KA", [FD + 1, L], BF16); d_KA = Dep("KA")
    VA = kb.sb("VA", [128, nb, FD + 1], BF16); d_VA = Dep("VA")
    lft = kb.sb("lft", [128, nb], F32); d_lft = Dep("lft")
    ct = kb.sb("ct", [128, nb], F32); d_ct = Dep("ct")
    tot = kb.sb("tot", [128, nb], F32); d_tot = Dep("tot")
    rall = kb.sb("rall", [128, nb], F32); d_rall = Dep("rall")
    biasr = kb.ring("bias", [128, nb], F32, 2)
    pr = kb.ring("P", [128, 512], BF16, 4)
    osb = kb.ring("osb", [FD + 1, 512], F32, 2)
    psS = kb.ring("psS", [128, 512], F32, 4, psum=True)
    psO = kb.ring("psO", [128, 512], F32, 2, psum=True)
    psM = kb.ring("psM", [128, 512], F32, 2, psum=True)
    si = 0
    oi = 0
    for bh in range(nbh):
        kb.dma("sp", QA[0:FD, :], qT[bh], W=[d_QA])
        kb.dma("sp", QA[FD:FD + 1, :], drow[bh:bh + 1, :], R=[d_drow], W=[d_QA])
        kb.dma("sp", KA[0:FD, :], kT[bh], W=[d_KA])
        kb.op("pool", lambda e: e.memset(KA[FD:FD + 1, :], 1.0), W=[d_KA])
        kb.dma("sp", VA[:, :, 0:FD], vv[bh].rearrange("(j p) d -> p j d", p=128), W=[d_VA])
        kb.op("pool", lambda e: e.memset(VA[:, :, FD:FD + 1], 1.0), W=[d_VA])
        kb.op("pool", lambda e: e.memset(VA[0:112, 0, :], 0.0), W=[d_VA])
        kb.dma("sp", lft[:], lfT[bh], W=[d_lft])
        pm0, d_pm0 = psM[0]
        pm1, d_pm1 = psM[1]
        kb.op("pe", lambda e: e.matmul(pm0[:, 0:nb], lhsT=trif[:], rhs=lft[:], start=True, stop=True),
              R=[d_tri, d_lft], W=[d_pm0])
        kb.op("pe", lambda e: e.matmul(pm1[:, 0:nb], lhsT=onesf[:], rhs=lft[:], start=True, stop=True),
              R=[d_ones, d_lft], W=[d_pm1])
        kb.op("dve", lambda e: e.tensor_copy(out=tot[:], in_=pm1[:, 0:nb]), R=[d_pm1], W=[d_tot])
        kb.op("dve", lambda e: e.tensor_tensor_scan(out=ct[:], data0=onerow[:], data1=tot[:], initial=0.0,
                                                     op0=ALU.mult, op1=ALU.add), R=[d_tot, d_or], W=[d_ct])
        kb.op("dve", lambda e: e.tensor_tensor(out=ct[:], in0=ct[:], in1=tot[:], op=ALU.subtract),
              R=[d_ct, d_tot], W=[d_ct])
        kb.op("dve", lambda e: e.tensor_tensor(out=ct[:], in0=ct[:], in1=pm0[:, 0:nb], op=ALU.add),
              R=[d_ct, d_pm0], W=[d_ct])
        kb.op("pe", lambda e: e.matmul(pm1[:, 0:nb], lhsT=sel0[:], rhs=ct[:], start=True, stop=True),
              R=[d_sel, d_ct], W=[d_pm1])
        kb.op("dve", lambda e: e.tensor_copy(out=rall[:], in_=pm1[:, 0:nb]), R=[d_pm1], W=[d_rall])
        steps = []
        for I in range(nI):
            j0 = 0 if I == 0 else 4 * I - 3
            nblk = 1 if I == 0 else 4
            nJ = j0 + nblk
            for J in range(nJ):
                steps.append((I, J, j0, nblk, nJ))
        LA = 2
        cur = {}
        for idx in range(len(steps) + LA):
            if idx < len(steps):
                I, J, j0, nblk, nJ = steps[idx]
                q0 = j0 * 128
                bias_t, d_bias = biasr[I % 2]
                if J == 0:
                    kb.op("dve", lambda e: e.tensor_scalar(out=bias_t[:, 0:nJ], in0=ct[:, 0:nJ], scalar1=-1.0,
                                                            scalar2=rall[:, j0:j0 + 1], op0=ALU.mult, op1=ALU.add),
                          R=[d_ct, d_rall], W=[d_bias])
                m = max(0, J - j0)
                c0 = m * 128
                c1 = nblk * 128
                ps, d_ps = psS[idx % 4]
                p_t, d_p = pr[idx % 4]
                kb.op("pe", lambda e: e.matmul(ps[:, c0:c1], lhsT=KA[:, J * 128:(J + 1) * 128],
                                               rhs=QA[:, q0 + c0:q0 + c1], start=True, stop=True),
                      R=[d_KA, d_QA], W=[d_ps])
                kb.op("act", lambda e: e.activation(out=p_t[:, c0:c1], in_=ps[:, c0:c1], func=AF.Exp,
                                                    bias=bias_t[:, J:J + 1], scale=1.0),
                      R=[d_ps, d_bias], W=[d_p])
                if J >= j0:
                    kb.op("dve", lambda e: e.tensor_tensor(out=p_t[:, c0:c0 + 128], in0=p_t[:, c0:c0 + 128],
                                                            in1=maskb[:], op=ALU.mult), R=[d_p, d_mask], W=[d_p])
            if idx >= LA:
                I, J, j0, nblk, nJ = steps[idx - LA]
                q0 = j0 * 128
                m = max(0, J - j0)
                c0 = m * 128
                c1 = nblk * 128
                p_t, d_p = pr[(idx - LA) % 4]
                po, d_po = psO[I % 2]
                kb.op("pe", lambda e: e.matmul(po[0:FD + 1, c0:c1], lhsT=VA[:, J, :], rhs=p_t[:, c0:c1],
                                               start=(J == 0), stop=(J == nJ - 1)),
                      R=[d_VA, d_p], W=[d_po])
                if J == nJ - 1:
                    o_t, d_o = osb[I % 2]
                    ncol = nblk * 128
                    kb.op("dve", lambda e: e.tensor_copy(out=o_t[:, 0:ncol], in_=po[0:FD + 1, 0:ncol]),
                          R=[d_po], W=[d_o])
                    kb.dma("sp", oT[bh, :, q0:q0 + ncol], o_t[:, 0:ncol], R=[d_o])
    kb.finish()
    return nc


BIGIDX = 1.0e6


def build_CE(stage, nt, has_meta, cap=CAP):
    nc = bass.Bass("TRN2", target_bir_lowering=False)
    rows = nt * 128
    nslot = NE * cap
    nst = cap // 128
    a_in = nc.dram_tensor("a_in", [rows, D], F32, kind="ExternalInput").ap()
    hp_in = nc.dram_tensor("hp_in", [rows, D], F32, kind="ExternalInput").ap()
    if stage == "C":
        den_in = nc.dram_tensor("den_in", [rows, FH], F32, kind="ExternalInput").ap()
    else:
        gs_in = nc.dram_tensor("gs_in", [rows, D], BF16, kind="ExternalInput").ap()
    valid_in = nc.dram_tensor("valid_in", [rows, 1], F32, kind="ExternalInput").ap()
    w_out = nc.dram_tensor("w_out", [D, D], F32, kind="ExternalInput").ap()
    mnorm = nc.dram_tensor("mnorm", [1, D], F32, kind="ExternalInput").ap()
    w_r = nc.dram_tensor("w_r", [D, 36], F32, kind="ExternalInput").ap()
    b_r = nc.dram_tensor("b_r", [1, 36], F32, kind="ExternalInput").ap()
    w_up = nc.dram_tensor("w_up", [NE, D, 2 * DE], F32, kind="ExternalInput").ap()
    w_dn = nc.dram_tensor("w_dn", [NE, DE, D], F32, kind="ExternalInput").ap()
    h2_out = nc.dram_tensor("h2_out", [rows, D], F32, kind="ExternalOutput").ap()
    if stage == "C":
        hnorm = nc.dram_tensor("hnorm", [1, D], F32, kind="ExternalInput").ap()
        hw_in = nc.dram_tensor("hw_in", [D, 4 * D], F32, kind="ExternalInput").ap()
        hbf = nc.dram_tensor("hbf", [1, D], F32, kind="ExternalInput").ap()
        lbl = nc.dram_tensor("lbl", [2, D], F32, kind="ExternalInput").ap()
        q1_out = nc.dram_tensor("q1_out", [rows, D], BF16, kind="ExternalOutput").ap()
        lf1_out = nc.dram_tensor("lf1_out", [rows, D], F32, kind="ExternalOutput").ap()
        v1_out = nc.dram_tensor("v1_out", [rows, D], BF16, kind="ExternalOutput").ap()
        gs_out = nc.dram_tensor("gs_out", [rows, D], BF16, kind="ExternalOutput").ap()
    xpad = nc.dram_tensor("xpad", [nslot, D], BF16, kind="Internal").ap()
    ypad = nc.dram_tensor("ypad", [nslot, D], BF16, kind="Internal").ap()
    h1s = nc.dram_tensor("h1s", [rows, D], F32, kind="Internal").ap()
    d_xpad, d_ypad, d_h1s = Dep("xpad"), Dep("ypad"), Dep("h1s")

    kb = KB(nc)
    identf, d_idf, ident, d_id = make_ident(kb)
    breg = nc.gpsimd.to_reg(nslot - 1)
    dest_i = kb.sb("dest_i", [128, nt, 2], I32); d_dest = Dep("dest_i")
    gw = kb.sb("gw", [128, nt, 2], F32); d_gw = Dep("gw")
    cntb = kb.sb("cntb", [128, NE], F32); d_cntb = Dep("cntb")
    usb = kb.sb("usb", [128, 128], BF16); d_us = Dep("usb")
    onesb = kb.sb("onesb", [128, 128], BF16); d_onesb = Dep("onesb")
    onesf = kb.sb("onesf", [128, 128], F32); d_onesf = Dep("onesf")
    tmpf = kb.sb("tmpf", [128, 128], F32); d_tmpf = Dep("tmpf")
    kb.op("pool", lambda e: e.memset(onesf[:], 1.0), W=[d_onesf])
    kb.op("pool", lambda e: e.affine_select(out=tmpf[:], in_=onesf[:], pattern=[[1, 128]], compare_op=ALU.is_gt,
                                            fill=0.0, base=0, channel_multiplier=-1), R=[d_onesf], W=[d_tmpf])
    kb.op("dve", lambda e: e.tensor_copy(out=usb[:], in_=tmpf[:]), R=[d_tmpf], W=[d_us])
    kb.op("dve", lambda e: e.tensor_copy(out=onesb[:], in_=onesf[:]), R=[d_onesf], W=[d_onesb])
    kb.op("pool", lambda e: e.iota(cntb[:], pattern=[[cap, NE]], base=0, channel_multiplier=0,
                                   allow_small_or_imprecise_dtypes=True), W=[d_cntb])
    for d in (d_us, d_onesb, d_onesf):
        d.ro = True
    mn_t, d_mn = load_bc(kb, "mnorm", mnorm, D)
    br_t, d_br = load_bc(kb, "b_r", b_r, 36)
    wr_t, d_wr = load_w(kb, "w_r", w_r, D, 36, dt=F32)
    d_wr.ro = True

    with kb.scope():
        wo_t, d_wo = load_w(kb, "w_out", w_out, D, D)
        d_wo.ro = True
        a_r = kb.ring("a", [128, D], F32, 2)
        hp_r = kb.ring("hp", [128, D], F32, 2)
        den_r = kb.ring("den", [128, FH], F32, 2)
        gs_r = kb.ring("gs", [128, D], BF16, 2)
        ob_r = kb.ring("ob", [128, D], BF16, 2)
        oT_r = kb.ring("oT", [128, 8, 128], BF16, 2)
        h1_r = kb.ring("h1", [128, D], F32, 2)
        scr_r = kb.ring("scr", [128, D], F32, 2)
        ss_r = kb.ring("ss", [128, 1], F32, 2)
        hmf_r = kb.ring("hmf", [128, D], F32, 2)
        hmb_r = kb.ring("hmb", [128, D], BF16, 2)
        hmT_r = kb.ring("hmT", [128, 8, 128], F32, 2)
        sm_r = kb.ring("sm", [128, 256], F32, 2)
        ab_r = kb.ring("ab", [128, NE], BF16, 2)
        val_r = kb.ring("val", [128, 2], F32, 2)
        pst_r = kb.ring("pst", [128, 8, 128], BF16, 2, psum=True)
        pstf_r = kb.ring("pstf", [128, 8, 128], F32, 1, psum=True)
        pmm_r = kb.ring("pmm", [128, 512], F32, 2, psum=True)
        prt_r = kb.ring("prt", [128, 512], F32, 2, psum=True)
        for t in range(nt):
            b = t % 2
            r0, r1 = t * 128, (t + 1) * 128
            a_t, d_a = a_r[b]
            hp_t, d_hp = hp_r[b]
            ob_t, d_ob = ob_r[b]
            kb.dma("sp", a_t[:], a_in[r0:r1, :], W=[d_a])
            kb.dma("sp", hp_t[:], hp_in[r0:r1, :], W=[d_hp])
            if stage == "C":
                den_t, d_den = den_r[b]
                kb.dma("sp", den_t[:], den_in[r0:r1, :], W=[d_den])
                kb.op("dve", lambda e: e.tensor_scalar(out=den_t[:], in0=den_t[:], scalar1=1e-30, scalar2=None,
                                                        op0=ALU.max), R=[d_den], W=[d_den])
                kb.op("dve", lambda e: e.reciprocal(out=den_t[:], in_=den_t[:]), R=[d_den], W=[d_den])
                kb.op("dve", lambda e: e.tensor_tensor(
                    out=ob_t[:].rearrange("p (h d) -> p h d", d=FD), in0=a_t[:].rearrange("p (h d) -> p h d", d=FD),
                    in1=den_t[:].unsqueeze(2).to_broadcast([128, FH, FD]), op=ALU.mult), R=[d_a, d_den], W=[d_ob])
            else:
                gs_t, d_gs = gs_r[b]
                kb.dma("sp", gs_t[:], gs_in[r0:r1, :], W=[d_gs])
                kb.op("dve", lambda e: e.tensor_tensor(out=ob_t[:], in0=a_t[:], in1=gs_t[:], op=ALU.mult),
                      R=[d_a, d_gs], W=[d_ob])
            oT_t, d_oT = oT_r[b]
            ps_t, d_pst = pst_r[b]
            transpose_chunks(kb, ob_t, d_ob, 8, ps_t, d_pst, oT_t, d_oT, ident, d_id, copy_eng="act")
            h1_t, d_h1 = h1_r[b]
            for half in range(2):
                p_t, d_p = pmm_r[half]
                mm_acc(kb, p_t[:], d_p, oT_t, d_oT, wo_t, d_wo, half * 512, half * 512 + 512)
                kb.op("dve", lambda e: e.tensor_tensor(out=h1_t[:, half * 512:(half + 1) * 512], in0=p_t[:],
                                                        in1=hp_t[:, half * 512:(half + 1) * 512], op=ALU.add),
                      R=[d_p, d_hp], W=[d_h1])
            kb.dma("sp", h1s[r0:r1, :], h1_t[:], R=[d_h1], W=[d_h1s])
            sc_t, d_sc = scr_r[b]
            ss_t, d_ss = ss_r[b]
            hmf_t, d_hmf = hmf_r[b]
            hmb_t, d_hmb = hmb_r[b]
            rms_rstd(kb, h1_t[:], d_h1, D, sc_t[:], d_sc, ss_t[:], d_ss)
            kb.op("dve", lambda e: e.scalar_tensor_tensor(out=hmf_t[:], in0=h1_t[:], scalar=ss_t[:, 0:1], in1=mn_t[:],
                                                           op0=ALU.mult, op1=ALU.mult),
                  R=[d_h1, d_ss, d_mn], W=[d_hmf])
            kb.op("pool", lambda e: e.tensor_copy(out=hmb_t[:], in_=hmf_t[:]), R=[d_hmf], W=[d_hmb])
            pf_t, d_pf = pstf_r[0]
            hmT_t, d_hmT = hmT_r[b]
            for c in range(8):
                kb.op("pe", lambda e: e.transpose(out=pf_t[:, c, :], in_=hmf_t[:, c * 128:(c + 1) * 128],
                                                  identity=identf[:]), R=[d_hmf, d_idf], W=[d_pf])
            kb.op("act", lambda e: e.copy(out=hmT_t[:], in_=pf_t[:]), R=[d_pf], W=[d_hmT])
            pr_t, d_pr = prt_r[b]
            for c in range(8):
                kb.op("pe", lambda e: e.matmul(pr_t[:, 0:36], lhsT=hmT_t[:, c, :], rhs=wr_t[:, c, :],
                                               start=(c == 0), stop=(c == 7)), R=[d_hmT, d_wr], W=[d_pr])
            sm, d_sm = sm_r[b]
            lg = sm[:, 0:36]
            gmax = sm[:, 36:37]
            ngmax = sm[:, 37:38]
            gsum = sm[:, 38:39]
            ggate = sm[:, 39:40]
            eg = sm[:, 40:44]
            ohg = sm[:, 44:48]
            esel = sm[:, 48:56]
            top8 = sm[:, 56:64]
            oh0 = sm[:, 64:72]
            oh1 = sm[:, 72:80]
            A0 = sm[:, 80:112]
            A1 = sm[:, 112:144]
            sbt = sm[:, 144:176]
            tmp = sm[:, 176:208]
            destf = sm[:, 208:210]
            diff = sm[:, 210:211]
            sg = sm[:, 211:212]
            S = [d_sm]
            dv = lambda fn, R=(), W=(): kb.op("dve", fn, R=list(R) + S, W=list(W) + S)
            dv(lambda e: e.tensor_tensor(out=lg, in0=pr_t[:, 0:36], in1=br_t[:], op=ALU.add), R=[d_pr, d_br])
            dv(lambda e: e.tensor_reduce(out=gmax, in_=lg[:, 0:4], axis=AX.X, op=ALU.max))
            dv(lambda e: e.tensor_scalar(out=ngmax, in0=gmax, scalar1=-1.0, scalar2=None, op0=ALU.mult))
            kb.op("act", lambda e: e.activation(out=eg, in_=lg[:, 0:4], func=AF.Exp, bias=ngmax, scale=1.0,
                                                accum_out=gsum), R=S, W=S)
            dv(lambda e: e.reciprocal(out=ggate, in_=gsum))
            dv(lambda e: e.tensor_scalar(out=ohg, in0=lg[:, 0:4], scalar1=gmax, scalar2=None, op0=ALU.is_equal))
            dv(lambda e: e.tensor_scalar(out=esel, in0=lg[:, 4:12], scalar1=ohg[:, 0:1], scalar2=None, op0=ALU.mult))
            for g in range(1, 4):
                dv(lambda e: e.scalar_tensor_tensor(out=esel, in0=lg[:, 4 + 8 * g:12 + 8 * g], scalar=ohg[:, g:g + 1],
                                                    in1=esel, op0=ALU.mult, op1=ALU.add))
            dv(lambda e: e.max(out=top8, in_=esel))
            dv(lambda e: e.tensor_scalar(out=oh0, in0=esel, scalar1=top8[:, 0:1], scalar2=None, op0=ALU.is_equal))
            dv(lambda e: e.tensor_scalar(out=oh1, in0=esel, scalar1=top8[:, 1:2], scalar2=None, op0=ALU.is_equal))
            for (Ak, ohk) in ((A0, oh0), (A1, oh1)):
                dv(lambda e: e.tensor_tensor(out=Ak.rearrange("p (g j) -> p g j", j=8),
                                             in0=ohg.unsqueeze(2).to_broadcast([128, 4, 8]),
                                             in1=ohk.unsqueeze(1).to_broadcast([128, 4, 8]), op=ALU.mult))
            use_valid = has_meta and t == 0
            if use_valid:
                val_t, d_val = val_r[b]
                kb.dma("sp", val_t[:, 0:1], valid_in[r0:r1, :], W=[d_val])
                kb.op("dve", lambda e: e.tensor_scalar(out=val_t[:, 1:2], in0=val_t[:, 0:1], scalar1=-BIGIDX,
                                                        scalar2=BIGIDX, op0=ALU.mult, op1=ALU.add),
                      R=[d_val], W=[d_val])
                for Ak in (A0, A1):
                    dv(lambda e: e.tensor_scalar(out=Ak, in0=Ak, scalar1=val_t[:, 0:1], scalar2=None, op0=ALU.mult),
                       R=[d_val])
            ab_t, d_ab = ab_r[b]
            kb.op("dve", lambda e: e.tensor_tensor(out=ab_t[:], in0=A0, in1=A1, op=ALU.add), R=S, W=[d_ab])
            kb.op("pe", lambda e: e.matmul(pr_t[:, 64:96], lhsT=usb[:], rhs=ab_t[:], start=True, stop=True),
                  R=[d_us, d_ab], W=[d_pr])
            kb.op("pe", lambda e: e.matmul(pr_t[:, 96:128], lhsT=onesb[:], rhs=ab_t[:], start=True, stop=True),
                  R=[d_onesb, d_ab], W=[d_pr])
            dv(lambda e: e.tensor_tensor(out=sbt, in0=pr_t[:, 64:96], in1=cntb[:], op=ALU.add), R=[d_pr, d_cntb])
            kb.op("dve", lambda e: e.tensor_tensor(out=cntb[:], in0=cntb[:], in1=pr_t[:, 96:128], op=ALU.add),
                  R=[d_pr, d_cntb] + S, W=[d_cntb])
            for k, Ak in enumerate((A0, A1)):
                dv(lambda e: e.tensor_tensor(out=tmp, in0=Ak, in1=sbt, op=ALU.mult))
                dv(lambda e: e.tensor_reduce(out=destf[:, k:k + 1], in_=tmp, axis=AX.X, op=ALU.add))
            if use_valid:
                dv(lambda e: e.tensor_scalar(out=destf, in0=destf, scalar1=val_t[:, 0:1], scalar2=val_t[:, 1:2],
                                             op0=ALU.mult, op1=ALU.add), R=[d_val])
            kb.op("dve", lambda e: e.tensor_copy(out=dest_i[:, t, :], in_=destf), R=S, W=[d_dest])
            dv(lambda e: e.tensor_tensor(out=diff, in0=top8[:, 0:1], in1=top8[:, 1:2], op=ALU.subtract))
            kb.op("act", lambda e: e.activation(out=sg, in_=diff, func=AF.Sigmoid), R=S, W=S)
            kb.op("dve", lambda e: e.tensor_tensor(out=gw[:, t, 0:1], in0=sg, in1=ggate, op=ALU.mult), R=S, W=[d_gw])
            kb.op("dve", lambda e: e.tensor_tensor(out=gw[:, t, 1:2], in0=ggate, in1=gw[:, t, 0:1], op=ALU.subtract),
                  R=S + [d_gw], W=[d_gw])
            for k in range(2):
                kb.idma(out=xpad[:, :], out_off=bass.IndirectOffsetOnAxis(ap=dest_i[:, t, k:k + 1], axis=0),
                        in_=hmb_t[:], in_off=None, R=[d_hmb, d_dest], W=[d_xpad],
                        bounds_check=breg, oob_is_err=False)

    with kb.scope():
        wup_r = kb.ring("wup", [128, 8, 2 * DE], BF16, 2)
        wdn_r = kb.ring("wdn", [128, 4, D], BF16, 2)
        xs_r = kb.ring("xs", [128, D], BF16, 3)
        xT_r = kb.ring("xT", [128, 8, cap], BF16, 2)
        sa_r = kb.ring("sa", [128, cap], F32, 2)
        aT_r = kb.ring("aT", [128, 4, cap], BF16, 2)
        yb_r = kb.ring("yb", [128, D], BF16, 2)
        pst_r = kb.ring("pst", [128, 8, 128], BF16, 2, psum=True)
        pau_r = kb.ring("pau", [128, 512], F32, 4, psum=True)
        py_r = kb.ring("py", [128, 512], F32, 2, psum=True)
        xi = 0
        yi = 0
        for ex in range(NE):
            b = ex % 2
            wup_t, d_wup = wup_r[b]
            wdn_t, d_wdn = wdn_r[b]
            wuv = w_up[ex].rearrange("(c p) n -> p c n", p=128)
            wdv = w_dn[ex].rearrange("(c p) n -> p c n", p=128)
            for c in range(8):
                kb.dma("pool", wup_t[:, c, :], wuv[:, c, :], W=[d_wup])
            for c in range(4):
                kb.dma("pool", wdn_t[:, c, :], wdv[:, c, :], W=[d_wdn])
            xT_t, d_xT = xT_r[b]
            for st in range(nst):
                xs_t, d_xs = xs_r[xi % 3]
                ps_t, d_pst = pst_r[xi % 2]
                xi += 1
                s0 = ex * cap + st * 128
                kb.dma("sp", xs_t[:], xpad[s0:s0 + 128, :], R=[d_xpad], W=[d_xs])
                for c in range(8):
                    kb.op("pe", lambda e: e.transpose(out=ps_t[:, c, :], in_=xs_t[:, c * 128:(c + 1) * 128],
                                                      identity=ident[:]), R=[d_xs, d_id], W=[d_pst])
                kb.op("act" if st % 2 else "dve",
                      (lambda e: e.copy(out=xT_t[:, :, st * 128:(st + 1) * 128], in_=ps_t[:])) if st % 2 else
                      (lambda e: e.tensor_copy(out=xT_t[:, :, st * 128:(st + 1) * 128], in_=ps_t[:])),
                      R=[d_pst], W=[d_xT])
            aT_t, d_aT = aT_r[b]
            for fc in range(4):
                pa, d_pa = pau_r[(2 * fc) % 4]
                pu, d_pu = pau_r[(2 * fc + 1) % 4]
                for c in range(8):
                    kb.op("pe", lambda e: e.matmul(pa[:, 0:cap], lhsT=wup_t[:, c, fc * 128:(fc + 1) * 128],
                                                   rhs=xT_t[:, c, :], start=(c == 0), stop=(c == 7)),
                          R=[d_wup, d_xT], W=[d_pa])
                for c in range(8):
                    kb.op("pe", lambda e: e.matmul(pu[:, 0:cap], lhsT=wup_t[:, c, DE + fc * 128:DE + (fc + 1) * 128],
                                                   rhs=xT_t[:, c, :], start=(c == 0), stop=(c == 7)),
                          R=[d_wup, d_xT], W=[d_pu])
                sa_t, d_sa = sa_r[fc % 2]
                kb.op("act", lambda e: e.activation(out=sa_t[:], in_=pa[:, 0:cap], func=AF.Silu), R=[d_pa], W=[d_sa])
                kb.op("dve", lambda e: e.tensor_tensor(out=aT_t[:, fc, :], in0=sa_t[:], in1=pu[:, 0:cap], op=ALU.mult),
                      R=[d_sa, d_pu], W=[d_aT])
            for st in range(nst):
                yb_t, d_yb = yb_r[yi % 2]
                yi += 1
                for half in range(2):
                    py, d_py = py_r[half]
                    for fc in range(4):
                        kb.op("pe", lambda e: e.matmul(py[:], lhsT=aT_t[:, fc, st * 128:(st + 1) * 128],
                                                       rhs=wdn_t[:, fc, half * 512:(half + 1) * 512],
                                                       start=(fc == 0), stop=(fc == 3)),
                              R=[d_aT, d_wdn], W=[d_py])
                    if half == 0:
                        kb.op("act", lambda e: e.copy(out=yb_t[:, 0:512], in_=py[:]), R=[d_py], W=[d_yb])
                    else:
                        kb.op("dve", lambda e: e.tensor_copy(out=yb_t[:, 512:1024], in_=py[:]), R=[d_py], W=[d_yb])
                s0 = ex * cap + st * 128
                kb.dma("sp", ypad[s0:s0 + 128, :], yb_t[:], R=[d_yb], W=[d_ypad])

    with kb.scope():
        h1_r = kb.ring("h1", [128, D], F32, 2)
        y_r = kb.ring("y", [128, D], BF16, 4)
        h2_r = kb.ring("h2", [128, D], F32, 2)
        for (y_t, d_y) in y_r:
            kb.op("pool", lambda e: e.memset(y_t[:], 0.0), W=[d_y])
        if stage == "C":
            hw_t, d_hw = load_w(kb, "hw_in", hw_in, D, 4 * D)
            d_hw.ro = True
            hn_t, d_hn = load_bc(kb, "hnorm", hnorm, D)
            hbf_t, d_hbf = load_bc(kb, "hbf", hbf, D)
            l0_t, d_l0 = load_bc(kb, "l0", lbl[0:1, :], D)
            l1_t, d_l1 = load_bc(kb, "l1", lbl[1:2, :], D)
            lb_t = kb.sb("lb", [128, D], F32); d_lb = Dep("lb")
            oml_t = kb.sb("oml", [128, D], F32); d_oml = Dep("oml")
            kb.op("dve", lambda e: e.tensor_tensor(out=lb_t[:], in0=l1_t[:], in1=l0_t[:], op=ALU.subtract),
                  R=[d_l0, d_l1], W=[d_lb])
            kb.op("act", lambda e: e.activation(out=oml_t[:], in_=lb_t[:], func=AF.Sigmoid, scale=-1.0),
                  R=[d_lb], W=[d_oml])
            kb.op("act", lambda e: e.activation(out=lb_t[:], in_=lb_t[:], func=AF.Sigmoid), R=[d_lb], W=[d_lb])
            d_lb.ro = True
            d_oml.ro = True
            scr_r = kb.ring("scr", [128, D], F32, 2)
            ss_r = kb.ring("ss", [128, 1], F32, 2)
            xn_r = kb.ring("xn", [128, D], BF16, 2)
            xT_r = kb.ring("xT", [128, 8, 128], BF16, 2)
            pst_r = kb.ring("pst", [128, 8, 128], BF16, 2, psum=True)
            pmm_r = kb.ring("pmm", [128, 512], F32, 4, psum=True)
            ob_r = kb.ring("obf", [128, D], BF16, 4)
            of_r = kb.ring("off", [128, D], F32, 2)
            zf_r = kb.ring("zf", [128, 512], F32, 2)
        pi = 0
        obi = 0
        for t in range(nt):
            b = t % 2
            r0, r1 = t * 128, (t + 1) * 128
            h1_t, d_h1 = h1_r[b]
            h2_t, d_h2 = h2_r[b]
            y0_t, d_y0 = y_r[2 * b]
            y1_t, d_y1 = y_r[2 * b + 1]
            kb.dma("sp", h1_t[:], h1s[r0:r1, :], R=[d_h1s], W=[d_h1])
            for k, (y_t, d_y) in enumerate(((y0_t, d_y0), (y1_t, d_y1))):
                kb.idma(out=y_t[:], out_off=None, in_=ypad[:, :],
                        in_off=bass.IndirectOffsetOnAxis(ap=dest_i[:, t, k:k + 1], axis=0),
                        R=[d_ypad, d_dest], W=[d_y], bounds_check=breg, oob_is_err=False)
            kb.op("dve", lambda e: e.scalar_tensor_tensor(out=h2_t[:], in0=y0_t[:], scalar=gw[:, t, 0:1], in1=h1_t[:],
                                                           op0=ALU.mult, op1=ALU.add), R=[d_y0, d_gw, d_h1], W=[d_h2])
            kb.op("dve", lambda e: e.scalar_tensor_tensor(out=h2_t[:], in0=y1_t[:], scalar=gw[:, t, 1:2], in1=h2_t[:],
                                                           op0=ALU.mult, op1=ALU.add), R=[d_y1, d_gw, d_h2], W=[d_h2])
            kb.dma("sp", h2_out[r0:r1, :], h2_t[:], R=[d_h2])
            if stage != "C":
                continue
            sc_t, d_sc = scr_r[b]
            ss_t, d_ss = ss_r[b]
            xn_t, d_xn = xn_r[b]
            xT_t, d_xT = xT_r[b]
            ps_t, d_pst = pst_r[b]
            rms_rstd(kb, h2_t[:], d_h2, D, sc_t[:], d_sc, ss_t[:], d_ss)
            kb.op("dve", lambda e: e.scalar_tensor_tensor(out=xn_t[:], in0=h2_t[:], scalar=ss_t[:, 0:1], in1=hn_t[:],
                                                           op0=ALU.mult, op1=ALU.mult), R=[d_h2, d_ss, d_hn], W=[d_xn])
            transpose_chunks(kb, xn_t, d_xn, 8, ps_t, d_pst, xT_t, d_xT, ident, d_id)
            for grp, (dram_o, kind) in enumerate(((q1_out, "silu"), (lf1_out, "f"), (v1_out, "copy"), (gs_out, "silu"))):
                if kind == "f":
                    o_t, d_o = of_r[b]
                else:
                    o_t, d_o = ob_r[obi % 4]
                    obi += 1
                for half in range(2):
                    p_t, d_p = pmm_r[pi % 4]
                    pi += 1
                    c0 = grp * 1024 + half * 512
                    hs = slice(half * 512, (half + 1) * 512)
                    mm_acc(kb, p_t[:], d_p, xT_t, d_xT, hw_t, d_hw, c0, c0 + 512)
                    if kind == "silu":
                        kb.op("act", lambda e: e.activation(out=o_t[:, hs], in_=p_t[:], func=AF.Silu), R=[d_p], W=[d_o])
                    elif kind == "copy":
                        kb.op("act", lambda e: e.copy(out=o_t[:, hs], in_=p_t[:]), R=[d_p], W=[d_o])
                    else:
                        z_t, d_z = zf_r[half]
                        kb.op("dve", lambda e: e.tensor_tensor(out=z_t[:], in0=p_t[:], in1=hbf_t[:, hs], op=ALU.add),
                              R=[d_p, d_hbf], W=[d_z])
                        kb.op("act", lambda e: e.activation(out=z_t[:], in_=z_t[:], func=AF.Sigmoid), R=[d_z], W=[d_z])
                        kb.op("dve", lambda e: e.tensor_tensor(out=z_t[:], in0=z_t[:], in1=oml_t[:, hs], op=ALU.mult),
                              R=[d_z, d_oml], W=[d_z])
                        kb.op("dve", lambda e: e.tensor_tensor(out=z_t[:], in0=z_t[:], in1=lb_t[:, hs], op=ALU.add),
                              R=[d_z, d_lb], W=[d_z])
                        kb.op("act", lambda e: e.activation(out=o_t[:, hs], in_=z_t[:], func=AF.Ln), R=[d_z], W=[d_o])
                kb.dma("sp", dram_o[r0:r1, :], o_t[:], R=[d_o])
    kb.finish()
    return nc


def build_D(nb, nbh):
    L = nb * 128
    nc = bass.Bass("TRN2", target_bir_lowering=False)
    qT = nc.dram_tensor("qT", [nbh, 128, L], BF16, kind="ExternalInput").ap()
    lfT = nc.dram_tensor("lfT", [nbh, 128, L], F32, kind="ExternalInput").ap()
    vv = nc.dram_tensor("v", [nbh, L, 128], BF16, kind="ExternalInput").ap()
    go = nc.dram_tensor("go", [1, 128], F32, kind="ExternalInput").ap()
    on = nc.dram_tensor("on", [nbh, L, 128], F32, kind="ExternalOutput").ap()
    kb = KB(nc)
    identf, d_idf, ident, d_id = make_ident(kb)
    go_t, d_go = load_bc(kb, "go", go, 128)
    onesf = kb.sb("onesf", [128, 128], F32); d_onesf = Dep("onesf")
    mf = kb.sb("mf", [128, 128], F32); d_mf = Dep("mf")
    mku = kb.sb("mku", [128, 128], U32); d_mk = Dep("mku")
    kb.op("pool", lambda e: e.memset(onesf[:], 1.0), W=[d_onesf])
    kb.op("pool", lambda e: e.affine_select(out=mf[:], in_=onesf[:], pattern=[[1, 128]], compare_op=ALU.is_ge,
                                            fill=0.0, base=0, channel_multiplier=-1), R=[d_onesf], W=[d_mf])
    kb.op("dve", lambda e: e.tensor_copy(out=mku[:], in_=mf[:]), R=[d_mf], W=[d_mk])
    d_mk.ro = True
    d_onesf.ro = True
    q_t = kb.sb("q", [128, L], BF16); d_q = Dep("q")
    lf_t = kb.sb("lf", [128, L], F32); d_lf = Dep("lf")
    v_t = kb.sb("v", [128, nb, 128], BF16); d_v = Dep("v")
    S_t = kb.sb("S", [128, 128], F32); d_S = Dep("S")
    Sb_r = kb.ring("Sb", [128, 128], BF16, 2)
    b_r = kb.ring("b", [128, 128], F32, 2)
    col_r = kb.ring("col", [128, 8], F32, 2)
    e1_r = kb.ring("e1", [128, 128], F32, 2)
    e2_r = kb.ring("e2", [128, 128], F32, 2)
    kk_r = kb.ring("kk", [128, 128], F32, 2)
    qd_r = kb.ring("qd", [128, 128], BF16, 2)
    kd_r = kb.ring("kd", [128, 128], BF16, 2)
    qb_r = kb.ring("qb", [128, 128], BF16, 2)
    ke_r = kb.ring("ke", [128, 128], BF16, 2)
    keT_r = kb.ring("keT", [128, 128], BF16, 2)
    scm_r = kb.ring("scm", [128, 128], BF16, 2)
    osq_r = kb.ring("osq", [128, 128], F32, 2)
    oss_r = kb.ring("oss", [128, 2], F32, 2)
    on_r = kb.ring("on", [128, 128], F32, 2)
    psc_r = kb.ring("psc", [128, 512], F32, 2, psum=True)
    po_r = kb.ring("po", [128, 512], F32, 2, psum=True)
    pt_r = kb.ring("pt", [128, 8, 128], BF16, 2, psum=True)
    pu_r = kb.ring("pu", [128, 512], F32, 2, psum=True)
    for (t_, d_) in scm_r:
        kb.op("pool", lambda e: e.memset(t_[:], 0.0), W=[d_])
    for bh in range(nbh):
        kb.dma("sp", q_t[:], qT[bh], W=[d_q])
        for hlf in range(4):
            c0 = (L // 4) * hlf
            kb.dma("sp", lf_t[:, c0:c0 + L // 4], lfT[bh][:, c0:c0 + L // 4], W=[d_lf])
        kb.dma("sp", v_t[:], vv[bh].rearrange("(j p) d -> p j d", p=128), W=[d_v])
        kb.op("pool", lambda e: e.memset(S_t[:], 0.0), W=[d_S])
        kb.op("pool", lambda e: e.memset(Sb_r[0][0][:], 0.0), W=[Sb_r[0][1]])
        for c in range(nb):
            r = c % 2
            cs = slice(c * 128, (c + 1) * 128)
            b_t, d_b = b_r[r]
            col, d_col = col_r[r]
            e1, d_e1 = e1_r[r]
            e2, d_e2 = e2_r[r]
            kk, d_kk = kk_r[r]
            qd, d_qd = qd_r[r]
            kd, d_kd = kd_r[r]
            qb, d_qb = qb_r[r]
            ke, d_ke = ke_r[r]
            keT, d_keT = keT_r[r]
            scm, d_scm = scm_r[r]
            kb.op("dve", lambda e: e.tensor_tensor_scan(out=b_t[:], data0=onesf[:], data1=lf_t[:, cs], initial=0.0,
                                                         op0=ALU.mult, op1=ALU.add), R=[d_lf, d_onesf], W=[d_b])
            kb.op("dve", lambda e: e.tensor_copy(out=col[:, 0:1], in_=b_t[:, 63:64]), R=[d_b], W=[d_col])
            kb.op("dve", lambda e: e.tensor_tensor(out=col[:, 1:2], in0=b_t[:, 127:128], in1=b_t[:, 63:64],
                                                    op=ALU.subtract), R=[d_b], W=[d_col])
            kb.op("dve", lambda e: e.tensor_copy(out=col[:, 2:3], in_=b_t[:, 127:128]), R=[d_b], W=[d_col])
            kb.op("dve", lambda e: e.tensor_scalar(out=col[:, 3:4], in0=b_t[:, 63:64], scalar1=-1.0, scalar2=None,
                                                    op0=ALU.mult), R=[d_b], W=[d_col])
            kb.op("act", lambda e: e.activation(out=col[:, 4:7], in_=col[:, 0:3], func=AF.Exp), R=[d_col], W=[d_col])
            kb.op("act", lambda e: e.activation(out=e1[:], in_=b_t[:], func=AF.Exp, bias=col[:, 3:4], scale=1.0),
                  R=[d_b, d_col], W=[d_e1])
            kb.op("act", lambda e: e.activation(out=e2[:], in_=b_t[:], func=AF.Exp, bias=col[:, 0:1], scale=-1.0),
                  R=[d_b, d_col], W=[d_e2])
            kb.op("act", lambda e: e.activation(out=kk[:], in_=lf_t[:, cs], func=AF.Exp), R=[d_lf], W=[d_kk])
            kb.op("pool", lambda e: e.tensor_scalar(out=kk[:], in0=kk[:], scalar1=-1.0, scalar2=1.0, op0=ALU.mult,
                                                     op1=ALU.add), R=[d_kk], W=[d_kk])
            kb.op("dve", lambda e: e.tensor_tensor(out=qd[:], in0=q_t[:, cs], in1=e1[:], op=ALU.mult),
                  R=[d_q, d_e1], W=[d_qd])
            kb.op("dve", lambda e: e.tensor_tensor(out=kd[:], in0=kk[:], in1=e2[:], op=ALU.mult),
                  R=[d_kk, d_e2], W=[d_kd])
            kb.op("dve", lambda e: e.scalar_tensor_tensor(out=qb[:], in0=q_t[:, cs], scalar=col[:, 4:5], in1=e1[:],
                                                           op0=ALU.mult, op1=ALU.mult), R=[d_q, d_col, d_e1], W=[d_qb])
            kb.op("dve", lambda e: e.scalar_tensor_tensor(out=ke[:], in0=kk[:], scalar=col[:, 5:6], in1=e2[:],
                                                           op0=ALU.mult, op1=ALU.mult), R=[d_kk, d_col, d_e2], W=[d_ke])
            psc, d_psc = psc_r[r]
            kb.op("pe", lambda e: e.matmul(psc[:, 0:128], lhsT=kd[:], rhs=qd[:], start=True, stop=True),
                  R=[d_kd, d_qd], W=[d_psc])
            kb.op("dve", lambda e: e.copy_predicated(out=scm[:], mask=mku[:], data=psc[:, 0:128]),
                  R=[d_psc, d_mk], W=[d_scm])
            po, d_po = po_r[r]
            Sb, d_Sb = Sb_r[c % 2]
            kb.op("pe", lambda e: e.matmul(po[:, 0:128], lhsT=scm[:], rhs=v_t[:, c, :], start=True, stop=False),
                  R=[d_scm, d_v], W=[d_po])
            kb.op("pe", lambda e: e.matmul(po[:, 0:128], lhsT=qb[:], rhs=Sb[:], start=False, stop=True),
                  R=[d_qb, d_Sb], W=[d_po])
            pt, d_pt = pt_r[r]
            kb.op("pe", lambda e: e.transpose(out=pt[:, 0, :], in_=ke[:], identity=ident[:]), R=[d_ke, d_id], W=[d_pt])
            kb.op("act", lambda e: e.copy(out=keT[:], in_=pt[:, 0, :]), R=[d_pt], W=[d_keT])
            pu, d_pu = pu_r[r]
            kb.op("pe", lambda e: e.matmul(pu[:, 0:128], lhsT=keT[:], rhs=v_t[:, c, :], start=True, stop=True),
                  R=[d_keT, d_v], W=[d_pu])
            kb.op("dve", lambda e: e.scalar_tensor_tensor(out=S_t[:], in0=S_t[:], scalar=col[:, 6:7], in1=pu[:, 0:128],
                                                           op0=ALU.mult, op1=ALU.add), R=[d_S, d_col, d_pu], W=[d_S])
            Sb2, d_Sb2 = Sb_r[(c + 1) % 2]
            kb.op("pool", lambda e: e.tensor_copy(out=Sb2[:], in_=S_t[:]), R=[d_S], W=[d_Sb2])
            osq, d_osq = osq_r[r]
            oss, d_oss = oss_r[r]
            on_t, d_on = on_r[r]
            kb.op("act", lambda e: e.activation(out=osq[:], in_=po[:, 0:128], func=AF.Square, accum_out=oss[:, 0:1]),
                  R=[d_po], W=[d_osq, d_oss])
            kb.op("act", lambda e: e.activation(out=oss[:, 1:2], in_=oss[:, 0:1], func=AF.Ln, scale=1.0 / 128, bias=EPS),
                  R=[d_oss], W=[d_oss])
            kb.op("act", lambda e: e.activation(out=oss[:, 1:2], in_=oss[:, 1:2], func=AF.Exp, scale=-0.5),
                  R=[d_oss], W=[d_oss])
            kb.op("dve", lambda e: e.scalar_tensor_tensor(out=on_t[:], in0=po[:, 0:128], scalar=oss[:, 1:2], in1=go_t[:],
                                                           op0=ALU.mult, op1=ALU.mult), R=[d_po, d_oss, d_go], W=[d_on])
            kb.dma("sp", on[bh, c * 128:(c + 1) * 128, :], on_t[:], R=[d_on])
    kb.finish()
    return nc


_CACHE = {}


def _prog(key, fn):
    if key not in _CACHE:
        _CACHE[key] = fn()
    return _CACHE[key]


def _run(nc, in_maps):
    res = run_bass_kernel_spmd(nc, in_maps, core_ids=list(range(NCORES)))
    return res.results


def kernel_unfused(**inp):
    inp = {k: np.asarray(v) for k, v in inp.items()}
    x = inp["x"]
    meta = inp["meta_tokens"]
    f32 = np.float32
    metatile = np.zeros((128, D), f32)
    metatile[128 - NMETA:] = meta
    seg = SEQ // 4

    def core_rows(full_b, c, with_meta=True):
        s = 128 + (c % 4) * seg
        body = full_b[s:s + seg]
        return np.concatenate([full_b[0:128], body], 0) if with_meta else body

    def assemble(per_core):
        out = []
        for b in range(2):
            parts = [per_core[4 * b][0:128]] + [per_core[4 * b + j][128:] for j in range(4)]
            out.append(np.concatenate(parts, 0))
        return out

    hA = [np.concatenate([metatile, x[c // 4, (c % 4) * seg:(c % 4 + 1) * seg]], 0) for c in range(NCORES)]
    valid = np.ones((NT * 128, 1), f32)
    valid[0:128 - NMETA] = 0.0

    ncA = _prog("A", lambda: build_A(NT))
    cA = {"gain": inp["fox_norm"][0][None], "w_in": inp["fox_w_in"][0],
          "gq": np.tile(inp["fox_q_norm"][0], FH)[None], "gk": np.tile(inp["fox_k_norm"][0], FH)[None],
          "bf": inp["fox_b_f"][0][None]}
    rA = _run(ncA, [dict(cA, h=hA[c]) for c in range(NCORES)])
    qf = assemble([np.asarray(r["qo"]) for r in rA])
    kf = assemble([np.asarray(r["ko"]) for r in rA])
    vf = assemble([np.asarray(r["vo"]) for r in rA])
    lff = assemble([np.asarray(r["lfo"]) for r in rA])

    ncB = _prog("B", lambda: build_B(NB, 4))
    imB = []
    for c in range(NCORES):
        b = c // 4
        hs = [(4 * c + i) % FH for i in range(4)]
        imB.append({
            "qT": np.ascontiguousarray(np.stack([qf[b][:, h * FD:(h + 1) * FD].T for h in hs])),
            "kT": np.ascontiguousarray(np.stack([kf[b][:, h * FD:(h + 1) * FD].T for h in hs])),
            "v": np.ascontiguousarray(np.stack([vf[b][:, h * FD:(h + 1) * FD] for h in hs])),
            "lfr": np.ascontiguousarray(np.stack([lff[b][:, h] for h in hs])),
            "lfT": np.ascontiguousarray(np.stack([lff[b][:, h].reshape(NB, 128).T for h in hs])),
        })
    rB = _run(ncB, imB)
    oun = [np.zeros((LP, D), f32) for _ in range(2)]
    den = [np.zeros((LP, FH), f32) for _ in range(2)]
    for c in range(NCORES):
        b = c // 4
        o = np.asarray(rB[c]["oT"])
        for i in range(4):
            h = (4 * c + i) % FH
            oun[b][:, h * FD:(h + 1) * FD] = o[i, 0:FD].T
            den[b][:, h] = o[i, FD]

    ncC = _prog("C", lambda: build_CE("C", NT, True))
    cC = {"valid_in": valid, "w_out": inp["fox_w_out"][0], "mnorm": inp["moe_norm"][0][None],
          "w_r": np.ascontiguousarray(np.concatenate([inp["moe_w_grp"][0], inp["moe_w_rt"][0]], 1)),
          "b_r": np.concatenate([inp["moe_b_grp"][0], inp["moe_b_rt"][0]])[None],
          "w_up": inp["moe_w_up"][0], "w_dn": inp["moe_w_down"][0],
          "hnorm": inp["hg_norm"][0][None], "hw_in": inp["hg_w_in"][0], "hbf": inp["hg_b_f"][0][None],
          "lbl": inp["hg_lb_logits"]}
    rC = _run(ncC, [dict(cC, a_in=core_rows(oun[c // 4], c), den_in=core_rows(den[c // 4], c), hp_in=hA[c])
                    for c in range(NCORES)])
    h2 = [np.asarray(r["h2_out"]) for r in rC]
    q1 = assemble([np.asarray(r["q1_out"]) for r in rC])
    lf1 = assemble([np.asarray(r["lf1_out"]) for r in rC])
    v1 = assemble([np.asarray(r["v1_out"]) for r in rC])
    gs = assemble([np.asarray(r["gs_out"]) for r in rC])
    npad = 128 - NMETA
    for b in range(2):
        q1[b][0:npad] = 0
        lf1[b][0:npad] = 0
        v1[b][0:npad] = 0

    ncD = _prog("D", lambda: build_D(NB, 2))
    imD = []
    for c in range(NCORES):
        prs = [2 * c, 2 * c + 1]
        imD.append({
            "qT": np.ascontiguousarray(np.stack([q1[p // HH][:, (p % HH) * 128:(p % HH + 1) * 128].T for p in prs])),
            "lfT": np.ascontiguousarray(np.stack([lf1[p // HH][:, (p % HH) * 128:(p % HH + 1) * 128].T for p in prs])),
            "v": np.ascontiguousarray(np.stack([v1[p // HH][:, (p % HH) * 128:(p % HH + 1) * 128] for p in prs])),
            "go": inp["hg_o_norm"][0][None],
        })
    rD = _run(ncD, imD)
    onf = [np.zeros((LP, D), f32) for _ in range(2)]
    for c in range(NCORES):
        o = np.asarray(rD[c]["on"])
        for i in range(2):
            p = 2 * c + i
            onf[p // HH][:, (p % HH) * 128:(p % HH + 1) * 128] = o[i]

    ncE = _prog("E", lambda: build_CE("E", NTX, False))
    cE = {"valid_in": np.ones((NTX * 128, 1), f32), "w_out": inp["hg_w_out"][0], "mnorm": inp["moe_norm"][1][None],
          "w_r": np.ascontiguousarray(np.concatenate([inp["moe_w_grp"][1], inp["moe_w_rt"][1]], 1)),
          "b_r": np.concatenate([inp["moe_b_grp"][1], inp["moe_b_rt"][1]])[None],
          "w_up": inp["moe_w_up"][1], "w_dn": inp["moe_w_down"][1]}
    rE = _run(ncE, [dict(cE, a_in=core_rows(onf[c // 4], c, False), gs_in=core_rows(gs[c // 4], c, False),
                         hp_in=h2[c][128:]) for c in range(NCORES)])
    out = np.zeros((2, SEQ, D), f32)
    for c in range(NCORES):
        out[c // 4, (c % 4) * seg:(c % 4 + 1) * seg] = np.asarray(rE[c]["h2_out"])
    return out


GROUPS = [[0, 1, 2, 3], [4, 5, 6, 7]]


def build_fused(ntx=NTX, cap=CAP, stop=None):
    assert ntx % 4 == 0
    nt = ntx + 1
    nb = 4 * ntx + 1
    L = nb * 128
    nch = ntx // 4
    TX = ntx * 128
    nI = ntx + 1
    nslot = NE * cap
    nst = cap // 128
    nc = bass.Bass("TRN2", target_bir_lowering=False)

    def din(name, shape, dt=F32):
        return nc.dram_tensor(name, list(shape), dt, kind="ExternalInput").ap()

    def dint(name, shape, dt=BF16):
        return nc.dram_tensor(name, list(shape), dt).ap()

    x_in = din("x_in", [nt * 128, D])
    valid_in = din("valid_in", [nt * 128, 1])
    fnorm = din("fnorm", [1, D])
    wq_d, wk_d, wv_d = din("wq", [D, 256]), din("wk", [D, 256]), din("wv", [D, 256])
    wf_d = din("wf", [D, 4])
    gqc_d, gkc_d = din("gqc", [128, 1]), din("gkc", [128, 1])
    bf4r_d, bf4c_d = din("bf4r", [1, 4]), din("bf4c", [4, 1])
    wo1_d = din("wo1", [D, D])
    idx2_d = din("idx2", [128, 8], I32)
    idx3_d = din("idx3", [128, 8], I32)
    moe_d = []
    for l in range(2):
        if stop in ("T1", "H1p", "H1a") or (stop in ("T2", "H2") and l == 1):
            moe_d.append(None)
            continue
        moe_d.append(dict(mnorm=din("mnorm%d" % l, [1, D]), w_r=din("w_r%d" % l, [D, 36]), b_r=din("b_r%d" % l, [1, 36]),
                          w_up=din("w_up%d" % l, [NE, D, 2 * DE]), w_dn=din("w_dn%d" % l, [NE, DE, D])))
    hnorm = din("hnorm", [1, D])
    hwq_d, hwf_d, hwv_d = din("hwq", [2, D, 128]), din("hwf", [2, D, 128]), din("hwv", [2, D, 128])
    hwg_d = din("hwg", [D, D])
    hbfc_d, l0c_d, l1c_d = din("hbfc", [2, 128, 1]), din("l0c", [2, 128, 1]), din("l1c", [2, 128, 1])
    go_d = din("go", [1, 128])
    wo2_d = din("wo2", [D, D])
    out_d = nc.dram_tensor("out", [TX, D], F32, kind="ExternalOutput").ap()

    MA, MC = dint("MA", [128, 8 * 128]), dint("MC", [128, 8 * 128])
    SA, RA = dint("SA", [nch * 128, 8 * 512]), dint("RA", [nch * 512, 8 * 512])
    SC, RC = dint("SC", [nch * 128, 8 * 512]), dint("RC", [nch * 512, 8 * 512])
    qTs, kTs, vs = dint("qTs", [4, FD + 1, L]), dint("kTs", [4, FD, L]), dint("vs", [L, 256])
    SBb, RBb = dint("SBb", [8 * 128, TX]), dint("RBb", [8 * 512, TX])
    SBm, RBm = dint("SBm", [256, 128]), dint("RBm", [1024, 128])
    h1s, h2s = dint("h1s", [nt * 128, D], F32), dint("h2s", [nt * 128, D], F32)
    xpad, ypad = dint("xpad", [nslot, D]), dint("ypad", [nslot, D])
    GS = dint("GS", [nch * 128, 8 * 512])
    SD, RD = dint("SD", [8 * 128, TX]), dint("RD", [8 * 512, TX])
    d_MA, d_MC = (Dep(n) for n in ("MA", "MC"))
    d_SA = (Dep("SAw"), [Dep("SA%d" % j) for j in range(nch)])
    d_SC = (Dep("SCw"), [Dep("SC%d" % j) for j in range(nch)])
    d_SBc = [Dep("SBb%d" % j) for j in range(8)]
    d_SDc = [Dep("SD%d" % j) for j in range(8)]
    d_RA = [Dep("RA%d" % j) for j in range(nch)]
    d_RC = [Dep("RC%d" % j) for j in range(nch)]
    d_qTs, d_kTs, d_vs, d_SBb, d_RBb, d_SBm, d_RBm = (Dep(n) for n in ("qTs", "kTs", "vs", "SBb", "RBb", "SBm", "RBm"))
    d_h1s, d_h2s, d_xpad, d_ypad, d_GS, d_SD, d_RD = (Dep(n) for n in ("h1s", "h2s", "xpad", "ypad", "GS", "SD", "RD"))

    kb = KB(nc)
    identf, d_idf, ident, d_id = make_ident(kb)
    breg = nc.gpsimd.to_reg(nslot - 1)
    breg2 = nc.gpsimd.to_reg(8 * 512 - 1)

    def dump_and_finish(items):
        for name, ap, dep in items:
            o = nc.dram_tensor("dbg_" + name, list(ap.shape), ap.dtype, kind="ExternalOutput").ap()
            kb.dma("sp", o, ap, R=[dep])
        kb.finish()
        return nc

    def emit_hnT(src_t, d_src, gain_t, d_gain, scr, ssr, xnr, pstr, hcr, t, Mloc, d_Mloc, Ssend, d_S, Rrecv, d_R, hc_hook=None):
        b = t % 2
        sc_t, d_sc = scr[b]
        ss_t, d_ss = ssr[b]
        xn_t, d_xn = xnr[b]
        ps_t, d_pst = pstr[b]
        rms_rstd(kb, src_t[:], d_src, D, sc_t[:], d_sc, ss_t[:], d_ss)
        kb.op("dve", lambda e: e.scalar_tensor_tensor(out=xn_t[:], in0=src_t[:], scalar=ss_t[:, 0:1], in1=gain_t[:],
                                                       op0=ALU.mult, op1=ALU.mult), R=[d_src, d_ss, d_gain], W=[d_xn])
        for c in range(8):
            kb.op("pe", lambda e: e.transpose(out=ps_t[:, c, :], in_=xn_t[:, c * 128:(c + 1) * 128], identity=ident[:]),
                  R=[d_xn, d_id], W=[d_pst])
        if t == 0:
            hc_t, d_hc = hcr[0]
            kb.op("dve", lambda e: e.tensor_copy(out=hc_t[:, :, 0:128], in_=ps_t[:]), R=[d_pst], W=[d_hc])
            kb.dma("sp", Mloc.rearrange("p (c t) -> p c t", c=8), hc_t[:, :, 0:128], R=[d_hc], W=[d_Mloc])
            return
        j, s = (t - 1) // 4, (t - 1) % 4
        hc_t, d_hc = hcr[(j + 1) % 2]
        kb.op("dve", lambda e: e.tensor_copy(out=hc_t[:, :, s * 128:(s + 1) * 128], in_=ps_t[:]), R=[d_pst], W=[d_hc])
        if s == 3:
            kb.dma("sp", Ssend[j * 128:(j + 1) * 128, :], hc_t[:].rearrange("p c t -> p (c t)"), R=[d_hc],
                   W=[d_S[0], d_S[1][j]])
            kb.cc(Ssend[j * 128:(j + 1) * 128, :], Rrecv[j * 512:(j + 1) * 512, :], GROUPS, R=[d_S[1][j]], W=[d_R[j]])
            if hc_hook is not None:
                hc_hook(j, hc_t, d_hc)

    lfall = kb.sb("lfall", [128, nb, 4], F32)
    d_lfall = Dep("lfall")
    with kb.scope():
        g_t, d_g = load_bc(kb, "fnorm", fnorm, D)
        wq_t, d_wq = load_w(kb, "wq", wq_d, D, 256)
        wk_t, d_wk = load_w(kb, "wk", wk_d, D, 256)
        wv_t, d_wv = load_w(kb, "wv", wv_d, D, 256)
        wf_t, d_wf = load_w(kb, "wf", wf_d, D, 4)
        for d in (d_wq, d_wk, d_wv, d_wf):
            d.ro = True
        gqc = kb.sb("gqc", [128, 1], F32); d_gqc = Dep("gqc")
        gkc = kb.sb("gkc", [128, 1], F32); d_gkc = Dep("gkc")
        bf4c = kb.sb("bf4c", [4, 1], F32); d_bf4c = Dep("bf4c")
        kb.dma("sp", gqc[:], gqc_d, W=[d_gqc])
        kb.dma("sp", gkc[:], gkc_d, W=[d_gkc])
        kb.dma("sp", bf4c[:], bf4c_d, W=[d_bf4c])
        kb.op("dve", lambda e: e.tensor_scalar(out=bf4c[:], in0=bf4c[:], scalar1=-1.0, scalar2=None, op0=ALU.mult),
              R=[d_bf4c], W=[d_bf4c])
        kb.op("dve", lambda e: e.tensor_scalar(out=gqc[:], in0=gqc[:], scalar1=float(FD ** -0.5), scalar2=None,
                                                op0=ALU.mult), R=[d_gqc], W=[d_gqc])
        bf4r, d_bf4r = load_bc(kb, "bf4r", bf4r_d, 4)
        blkf = kb.sb("blkf", [128, 128], F32); d_blkf = Dep("blkf")
        blk = kb.sb("blk", [128, 128], BF16); d_blk = Dep("blk")
        ones1 = kb.sb("ones1", [128, 512], F32); d_ones1 = Dep("ones1")
        kb.op("pool", lambda e: e.memset(blkf[:], 0.0), W=[d_blkf])
        kb.op("pool", lambda e: e.memset(blkf[0:64, 0:64], 1.0), W=[d_blkf])
        kb.op("pool", lambda e: e.memset(blkf[64:128, 64:128], 1.0), W=[d_blkf])
        kb.op("dve", lambda e: e.tensor_copy(out=blk[:], in_=blkf[:]), R=[d_blkf], W=[d_blk])
        kb.op("pool", lambda e: e.memset(ones1[:], 1.0), W=[d_ones1])
        d_blk.ro = True
        d_ones1.ro = True
        xin = kb.ring("xin", [128, D], F32, 4)
        scr = kb.ring("scr", [128, D], F32, 2)
        ssr = kb.ring("ss", [128, 1], F32, 2)
        xnr = kb.ring("xn", [128, D], BF16, 2)
        hcr = kb.ring("hc", [128, 8, 512], BF16, 2)
        pstr = kb.ring("pst", [128, 8, 128], BF16, 2, psum=True)
        def t1_load(t):
            x_t, d_x = xin[t % 4]
            kb.dma("sp", x_t[:], x_in[t * 128:(t + 1) * 128, :], W=[d_x])
        for t in range(min(3, nt)):
            t1_load(t)
        for t in range(nt):
            if t + 3 < nt:
                t1_load(t + 3)
            x_t, d_x = xin[t % 4]
            emit_hnT(x_t, d_x, g_t, d_g, scr, ssr, xnr, pstr, hcr, t, MA, d_MA, SA, d_SA, RA, d_RA)

        if stop == "T1":
            kb.barrier()
            return dump_and_finish([("RA", RA, d_RA[nch - 1]), ("MA", MA, d_MA)])
        hgr = kb.ring("hg", [128, 8, 512], BF16, 2)
        sqr = kb.ring("sq", [128, 512], BF16, 2)
        rsr = kb.ring("rs", [128, 512], F32, 2)
        qnr = kb.ring("qn", [128, 512], BF16, 2)
        vsr = kb.ring("vsb", [128, 256], BF16, 2)
        lzr = kb.ring("lz", [128, 4], F32, 2)
        lrr = kb.ring("lr", [4, 512], F32, 2)
        drr = kb.ring("dr", [4, 512], BF16, 2)
        pqr = kb.ring("pq", [128, 512], F32, 2, psum=True)
        pssr = kb.ring("pss", [128, 512], F32, 1, psum=True)
        pvr = kb.ring("pv", [128, 512], F32, 2, psum=True)
        pfr = kb.ring("pf", [128, 512], F32, 1, psum=True)
        qi = 0
        vi = 0
        def h1_load(G):
            hg, d_hg = hgr[G % 2]
            if G == 0:
                kb.dma("sp", hg[:, :, 0:128], MA.rearrange("p (c t) -> p c t", c=8), R=[d_MA], W=[d_hg])
            else:
                j, rank = (G - 1) // 4, (G - 1) % 4
                r0 = j * 512 + rank * 128
                kb.dma("sp", hg[:].rearrange("p c t -> p (c t)"), RA[r0:r0 + 128, :], R=[d_RA[j]], W=[d_hg])
        h1_load(0)
        for G in range(1 + 4 * nch):
            hg, d_hg = hgr[G % 2]
            if G + 1 < 1 + 4 * nch:
                h1_load(G + 1)
            if G == 0:
                n, tok0 = 128, 0
            else:
                n, tok0 = 512, 128 + ((G - 1) % 4) * TX + ((G - 1) // 4) * 512
            for (w_t, d_w, gc, d_gc, dst, d_dst) in ((wq_t, d_wq, gqc, d_gqc, qTs, d_qTs), (wk_t, d_wk, gkc, d_gkc, kTs, d_kTs)):
                for pr in range(2):
                    pq, d_pq = pqr[qi % 2]
                    pss, d_pss = pssr[0]
                    sq, d_sq = sqr[qi % 2]
                    rs, d_rs = rsr[qi % 2]
                    qn, d_qn = qnr[qi % 2]
                    qi += 1
                    for c in range(8):
                        kb.op("pe", lambda e: e.matmul(pq[:, 0:n], lhsT=w_t[:, c, pr * 128:(pr + 1) * 128], rhs=hg[:, c, 0:n],
                                                       start=(c == 0), stop=(c == 7)), R=[d_w, d_hg], W=[d_pq])
                    kb.op("act", lambda e: e.activation(out=sq[:, 0:n], in_=pq[:, 0:n], func=AF.Square), R=[d_pq], W=[d_sq])
                    kb.op("pe", lambda e: e.matmul(pss[:, 0:n], lhsT=blk[:], rhs=sq[:, 0:n], start=True, stop=True),
                          R=[d_blk, d_sq], W=[d_pss])
                    kb.op("act", lambda e: e.activation(out=rs[:, 0:n], in_=pss[:, 0:n], func=AF.Ln, scale=1.0 / FD, bias=EPS),
                          R=[d_pss], W=[d_rs])
                    kb.op("act", lambda e: e.activation(out=rs[:, 0:n], in_=rs[:, 0:n], func=AF.Exp, scale=-0.5), R=[d_rs], W=[d_rs])
                    kb.op("dve", lambda e: e.scalar_tensor_tensor(out=qn[:, 0:n], in0=pq[:, 0:n], scalar=gc[:, 0:1], in1=rs[:, 0:n],
                                                                   op0=ALU.mult, op1=ALU.mult), R=[d_pq, d_gc, d_rs], W=[d_qn])
                    for hh in range(2):
                        kb.dma("sp", dst[2 * pr + hh, 0:FD, tok0:tok0 + n], qn[hh * 64:(hh + 1) * 64, 0:n], R=[d_qn], W=[d_dst])
            for s in range(n // 128):
                pv, d_pv = pvr[vi % 2]
                vsb, d_vsb = vsr[vi % 2]
                lz, d_lz = lzr[vi % 2]
                vi += 1
                for c in range(8):
                    kb.op("pe", lambda e: e.matmul(pv[:, 0:256], lhsT=hg[:, c, s * 128:(s + 1) * 128], rhs=wv_t[:, c, :],
                                                   start=(c == 0), stop=(c == 7)), R=[d_wv, d_hg], W=[d_pv])
                for c in range(8):
                    kb.op("pe", lambda e: e.matmul(pv[:, 256:260], lhsT=hg[:, c, s * 128:(s + 1) * 128], rhs=wf_t[:, c, :],
                                                   start=(c == 0), stop=(c == 7)), R=[d_wf, d_hg], W=[d_pv])
                kb.op("act", lambda e: e.copy(out=vsb[:], in_=pv[:, 0:256]), R=[d_pv], W=[d_vsb])
                kb.dma("sp", vs[tok0 + s * 128:tok0 + (s + 1) * 128, :], vsb[:], R=[d_vsb], W=[d_vs])
                blk_i = tok0 // 128 + s
                kb.op("dve", lambda e: e.tensor_tensor(out=lz[:], in0=pv[:, 256:260], in1=bf4r[:], op=ALU.add),
                      R=[d_pv, d_bf4r], W=[d_lz])
                kb.op("act", lambda e: e.activation(out=lz[:], in_=lz[:], func=AF.Exp, scale=-1.0), R=[d_lz], W=[d_lz])
                kb.op("act", lambda e: e.activation(out=lz[:], in_=lz[:], func=AF.Ln, bias=1.0), R=[d_lz], W=[d_lz])
                kb.op("dve", lambda e: e.tensor_scalar(out=lfall[:, blk_i, :], in0=lz[:], scalar1=-1.0, scalar2=None,
                                                        op0=ALU.mult), R=[d_lz], W=[d_lfall])
            pf, d_pf = pfr[0]
            lr, d_lr = lrr[G % 2]
            dr, d_dr = drr[G % 2]
            for c in range(8):
                kb.op("pe", lambda e: e.matmul(pf[0:4, 0:n], lhsT=wf_t[:, c, :], rhs=hg[:, c, 0:n], start=(c == 0), stop=(c == 7)),
                      R=[d_wf, d_hg], W=[d_pf])
            kb.op("act", lambda e: e.activation(out=lr[:, 0:n], in_=pf[0:4, 0:n], func=AF.Exp, scale=-1.0, bias=bf4c[:, 0:1]),
                  R=[d_pf, d_bf4c], W=[d_lr])
            kb.op("act", lambda e: e.activation(out=lr[:, 0:n], in_=lr[:, 0:n], func=AF.Ln, bias=1.0), R=[d_lr], W=[d_lr])
            kb.op("dve", lambda e: e.tensor_tensor_scan(out=lr[:, 0:n], data0=ones1[0:4, 0:n], data1=lr[:, 0:n], initial=0.0,
                                                         op0=ALU.mult, op1=ALU.subtract), R=[d_lr, d_ones1], W=[d_lr])
            kb.op("dve", lambda e: e.tensor_copy(out=dr[:, 0:n], in_=lr[:, 0:n]), R=[d_lr], W=[d_dr])
            kb.dma("sp", qTs[:, FD, tok0:tok0 + n], dr[:, 0:n], R=[d_dr], W=[d_qTs])

    if stop == "H1p":
        return dump_and_finish([("qTs", qTs, d_qTs), ("kTs", kTs, d_kTs), ("vs", vs, d_vs)])
    with kb.scope():
        trif = kb.sb("trif", [128, 128], F32); d_tri = Dep("trif")
        onesf = kb.sb("onesf", [128, 128], F32); d_ones = Dep("onesf")
        sel0 = kb.sb("sel0", [128, 128], F32); d_sel = Dep("sel0")
        maskb = kb.sb("maskb", [128, 128], BF16); d_mask = Dep("maskb")
        onerow = kb.sb("onerow", [128, nb], F32); d_or = Dep("onerow")
        kb.op("pool", lambda e: e.memset(onesf[:], 1.0), W=[d_ones])
        kb.op("pool", lambda e: e.memset(onerow[:], 1.0), W=[d_or])
        kb.op("pool", lambda e: e.affine_select(out=trif[:], in_=onesf[:], pattern=[[1, 128]], compare_op=ALU.is_ge,
                                                fill=0.0, base=0, channel_multiplier=-1), R=[d_ones], W=[d_tri])
        kb.op("pool", lambda e: e.affine_select(out=sel0[:], in_=onesf[:], pattern=[[0, 128]], compare_op=ALU.is_ge,
                                                fill=0.0, base=0, channel_multiplier=-1), R=[d_ones], W=[d_sel])
        kb.op("dve", lambda e: e.tensor_copy(out=maskb[:], in_=trif[:]), R=[d_tri], W=[d_mask])
        for d in (d_tri, d_ones, d_sel, d_mask, d_or):
            d.ro = True
        QA = kb.sb("QA", [FD + 1, L], BF16)
        tpc = ntx // 4
        qb_ = [0] + [128 + (c + 1) * tpc * 512 for c in range(3)] + [L]
        d_QAc = [Dep("QA%d" % c) for c in range(4)]

        def q_chunk(I):
            return 0 if I == 0 else (I - 1) // tpc

        def q_load(hx, c):
            kb.dma("sp", QA[:, qb_[c]:qb_[c + 1]], qTs[hx][:, qb_[c]:qb_[c + 1]], R=[d_qTs], W=[d_QAc[c]])
        KAr = kb.ring("KA", [FD + 1, L], BF16, 2)
        VAr = kb.ring("VA", [128, nb, 128], BF16, 2)
        lft = kb.sb("lft", [128, nb], F32); d_lft = Dep("lft")
        ct = kb.sb("ct", [128, nb], F32); d_ct = Dep("ct")
        tot = kb.sb("tot", [128, nb], F32); d_tot = Dep("tot")
        rall = kb.sb("rall", [128, nb], F32); d_rall = Dep("rall")
        biasr = kb.ring("bias", [128, nb], F32, 2)
        pr_ = kb.ring("P", [128, 512], BF16, 4)
        denr = kb.ring("den", [128, 512], F32, 2)
        rdr = kb.ring("rden", [64, 512], F32, 2)
        onr = kb.ring("onb", [64, 512], BF16, 2)
        psS = kb.ring("psS", [128, 512], F32, 4, psum=True)
        psO = kb.ring("psO", [128, 512], F32, 2, psum=True)
        psM = kb.ring("psM", [128, 512], F32, 2, psum=True)
        for (KA_, d_KA_), (VA_, d_VA_) in zip(KAr, VAr):
            kb.op("pool", lambda e: e.memset(KA_[FD:FD + 1, :], 1.0), W=[d_KA_])
            kb.op("pool", lambda e: e.memset(VA_[:, :, FD:128], 1.0), W=[d_VA_])

        def kv_load(hx):
            KA_, d_KA_ = KAr[hx % 2]
            VA_, d_VA_ = VAr[hx % 2]
            kb.dma("sp", KA_[0:FD, :], kTs[hx], R=[d_kTs], W=[d_KA_])
            kb.dma("sp", VA_[:, :, 0:FD], vs[:, hx * FD:(hx + 1) * FD].rearrange("(j p) d -> p j d", p=128), R=[d_vs], W=[d_VA_])
            kb.op("pool", lambda e: e.memset(VA_[0:112, 0, :], 0.0), W=[d_VA_])

        for c in range(4):
            q_load(0, c)
        kv_load(0)
        for h4 in range(4):
            pr4, hh4 = h4 // 2, h4 % 2
            KA, d_KA = KAr[h4 % 2]
            VA, d_VA = VAr[h4 % 2]
            if h4 + 1 < 4:
                kv_load(h4 + 1)
            kb.op("dve", lambda e: e.tensor_copy(out=lft[:], in_=lfall[:, :, h4]), R=[d_lfall], W=[d_lft])
            pm0, d_pm0 = psM[0]
            pm1, d_pm1 = psM[1]
            kb.op("pe", lambda e: e.matmul(pm0[:, 0:nb], lhsT=trif[:], rhs=lft[:], start=True, stop=True),
                  R=[d_tri, d_lft], W=[d_pm0])
            kb.op("pe", lambda e: e.matmul(pm1[:, 0:nb], lhsT=onesf[:], rhs=lft[:], start=True, stop=True),
                  R=[d_ones, d_lft], W=[d_pm1])
            kb.op("dve", lambda e: e.tensor_copy(out=tot[:], in_=pm1[:, 0:nb]), R=[d_pm1], W=[d_tot])
            kb.op("dve", lambda e: e.tensor_tensor_scan(out=ct[:], data0=onerow[:], data1=tot[:], initial=0.0,
                                                         op0=ALU.mult, op1=ALU.add), R=[d_tot, d_or], W=[d_ct])
            kb.op("dve", lambda e: e.tensor_tensor(out=ct[:], in0=ct[:], in1=tot[:], op=ALU.subtract),
                  R=[d_ct, d_tot], W=[d_ct])
            kb.op("dve", lambda e: e.tensor_tensor(out=ct[:], in0=ct[:], in1=pm0[:, 0:nb], op=ALU.add),
                  R=[d_ct, d_pm0], W=[d_ct])
            kb.op("pe", lambda e: e.matmul(pm1[:, 0:nb], lhsT=sel0[:], rhs=ct[:], start=True, stop=True),
                  R=[d_sel, d_ct], W=[d_pm1])
            kb.op("dve", lambda e: e.tensor_copy(out=rall[:], in_=pm1[:, 0:nb]), R=[d_pm1], W=[d_rall])
            steps = []
            for I in range(nI):
                j0 = 0 if I == 0 else 4 * I - 3
                nblk = 1 if I == 0 else 4
                nJ = j0 + nblk
                for J in range(nJ):
                    steps.append((I, J, j0, nblk, nJ))
            LA = 2
            for idx in range(len(steps) + LA):
                if idx < len(steps):
                    I, J, j0, nblk, nJ = steps[idx]
                    q0 = j0 * 128
                    bias_t, d_bias = biasr[I % 2]
                    if J == 0:
                        kb.op("dve", lambda e: e.tensor_scalar(out=bias_t[:, 0:nJ], in0=ct[:, 0:nJ], scalar1=-1.0,
                                                                scalar2=rall[:, j0:j0 + 1], op0=ALU.mult, op1=ALU.add),
                              R=[d_ct, d_rall], W=[d_bias])
                    m = max(0, J - j0)
                    c0 = m * 128
                    c1 = nblk * 128
                    ps, d_ps = psS[idx % 4]
                    p_t, d_p = pr_[idx % 4]
                    kb.op("pe", lambda e: e.matmul(ps[:, c0:c1], lhsT=KA[:, J * 128:(J + 1) * 128],
                                                   rhs=QA[:, q0 + c0:q0 + c1], start=True, stop=True),
                          R=[d_KA, d_QAc[q_chunk(I)]], W=[d_ps])
                    kb.op("act", lambda e: e.activation(out=p_t[:, c0:c1], in_=ps[:, c0:c1], func=AF.Exp,
                                                        bias=bias_t[:, J:J + 1], scale=1.0),
                          R=[d_ps, d_bias], W=[d_p])
                    if J >= j0:
                        kb.op("dve", lambda e: e.tensor_tensor(out=p_t[:, c0:c0 + 128], in0=p_t[:, c0:c0 + 128],
                                                                in1=maskb[:], op=ALU.mult), R=[d_p, d_mask], W=[d_p])
                    if J == nJ - 1 and I > 0 and I % tpc == 0 and h4 + 1 < 4:
                        q_load(h4 + 1, I // tpc - 1)
                if idx >= LA:
                    I, J, j0, nblk, nJ = steps[idx - LA]
                    m = max(0, J - j0)
                    c0 = m * 128
                    c1 = nblk * 128
                    p_t, d_p = pr_[(idx - LA) % 4]
                    po, d_po = psO[I % 2]
                    kb.op("pe", lambda e: e.matmul(po[:, c0:c1], lhsT=VA[:, J, :], rhs=p_t[:, c0:c1],
                                                   start=(J == 0), stop=(J == nJ - 1)), R=[d_VA, d_p], W=[d_po])
                    if J == nJ - 1:
                        ncol = nblk * 128
                        den, d_den = denr[I % 2]
                        rd, d_rd = rdr[I % 2]
                        onb, d_onb = onr[I % 2]
                        kb.op("dve", lambda e: e.tensor_scalar(out=den[64:128, 0:ncol], in0=po[64:128, 0:ncol], scalar1=1e-30,
                                                                scalar2=None, op0=ALU.max), R=[d_po], W=[d_den])
                        kb.dma("sp", rd[:, 0:ncol], den[64:128, 0:ncol], R=[d_den], W=[d_rd])
                        kb.op("dve", lambda e: e.reciprocal(out=rd[:, 0:ncol], in_=rd[:, 0:ncol]), R=[d_rd], W=[d_rd])
                        kb.op("dve", lambda e: e.tensor_tensor(out=onb[:, 0:ncol], in0=po[0:64, 0:ncol], in1=rd[:, 0:ncol],
                                                                op=ALU.mult), R=[d_po, d_rd], W=[d_onb])
                        if I == 0:
                            kb.dma("sp", SBm[h4 * 64:(h4 + 1) * 64, :], onb[:, 0:128], R=[d_onb], W=[d_SBm])
                        else:
                            off = (I - 1) * 512
                            qq, col = off // TX, off % TX
                            r0 = (pr4 * 4 + qq) * 128 + hh4 * 64
                            cidx = pr4 * 4 + qq
                            kb.dma("sp", SBb[r0:r0 + 64, col:col + 512], onb[:, 0:512], R=[d_onb], W=[d_SBb, d_SBc[cidx]])
                            if hh4 == 1 and (off + 512) % TX == 0:
                                kb.cc(SBb[cidx * 128:(cidx + 1) * 128, :], RBb[cidx * 512:(cidx + 1) * 512, :], GROUPS,
                                      R=[d_SBc[cidx]], W=[d_RBb])
        kb.cc(SBm[:, :], RBm[:, :], GROUPS, R=[d_SBm], W=[d_RBm])

    if stop == "H1a":
        return dump_and_finish([("RBb", RBb, d_RBb), ("RBm", RBm, d_RBm)])
    usb = kb.sb("usb", [128, 128], BF16); d_us = Dep("usb")
    onesb = kb.sb("onesb", [128, 128], BF16); d_onesb = Dep("onesb")
    onesf2 = kb.sb("onesf2", [128, 128], F32); d_onesf2 = Dep("onesf2")
    tmpf = kb.sb("tmpf", [128, 128], F32); d_tmpf = Dep("tmpf")
    kb.op("pool", lambda e: e.memset(onesf2[:], 1.0), W=[d_onesf2])
    kb.op("pool", lambda e: e.affine_select(out=tmpf[:], in_=onesf2[:], pattern=[[1, 128]], compare_op=ALU.is_gt,
                                            fill=0.0, base=0, channel_multiplier=-1), R=[d_onesf2], W=[d_tmpf])
    kb.op("dve", lambda e: e.tensor_copy(out=usb[:], in_=tmpf[:]), R=[d_tmpf], W=[d_us])
    kb.op("dve", lambda e: e.tensor_copy(out=onesb[:], in_=onesf2[:]), R=[d_onesf2], W=[d_onesb])
    for d in (d_us, d_onesb, d_onesf2):
        d.ro = True
    dest_i = kb.sb("dest_i", [128, nt, 2], I32); d_dest = Dep("dest_i")
    gw = kb.sb("gw", [128, nt, 2], F32); d_gw = Dep("gw")
    cntb = kb.sb("cntb", [128, NE], F32); d_cntb = Dep("cntb")

    def tok_stage(l, tiles, has_meta, setup_lhsT, hp_ap, wo_d, pass3_setup, pass3_tile):
        md = moe_d[l]
        kb.op("pool", lambda e: e.iota(cntb[:], pattern=[[cap, NE]], base=0, channel_multiplier=0,
                                       allow_small_or_imprecise_dtypes=True), W=[d_cntb])
        with kb.scope():
            mn_t, d_mn = load_bc(kb, "mnorm", md["mnorm"], D)
            br_t, d_br = load_bc(kb, "b_r", md["b_r"], 36)
            wr_t, d_wr = load_w(kb, "w_r", md["w_r"], D, 36, dt=F32)
            d_wr.ro = True
            with kb.scope():
                wo_t, d_wo = load_w(kb, "w_out", wo_d, D, D)
                d_wo.ro = True
                get_lhsT = setup_lhsT()
                hp_r = kb.ring("hp", [128, D], F32, 4)
                h1_r = kb.ring("h1", [128, D], F32, 2)
                scr_r = kb.ring("scr", [128, D], F32, 2)
                ss_r = kb.ring("ss", [128, 1], F32, 2)
                hmf_r = kb.ring("hmf", [128, D], F32, 2)
                hmb_r = kb.ring("hmb", [128, D], BF16, 6)
                hmT_r = kb.ring("hmT", [128, 8, 128], F32, 2)
                sm_r = kb.ring("sm", [128, 256], F32, 4)
                ab_r = kb.ring("ab", [128, NE], BF16, 4)
                val_r = kb.ring("val", [128, 2], F32, 4)
                pstf_r = kb.ring("pstf", [128, 8, 128], F32, 1, psum=True)
                pmm_r = kb.ring("pmm", [128, 512], F32, 2, psum=True)
                prt_r = kb.ring("prt", [128, 512], F32, 4, psum=True)
                def p1_load(ti):
                    hp_t, d_hp = hp_r[ti % 4]
                    kb.dma("sp", hp_t[:], hp_ap(tiles[ti]), R=[d_h2s], W=[d_hp])
                    if hasattr(get_lhsT, "prefetch"):
                        get_lhsT.prefetch(tiles[ti])

                def front(ti):
                    t = tiles[ti]
                    b = ti % 2
                    r0, r1 = t * 128, (t + 1) * 128
                    hp_t, d_hp = hp_r[ti % 4]
                    if ti + 3 < len(tiles):
                        p1_load(ti + 3)
                    lhsT, d_lhsT = get_lhsT(t)
                    h1_t, d_h1 = h1_r[b]
                    for half in range(2):
                        p_t, d_p = pmm_r[half]
                        for c in range(8):
                            kb.op("pe", lambda e: e.matmul(p_t[:], lhsT=lhsT(c), rhs=wo_t[:, c, half * 512:(half + 1) * 512],
                                                           start=(c == 0), stop=(c == 7)), R=d_lhsT + [d_wo], W=[d_p])
                        kb.op("dve", lambda e: e.tensor_tensor(out=h1_t[:, half * 512:(half + 1) * 512], in0=p_t[:],
                                                                in1=hp_t[:, half * 512:(half + 1) * 512], op=ALU.add),
                              R=[d_p, d_hp], W=[d_h1])
                    kb.dma("sp", h1s[r0:r1, :], h1_t[:], R=[d_h1], W=[d_h1s])
                    sc_t, d_sc = scr_r[b]
                    ss_t, d_ss = ss_r[b]
                    hmf_t, d_hmf = hmf_r[b]
                    hmb_t, d_hmb = hmb_r[ti % 6]
                    rms_rstd(kb, h1_t[:], d_h1, D, sc_t[:], d_sc, ss_t[:], d_ss)
                    kb.op("dve", lambda e: e.scalar_tensor_tensor(out=hmf_t[:], in0=h1_t[:], scalar=ss_t[:, 0:1], in1=mn_t[:],
                                                                   op0=ALU.mult, op1=ALU.mult),
                          R=[d_h1, d_ss, d_mn], W=[d_hmf])
                    kb.op("pool", lambda e: e.tensor_copy(out=hmb_t[:], in_=hmf_t[:]), R=[d_hmf], W=[d_hmb])

                def front2(ti):
                    t = tiles[ti]
                    b = ti % 2
                    r0, r1 = t * 128, (t + 1) * 128
                    hmf_t, d_hmf = hmf_r[b]
                    hmb_t, d_hmb = hmb_r[ti % 6]
                    pf_t, d_pf = pstf_r[0]
                    hmT_t, d_hmT = hmT_r[b]
                    for c in range(8):
                        kb.op("pe", lambda e: e.transpose(out=pf_t[:, c, :], in_=hmf_t[:, c * 128:(c + 1) * 128],
                                                          identity=identf[:]), R=[d_hmf, d_idf], W=[d_pf])
                    kb.op("act", lambda e: e.copy(out=hmT_t[:], in_=pf_t[:]), R=[d_pf], W=[d_hmT])
                    pr_t, d_pr = prt_r[ti % 4]
                    for c in range(8):
                        kb.op("pe", lambda e: e.matmul(pr_t[:, 0:36], lhsT=hmT_t[:, c, :], rhs=wr_t[:, c, :],
                                                       start=(c == 0), stop=(c == 7)), R=[d_hmT, d_wr], W=[d_pr])
                    return dict(t=t, b=ti % 4, r0=r0, r1=r1, hmb_t=hmb_t, d_hmb=d_hmb, pr_t=pr_t, d_pr=d_pr)

                def route_ops(cx):
                    t, b, r0, r1 = cx["t"], cx["b"], cx["r0"], cx["r1"]
                    hmb_t, d_hmb, pr_t, d_pr = cx["hmb_t"], cx["d_hmb"], cx["pr_t"], cx["d_pr"]
                    ops = []
                    add = ops.append
                    sm, d_sm = sm_r[b]
                    lg = sm[:, 0:36]
                    gmax = sm[:, 36:37]
                    ngmax = sm[:, 37:38]
                    gsum = sm[:, 38:39]
                    ggate = sm[:, 39:40]
                    eg = sm[:, 40:44]
                    ohg = sm[:, 44:48]
                    esel = sm[:, 48:56]
                    top8 = sm[:, 56:64]
                    oh0 = sm[:, 64:72]
                    oh1 = sm[:, 72:80]
                    A0 = sm[:, 80:112]
                    A1 = sm[:, 112:144]
                    sbt = sm[:, 144:176]
                    tmp = sm[:, 176:208]
                    destf = sm[:, 208:210]
                    diff = sm[:, 210:211]
                    sg = sm[:, 211:212]
                    S = [d_sm]
                    dv = lambda fn, R=(), W=(): kb.op("dve", fn, R=list(R) + S, W=list(W) + S)
                    add(lambda: dv(lambda e: e.tensor_tensor(out=lg, in0=pr_t[:, 0:36], in1=br_t[:], op=ALU.add), R=[d_pr, d_br]))
                    add(lambda: dv(lambda e: e.tensor_reduce(out=gmax, in_=lg[:, 0:4], axis=AX.X, op=ALU.max)))
                    add(lambda: dv(lambda e: e.tensor_scalar(out=ngmax, in0=gmax, scalar1=-1.0, scalar2=None, op0=ALU.mult)))
                    add(lambda: kb.op("act", lambda e: e.activation(out=eg, in_=lg[:, 0:4], func=AF.Exp, bias=ngmax, scale=1.0,
                                                                    accum_out=gsum), R=S, W=S))
                    add(lambda: dv(lambda e: e.reciprocal(out=ggate, in_=gsum)))
                    add(lambda: dv(lambda e: e.tensor_scalar(out=ohg, in0=lg[:, 0:4], scalar1=gmax, scalar2=None, op0=ALU.is_equal)))
                    add(lambda: dv(lambda e: e.tensor_scalar(out=esel, in0=lg[:, 4:12], scalar1=ohg[:, 0:1], scalar2=None, op0=ALU.mult)))
                    for g in range(1, 4):
                        add(lambda g=g: dv(lambda e: e.scalar_tensor_tensor(out=esel, in0=lg[:, 4 + 8 * g:12 + 8 * g],
                                                                            scalar=ohg[:, g:g + 1], in1=esel, op0=ALU.mult, op1=ALU.add)))
                    add(lambda: dv(lambda e: e.max(out=top8, in_=esel)))
                    add(lambda: dv(lambda e: e.tensor_scalar(out=oh0, in0=esel, scalar1=top8[:, 0:1], scalar2=None, op0=ALU.is_equal)))
                    add(lambda: dv(lambda e: e.tensor_scalar(out=oh1, in0=esel, scalar1=top8[:, 1:2], scalar2=None, op0=ALU.is_equal)))
                    for (Ak, ohk) in ((A0, oh0), (A1, oh1)):
                        add(lambda Ak=Ak, ohk=ohk: dv(lambda e: e.tensor_tensor(
                            out=Ak.rearrange("p (g j) -> p g j", j=8), in0=ohg.unsqueeze(2).to_broadcast([128, 4, 8]),
                            in1=ohk.unsqueeze(1).to_broadcast([128, 4, 8]), op=ALU.mult)))
                    use_valid = has_meta and t == 0
                    val_t, d_val = val_r[b]
                    if use_valid:
                        add(lambda: kb.dma("sp", val_t[:, 0:1], valid_in[r0:r1, :], W=[d_val]))
                        add(lambda: kb.op("dve", lambda e: e.tensor_scalar(out=val_t[:, 1:2], in0=val_t[:, 0:1], scalar1=-BIGIDX,
                                                                            scalar2=BIGIDX, op0=ALU.mult, op1=ALU.add),
                                          R=[d_val], W=[d_val]))
                        for Ak in (A0, A1):
                            add(lambda Ak=Ak: dv(lambda e: e.tensor_scalar(out=Ak, in0=Ak, scalar1=val_t[:, 0:1], scalar2=None,
                                                                           op0=ALU.mult), R=[d_val]))
                    ab_t, d_ab = ab_r[b]
                    add(lambda: kb.op("dve", lambda e: e.tensor_tensor(out=ab_t[:], in0=A0, in1=A1, op=ALU.add), R=S, W=[d_ab]))
                    add(lambda: kb.op("pe", lambda e: e.matmul(pr_t[:, 64:96], lhsT=usb[:], rhs=ab_t[:], start=True, stop=True),
                                      R=[d_us, d_ab], W=[d_pr]))
                    add(lambda: kb.op("pe", lambda e: e.matmul(pr_t[:, 96:128], lhsT=onesb[:], rhs=ab_t[:], start=True, stop=True),
                                      R=[d_onesb, d_ab], W=[d_pr]))

                    def cnt_ops():
                        dv(lambda e: e.tensor_tensor(out=sbt, in0=pr_t[:, 64:96], in1=cntb[:], op=ALU.add), R=[d_pr, d_cntb])
                        kb.op("dve", lambda e: e.tensor_tensor(out=cntb[:], in0=cntb[:], in1=pr_t[:, 96:128], op=ALU.add),
                              R=[d_pr, d_cntb] + S, W=[d_cntb])
                    add(cnt_ops)
                    for k, Ak in enumerate((A0, A1)):
                        add(lambda Ak=Ak: dv(lambda e: e.tensor_tensor(out=tmp, in0=Ak, in1=sbt, op=ALU.mult)))
                        add(lambda k=k: dv(lambda e: e.tensor_reduce(out=destf[:, k:k + 1], in_=tmp, axis=AX.X, op=ALU.add)))
                    if use_valid:
                        add(lambda: dv(lambda e: e.tensor_scalar(out=destf, in0=destf, scalar1=val_t[:, 0:1], scalar2=val_t[:, 1:2],
                                                                 op0=ALU.mult, op1=ALU.add), R=[d_val]))
                    add(lambda: kb.op("dve", lambda e: e.tensor_copy(out=dest_i[:, t, :], in_=destf), R=S, W=[d_dest]))
                    add(lambda: dv(lambda e: e.tensor_tensor(out=diff, in0=top8[:, 0:1], in1=top8[:, 1:2], op=ALU.subtract)))
                    add(lambda: kb.op("act", lambda e: e.activation(out=sg, in_=diff, func=AF.Sigmoid), R=S, W=S))
                    add(lambda: kb.op("dve", lambda e: e.tensor_tensor(out=gw[:, t, 0:1], in0=sg, in1=ggate, op=ALU.mult), R=S, W=[d_gw]))
                    add(lambda: kb.op("dve", lambda e: e.tensor_tensor(out=gw[:, t, 1:2], in0=ggate, in1=gw[:, t, 0:1], op=ALU.subtract),
                                      R=S + [d_gw], W=[d_gw]))
                    for k in range(2):
                        add(lambda k=k: kb.idma(out=xpad[:, :], out_off=bass.IndirectOffsetOnAxis(ap=dest_i[:, t, k:k + 1], axis=0),
                                                in_=hmb_t[:], in_off=None, R=[d_hmb, d_dest], W=[d_xpad],
                                                bounds_check=breg, oob_is_err=False))
                    return ops

                p1_load(0)

                def emit_routes(pair):
                    lists = [route_ops(cxs[i]) for i in pair]
                    for k in range(max(len(l) for l in lists)):
                        for l in lists:
                            if k < len(l):
                                l[k]()

                cxs = {}
                ntl = len(tiles)
                for ti in range(1, min(3, ntl)):
                    p1_load(ti)
                front(0)
                pending = None
                for ti in range(ntl):
                    if ti + 1 < ntl:
                        front(ti + 1)
                    cxs[ti] = front2(ti)
                    if ti % 2 == 1 or ti == ntl - 1:
                        pair = (ti - 1, ti) if ti % 2 == 1 else (ti,)
                        if pending is not None:
                            emit_routes(pending)
                        pending = pair
                emit_routes(pending)
            with kb.scope():
                wup_r = kb.ring("wup", [128, 8, 2 * DE], BF16, 2)
                wdn_r = kb.ring("wdn", [128, 4, D], BF16, 2)
                wst_r = kb.ring("wst", [128, D], F32, 12)
                wdeps = [[Dep("w%d_%d" % (bb, cc)) for cc in range(12)] for bb in range(2)]
                xs_r = kb.ring("xs", [128, D], BF16, 2 * nst)
                xT_r = kb.ring("xT", [128, 8, cap], BF16, 2)
                sa_r = kb.ring("sa", [128, cap], F32, 2)
                aT_r = kb.ring("aT", [128, 4, cap], BF16, 2)
                yb_r = kb.ring("yb", [128, D], BF16, 2)
                pst_r = kb.ring("pst", [128, 8, 128], BF16, 2, psum=True)
                pau_r = kb.ring("pau", [128, 512], F32, 4, psum=True)
                py_r = kb.ring("py", [128, 512], F32, 2, psum=True)
                CE = ("act", "dve", "act", "dve", "act", "dve", "act", "dve", "act", "dve", "act", "dve")

                def w_dma(ex):
                    wuv = md["w_up"][ex].rearrange("(c p) n -> p c n", p=128)
                    wdv = md["w_dn"][ex].rearrange("(c p) n -> p c n", p=128)
                    for c in range(12):
                        stg, d_stg = wst_r[c]
                        kb.dma("sp", stg[:], wuv[:, c, :] if c < 8 else wdv[:, c - 8, :], W=[d_stg])

                def w_cast(ex, cs):
                    bb = ex % 2
                    for c in cs:
                        stg, d_stg = wst_r[c]
                        dst_ap = wup_r[bb][0][:, c, :] if c < 8 else wdn_r[bb][0][:, c - 8, :]
                        if CE[c] == "act":
                            kb.op("act", lambda e: e.copy(out=dst_ap, in_=stg[:]), R=[d_stg], W=[wdeps[bb][c]])
                        else:
                            kb.op("dve", lambda e: e.tensor_copy(out=dst_ap, in_=stg[:]), R=[d_stg], W=[wdeps[bb][c]])

                def x_dma(ex):
                    for st in range(nst):
                        xs_t, d_xs = xs_r[(ex % 2) * nst + st]
                        s0 = ex * cap + st * 128
                        kb.dma("sp", xs_t[:], xpad[s0:s0 + 128, :], R=[d_xpad], W=[d_xs])

                w_dma(0)
                x_dma(0)
                w_cast(0, range(12))
                yi = 0
                for ex in range(NE):
                    b = ex % 2
                    wup_t = wup_r[b][0]
                    wdn_t = wdn_r[b][0]
                    if ex + 1 < NE:
                        w_dma(ex + 1)
                        x_dma(ex + 1)
                    xT_t, d_xT = xT_r[b]
                    for st in range(nst):
                        xs_t, d_xs = xs_r[b * nst + st]
                        ps_t, d_pst = pst_r[st % 2]
                        for c in range(8):
                            kb.op("pe", lambda e: e.transpose(out=ps_t[:, c, :], in_=xs_t[:, c * 128:(c + 1) * 128],
                                                              identity=ident[:]), R=[d_xs, d_id], W=[d_pst])
                        kb.op("act" if st % 2 else "dve",
                              (lambda e: e.copy(out=xT_t[:, :, st * 128:(st + 1) * 128], in_=ps_t[:])) if st % 2 else
                              (lambda e: e.tensor_copy(out=xT_t[:, :, st * 128:(st + 1) * 128], in_=ps_t[:])),
                              R=[d_pst], W=[d_xT])
                    aT_t, d_aT = aT_r[b]
                    for fc in range(4):
                        pa, d_pa = pau_r[(2 * fc) % 4]
                        pu, d_pu = pau_r[(2 * fc + 1) % 4]
                        for c in range(8):
                            kb.op("pe", lambda e: e.matmul(pa[:, 0:cap], lhsT=wup_t[:, c, fc * 128:(fc + 1) * 128],
                                                           rhs=xT_t[:, c, :], start=(c == 0), stop=(c == 7)),
                                  R=[wdeps[b][c], d_xT], W=[d_pa])
                        for c in range(8):
                            kb.op("pe", lambda e: e.matmul(pu[:, 0:cap], lhsT=wup_t[:, c, DE + fc * 128:DE + (fc + 1) * 128],
                                                           rhs=xT_t[:, c, :], start=(c == 0), stop=(c == 7)),
                                  R=[wdeps[b][c], d_xT], W=[d_pu])
                        sa_t, d_sa = sa_r[fc % 2]
                        kb.op("act", lambda e: e.activation(out=sa_t[:], in_=pa[:, 0:cap], func=AF.Silu), R=[d_pa], W=[d_sa])
                        kb.op("dve", lambda e: e.tensor_tensor(out=aT_t[:, fc, :], in0=sa_t[:], in1=pu[:, 0:cap], op=ALU.mult),
                              R=[d_sa, d_pu], W=[d_aT])
                    if ex + 1 < NE:
                        w_cast(ex + 1, range(0, 8))
                    for st in range(nst):
                        yb_t, d_yb = yb_r[yi % 2]
                        yi += 1
                        for half in range(2):
                            py, d_py = py_r[half]
                            for fc in range(4):
                                kb.op("pe", lambda e: e.matmul(py[:], lhsT=aT_t[:, fc, st * 128:(st + 1) * 128],
                                                               rhs=wdn_t[:, fc, half * 512:(half + 1) * 512],
                                                               start=(fc == 0), stop=(fc == 3)),
                                      R=[d_aT, wdeps[b][8 + fc]], W=[d_py])
                            if half == 0:
                                kb.op("act", lambda e: e.copy(out=yb_t[:, 0:512], in_=py[:]), R=[d_py], W=[d_yb])
                            else:
                                kb.op("dve", lambda e: e.tensor_copy(out=yb_t[:, 512:1024], in_=py[:]), R=[d_py], W=[d_yb])
                        s0 = ex * cap + st * 128
                        kb.dma("sp", ypad[s0:s0 + 128, :], yb_t[:], R=[d_yb], W=[d_ypad])
                    if ex + 1 < NE:
                        w_cast(ex + 1, range(8, 12))
            with kb.scope():
                h1_r = kb.ring("h1", [128, D], F32, 4)
                y_r = kb.ring("y", [128, D], BF16, 8)
                h2_r = kb.ring("h2", [128, D], F32, 2)
                for (y_t, d_y) in y_r:
                    kb.op("pool", lambda e: e.memset(y_t[:], 0.0), W=[d_y])
                p3 = pass3_setup()
                def p3_load(ti):
                    tt = tiles[ti]
                    bb = ti % 4
                    h1_t, d_h1 = h1_r[bb]
                    kb.dma("sp", h1_t[:], h1s[tt * 128:(tt + 1) * 128, :], R=[d_h1s], W=[d_h1])
                    for k in range(2):
                        y_t, d_y = y_r[2 * bb + k]
                        kb.idma(out=y_t[:], out_off=None, in_=ypad[:, :],
                                in_off=bass.IndirectOffsetOnAxis(ap=dest_i[:, tt, k:k + 1], axis=0),
                                R=[d_ypad, d_dest], W=[d_y], bounds_check=breg, oob_is_err=False)
                for ti in range(min(3, len(tiles))):
                    p3_load(ti)
                for ti, t in enumerate(tiles):
                    b = ti % 2
                    r0, r1 = t * 128, (t + 1) * 128
                    h1_t, d_h1 = h1_r[ti % 4]
                    h2_t, d_h2 = h2_r[b]
                    y0_t, d_y0 = y_r[2 * (ti % 4)]
                    y1_t, d_y1 = y_r[2 * (ti % 4) + 1]
                    if ti + 3 < len(tiles):
                        p3_load(ti + 3)
                    kb.op("dve", lambda e: e.scalar_tensor_tensor(out=h2_t[:], in0=y0_t[:], scalar=gw[:, t, 0:1], in1=h1_t[:],
                                                                   op0=ALU.mult, op1=ALU.add), R=[d_y0, d_gw, d_h1], W=[d_h2])
                    kb.op("dve", lambda e: e.scalar_tensor_tensor(out=h2_t[:], in0=y1_t[:], scalar=gw[:, t, 1:2], in1=h2_t[:],
                                                                   op0=ALU.mult, op1=ALU.add), R=[d_y1, d_gw, d_h2], W=[d_h2])
                    pass3_tile(p3, t, h2_t, d_h2)

    def t2_setup_lhsT():
        idx2 = kb.sb("idx2", [128, 8], I32); d_idx2 = Dep("idx2")
        kb.dma("sp", idx2[:], idx2_d, W=[d_idx2])
        oTall = kb.sb("oTall", [128, 8, TX], BF16); d_oTall = Dep("oTall")
        oTm = kb.sb("oTm", [128, 8, 128], BF16); d_oTm = Dep("oTm")
        for c8 in range(8):
            kb.idma(out=oTall[:, c8, :], out_off=None, in_=RBb[:, :],
                    in_off=bass.IndirectOffsetOnAxis(ap=idx2[:, c8:c8 + 1], axis=0),
                    R=[d_RBb, d_idx2], W=[d_oTall], bounds_check=breg2, oob_is_err=False)
        kb.dma("sp", oTm[:], RBm.rearrange("(c p) t -> p c t", p=128), R=[d_RBm], W=[d_oTm])

        def get(t):
            if t == 0:
                return (lambda c: oTm[:, c, :]), [d_oTm]
            return (lambda c: oTall[:, c, (t - 1) * 128:t * 128]), [d_oTall]
        return get

    def t2_pass3_setup():
        p = {}
        p["wg"], p["d_wg"] = load_w(kb, "hwg", hwg_d, D, D)
        p["d_wg"].ro = True
        p["hn"], p["d_hn"] = load_bc(kb, "hnorm", hnorm, D)
        p["scr"] = kb.ring("scr", [128, D], F32, 2)
        p["ss"] = kb.ring("ss", [128, 1], F32, 2)
        p["xn"] = kb.ring("xn", [128, D], BF16, 2)
        p["hc"] = kb.ring("hc", [128, 8, 512], BF16, 2)
        p["gsb"] = kb.ring("gsb", [128, 8, 512], BF16, 2)
        p["pst"] = kb.ring("pst", [128, 8, 128], BF16, 2, psum=True)
        p["pg"] = kb.ring("pg", [128, 512], F32, 2, psum=True)
        return p

    def t2_pass3_tile(p, t, h2_t, d_h2):
        kb.dma("sp", h2s[t * 128:(t + 1) * 128, :], h2_t[:], R=[d_h2], W=[d_h2s])

        def hook(j, hc_t, d_hc):
            gsb, d_gsb = p["gsb"][j % 2]
            for g in range(8):
                pg, d_pg = p["pg"][g % 2]
                for c in range(8):
                    kb.op("pe", lambda e: e.matmul(pg[:], lhsT=p["wg"][:, c, g * 128:(g + 1) * 128], rhs=hc_t[:, c, :],
                                                   start=(c == 0), stop=(c == 7)), R=[p["d_wg"], d_hc], W=[d_pg])
                kb.op("act", lambda e: e.activation(out=gsb[:, g, :], in_=pg[:], func=AF.Silu), R=[d_pg], W=[d_gsb])
            kb.dma("sp", GS[j * 128:(j + 1) * 128, :], gsb[:].rearrange("p g t -> p (g t)"), R=[d_gsb], W=[d_GS])
        emit_hnT(h2_t, d_h2, p["hn"], p["d_hn"], p["scr"], p["ss"], p["xn"], p["pst"], p["hc"], t,
                 MC, d_MC, SC, d_SC, RC, d_RC, hc_hook=hook)

    tok_stage(0, list(range(nt)), True, t2_setup_lhsT, lambda t: x_in[t * 128:(t + 1) * 128, :], wo1_d,
              t2_pass3_setup, t2_pass3_tile)

    if stop == "T2":
        return dump_and_finish([("h2s", h2s, d_h2s), ("RC", RC, d_RC[nch - 1]), ("GS", GS, d_GS)])
    for i2 in range(2):
        with kb.scope():
            q_t = kb.sb("q", [128, L], BF16); d_q = Dep("q")
            lf_t = kb.sb("lf", [128, L], F32); d_lf = Dep("lf")
            v_t = kb.sb("v", [128, nb, 128], BF16); d_v = Dep("v")
            go_t, d_go = load_bc(kb, "go", go_d, 128)
            with kb.scope():
                hwq_t, d_hwq = load_w(kb, "hwq", hwq_d[i2], D, 128)
                hwf_t, d_hwf = load_w(kb, "hwf", hwf_d[i2], D, 128)
                hwv_t, d_hwv = load_w(kb, "hwv", hwv_d[i2], D, 128)
                for d in (d_hwq, d_hwf, d_hwv):
                    d.ro = True
                cols = kb.sb("cols", [128, 8], F32); d_cols = Dep("cols")
                kb.dma("sp", cols[:, 0:1], hbfc_d[i2], W=[d_cols])
                kb.dma("sp", cols[:, 1:2], l0c_d[i2], W=[d_cols])
                kb.dma("sp", cols[:, 2:3], l1c_d[i2], W=[d_cols])
                kb.op("dve", lambda e: e.tensor_tensor(out=cols[:, 3:4], in0=cols[:, 2:3], in1=cols[:, 1:2], op=ALU.subtract),
                      R=[d_cols], W=[d_cols])
                kb.op("act", lambda e: e.activation(out=cols[:, 4:5], in_=cols[:, 3:4], func=AF.Sigmoid), R=[d_cols], W=[d_cols])
                kb.op("act", lambda e: e.activation(out=cols[:, 5:6], in_=cols[:, 3:4], func=AF.Sigmoid, scale=-1.0),
                      R=[d_cols], W=[d_cols])
                d_cols.ro = True
                hgr = kb.ring("hg", [128, 8, 512], BF16, 2)
                sgr = kb.ring("sg", [128, 512], F32, 2)
                pqr = kb.ring("pq", [128, 512], F32, 2, psum=True)
                pfr = kb.ring("pf", [128, 512], F32, 2, psum=True)
                pvr = kb.ring("pv", [128, 512], F32, 2, psum=True)
                vi = 0
                def h2_load(G):
                    hg, d_hg = hgr[G % 2]
                    if G == 0:
                        kb.dma("sp", hg[:, :, 0:128], MC.rearrange("p (c t) -> p c t", c=8), R=[d_MC], W=[d_hg])
                    else:
                        rank, j = (G - 1) // nch, (G - 1) % nch
                        r0 = j * 512 + rank * 128
                        kb.dma("sp", hg[:].rearrange("p c t -> p (c t)"), RC[r0:r0 + 128, :], R=[d_RC[j]], W=[d_hg])
                h2_load(0)
                for G in range(1 + 4 * nch):
                    hg, d_hg = hgr[G % 2]
                    if G + 1 < 1 + 4 * nch:
                        h2_load(G + 1)
                    if G == 0:
                        n, tok0 = 128, 0
                    else:
                        n, tok0 = 512, 128 + (G - 1) * 512
                    pq, d_pq = pqr[G % 2]
                    pf, d_pf = pfr[G % 2]
                    sg, d_sg = sgr[G % 2]
                    for c in range(8):
                        kb.op("pe", lambda e: e.matmul(pq[:, 0:n], lhsT=hwq_t[:, c, :], rhs=hg[:, c, 0:n], start=(c == 0), stop=(c == 7)),
                              R=[d_hwq, d_hg], W=[d_pq])
                    kb.op("act", lambda e: e.activation(out=q_t[:, tok0:tok0 + n], in_=pq[:, 0:n], func=AF.Silu), R=[d_pq], W=[d_q])
                    for c in range(8):
                        kb.op("pe", lambda e: e.matmul(pf[:, 0:n], lhsT=hwf_t[:, c, :], rhs=hg[:, c, 0:n], start=(c == 0), stop=(c == 7)),
                              R=[d_hwf, d_hg], W=[d_pf])
                    kb.op("act", lambda e: e.activation(out=sg[:, 0:n], in_=pf[:, 0:n], func=AF.Sigmoid, bias=cols[:, 0:1], scale=1.0),
                          R=[d_pf, d_cols], W=[d_sg])
                    kb.op("dve", lambda e: e.tensor_scalar(out=sg[:, 0:n], in0=sg[:, 0:n], scalar1=cols[:, 5:6], scalar2=cols[:, 4:5],
                                                            op0=ALU.mult, op1=ALU.add), R=[d_sg, d_cols], W=[d_sg])
                    kb.op("act", lambda e: e.activation(out=lf_t[:, tok0:tok0 + n], in_=sg[:, 0:n], func=AF.Ln), R=[d_sg], W=[d_lf])
                    for s_ in range(n // 128):
                        pv, d_pv = pvr[vi % 2]
                        vi += 1
                        for c in range(8):
                            kb.op("pe", lambda e: e.matmul(pv[:, 0:128], lhsT=hg[:, c, s_ * 128:(s_ + 1) * 128], rhs=hwv_t[:, c, :],
                                                           start=(c == 0), stop=(c == 7)), R=[d_hwv, d_hg], W=[d_pv])
                        kb.op("dve", lambda e: e.tensor_copy(out=v_t[:, tok0 // 128 + s_, :], in_=pv[:, 0:128]), R=[d_pv], W=[d_v])
                kb.op("pool", lambda e: e.memset(lf_t[:, 0:128 - NMETA], 0.0), W=[d_lf])
            with kb.scope():
                onesf3 = kb.sb("onesf3", [128, 128], F32); d_onesf3 = Dep("onesf3")
                mf = kb.sb("mf", [128, 128], F32); d_mf = Dep("mf")
                mku = kb.sb("mku", [128, 128], U32); d_mk = Dep("mku")
                kb.op("pool", lambda e: e.memset(onesf3[:], 1.0), W=[d_onesf3])
                kb.op("pool", lambda e: e.affine_select(out=mf[:], in_=onesf3[:], pattern=[[1, 128]], compare_op=ALU.is_ge,
                                                        fill=0.0, base=0, channel_multiplier=-1), R=[d_onesf3], W=[d_mf])
                kb.op("dve", lambda e: e.tensor_copy(out=mku[:], in_=mf[:]), R=[d_mf], W=[d_mk])
                d_mk.ro = True
                d_onesf3.ro = True
                S_t = kb.sb("S", [128, 128], F32); d_S = Dep("S")
                Sb_r = kb.ring("Sb", [128, 128], BF16, 2)
                b_r = kb.ring("b", [128, 128], F32, 3)
                col_r = kb.ring("col", [128, 8], F32, 3)
                e1_r = kb.ring("e1", [128, 128], F32, 3)
                e2_r = kb.ring("e2", [128, 128], F32, 3)
                kk_r = kb.ring("kk", [128, 128], F32, 3)
                qd_r = kb.ring("qd", [128, 128], BF16, 3)
                kd_r = kb.ring("kd", [128, 128], BF16, 3)
                qb_r = kb.ring("qb", [128, 128], BF16, 3)
                ke_r = kb.ring("ke", [128, 128], BF16, 3)
                keT_r = kb.ring("keT", [128, 128], BF16, 2)
                scm_r = kb.ring("scm", [128, 128], BF16, 3)
                osq_r = kb.ring("osq", [128, 128], F32, 2)
                oss_r = kb.ring("oss", [128, 2], F32, 3)
                on_r = kb.ring("on", [128, 128], BF16, 3)
                stg_r = kb.ring("stg", [128, TX], BF16, 2)
                psc_b = [kb.ps("psc%d" % k, [128, 512], F32) for k in range(2)]
                po_b = [kb.ps("po%d" % k, [128, 512], F32) for k in range(2)]
                pu_b = [kb.ps("pu%d" % k, [128, 512], F32) for k in range(2)]
                pt_b = [kb.ps("pt%d" % k, [128, 8, 128], BF16) for k in range(2)]
                psc_r = [(psc_b[k % 2][:, 0:128], Dep("psc%d" % k)) for k in range(2)] * 2
                po_r = [(po_b[k % 2][:, 0:128], Dep("po%d" % k)) for k in range(2)] * 2
                pu_r = [(pu_b[k % 2][:, 0:128], Dep("pu%d" % k)) for k in range(2)] * 2
                ptk_r = [(pt_b[k % 2][:, 0, :], Dep("ptk%d" % k)) for k in range(2)] * 2
                pto_r = [(pt_b[k % 2][:, 1, :], Dep("pto%d" % k)) for k in range(2)] * 2
                for (t_, d_) in scm_r:
                    kb.op("pool", lambda e: e.memset(t_[:], 0.0), W=[d_])
                kb.op("pool", lambda e: e.memset(S_t[:], 0.0), W=[d_S])
                kb.op("pool", lambda e: e.memset(Sb_r[0][0][:], 0.0), W=[Sb_r[0][1]])

                def st_a(c):
                    cs = slice(c * 128, (c + 1) * 128)
                    b_t, d_b = b_r[c % 3]
                    col, d_col = col_r[c % 3]
                    e1, d_e1 = e1_r[c % 3]
                    e2, d_e2 = e2_r[c % 3]
                    kk, d_kk = kk_r[c % 3]
                    qd, d_qd = qd_r[c % 3]
                    kd, d_kd = kd_r[c % 3]
                    qb, d_qb = qb_r[c % 3]
                    ke, d_ke = ke_r[c % 3]
                    kb.op("dve", lambda e: e.tensor_tensor_scan(out=b_t[:], data0=onesf3[:], data1=lf_t[:, cs], initial=0.0,
                                                                 op0=ALU.mult, op1=ALU.add), R=[d_lf, d_onesf3], W=[d_b])
                    kb.op("dve", lambda e: e.tensor_copy(out=col[:, 0:1], in_=b_t[:, 63:64]), R=[d_b], W=[d_col])
                    kb.op("dve", lambda e: e.tensor_tensor(out=col[:, 1:2], in0=b_t[:, 127:128], in1=b_t[:, 63:64],
                                                            op=ALU.subtract), R=[d_b], W=[d_col])
                    kb.op("dve", lambda e: e.tensor_copy(out=col[:, 2:3], in_=b_t[:, 127:128]), R=[d_b], W=[d_col])
                    kb.op("dve", lambda e: e.tensor_scalar(out=col[:, 3:4], in0=b_t[:, 63:64], scalar1=-1.0, scalar2=None,
                                                            op0=ALU.mult), R=[d_b], W=[d_col])
                    kb.op("act", lambda e: e.activation(out=col[:, 4:7], in_=col[:, 0:3], func=AF.Exp), R=[d_col], W=[d_col])
                    kb.op("act", lambda e: e.activation(out=e1[:], in_=b_t[:], func=AF.Exp, bias=col[:, 3:4], scale=1.0),
                          R=[d_b, d_col], W=[d_e1])
                    kb.op("act", lambda e: e.activation(out=e2[:], in_=b_t[:], func=AF.Exp, bias=col[:, 0:1], scale=-1.0),
                          R=[d_b, d_col], W=[d_e2])
                    kb.op("act", lambda e: e.activation(out=kk[:], in_=lf_t[:, cs], func=AF.Exp), R=[d_lf], W=[d_kk])
                    kb.op("pool", lambda e: e.tensor_scalar(out=kk[:], in0=kk[:], scalar1=-1.0, scalar2=1.0, op0=ALU.mult,
                                                             op1=ALU.add), R=[d_kk], W=[d_kk])
                    kb.op("dve", lambda e: e.tensor_tensor(out=qd[:], in0=q_t[:, cs], in1=e1[:], op=ALU.mult),
                          R=[d_q, d_e1], W=[d_qd])
                    kb.op("dve", lambda e: e.tensor_tensor(out=kd[:], in0=kk[:], in1=e2[:], op=ALU.mult),
                          R=[d_kk, d_e2], W=[d_kd])
                    kb.op("dve", lambda e: e.scalar_tensor_tensor(out=qb[:], in0=q_t[:, cs], scalar=col[:, 4:5], in1=e1[:],
                                                                   op0=ALU.mult, op1=ALU.mult), R=[d_q, d_col, d_e1], W=[d_qb])
                    kb.op("dve", lambda e: e.scalar_tensor_tensor(out=ke[:], in0=kk[:], scalar=col[:, 5:6], in1=e2[:],
                                                                   op0=ALU.mult, op1=ALU.mult), R=[d_kk, d_col, d_e2], W=[d_ke])

                def st_f(c):
                    qd, d_qd = qd_r[c % 3]
                    kd, d_kd = kd_r[c % 3]
                    ke, d_ke = ke_r[c % 3]
                    keT, d_keT = keT_r[c % 2]
                    scm, d_scm = scm_r[c % 3]
                    psc, d_psc = psc_r[c % 4]
                    ptk, d_ptk = ptk_r[c % 4]
                    pu, d_pu = pu_r[c % 4]
                    kb.op("pe", lambda e: e.matmul(psc, lhsT=kd[:], rhs=qd[:], start=True, stop=True), R=[d_kd, d_qd], W=[d_psc])
                    kb.op("dve", lambda e: e.copy_predicated(out=scm[:], mask=mku[:], data=psc), R=[d_psc, d_mk], W=[d_scm])
                    kb.op("pe", lambda e: e.transpose(out=ptk, in_=ke[:], identity=ident[:]), R=[d_ke, d_id], W=[d_ptk])
                    kb.op("act", lambda e: e.copy(out=keT[:], in_=ptk), R=[d_ptk], W=[d_keT])
                    kb.op("pe", lambda e: e.matmul(pu, lhsT=keT[:], rhs=v_t[:, c, :], start=True, stop=True), R=[d_keT, d_v], W=[d_pu])

                def st_k(c):
                    col, d_col = col_r[c % 3]
                    qb, d_qb = qb_r[c % 3]
                    scm, d_scm = scm_r[c % 3]
                    po, d_po = po_r[c % 4]
                    pu, d_pu = pu_r[c % 4]
                    Sb, d_Sb = Sb_r[c % 2]
                    kb.op("pe", lambda e: e.matmul(po, lhsT=scm[:], rhs=v_t[:, c, :], start=True, stop=False), R=[d_scm, d_v], W=[d_po])
                    kb.op("pe", lambda e: e.matmul(po, lhsT=qb[:], rhs=Sb[:], start=False, stop=True), R=[d_qb, d_Sb], W=[d_po])
                    kb.op("dve", lambda e: e.scalar_tensor_tensor(out=S_t[:], in0=S_t[:], scalar=col[:, 6:7], in1=pu,
                                                                   op0=ALU.mult, op1=ALU.add), R=[d_S, d_col, d_pu], W=[d_S])
                    Sb2, d_Sb2 = Sb_r[(c + 1) % 2]
                    kb.op("pool", lambda e: e.tensor_copy(out=Sb2[:], in_=S_t[:]), R=[d_S], W=[d_Sb2])

                def st_n(c):
                    po, d_po = po_r[c % 4]
                    osq, d_osq = osq_r[c % 2]
                    oss, d_oss = oss_r[c % 3]
                    on_t, d_on = on_r[c % 3]
                    kb.op("act", lambda e: e.activation(out=osq[:], in_=po, func=AF.Square, accum_out=oss[:, 0:1]),
                          R=[d_po], W=[d_osq, d_oss])
                    kb.op("act", lambda e: e.activation(out=oss[:, 1:2], in_=oss[:, 0:1], func=AF.Ln, scale=1.0 / 128, bias=EPS),
                          R=[d_oss], W=[d_oss])
                    kb.op("act", lambda e: e.activation(out=oss[:, 1:2], in_=oss[:, 1:2], func=AF.Exp, scale=-0.5),
                          R=[d_oss], W=[d_oss])
                    kb.op("dve", lambda e: e.scalar_tensor_tensor(out=on_t[:], in0=po, scalar=oss[:, 1:2], in1=go_t[:],
                                                                   op0=ALU.mult, op1=ALU.mult), R=[d_po, d_oss, d_go], W=[d_on])

                def st_t(c):
                    on_t, d_on = on_r[c % 3]
                    pto, d_pto = pto_r[c % 4]
                    xb = c - 1
                    qq, tb = xb // ntx, xb % ntx
                    stg, d_stg = stg_r[qq % 2]
                    kb.op("pe", lambda e: e.transpose(out=pto, in_=on_t[:], identity=ident[:]), R=[d_on, d_id], W=[d_pto])
                    kb.op("act", lambda e: e.copy(out=stg[:, tb * 128:(tb + 1) * 128], in_=pto), R=[d_pto], W=[d_stg])
                    if tb == ntx - 1:
                        cidx = i2 * 4 + qq
                        kb.dma("sp", SD[cidx * 128:(cidx + 1) * 128, :], stg[:], R=[d_stg], W=[d_SD, d_SDc[cidx]])
                        kb.cc(SD[cidx * 128:(cidx + 1) * 128, :], RD[cidx * 512:(cidx + 1) * 512, :], GROUPS, R=[d_SDc[cidx]], W=[d_RD])

                st_a(0)
                if nb > 1:
                    st_a(1)
                st_f(0)
                for i in range(nb + 2):
                    if i + 2 < nb:
                        st_a(i + 2)
                    if i + 1 < nb:
                        st_f(i + 1)
                    if i < nb:
                        st_k(i)
                    if 1 <= i - 1 < nb:
                        st_n(i - 1)
                    if 1 <= i - 2 < nb:
                        st_t(i - 2)

    if stop == "H2":
        return dump_and_finish([("RD", RD, d_RD)])
    def t3_setup_lhsT():
        idx3 = kb.sb("idx3", [128, 8], I32); d_idx3 = Dep("idx3")
        kb.dma("sp", idx3[:], idx3_d, W=[d_idx3])
        onall = kb.sb("onall", [128, 8, TX], BF16); d_onall = Dep("onall")
        for c8 in range(8):
            kb.idma(out=onall[:, c8, :], out_off=None, in_=RD[:, :],
                    in_off=bass.IndirectOffsetOnAxis(ap=idx3[:, c8:c8 + 1], axis=0),
                    R=[d_RD, d_idx3], W=[d_onall], bounds_check=breg2, oob_is_err=False)
        gsr = kb.ring("gsT", [128, 8, 512], BF16, 2)
        ogr = kb.ring("og", [128, 8, 128], BF16, 2)

        def prefetch(t):
            if (t - 1) % 4 == 0:
                jj = (t - 1) // 4
                gs_t, d_gs = gsr[jj % 2]
                kb.dma("sp", gs_t[:].rearrange("p g t -> p (g t)"), GS[jj * 128:(jj + 1) * 128, :], R=[d_GS], W=[d_gs])

        def get(t):
            jj, ss_ = (t - 1) // 4, (t - 1) % 4
            gs_t, d_gs = gsr[jj % 2]
            og_t, d_og = ogr[t % 2]
            kb.op("dve", lambda e: e.tensor_tensor(out=og_t[:], in0=onall[:, :, (t - 1) * 128:t * 128],
                                                    in1=gs_t[:, :, ss_ * 128:(ss_ + 1) * 128], op=ALU.mult),
                  R=[d_onall, d_gs], W=[d_og])
            return (lambda c: og_t[:, c, :]), [d_og]
        get.prefetch = prefetch
        return get

    def t3_pass3_tile(p, t, h2_t, d_h2):
        kb.dma("sp", out_d[(t - 1) * 128:t * 128, :], h2_t[:], R=[d_h2])

    tok_stage(1, list(range(1, nt)), False, t3_setup_lhsT, lambda t: h2s[t * 128:(t + 1) * 128, :], wo2_d,
              lambda: None, t3_pass3_tile)
    kb.finish()
    return nc


def fused_in_maps(inp, ntx):
    f32 = np.float32
    TX = ntx * 128
    nt = ntx + 1
    x = inp["x"]
    metatile = np.zeros((128, D), f32)
    metatile[128 - NMETA:] = inp["meta_tokens"]
    valid = np.ones((nt * 128, 1), f32)
    valid[0:128 - NMETA] = 0.0
    fw = inp["fox_w_in"][0]
    hw = inp["hg_w_in"][0]
    common = {
        "valid_in": valid, "fnorm": inp["fox_norm"][0][None],
        "gqc": np.tile(inp["fox_q_norm"][0], 2)[:, None].astype(f32), "gkc": np.tile(inp["fox_k_norm"][0], 2)[:, None].astype(f32),
        "wo1": inp["fox_w_out"][0], "wo2": inp["hg_w_out"][0], "hnorm": inp["hg_norm"][0][None],
        "hwg": np.ascontiguousarray(hw[:, 3 * D:4 * D]), "go": inp["hg_o_norm"][0][None],
    }
    for l in range(2):
        common["mnorm%d" % l] = inp["moe_norm"][l][None]
        common["w_r%d" % l] = np.ascontiguousarray(np.concatenate([inp["moe_w_grp"][l], inp["moe_w_rt"][l]], 1))
        common["b_r%d" % l] = np.concatenate([inp["moe_b_grp"][l], inp["moe_b_rt"][l]])[None]
        common["w_up%d" % l] = inp["moe_w_up"][l]
        common["w_dn%d" % l] = inp["moe_w_down"][l]
    maps = []
    p = np.arange(128)
    for c in range(NCORES):
        b, r = c // 4, c % 4
        m = dict(common)
        m["x_in"] = np.concatenate([metatile, x[b, r * TX:(r + 1) * TX]], 0)
        hc = slice(4 * r * FD, (4 * r + 4) * FD)
        m["wq"] = np.ascontiguousarray(fw[:, 0:D][:, hc])
        m["wk"] = np.ascontiguousarray(fw[:, D:2 * D][:, hc])
        m["wv"] = np.ascontiguousarray(fw[:, 2 * D:3 * D][:, hc])
        m["wf"] = np.ascontiguousarray(fw[:, 3 * D + 4 * r:3 * D + 4 * r + 4])
        bf4 = inp["fox_b_f"][0][4 * r:4 * r + 4]
        m["bf4r"] = bf4[None].astype(f32)
        m["bf4c"] = bf4[:, None].astype(f32)
        idx2 = np.zeros((128, 8), np.int32)
        idx3 = np.zeros((128, 8), np.int32)
        for c8 in range(8):
            rank, sub = c8 // 2, c8 % 2
            idx2[:, c8] = (sub * 4 + r) * 512 + rank * 128 + p
            idx3[:, c8] = (sub * 4 + r) * 512 + rank * 128 + p
        m["idx2"] = idx2
        m["idx3"] = idx3
        hs = [slice((2 * r + i) * 128, (2 * r + i + 1) * 128) for i in range(2)]
        m["hwq"] = np.ascontiguousarray(np.stack([hw[:, 0:D][:, s] for s in hs]))
        m["hwf"] = np.ascontiguousarray(np.stack([hw[:, D:2 * D][:, s] for s in hs]))
        m["hwv"] = np.ascontiguousarray(np.stack([hw[:, 2 * D:3 * D][:, s] for s in hs]))
        m["hbfc"] = np.stack([inp["hg_b_f"][0][s][:, None] for s in hs]).astype(f32)
        m["l0c"] = np.stack([inp["hg_lb_logits"][0][s][:, None] for s in hs]).astype(f32)
        m["l1c"] = np.stack([inp["hg_lb_logits"][1][s][:, None] for s in hs]).astype(f32)
        maps.append(m)
    return maps


def kernel_fused(inp, ntx=NTX, cap=CAP, stop=None):
    inp = {k: np.asarray(v) for k, v in inp.items()}
    nc = _prog(("F", ntx, cap, stop), lambda: build_fused(ntx, cap, stop))
    maps = fused_in_maps(inp, ntx)
    if stop is not None:
        drop = []
        for l in range(2):
            if stop in ("T1", "H1p", "H1a") or (stop in ("T2", "H2") and l == 1):
                drop += [k + str(l) for k in ("mnorm", "w_r", "b_r", "w_up", "w_dn")]
        maps = [{k: v for k, v in m.items() if k not in drop} for m in maps]
    res = _run(nc, maps)
    if stop is not None:
        return res
    TX = ntx * 128
    out = np.zeros((2, 4 * TX, D), np.float32)
    for c in range(NCORES):
        out[c // 4, (c % 4) * TX:(c % 4 + 1) * TX] = np.asarray(res[c]["out"])
    return out


def kernel(**inp):
    return kernel_fused(inp, NTX, CAP)
```

```python
import contextlib
import numpy as np
import ml_dtypes
import concourse.bass as bass
import concourse.mybir as mybir
from concourse.bass_utils import run_bass_kernel_spmd

F32 = mybir.dt.float32
BF16 = mybir.dt.bfloat16
I32 = mybir.dt.int32
U32 = mybir.dt.uint32
AF = mybir.ActivationFunctionType
ALU = mybir.AluOpType
AX = mybir.AxisListType
NPBF = ml_dtypes.bfloat16

D = 1024
NCORES = 8
SEQ = 16384
NMETA = 16
BLK = 128
EPS = 1e-6
FH = 16
FD = 64
HH = 8
NE = 32
CAP = 384
DE = 512
NTX = 32
NT = NTX + 1
LP = SEQ + BLK
NB = LP // BLK


class Dep:
    __slots__ = ("w", "r", "name", "ro")

    def __init__(self, name=""):
        self.w = None
        self.r = []
        self.name = name
        self.ro = False


class _E:
    def __init__(self, name, eng, sem):
        self.name = name
        self.eng = eng
        self.sem = sem
        self.n = 0
        self.waited = {}
        self.pool = []
        self.pool_i = 0


class KB:
    def __init__(self, nc, dma_pool=(("sp", 16), ("pool", 16), ("act", 4)), same_eng_sync=True):
        self.nc = nc
        self.stacks = [contextlib.ExitStack()]
        self.same = same_eng_sync
        self.e = {}
        for name, eng in (("pe", nc.tensor), ("dve", nc.vector), ("act", nc.scalar),
                          ("pool", nc.gpsimd), ("sp", nc.sync)):
            sem = self.stacks[0].enter_context(nc.semaphore("c_" + name))
            self.e[name] = _E(name, eng, sem)
        for q, n in dma_pool:
            for i in range(n):
                sem = self.stacks[0].enter_context(nc.semaphore("d_%s%d" % (q, i)))
                self.e[q].pool.append([sem, 0])
        self.ninst = 0
        self.uid = 0

    def sb(self, name, shape, dt):
        self.uid += 1
        return self.stacks[-1].enter_context(self.nc.sbuf_tensor("%s_%d" % (name, self.uid), list(shape), dt))

    def ps(self, name, shape, dt):
        self.uid += 1
        return self.stacks[-1].enter_context(self.nc.psum_tensor("%s_%d" % (name, self.uid), list(shape), dt))

    def ring(self, name, shape, dt, n, psum=False):
        out = []
        for i in range(n):
            t = (self.ps if psum else self.sb)("%s%d" % (name, i), shape, dt)
            out.append((t, Dep("%s%d" % (name, i))))
        return out

    @contextlib.contextmanager
    def scope(self):
        self.stacks.append(contextlib.ExitStack())
        try:
            yield
        finally:
            self.barrier()
            self.stacks.pop().close()

    def _wait(self, E, ev, own_ok=False):
        if ev is None:
            return
        sem, val = ev
        if sem is E.sem and not own_ok:
            return
        k = id(sem)
        if E.waited.get(k, 0) >= val:
            return
        E.eng.wait_ge(sem, val)
        E.waited[k] = val

    def _deps(self, E, R, W):
        same = self.same and E.name != "pe"
        for d in R:
            self._wait(E, d.w, own_ok=same)
        for d in W:
            self._wait(E, d.w, own_ok=same)
            for ev in d.r:
                self._wait(E, ev, own_ok=False)

    def _record(self, ev, R, W):
        for d in R:
            if not d.ro:
                for i, (sem, val) in enumerate(d.r):
                    if sem is ev[0]:
                        if ev[1] > val:
                            d.r[i] = ev
                        break
                else:
                    d.r.append(ev)
        for d in W:
            d.w = ev
            d.r = []

    def op(self, en, fn, R=(), W=()):
        E = self.e[en]
        self._deps(E, R, W)
        ins = fn(E.eng)
        E.n += 1
        ins.then_inc(E.sem, 1)
        ev = (E.sem, E.n)
        self._record(ev, R, W)
        self.ninst += 1
        return ev

    def _dma_issue(self, E, issue, R, W):
        self._deps(E, R, W)
        slot = E.pool[E.pool_i % len(E.pool)]
        E.pool_i += 1
        sem, cur = slot
        if cur:
            self._wait(E, (sem, cur))
        ins = issue(E.eng)
        ins.then_inc(sem, 16)
        slot[1] = cur + 16
        ev = (sem, cur + 16)
        self._record(ev, R, W)
        self.ninst += 1
        return ev

    def dma(self, q, out, in_, R=(), W=(), **kw):
        return self._dma_issue(self.e[q], lambda e: e.dma_start(out=out, in_=in_, **kw), R, W)

    def idma(self, out, out_off, in_, in_off, R=(), W=(), **kw):
        return self._dma_issue(
            self.e["pool"],
            lambda e: e.indirect_dma_start(out=out, out_offset=out_off, in_=in_, in_offset=in_off, **kw),
            R, W)

    def cc(self, in_ap, out_ap, groups, R=(), W=()):
        E = self.e["pool"]
        self._deps(E, R, W)
        if not hasattr(self, "ccsem"):
            self.ccsem = self.stacks[0].enter_context(self.nc.semaphore("ccsem"))
            self.ccn = 0
        ins = E.eng.collective_compute("AllGather", ALU.bypass, replica_groups=groups, ins=[in_ap], outs=[out_ap], dma_qos="P2")
        ins.then_inc(self.ccsem, 1)
        self.ccn += 1
        ev = (self.ccsem, self.ccn)
        self._record(ev, R, W)
        self.ninst += 1
        return ev

    def barrier(self):
        evs = [(E.sem, E.n) for E in self.e.values() if E.n]
        for q in ("sp", "pool", "act"):
            for sem, cur in self.e[q].pool:
                if cur:
                    evs.append((sem, cur))
        if getattr(self, "ccn", 0):
            evs.append((self.ccsem, self.ccn))
        for E in self.e.values():
            for ev in evs:
                self._wait(E, ev, own_ok=False)

    def finish(self):
        self.barrier()
        self.e["sp"].eng.nop()
        while self.stacks:
            self.stacks.pop().close()


def make_ident(kb, n=128):
    identf = kb.sb("identf", [128, 128], F32)
    ident = kb.sb("ident", [128, 128], BF16)
    d1, d2 = Dep("identf"), Dep("ident")
    kb.op("pool", lambda e: e.memset(identf[:], 0.0), W=[d1])
    kb.op("pool", lambda e: e.affine_select(out=identf[:], in_=identf[:], pattern=[[-1, 128]],
                                            compare_op=ALU.not_equal, fill=1.0, base=0,
                                            channel_multiplier=1), R=[d1], W=[d1])
    kb.op("dve", lambda e: e.tensor_copy(out=ident[:], in_=identf[:]), R=[d1], W=[d2])
    d1.ro = True
    d2.ro = True
    return identf, d1, ident, d2


def load_bc(kb, name, src_ap, n, q="sp"):
    t = kb.sb(name, [128, n], F32)
    d = Dep(name)
    kb.dma(q, t[:], src_ap.to_broadcast([128, n]), W=[d])
    d.ro = True
    return t, d


def load_w(kb, name, w_ap, kin, nout, dt=BF16):
    kc = kin // 128
    t = kb.sb(name, [128, kc, nout], dt)
    d = Dep(name)
    wv = w_ap.rearrange("(c p) n -> p c n", p=128)
    for c in range(kc):
        kb.dma("pool" if dt != F32 else "sp", t[:, c, :], wv[:, c, :], W=[d])
    return t, d


def rms_rstd(kb, src, d_src, n, scr, d_scr, ss, d_ss):
    kb.op("act", lambda e: e.activation(out=scr, in_=src, func=AF.Square, accum_out=ss),
          R=[d_src], W=[d_scr, d_ss])
    kb.op("act", lambda e: e.activation(out=ss, in_=ss, func=AF.Ln, scale=1.0 / n, bias=EPS),
          R=[d_ss], W=[d_ss])
    kb.op("act", lambda e: e.activation(out=ss, in_=ss, func=AF.Exp, scale=-0.5), R=[d_ss], W=[d_ss])


def transpose_chunks(kb, src, d_src, nchunk, pst, d_pst, dst, d_dst, ident, d_id, copy_eng="dve"):
    for c in range(nchunk):
        kb.op("pe", lambda e: e.transpose(out=pst[:, c, :], in_=src[:, c * 128:(c + 1) * 128], identity=ident[:]),
              R=[d_src, d_id], W=[d_pst])
    if copy_eng == "act":
        kb.op("act", lambda e: e.copy(out=dst[:, 0:nchunk, :], in_=pst[:, 0:nchunk, :]), R=[d_pst], W=[d_dst])
    else:
        kb.op("dve", lambda e: e.tensor_copy(out=dst[:, 0:nchunk, :], in_=pst[:, 0:nchunk, :]), R=[d_pst], W=[d_dst])


def mm_acc(kb, ps_ap, d_ps, xT, d_xT, w, d_w, c0, c1, kc=8):
    for c in range(kc):
        kb.op("pe", lambda e: e.matmul(ps_ap, lhsT=xT[:, c, :], rhs=w[:, c, c0:c1], start=(c == 0), stop=(c == kc - 1)),
              R=[d_xT, d_w], W=[d_ps])


def build_A(nt):
    nc = bass.Bass("TRN2", target_bir_lowering=False)
    rows = nt * 128
    h_in = nc.dram_tensor("h", [rows, D], F32, kind="ExternalInput").ap()
    gain = nc.dram_tensor("gain", [1, D], F32, kind="ExternalInput").ap()
    w_in = nc.dram_tensor("w_in", [D, 3088], F32, kind="ExternalInput").ap()
    gq = nc.dram_tensor("gq", [1, D], F32, kind="ExternalInput").ap()
    gk = nc.dram_tensor("gk", [1, D], F32, kind="ExternalInput").ap()
    bfv = nc.dram_tensor("bf", [1, FH], F32, kind="ExternalInput").ap()
    qo = nc.dram_tensor("qo", [rows, D], BF16, kind="ExternalOutput").ap()
    ko = nc.dram_tensor("ko", [rows, D], BF16, kind="ExternalOutput").ap()
    vo = nc.dram_tensor("vo", [rows, D], BF16, kind="ExternalOutput").ap()
    lfo = nc.dram_tensor("lfo", [rows, FH], F32, kind="ExternalOutput").ap()
    d_out = Dep("out")
    kb = KB(nc)
    identf, d_idf, ident, d_id = make_ident(kb)
    g_t, d_g = load_bc(kb, "gain", gain, D)
    gq_t, d_gq = load_bc(kb, "gq", gq, D)
    gk_t, d_gk = load_bc(kb, "gk", gk, D)
    bf_t, d_bf = load_bc(kb, "bf", bfv, FH)
    w_t, d_w = load_w(kb, "w_in", w_in, D, 3088)
    d_w.ro = True
    xin = kb.ring("xin", [128, D], F32, 2)
    scr = kb.ring("scr", [128, D], F32, 2)
    ssr = kb.ring("ss", [128, 1], F32, 2)
    xnr = kb.ring("xn", [128, D], BF16, 2)
    xTr = kb.ring("xT", [128, 8, 128], BF16, 2)
    pstr = kb.ring("pst", [128, 8, 128], BF16, 2, psum=True)
    pmm = kb.ring("pmm", [128, 512], F32, 6, psum=True)
    qf = kb.ring("qf", [128, D], F32, 2)
    s16 = kb.ring("s16", [128, FH], F32, 2)
    qn = kb.ring("qn", [128, D], BF16, 2)
    kn = kb.ring("kn", [128, D], BF16, 2)
    vb = kb.ring("vb", [128, D], BF16, 2)
    lft = kb.ring("lft", [128, FH], F32, 2)
    pi = 0
    for t in range(nt):
        b = t % 2
        x_t, d_x = xin[b]
        sc_t, d_sc = scr[b]
        ss_t, d_ss = ssr[b]
        xn_t, d_xn = xnr[b]
        xT_t, d_xT = xTr[b]
        ps_t, d_pst = pstr[b]
        kb.dma("sp", x_t[:], h_in[t * 128:(t + 1) * 128, :], W=[d_x])
        rms_rstd(kb, x_t[:], d_x, D, sc_t[:], d_sc, ss_t[:], d_ss)
        kb.op("dve", lambda e: e.scalar_tensor_tensor(out=xn_t[:], in0=x_t[:], scalar=ss_t[:, 0:1], in1=g_t[:],
                                                       op0=ALU.mult, op1=ALU.mult),
              R=[d_x, d_ss, d_g], W=[d_xn])
        transpose_chunks(kb, xn_t, d_xn, 8, ps_t, d_pst, xT_t, d_xT, ident, d_id)
        for which, (g2_t, d_g2, o_ring, o_dram, scl) in enumerate(
                ((gq_t, d_gq, qn, qo, FD ** -0.5), (gk_t, d_gk, kn, ko, 1.0))):
            qf_t, d_qf = qf[which]
            for half in range(2):
                p_t, d_p = pmm[pi % 6]
                pi += 1
                c0 = which * 1024 + half * 512
                mm_acc(kb, p_t[:], d_p, xT_t, d_xT, w_t, d_w, c0, c0 + 512)
                kb.op("act", lambda e: e.copy(out=qf_t[:, half * 512:(half + 1) * 512], in_=p_t[:]),
                      R=[d_p], W=[d_qf])
            s_t, d_s = s16[which]
            kb.op("pool", lambda e: e.tensor_tensor(out=sc_t[:], in0=qf_t[:], in1=qf_t[:], op=ALU.mult),
                  R=[d_qf], W=[d_sc])
            kb.op("dve", lambda e: e.tensor_reduce(out=s_t[:], in_=sc_t[:].rearrange("p (h d) -> p h d", d=FD),
                                                    axis=AX.X, op=ALU.add), R=[d_sc], W=[d_s])
            kb.op("act", lambda e: e.activation(out=s_t[:], in_=s_t[:], func=AF.Sqrt, scale=1.0 / FD, bias=EPS),
                  R=[d_s], W=[d_s])
            kb.op("dve", lambda e: e.reciprocal(out=s_t[:], in_=s_t[:]), R=[d_s], W=[d_s])
            kb.op("dve", lambda e: e.tensor_tensor(
                out=sc_t[:].rearrange("p (h d) -> p h d", d=FD), in0=qf_t[:].rearrange("p (h d) -> p h d", d=FD),
                in1=s_t[:].unsqueeze(2).to_broadcast([128, FH, FD]), op=ALU.mult), R=[d_qf, d_s], W=[d_sc])
            o_t, d_o = o_ring[b]
            kb.op("dve", lambda e: e.scalar_tensor_tensor(out=o_t[:], in0=sc_t[:], scalar=float(scl), in1=g2_t[:],
                                                           op0=ALU.mult, op1=ALU.mult),
                  R=[d_sc, d_g2], W=[d_o])
            kb.dma("sp", o_dram[t * 128:(t + 1) * 128, :], o_t[:], R=[d_o])
        v_t, d_v = vb[b]
        for half in range(2):
            p_t, d_p = pmm[pi % 6]
            pi += 1
            c0 = 2048 + half * 512
            mm_acc(kb, p_t[:], d_p, xT_t, d_xT, w_t, d_w, c0, c0 + 512)
            kb.op("act", lambda e: e.copy(out=v_t[:, half * 512:(half + 1) * 512], in_=p_t[:]), R=[d_p], W=[d_v])
        kb.dma("sp", vo[t * 128:(t + 1) * 128, :], v_t[:], R=[d_v])
        p_t, d_p = pmm[pi % 6]
        pi += 1
        mm_acc(kb, p_t[:, 0:FH], d_p, xT_t, d_xT, w_t, d_w, 3072, 3088)
        l_t, d_l = lft[b]
        kb.op("dve", lambda e: e.tensor_tensor(out=l_t[:], in0=p_t[:, 0:FH], in1=bf_t[:], op=ALU.add),
              R=[d_p, d_bf], W=[d_l])
        kb.op("act", lambda e: e.activation(out=l_t[:], in_=l_t[:], func=AF.Exp, scale=-1.0), R=[d_l], W=[d_l])
        kb.op("act", lambda e: e.activation(out=l_t[:], in_=l_t[:], func=AF.Ln, bias=1.0), R=[d_l], W=[d_l])
        kb.op("dve", lambda e: e.tensor_scalar(out=l_t[:], in0=l_t[:], scalar1=-1.0, scalar2=None, op0=ALU.mult),
              R=[d_l], W=[d_l])
        kb.dma("sp", lfo[t * 128:(t + 1) * 128, :], l_t[:], R=[d_l])
    kb.finish()
    return nc


def build_B(nb, nbh):
    assert (nb - 1) % 4 == 0
    L = nb * 128
    nI = (nb - 1) // 4 + 1
    nc = bass.Bass("TRN2", target_bir_lowering=False)
    qT = nc.dram_tensor("qT", [nbh, FD, L], BF16, kind="ExternalInput").ap()
    kT = nc.dram_tensor("kT", [nbh, FD, L], BF16, kind="ExternalInput").ap()
    vv = nc.dram_tensor("v", [nbh, L, FD], BF16, kind="ExternalInput").ap()
    lfr = nc.dram_tensor("lfr", [nbh, L], F32, kind="ExternalInput").ap()
    lfT = nc.dram_tensor("lfT", [nbh, 128, nb], F32, kind="ExternalInput").ap()
    oT = nc.dram_tensor("oT", [nbh, FD + 1, L], F32, kind="ExternalOutput").ap()
    kb = KB(nc)
    trif = kb.sb("trif", [128, 128], F32); d_tri = Dep("trif")
    onesf = kb.sb("onesf", [128, 128], F32); d_ones = Dep("onesf")
    sel0 = kb.sb("sel0", [128, 128], F32); d_sel = Dep("sel0")
    maskb = kb.sb("maskb", [128, 128], BF16); d_mask = Dep("maskb")
    onerow = kb.sb("onerow", [128, nb], F32); d_or = Dep("onerow")
    kb.op("pool", lambda e: e.memset(onesf[:], 1.0), W=[d_ones])
    kb.op("pool", lambda e: e.memset(onerow[:], 1.0), W=[d_or])
    kb.op("pool", lambda e: e.affine_select(out=trif[:], in_=onesf[:], pattern=[[1, 128]], compare_op=ALU.is_ge,
                                            fill=0.0, base=0, channel_multiplier=-1), R=[d_ones], W=[d_tri])
    kb.op("pool", lambda e: e.affine_select(out=sel0[:], in_=onesf[:], pattern=[[0, 128]], compare_op=ALU.is_ge,
                                            fill=0.0, base=0, channel_multiplier=-1), R=[d_ones], W=[d_sel])
    kb.op("dve", lambda e: e.tensor_copy(out=maskb[:], in_=trif[:]), R=[d_tri], W=[d_mask])
    for d in (d_tri, d_ones, d_sel, d_mask, d_or):
        d.ro = True
    crow = kb.sb("crow", [nbh, L], F32); d_crow = Dep("crow")
    drow = kb.sb("drow", [nbh, L], BF16); d_drow = Dep("drow")
    kb.dma("sp", crow[:], lfr, W=[d_crow])
    kb.op("dve", lambda e: e.tensor_tensor_scan(out=crow[:], data0=onerow[0:nbh, 0:1].to_broadcast([nbh, L]),
                                                 data1=crow[:], initial=0.0, op0=ALU.mult, op1=ALU.add),
          R=[d_crow, d_or], W=[d_crow])
    kb.op("dve", lambda e: e.tensor_scalar(out=drow[:, 0:128], in0=crow[:, 0:128], scalar1=crow[:, 0:1], scalar2=None,
                                            op0=ALU.subtract), R=[d_crow], W=[d_drow])
    if nI > 1:
        kb.op("dve", lambda e: e.tensor_tensor(
            out=drow[:, 128:L].rearrange("p (i c) -> p i c", c=512),
            in0=crow[:, 128:L].rearrange("p (i c) -> p i c", c=512),
            in1=crow[:, 128:L].rearrange("p (i c) -> p i c", c=512)[:, :, 0:1].to_broadcast([nbh, nI - 1, 512]),
            op=ALU.subtract), R=[d_crow], W=[d_drow])
    QA = kb.sb("QA", [FD + 1, L], BF16); d_QA = Dep("QA")
    KA = kb.sb("KA", [FD + 1, L], BF16); d_KA = Dep("KA")
    VA = kb.sb("VA", [128, nb, FD + 1], BF16); d_VA = Dep("VA")
    lft = kb.sb("lft", [128, nb], F32); d_lft = Dep("lft")
    ct = kb.sb("ct", [128, nb], F32); d_ct = Dep("ct")
    tot = kb.sb("tot", [128, nb], F32); d_tot = Dep("tot")
    rall = kb.sb("rall", [128, nb], F32); d_rall = Dep("rall")
    biasr = kb.ring("bias", [128, nb], F32, 2)
    pr = kb.ring("P", [128, 512], BF16, 4)
    osb = kb.ring("osb", [FD + 1, 512], F32, 2)
    psS = kb.ring("psS", [128, 512], F32, 4, psum=True)
    psO = kb.ring("psO", [128, 512], F32, 2, psum=True)
    psM = kb.ring("psM", [128, 512], F32, 2, psum=True)
    si = 0
    oi = 0
    for bh in range(nbh):
        kb.dma("sp", QA[0:FD, :], qT[bh], W=[d_QA])
        kb.dma("sp", QA[FD:FD + 1, :], drow[bh:bh + 1, :], R=[d_drow], W=[d_QA])
        kb.dma("sp", KA[0:FD, :], kT[bh], W=[d_KA])
        kb.op("pool", lambda e: e.memset(KA[FD:FD + 1, :], 1.0), W=[d_KA])
        kb.dma("sp", VA[:, :, 0:FD], vv[bh].rearrange("(j p) d -> p j d", p=128), W=[d_VA])
        kb.op("pool", lambda e: e.memset(VA[:, :, FD:FD + 1], 1.0), W=[d_VA])
        kb.op("pool", lambda e: e.memset(VA[0:112, 0, :], 0.0), W=[d_VA])
        kb.dma("sp", lft[:], lfT[bh], W=[d_lft])
        pm0, d_pm0 = psM[0]
        pm1, d_pm1 = psM[1]
        kb.op("pe", lambda e: e.matmul(pm0[:, 0:nb], lhsT=trif[:], rhs=lft[:], start=True, stop=True),
              R=[d_tri, d_lft], W=[d_pm0])
        kb.op("pe", lambda e: e.matmul(pm1[:, 0:nb], lhsT=onesf[:], rhs=lft[:], start=True, stop=True),
              R=[d_ones, d_lft], W=[d_pm1])
        kb.op("dve", lambda e: e.tensor_copy(out=tot[:], in_=pm1[:, 0:nb]), R=[d_pm1], W=[d_tot])
        kb.op("dve", lambda e: e.tensor_tensor_scan(out=ct[:], data0=onerow[:], data1=tot[:], initial=0.0,
                                                     op0=ALU.mult, op1=ALU.add), R=[d_tot, d_or], W=[d_ct])
        kb.op("dve", lambda e: e.tensor_tensor(out=ct[:], in0=ct[:], in1=tot[:], op=ALU.subtract),
              R=[d_ct, d_tot], W=[d_ct])
        kb.op("dve", lambda e: e.tensor_tensor(out=ct[:], in0=ct[:], in1=pm0[:, 0:nb], op=ALU.add),
              R=[d_ct, d_pm0], W=[d_ct])
        kb.op("pe", lambda e: e.matmul(pm1[:, 0:nb], lhsT=sel0[:], rhs=ct[:], start=True, stop=True),
              R=[d_sel, d_ct], W=[d_pm1])
        kb.op("dve", lambda e: e.tensor_copy(out=rall[:], in_=pm1[:, 0:nb]), R=[d_pm1], W=[d_rall])
        steps = []
        for I in range(nI):
            j0 = 0 if I == 0 else 4 * I - 3
            nblk = 1 if I == 0 else 4
            nJ = j0 + nblk
            for J in range(nJ):
                steps.append((I, J, j0, nblk, nJ))
        LA = 2
        cur = {}
        for idx in range(len(steps) + LA):
            if idx < len(steps):
                I, J, j0, nblk, nJ = steps[idx]
                q0 = j0 * 128
                bias_t, d_bias = biasr[I % 2]
                if J == 0:
                    kb.op("dve", lambda e: e.tensor_scalar(out=bias_t[:, 0:nJ], in0=ct[:, 0:nJ], scalar1=-1.0,
                                                            scalar2=rall[:, j0:j0 + 1], op0=ALU.mult, op1=ALU.add),
                          R=[d_ct, d_rall], W=[d_bias])
                m = max(0, J - j0)
                c0 = m * 128
                c1 = nblk * 128
                ps, d_ps = psS[idx % 4]
                p_t, d_p = pr[idx % 4]
                kb.op("pe", lambda e: e.matmul(ps[:, c0:c1], lhsT=KA[:, J * 128:(J + 1) * 128],
                                               rhs=QA[:, q0 + c0:q0 + c1], start=True, stop=True),
                      R=[d_KA, d_QA], W=[d_ps])
                kb.op("act", lambda e: e.activation(out=p_t[:, c0:c1], in_=ps[:, c0:c1], func=AF.Exp,
                                                    bias=bias_t[:, J:J + 1], scale=1.0),
                      R=[d_ps, d_bias], W=[d_p])
                if J >= j0:
                    kb.op("dve", lambda e: e.tensor_tensor(out=p_t[:, c0:c0 + 128], in0=p_t[:, c0:c0 + 128],
                                                            in1=maskb[:], op=ALU.mult), R=[d_p, d_mask], W=[d_p])
            if idx >= LA:
                I, J, j0, nblk, nJ = steps[idx - LA]
                q0 = j0 * 128
                m = max(0, J - j0)
                c0 = m * 128
                c1 = nblk * 128
                p_t, d_p = pr[(idx - LA) % 4]
                po, d_po = psO[I % 2]
                kb.op("pe", lambda e: e.matmul(po[0:FD + 1, c0:c1], lhsT=VA[:, J, :], rhs=p_t[:, c0:c1],
                                               start=(J == 0), stop=(J == nJ - 1)),
                      R=[d_VA, d_p], W=[d_po])
                if J == nJ - 1:
                    o_t, d_o = osb[I % 2]
                    ncol = nblk * 128
                    kb.op("dve", lambda e: e.tensor_copy(out=o_t[:, 0:ncol], in_=po[0:FD + 1, 0:ncol]),
                          R=[d_po], W=[d_o])
                    kb.dma("sp", oT[bh, :, q0:q0 + ncol], o_t[:, 0:ncol], R=[d_o])
    kb.finish()
    return nc


BIGIDX = 1.0e6


def build_CE(stage, nt, has_meta, cap=CAP):
    nc = bass.Bass("TRN2", target_bir_lowering=False)
    rows = nt * 128
    nslot = NE * cap
    nst = cap // 128
    a_in = nc.dram_tensor("a_in", [rows, D], F32, kind="ExternalInput").ap()
    hp_in = nc.dram_tensor("hp_in", [rows, D], F32, kind="ExternalInput").ap()
    if stage == "C":
        den_in = nc.dram_tensor("den_in", [rows, FH], F32, kind="ExternalInput").ap()
    else:
        gs_in = nc.dram_tensor("gs_in", [rows, D], BF16, kind="ExternalInput").ap()
    valid_in = nc.dram_tensor("valid_in", [rows, 1], F32, kind="ExternalInput").ap()
    w_out = nc.dram_tensor("w_out", [D, D], F32, kind="ExternalInput").ap()
    mnorm = nc.dram_tensor("mnorm", [1, D], F32, kind="ExternalInput").ap()
    w_r = nc.dram_tensor("w_r", [D, 36], F32, kind="ExternalInput").ap()
    b_r = nc.dram_tensor("b_r", [1, 36], F32, kind="ExternalInput").ap()
    w_up = nc.dram_tensor("w_up", [NE, D, 2 * DE], F32, kind="ExternalInput").ap()
    w_dn = nc.dram_tensor("w_dn", [NE, DE, D], F32, kind="ExternalInput").ap()
    h2_out = nc.dram_tensor("h2_out", [rows, D], F32, kind="ExternalOutput").ap()
    if stage == "C":
        hnorm = nc.dram_tensor("hnorm", [1, D], F32, kind="ExternalInput").ap()
        hw_in = nc.dram_tensor("hw_in", [D, 4 * D], F32, kind="ExternalInput").ap()
        hbf = nc.dram_tensor("hbf", [1, D], F32, kind="ExternalInput").ap()
        lbl = nc.dram_tensor("lbl", [2, D], F32, kind="ExternalInput").ap()
        q1_out = nc.dram_tensor("q1_out", [rows, D], BF16, kind="ExternalOutput").ap()
        lf1_out = nc.dram_tensor("lf1_out", [rows, D], F32, kind="ExternalOutput").ap()
        v1_out = nc.dram_tensor("v1_out", [rows, D], BF16, kind="ExternalOutput").ap()
        gs_out = nc.dram_tensor("gs_out", [rows, D], BF16, kind="ExternalOutput").ap()
    xpad = nc.dram_tensor("xpad", [nslot, D], BF16, kind="Internal").ap()
    ypad = nc.dram_tensor("ypad", [nslot, D], BF16, kind="Internal").ap()
    h1s = nc.dram_tensor("h1s", [rows, D], F32, kind="Internal").ap()
    d_xpad, d_ypad, d_h1s = Dep("xpad"), Dep("ypad"), Dep("h1s")

    kb = KB(nc)
    identf, d_idf, ident, d_id = make_ident(kb)
    breg = nc.gpsimd.to_reg(nslot - 1)
    dest_i = kb.sb("dest_i", [128, nt, 2], I32); d_dest = Dep("dest_i")
    gw = kb.sb("gw", [128, nt, 2], F32); d_gw = Dep("gw")
    cntb = kb.sb("cntb", [128, NE], F32); d_cntb = Dep("cntb")
    usb = kb.sb("usb", [128, 128], BF16); d_us = Dep("usb")
    onesb = kb.sb("onesb", [128, 128], BF16); d_onesb = Dep("onesb")
    onesf = kb.sb("onesf", [128, 128], F32); d_onesf = Dep("onesf")
    tmpf = kb.sb("tmpf", [128, 128], F32); d_tmpf = Dep("tmpf")
    kb.op("pool", lambda e: e.memset(onesf[:], 1.0), W=[d_onesf])
    kb.op("pool", lambda e: e.affine_select(out=tmpf[:], in_=onesf[:], pattern=[[1, 128]], compare_op=ALU.is_gt,
                                            fill=0.0, base=0, channel_multiplier=-1), R=[d_onesf], W=[d_tmpf])
    kb.op("dve", lambda e: e.tensor_copy(out=usb[:], in_=tmpf[:]), R=[d_tmpf], W=[d_us])
    kb.op("dve", lambda e: e.tensor_copy(out=onesb[:], in_=onesf[:]), R=[d_onesf], W=[d_onesb])
    kb.op("pool", lambda e: e.iota(cntb[:], pattern=[[cap, NE]], base=0, channel_multiplier=0,
                                   allow_small_or_imprecise_dtypes=True), W=[d_cntb])
    for d in (d_us, d_onesb, d_onesf):
        d.ro = True
    mn_t, d_mn = load_bc(kb, "mnorm", mnorm, D)
    br_t, d_br = load_bc(kb, "b_r", b_r, 36)
    wr_t, d_wr = load_w(kb, "w_r", w_r, D, 36, dt=F32)
    d_wr.ro = True

    with kb.scope():
        wo_t, d_wo = load_w(kb, "w_out", w_out, D, D)
        d_wo.ro = True
        a_r = kb.ring("a", [128, D], F32, 2)
        hp_r = kb.ring("hp", [128, D], F32, 2)
        den_r = kb.ring("den", [128, FH], F32, 2)
        gs_r = kb.ring("gs", [128, D], BF16, 2)
        ob_r = kb.ring("ob", [128, D], BF16, 2)
        oT_r = kb.ring("oT", [128, 8, 128], BF16, 2)
        h1_r = kb.ring("h1", [128, D], F32, 2)
        scr_r = kb.ring("scr", [128, D], F32, 2)
        ss_r = kb.ring("ss", [128, 1], F32, 2)
        hmf_r = kb.ring("hmf", [128, D], F32, 2)
        hmb_r = kb.ring("hmb", [128, D], BF16, 2)
        hmT_r = kb.ring("hmT", [128, 8, 128], F32, 2)
        sm_r = kb.ring("sm", [128, 256], F32, 2)
        ab_r = kb.ring("ab", [128, NE], BF16, 2)
        val_r = kb.ring("val", [128, 2], F32, 2)
        pst_r = kb.ring("pst", [128, 8, 128], BF16, 2, psum=True)
        pstf_r = kb.ring("pstf", [128, 8, 128], F32, 1, psum=True)
        pmm_r = kb.ring("pmm", [128, 512], F32, 2, psum=True)
        prt_r = kb.ring("prt", [128, 512], F32, 2, psum=True)
        for t in range(nt):
            b = t % 2
            r0, r1 = t * 128, (t + 1) * 128
            a_t, d_a = a_r[b]
            hp_t, d_hp = hp_r[b]
            ob_t, d_ob = ob_r[b]
            kb.dma("sp", a_t[:], a_in[r0:r1, :], W=[d_a])
            kb.dma("sp", hp_t[:], hp_in[r0:r1, :], W=[d_hp])
            if stage == "C":
                den_t, d_den = den_r[b]
                kb.dma("sp", den_t[:], den_in[r0:r1, :], W=[d_den])
                kb.op("dve", lambda e: e.tensor_scalar(out=den_t[:], in0=den_t[:], scalar1=1e-30, scalar2=None,
                                                        op0=ALU.max), R=[d_den], W=[d_den])
                kb.op("dve", lambda e: e.reciprocal(out=den_t[:], in_=den_t[:]), R=[d_den], W=[d_den])
                kb.op("dve", lambda e: e.tensor_tensor(
                    out=ob_t[:].rearrange("p (h d) -> p h d", d=FD), in0=a_t[:].rearrange("p (h d) -> p h d", d=FD),
                    in1=den_t[:].unsqueeze(2).to_broadcast([128, FH, FD]), op=ALU.mult), R=[d_a, d_den], W=[d_ob])
            else:
                gs_t, d_gs = gs_r[b]
                kb.dma("sp", gs_t[:], gs_in[r0:r1, :], W=[d_gs])
                kb.op("dve", lambda e: e.tensor_tensor(out=ob_t[:], in0=a_t[:], in1=gs_t[:], op=ALU.mult),
                      R=[d_a, d_gs], W=[d_ob])
            oT_t, d_oT = oT_r[b]
            ps_t, d_pst = pst_r[b]
            transpose_chunks(kb, ob_t, d_ob, 8, ps_t, d_pst, oT_t, d_oT, ident, d_id, copy_eng="act")
            h1_t, d_h1 = h1_r[b]
            for half in range(2):
                p_t, d_p = pmm_r[half]
                mm_acc(kb, p_t[:], d_p, oT_t, d_oT, wo_t, d_wo, half * 512, half * 512 + 512)
                kb.op("dve", lambda e: e.tensor_tensor(out=h1_t[:, half * 512:(half + 1) * 512], in0=p_t[:],
                                                        in1=hp_t[:, half * 512:(half + 1) * 512], op=ALU.add),
                      R=[d_p, d_hp], W=[d_h1])
            kb.dma("sp", h1s[r0:r1, :], h1_t[:], R=[d_h1], W=[d_h1s])
            sc_t, d_sc = scr_r[b]
            ss_t, d_ss = ss_r[b]
            hmf_t, d_hmf = hmf_r[b]
            hmb_t, d_hmb = hmb_r[b]
            rms_rstd(kb, h1_t[:], d_h1, D, sc_t[:], d_sc, ss_t[:], d_ss)
            kb.op("dve", lambda e: e.scalar_tensor_tensor(out=hmf_t[:], in0=h1_t[:], scalar=ss_t[:, 0:1], in1=mn_t[:],
                                                           op0=ALU.mult, op1=ALU.mult),
                  R=[d_h1, d_ss, d_mn], W=[d_hmf])
            kb.op("pool", lambda e: e.tensor_copy(out=hmb_t[:], in_=hmf_t[:]), R=[d_hmf], W=[d_hmb])
            pf_t, d_pf = pstf_r[0]
            hmT_t, d_hmT = hmT_r[b]
            for c in range(8):
                kb.op("pe", lambda e: e.transpose(out=pf_t[:, c, :], in_=hmf_t[:, c * 128:(c + 1) * 128],
                                                  identity=identf[:]), R=[d_hmf, d_idf], W=[d_pf])
            kb.op("act", lambda e: e.copy(out=hmT_t[:], in_=pf_t[:]), R=[d_pf], W=[d_hmT])
            pr_t, d_pr = prt_r[b]
            for c in range(8):
                kb.op("pe", lambda e: e.matmul(pr_t[:, 0:36], lhsT=hmT_t[:, c, :], rhs=wr_t[:, c, :],
                                               start=(c == 0), stop=(c == 7)), R=[d_hmT, d_wr], W=[d_pr])
            sm, d_sm = sm_r[b]
            lg = sm[:, 0:36]
            gmax = sm[:, 36:37]
            ngmax = sm[:, 37:38]
            gsum = sm[:, 38:39]
            ggate = sm[:, 39:40]
            eg = sm[:, 40:44]
            ohg = sm[:, 44:48]
            esel = sm[:, 48:56]
            top8 = sm[:, 56:64]
            oh0 = sm[:, 64:72]
            oh1 = sm[:, 72:80]
            A0 = sm[:, 80:112]
            A1 = sm[:, 112:144]
            sbt = sm[:, 144:176]
            tmp = sm[:, 176:208]
            destf = sm[:, 208:210]
            diff = sm[:, 210:211]
            sg = sm[:, 211:212]
            S = [d_sm]
            dv = lambda fn, R=(), W=(): kb.op("dve", fn, R=list(R) + S, W=list(W) + S)
            dv(lambda e: e.tensor_tensor(out=lg, in0=pr_t[:, 0:36], in1=br_t[:], op=ALU.add), R=[d_pr, d_br])
            dv(lambda e: e.tensor_reduce(out=gmax, in_=lg[:, 0:4], axis=AX.X, op=ALU.max))
            dv(lambda e: e.tensor_scalar(out=ngmax, in0=gmax, scalar1=-1.0, scalar2=None, op0=ALU.mult))
            kb.op("act", lambda e: e.activation(out=eg, in_=lg[:, 0:4], func=AF.Exp, bias=ngmax, scale=1.0,
                                                accum_out=gsum), R=S, W=S)
            dv(lambda e: e.reciprocal(out=ggate, in_=gsum))
            dv(lambda e: e.tensor_scalar(out=ohg, in0=lg[:, 0:4], scalar1=gmax, scalar2=None, op0=ALU.is_equal))
            dv(lambda e: e.tensor_scalar(out=esel, in0=lg[:, 4:12], scalar1=ohg[:, 0:1], scalar2=None, op0=ALU.mult))
            for g in range(1, 4):
                dv(lambda e: e.scalar_tensor_tensor(out=esel, in0=lg[:, 4 + 8 * g:12 + 8 * g], scalar=ohg[:, g:g + 1],
                                                    in1=esel, op0=ALU.mult, op1=ALU.add))
            dv(lambda e: e.max(out=top8, in_=esel))
            dv(lambda e: e.tensor_scalar(out=oh0, in0=esel, scalar1=top8[:, 0:1], scalar2=None, op0=ALU.is_equal))
            dv(lambda e: e.tensor_scalar(out=oh1, in0=esel, scalar1=top8[:, 1:2], scalar2=None, op0=ALU.is_equal))
            for (Ak, ohk) in ((A0, oh0), (A1, oh1)):
                dv(lambda e: e.tensor_tensor(out=Ak.rearrange("p (g j) -> p g j", j=8),
                                             in0=ohg.unsqueeze(2).to_broadcast([128, 4, 8]),
                                             in1=ohk.unsqueeze(1).to_broadcast([128, 4, 8]), op=ALU.mult))
            use_valid = has_meta and t == 0
            if use_valid:
                val_t, d_val = val_r[b]
                kb.dma("sp", val_t[:, 0:1], valid_in[r0:r1, :], W=[d_val])
                kb.op("dve", lambda e: e.tensor_scalar(out=val_t[:, 1:2], in0=val_t[:, 0:1], scalar1=-BIGIDX,
                                                        scalar2=BIGIDX, op0=ALU.mult, op1=ALU.add),
                      R=[d_val], W=[d_val])
                for Ak in (A0, A1):
                    dv(lambda e: e.tensor_scalar(out=Ak, in0=Ak, scalar1=val_t[:, 0:1], scalar2=None, op0=ALU.mult),
                       R=[d_val])
            ab_t, d_ab = ab_r[b]
            kb.op("dve", lambda e: e.tensor_tensor(out=ab_t[:], in0=A0, in1=A1, op=ALU.add), R=S, W=[d_ab])
            kb.op("pe", lambda e: e.matmul(pr_t[:, 64:96], lhsT=usb[:], rhs=ab_t[:], start=True, stop=True),
                  R=[d_us, d_ab], W=[d_pr])
            kb.op("pe", lambda e: e.matmul(pr_t[:, 96:128], lhsT=onesb[:], rhs=ab_t[:], start=True, stop=True),
                  R=[d_onesb, d_ab], W=[d_pr])
            dv(lambda e: e.tensor_tensor(out=sbt, in0=pr_t[:, 64:96], in1=cntb[:], op=ALU.add), R=[d_pr, d_cntb])
            kb.op("dve", lambda e: e.tensor_tensor(out=cntb[:], in0=cntb[:], in1=pr_t[:, 96:128], op=ALU.add),
                  R=[d_pr, d_cntb] + S, W=[d_cntb])
            for k, Ak in enumerate((A0, A1)):
                dv(lambda e: e.tensor_tensor(out=tmp, in0=Ak, in1=sbt, op=ALU.mult))
                dv(lambda e: e.tensor_reduce(out=destf[:, k:k + 1], in_=tmp, axis=AX.X, op=ALU.add))
            if use_valid:
                dv(lambda e: e.tensor_scalar(out=destf, in0=destf, scalar1=val_t[:, 0:1], scalar2=val_t[:, 1:2],
                                             op0=ALU.mult, op1=ALU.add), R=[d_val])
            kb.op("dve", lambda e: e.tensor_copy(out=dest_i[:, t, :], in_=destf), R=S, W=[d_dest])
            dv(lambda e: e.tensor_tensor(out=diff, in0=top8[:, 0:1], in1=top8[:, 1:2], op=ALU.subtract))
            kb.op("act", lambda e: e.activation(out=sg, in_=diff, func=AF.Sigmoid), R=S, W=S)
            kb.op("dve", lambda e: e.tensor_tensor(out=gw[:, t, 0:1], in0=sg, in1=ggate, op=ALU.mult), R=S, W=[d_gw])
            kb.op("dve", lambda e: e.tensor_tensor(out=gw[:, t, 1:2], in0=ggate, in1=gw[:, t, 0:1], op=ALU.subtract),
                  R=S + [d_gw], W=[d_gw])
            for k in range(2):
                kb.idma(out=xpad[:, :], out_off=bass.IndirectOffsetOnAxis(ap=dest_i[:, t, k:k + 1], axis=0),
                        in_=hmb_t[:], in_off=None, R=[d_hmb, d_dest], W=[d_xpad],
                        bounds_check=breg, oob_is_err=False)

    with kb.scope():
        wup_r = kb.ring("wup", [128, 8, 2 * DE], BF16, 2)
        wdn_r = kb.ring("wdn", [128, 4, D], BF16, 2)
        xs_r = kb.ring("xs", [128, D], BF16, 3)
        xT_r = kb.ring("xT", [128, 8, cap], BF16, 2)
        sa_r = kb.ring("sa", [128, cap], F32, 2)
        aT_r = kb.ring("aT", [128, 4, cap], BF16, 2)
        yb_r = kb.ring("yb", [128, D], BF16, 2)
        pst_r = kb.ring("pst", [128, 8, 128], BF16, 2, psum=True)
        pau_r = kb.ring("pau", [128, 512], F32, 4, psum=True)
        py_r = kb.ring("py", [128, 512], F32, 2, psum=True)
        xi = 0
        yi = 0
        for ex in range(NE):
            b = ex % 2
            wup_t, d_wup = wup_r[b]
            wdn_t, d_wdn = wdn_r[b]
            wuv = w_up[ex].rearrange("(c p) n -> p c n", p=128)
            wdv = w_dn[ex].rearrange("(c p) n -> p c n", p=128)
            for c in range(8):
                kb.dma("pool", wup_t[:, c, :], wuv[:, c, :], W=[d_wup])
            for c in range(4):
                kb.dma("pool", wdn_t[:, c, :], wdv[:, c, :], W=[d_wdn])
            xT_t, d_xT = xT_r[b]
            for st in range(nst):
                xs_t, d_xs = xs_r[xi % 3]
                ps_t, d_pst = pst_r[xi % 2]
                xi += 1
                s0 = ex * cap + st * 128
                kb.dma("sp", xs_t[:], xpad[s0:s0 + 128, :], R=[d_xpad], W=[d_xs])
                for c in range(8):
                    kb.op("pe", lambda e: e.transpose(out=ps_t[:, c, :], in_=xs_t[:, c * 128:(c + 1) * 128],
                                                      identity=ident[:]), R=[d_xs, d_id], W=[d_pst])
                kb.op("act" if st % 2 else "dve",
                      (lambda e: e.copy(out=xT_t[:, :, st * 128:(st + 1) * 128], in_=ps_t[:])) if st % 2 else
                      (lambda e: e.tensor_copy(out=xT_t[:, :, st * 128:(st + 1) * 128], in_=ps_t[:])),
                      R=[d_pst], W=[d_xT])
            aT_t, d_aT = aT_r[b]
            for fc in range(4):
                pa, d_pa = pau_r[(2 * fc) % 4]
                pu, d_pu = pau_r[(2 * fc + 1) % 4]
                for c in range(8):
                    kb.op("pe", lambda e: e.matmul(pa[:, 0:cap], lhsT=wup_t[:, c, fc * 128:(fc + 1) * 128],
                                                   rhs=xT_t[:, c, :], start=(c == 0), stop=(c == 7)),
                          R=[d_wup, d_xT], W=[d_pa])
                for c in range(8):
                    kb.op("pe", lambda e: e.matmul(pu[:, 0:cap], lhsT=wup_t[:, c, DE + fc * 128:DE + (fc + 1) * 128],
                                                   rhs=xT_t[:, c, :], start=(c == 0), stop=(c == 7)),
                          R=[d_wup, d_xT], W=[d_pu])
                sa_t, d_sa = sa_r[fc % 2]
                kb.op("act", lambda e: e.activation(out=sa_t[:], in_=pa[:, 0:cap], func=AF.Silu), R=[d_pa], W=[d_sa])
                kb.op("dve", lambda e: e.tensor_tensor(out=aT_t[:, fc, :], in0=sa_t[:], in1=pu[:, 0:cap], op=ALU.mult),
                      R=[d_sa, d_pu], W=[d_aT])
            for st in range(nst):
                yb_t, d_yb = yb_r[yi % 2]
                yi += 1
                for half in range(2):
                    py, d_py = py_r[half]
                    for fc in range(4):
                        kb.op("pe", lambda e: e.matmul(py[:], lhsT=aT_t[:, fc, st * 128:(st + 1) * 128],
                                                       rhs=wdn_t[:, fc, half * 512:(half + 1) * 512],
                                                       start=(fc == 0), stop=(fc == 3)),
                              R=[d_aT, d_wdn], W=[d_py])
                    if half == 0:
                        kb.op("act", lambda e: e.copy(out=yb_t[:, 0:512], in_=py[:]), R=[d_py], W=[d_yb])
                    else:
                        kb.op("dve", lambda e: e.tensor_copy(out=yb_t[:, 512:1024], in_=py[:]), R=[d_py], W=[d_yb])
                s0 = ex * cap + st * 128
                kb.dma("sp", ypad[s0:s0 + 128, :], yb_t[:], R=[d_yb], W=[d_ypad])

    with kb.scope():
        h1_r = kb.ring("h1", [128, D], F32, 2)
        y_r = kb.ring("y", [128, D], BF16, 4)
        h2_r = kb.ring("h2", [128, D], F32, 2)
        for (y_t, d_y) in y_r:
            kb.op("pool", lambda e: e.memset(y_t[:], 0.0), W=[d_y])
        if stage == "C":
            hw_t, d_hw = load_w(kb, "hw_in", hw_in, D, 4 * D)
            d_hw.ro = True
            hn_t, d_hn = load_bc(kb, "hnorm", hnorm, D)
            hbf_t, d_hbf = load_bc(kb, "hbf", hbf, D)
            l0_t, d_l0 = load_bc(kb, "l0", lbl[0:1, :], D)
            l1_t, d_l1 = load_bc(kb, "l1", lbl[1:2, :], D)
            lb_t = kb.sb("lb", [128, D], F32); d_lb = Dep("lb")
            oml_t = kb.sb("oml", [128, D], F32); d_oml = Dep("oml")
            kb.op("dve", lambda e: e.tensor_tensor(out=lb_t[:], in0=l1_t[:], in1=l0_t[:], op=ALU.subtract),
                  R=[d_l0, d_l1], W=[d_lb])
            kb.op("act", lambda e: e.activation(out=oml_t[:], in_=lb_t[:], func=AF.Sigmoid, scale=-1.0),
                  R=[d_lb], W=[d_oml])
            kb.op("act", lambda e: e.activation(out=lb_t[:], in_=lb_t[:], func=AF.Sigmoid), R=[d_lb], W=[d_lb])
            d_lb.ro = True
            d_oml.ro = True
            scr_r = kb.ring("scr", [128, D], F32, 2)
            ss_r = kb.ring("ss", [128, 1], F32, 2)
            xn_r = kb.ring("xn", [128, D], BF16, 2)
            xT_r = kb.ring("xT", [128, 8, 128], BF16, 2)
            pst_r = kb.ring("pst", [128, 8, 128], BF16, 2, psum=True)
            pmm_r = kb.ring("pmm", [128, 512], F32, 4, psum=True)
            ob_r = kb.ring("obf", [128, D], BF16, 4)
            of_r = kb.ring("off", [128, D], F32, 2)
            zf_r = kb.ring("zf", [128, 512], F32, 2)
        pi = 0
        obi = 0
        for t in range(nt):
            b = t % 2
            r0, r1 = t * 128, (t + 1) * 128
            h1_t, d_h1 = h1_r[b]
            h2_t, d_h2 = h2_r[b]
            y0_t, d_y0 = y_r[2 * b]
            y1_t, d_y1 = y_r[2 * b + 1]
            kb.dma("sp", h1_t[:], h1s[r0:r1, :], R=[d_h1s], W=[d_h1])
            for k, (y_t, d_y) in enumerate(((y0_t, d_y0), (y1_t, d_y1))):
                kb.idma(out=y_t[:], out_off=None, in_=ypad[:, :],
                        in_off=bass.IndirectOffsetOnAxis(ap=dest_i[:, t, k:k + 1], axis=0),
                        R=[d_ypad, d_dest], W=[d_y], bounds_check=breg, oob_is_err=False)
            kb.op("dve", lambda e: e.scalar_tensor_tensor(out=h2_t[:], in0=y0_t[:], scalar=gw[:, t, 0:1], in1=h1_t[:],
                                                           op0=ALU.mult, op1=ALU.add), R=[d_y0, d_gw, d_h1], W=[d_h2])
            kb.op("dve", lambda e: e.scalar_tensor_tensor(out=h2_t[:], in0=y1_t[:], scalar=gw[:, t, 1:2], in1=h2_t[:],
                                                           op0=ALU.mult, op1=ALU.add), R=[d_y1, d_gw, d_h2], W=[d_h2])
            kb.dma("sp", h2_out[r0:r1, :], h2_t[:], R=[d_h2])
            if stage != "C":
                continue
            sc_t, d_sc = scr_r[b]
            ss_t, d_ss = ss_r[b]
            xn_t, d_xn = xn_r[b]
            xT_t, d_xT = xT_r[b]
            ps_t, d_pst = pst_r[b]
            rms_rstd(kb, h2_t[:], d_h2, D, sc_t[:], d_sc, ss_t[:], d_ss)
            kb.op("dve", lambda e: e.scalar_tensor_tensor(out=xn_t[:], in0=h2_t[:], scalar=ss_t[:, 0:1], in1=hn_t[:],
                                                           op0=ALU.mult, op1=ALU.mult), R=[d_h2, d_ss, d_hn], W=[d_xn])
            transpose_chunks(kb, xn_t, d_xn, 8, ps_t, d_pst, xT_t, d_xT, ident, d_id)
            for grp, (dram_o, kind) in enumerate(((q1_out, "silu"), (lf1_out, "f"), (v1_out, "copy"), (gs_out, "silu"))):
                if kind == "f":
                    o_t, d_o = of_r[b]
                else:
                    o_t, d_o = ob_r[obi % 4]
                    obi += 1
                for half in range(2):
                    p_t, d_p = pmm_r[pi % 4]
                    pi += 1
                    c0 = grp * 1024 + half * 512
                    hs = slice(half * 512, (half + 1) * 512)
                    mm_acc(kb, p_t[:], d_p, xT_t, d_xT, hw_t, d_hw, c0, c0 + 512)
                    if kind == "silu":
                        kb.op("act", lambda e: e.activation(out=o_t[:, hs], in_=p_t[:], func=AF.Silu), R=[d_p], W=[d_o])
                    elif kind == "copy":
                        kb.op("act", lambda e: e.copy(out=o_t[:, hs], in_=p_t[:]), R=[d_p], W=[d_o])
                    else:
                        z_t, d_z = zf_r[half]
                        kb.op("dve", lambda e: e.tensor_tensor(out=z_t[:], in0=p_t[:], in1=hbf_t[:, hs], op=ALU.add),
                              R=[d_p, d_hbf], W=[d_z])
                        kb.op("act", lambda e: e.activation(out=z_t[:], in_=z_t[:], func=AF.Sigmoid), R=[d_z], W=[d_z])
                        kb.op("dve", lambda e: e.tensor_tensor(out=z_t[:], in0=z_t[:], in1=oml_t[:, hs], op=ALU.mult),
                              R=[d_z, d_oml], W=[d_z])
                        kb.op("dve", lambda e: e.tensor_tensor(out=z_t[:], in0=z_t[:], in1=lb_t[:, hs], op=ALU.add),
                              R=[d_z, d_lb], W=[d_z])
                        kb.op("act", lambda e: e.activation(out=o_t[:, hs], in_=z_t[:], func=AF.Ln), R=[d_z], W=[d_o])
                kb.dma("sp", dram_o[r0:r1, :], o_t[:], R=[d_o])
    kb.finish()
    return nc


def build_D(nb, nbh):
    L = nb * 128
    nc = bass.Bass("TRN2", target_bir_lowering=False)
    qT = nc.dram_tensor("qT", [nbh, 128, L], BF16, kind="ExternalInput").ap()
    lfT = nc.dram_tensor("lfT", [nbh, 128, L], F32, kind="ExternalInput").ap()
    vv = nc.dram_tensor("v", [nbh, L, 128], BF16, kind="ExternalInput").ap()
    go = nc.dram_tensor("go", [1, 128], F32, kind="ExternalInput").ap()
    on = nc.dram_tensor("on", [nbh, L, 128], F32, kind="ExternalOutput").ap()
    kb = KB(nc)
    identf, d_idf, ident, d_id = make_ident(kb)
    go_t, d_go = load_bc(kb, "go", go, 128)
    onesf = kb.sb("onesf", [128, 128], F32); d_onesf = Dep("onesf")
    mf = kb.sb("mf", [128, 128], F32); d_mf = Dep("mf")
    mku = kb.sb("mku", [128, 128], U32); d_mk = Dep("mku")
    kb.op("pool", lambda e: e.memset(onesf[:], 1.0), W=[d_onesf])
    kb.op("pool", lambda e: e.affine_select(out=mf[:], in_=onesf[:], pattern=[[1, 128]], compare_op=ALU.is_ge,
                                            fill=0.0, base=0, channel_multiplier=-1), R=[d_onesf], W=[d_mf])
    kb.op("dve", lambda e: e.tensor_copy(out=mku[:], in_=mf[:]), R=[d_mf], W=[d_mk])
    d_mk.ro = True
    d_onesf.ro = True
    q_t = kb.sb("q", [128, L], BF16); d_q = Dep("q")
    lf_t = kb.sb("lf", [128, L], F32); d_lf = Dep("lf")
    v_t = kb.sb("v", [128, nb, 128], BF16); d_v = Dep("v")
    S_t = kb.sb("S", [128, 128], F32); d_S = Dep("S")
    Sb_r = kb.ring("Sb", [128, 128], BF16, 2)
    b_r = kb.ring("b", [128, 128], F32, 2)
    col_r = kb.ring("col", [128, 8], F32, 2)
    e1_r = kb.ring("e1", [128, 128], F32, 2)
    e2_r = kb.ring("e2", [128, 128], F32, 2)
    kk_r = kb.ring("kk", [128, 128], F32, 2)
    qd_r = kb.ring("qd", [128, 128], BF16, 2)
    kd_r = kb.ring("kd", [128, 128], BF16, 2)
    qb_r = kb.ring("qb", [128, 128], BF16, 2)
    ke_r = kb.ring("ke", [128, 128], BF16, 2)
    keT_r = kb.ring("keT", [128, 128], BF16, 2)
    scm_r = kb.ring("scm", [128, 128], BF16, 2)
    osq_r = kb.ring("osq", [128, 128], F32, 2)
    oss_r = kb.ring("oss", [128, 2], F32, 2)
    on_r = kb.ring("on", [128, 128], F32, 2)
    psc_r = kb.ring("psc", [128, 512], F32, 2, psum=True)
    po_r = kb.ring("po", [128, 512], F32, 2, psum=True)
    pt_r = kb.ring("pt", [128, 8, 128], BF16, 2, psum=True)
    pu_r = kb.ring("pu", [128, 512], F32, 2, psum=True)
    for (t_, d_) in scm_r:
        kb.op("pool", lambda e: e.memset(t_[:], 0.0), W=[d_])
    for bh in range(nbh):
        kb.dma("sp", q_t[:], qT[bh], W=[d_q])
        for hlf in range(4):
            c0 = (L // 4) * hlf
            kb.dma("sp", lf_t[:, c0:c0 + L // 4], lfT[bh][:, c0:c0 + L // 4], W=[d_lf])
        kb.dma("sp", v_t[:], vv[bh].rearrange("(j p) d -> p j d", p=128), W=[d_v])
        kb.op("pool", lambda e: e.memset(S_t[:], 0.0), W=[d_S])
        kb.op("pool", lambda e: e.memset(Sb_r[0][0][:], 0.0), W=[Sb_r[0][1]])
        for c in range(nb):
            r = c % 2
            cs = slice(c * 128, (c + 1) * 128)
            b_t, d_b = b_r[r]
            col, d_col = col_r[r]
            e1, d_e1 = e1_r[r]
            e2, d_e2 = e2_r[r]
            kk, d_kk = kk_r[r]
            qd, d_qd = qd_r[r]
            kd, d_kd = kd_r[r]
            qb, d_qb = qb_r[r]
            ke, d_ke = ke_r[r]
            keT, d_keT = keT_r[r]
            scm, d_scm = scm_r[r]
            kb.op("dve", lambda e: e.tensor_tensor_scan(out=b_t[:], data0=onesf[:], data1=lf_t[:, cs], initial=0.0,
                                                         op0=ALU.mult, op1=ALU.add), R=[d_lf, d_onesf], W=[d_b])
            kb.op("dve", lambda e: e.tensor_copy(out=col[:, 0:1], in_=b_t[:, 63:64]), R=[d_b], W=[d_col])
            kb.op("dve", lambda e: e.tensor_tensor(out=col[:, 1:2], in0=b_t[:, 127:128], in1=b_t[:, 63:64],
                                                    op=ALU.subtract), R=[d_b], W=[d_col])
            kb.op("dve", lambda e: e.tensor_copy(out=col[:, 2:3], in_=b_t[:, 127:128]), R=[d_b], W=[d_col])
            kb.op("dve", lambda e: e.tensor_scalar(out=col[:, 3:4], in0=b_t[:, 63:64], scalar1=-1.0, scalar2=None,
                                                    op0=ALU.mult), R=[d_b], W=[d_col])
            kb.op("act", lambda e: e.activation(out=col[:, 4:7], in_=col[:, 0:3], func=AF.Exp), R=[d_col], W=[d_col])
            kb.op("act", lambda e: e.activation(out=e1[:], in_=b_t[:], func=AF.Exp, bias=col[:, 3:4], scale=1.0),
                  R=[d_b, d_col], W=[d_e1])
            kb.op("act", lambda e: e.activation(out=e2[:], in_=b_t[:], func=AF.Exp, bias=col[:, 0:1], scale=-1.0),
                  R=[d_b, d_col], W=[d_e2])
            kb.op("act", lambda e: e.activation(out=kk[:], in_=lf_t[:, cs], func=AF.Exp), R=[d_lf], W=[d_kk])
            kb.op("pool", lambda e: e.tensor_scalar(out=kk[:], in0=kk[:], scalar1=-1.0, scalar2=1.0, op0=ALU.mult,
                                                     op1=ALU.add), R=[d_kk], W=[d_kk])
            kb.op("dve", lambda e: e.tensor_tensor(out=qd[:], in0=q_t[:, cs], in1=e1[:], op=ALU.mult),
                  R=[d_q, d_e1], W=[d_qd])
            kb.op("dve", lambda e: e.tensor_tensor(out=kd[:], in0=kk[:], in1=e2[:], op=ALU.mult),
                  R=[d_kk, d_e2], W=[d_kd])
            kb.op("dve", lambda e: e.scalar_tensor_tensor(out=qb[:], in0=q_t[:, cs], scalar=col[:, 4:5], in1=e1[:],
                                                           op0=ALU.mult, op1=ALU.mult), R=[d_q, d_col, d_e1], W=[d_qb])
            kb.op("dve", lambda e: e.scalar_tensor_tensor(out=ke[:], in0=kk[:], scalar=col[:, 5:6], in1=e2[:],
                                                           op0=ALU.mult, op1=ALU.mult), R=[d_kk, d_col, d_e2], W=[d_ke])
            psc, d_psc = psc_r[r]
            kb.op("pe", lambda e: e.matmul(psc[:, 0:128], lhsT=kd[:], rhs=qd[:], start=True, stop=True),
                  R=[d_kd, d_qd], W=[d_psc])
            kb.op("dve", lambda e: e.copy_predicated(out=scm[:], mask=mku[:], data=psc[:, 0:128]),
                  R=[d_psc, d_mk], W=[d_scm])
            po, d_po = po_r[r]
            Sb, d_Sb = Sb_r[c % 2]
            kb.op("pe", lambda e: e.matmul(po[:, 0:128], lhsT=scm[:], rhs=v_t[:, c, :], start=True, stop=False),
                  R=[d_scm, d_v], W=[d_po])
            kb.op("pe", lambda e: e.matmul(po[:, 0:128], lhsT=qb[:], rhs=Sb[:], start=False, stop=True),
                  R=[d_qb, d_Sb], W=[d_po])
            pt, d_pt = pt_r[r]
            kb.op("pe", lambda e: e.transpose(out=pt[:, 0, :], in_=ke[:], identity=ident[:]), R=[d_ke, d_id], W=[d_pt])
            kb.op("act", lambda e: e.copy(out=keT[:], in_=pt[:, 0, :]), R=[d_pt], W=[d_keT])
            pu, d_pu = pu_r[r]
            kb.op("pe", lambda e: e.matmul(pu[:, 0:128], lhsT=keT[:], rhs=v_t[:, c, :], start=True, stop=True),
                  R=[d_keT, d_v], W=[d_pu])
            kb.op("dve", lambda e: e.scalar_tensor_tensor(out=S_t[:], in0=S_t[:], scalar=col[:, 6:7], in1=pu[:, 0:128],
                                                           op0=ALU.mult, op1=ALU.add), R=[d_S, d_col, d_pu], W=[d_S])
            Sb2, d_Sb2 = Sb_r[(c + 1) % 2]
            kb.op("pool", lambda e: e.tensor_copy(out=Sb2[:], in_=S_t[:]), R=[d_S], W=[d_Sb2])
            osq, d_osq = osq_r[r]
            oss, d_oss = oss_r[r]
            on_t, d_on = on_r[r]
            kb.op("act", lambda e: e.activation(out=osq[:], in_=po[:, 0:128], func=AF.Square, accum_out=oss[:, 0:1]),
                  R=[d_po], W=[d_osq, d_oss])
            kb.op("act", lambda e: e.activation(out=oss[:, 1:2], in_=oss[:, 0:1], func=AF.Ln, scale=1.0 / 128, bias=EPS),
                  R=[d_oss], W=[d_oss])
            kb.op("act", lambda e: e.activation(out=oss[:, 1:2], in_=oss[:, 1:2], func=AF.Exp, scale=-0.5),
                  R=[d_oss], W=[d_oss])
            kb.op("dve", lambda e: e.scalar_tensor_tensor(out=on_t[:], in0=po[:, 0:128], scalar=oss[:, 1:2], in1=go_t[:],
                                                           op0=ALU.mult, op1=ALU.mult), R=[d_po, d_oss, d_go], W=[d_on])
            kb.dma("sp", on[bh, c * 128:(c + 1) * 128, :], on_t[:], R=[d_on])
    kb.finish()
    return nc


_CACHE = {}


def _prog(key, fn):
    if key not in _CACHE:
        _CACHE[key] = fn()
    return _CACHE[key]


def _run(nc, in_maps):
    res = run_bass_kernel_spmd(nc, in_maps, core_ids=list(range(NCORES)))
    return res.results


def kernel_unfused(**inp):
    inp = {k: np.asarray(v) for k, v in inp.items()}
    x = inp["x"]
    meta = inp["meta_tokens"]
    f32 = np.float32
    metatile = np.zeros((128, D), f32)
    metatile[128 - NMETA:] = meta
    seg = SEQ // 4

    def core_rows(full_b, c, with_meta=True):
        s = 128 + (c % 4) * seg
        body = full_b[s:s + seg]
        return np.concatenate([full_b[0:128], body], 0) if with_meta else body

    def assemble(per_core):
        out = []
        for b in range(2):
            parts = [per_core[4 * b][0:128]] + [per_core[4 * b + j][128:] for j in range(4)]
            out.append(np.concatenate(parts, 0))
        return out

    hA = [np.concatenate([metatile, x[c // 4, (c % 4) * seg:(c % 4 + 1) * seg]], 0) for c in range(NCORES)]
    valid = np.ones((NT * 128, 1), f32)
    valid[0:128 - NMETA] = 0.0

    ncA = _prog("A", lambda: build_A(NT))
    cA = {"gain": inp["fox_norm"][0][None], "w_in": inp["fox_w_in"][0],
          "gq": np.tile(inp["fox_q_norm"][0], FH)[None], "gk": np.tile(inp["fox_k_norm"][0], FH)[None],
          "bf": inp["fox_b_f"][0][None]}
    rA = _run(ncA, [dict(cA, h=hA[c]) for c in range(NCORES)])
    qf = assemble([np.asarray(r["qo"]) for r in rA])
    kf = assemble([np.asarray(r["ko"]) for r in rA])
    vf = assemble([np.asarray(r["vo"]) for r in rA])
    lff = assemble([np.asarray(r["lfo"]) for r in rA])

    ncB = _prog("B", lambda: build_B(NB, 4))
    imB = []
    for c in range(NCORES):
        b = c // 4
        hs = [(4 * c + i) % FH for i in range(4)]
        imB.append({
            "qT": np.ascontiguousarray(np.stack([qf[b][:, h * FD:(h + 1) * FD].T for h in hs])),
            "kT": np.ascontiguousarray(np.stack([kf[b][:, h * FD:(h + 1) * FD].T for h in hs])),
            "v": np.ascontiguousarray(np.stack([vf[b][:, h * FD:(h + 1) * FD] for h in hs])),
            "lfr": np.ascontiguousarray(np.stack([lff[b][:, h] for h in hs])),
            "lfT": np.ascontiguousarray(np.stack([lff[b][:, h].reshape(NB, 128).T for h in hs])),
        })
    rB = _run(ncB, imB)
    oun = [np.zeros((LP, D), f32) for _ in range(2)]
    den = [np.zeros((LP, FH), f32) for _ in range(2)]
    for c in range(NCORES):
        b = c // 4
        o = np.asarray(rB[c]["oT"])
        for i in range(4):
            h = (4 * c + i) % FH
            oun[b][:, h * FD:(h + 1) * FD] = o[i, 0:FD].T
            den[b][:, h] = o[i, FD]

    ncC = _prog("C", lambda: build_CE("C", NT, True))
    cC = {"valid_in": valid, "w_out": inp["fox_w_out"][0], "mnorm": inp["moe_norm"][0][None],
          "w_r": np.ascontiguousarray(np.concatenate([inp["moe_w_grp"][0], inp["moe_w_rt"][0]], 1)),
          "b_r": np.concatenate([inp["moe_b_grp"][0], inp["moe_b_rt"][0]])[None],
          "w_up": inp["moe_w_up"][0], "w_dn": inp["moe_w_down"][0],
          "hnorm": inp["hg_norm"][0][None], "hw_in": inp["hg_w_in"][0], "hbf": inp["hg_b_f"][0][None],
          "lbl": inp["hg_lb_logits"]}
    rC = _run(ncC, [dict(cC, a_in=core_rows(oun[c // 4], c), den_in=core_rows(den[c // 4], c), hp_in=hA[c])
                    for c in range(NCORES)])
    h2 = [np.asarray(r["h2_out"]) for r in rC]
    q1 = assemble([np.asarray(r["q1_out"]) for r in rC])
    lf1 = assemble([np.asarray(r["lf1_out"]) for r in rC])
    v1 = assemble([np.asarray(r["v1_out"]) for r in rC])
    gs = assemble([np.asarray(r["gs_out"]) for r in rC])
    npad = 128 - NMETA
    for b in range(2):
        q1[b][0:npad] = 0
        lf1[b][0:npad] = 0
        v1[b][0:npad] = 0

    ncD = _prog("D", lambda: build_D(NB, 2))
    imD = []
    for c in range(NCORES):
        prs = [2 * c, 2 * c + 1]
        imD.append({
            "qT": np.ascontiguousarray(np.stack([q1[p // HH][:, (p % HH) * 128:(p % HH + 1) * 128].T for p in prs])),
            "lfT": np.ascontiguousarray(np.stack([lf1[p // HH][:, (p % HH) * 128:(p % HH + 1) * 128].T for p in prs])),
            "v": np.ascontiguousarray(np.stack([v1[p // HH][:, (p % HH) * 128:(p % HH + 1) * 128] for p in prs])),
            "go": inp["hg_o_norm"][0][None],
        })
    rD = _run(ncD, imD)
    onf = [np.zeros((LP, D), f32) for _ in range(2)]
    for c in range(NCORES):
        o = np.asarray(rD[c]["on"])
        for i in range(2):
            p = 2 * c + i
            onf[p // HH][:, (p % HH) * 128:(p % HH + 1) * 128] = o[i]

    ncE = _prog("E", lambda: build_CE("E", NTX, False))
    cE = {"valid_in": np.ones((NTX * 128, 1), f32), "w_out": inp["hg_w_out"][0], "mnorm": inp["moe_norm"][1][None],
          "w_r": np.ascontiguousarray(np.concatenate([inp["moe_w_grp"][1], inp["moe_w_rt"][1]], 1)),
          "b_r": np.concatenate([inp["moe_b_grp"][1], inp["moe_b_rt"][1]])[None],
          "w_up": inp["moe_w_up"][1], "w_dn": inp["moe_w_down"][1]}
    rE = _run(ncE, [dict(cE, a_in=core_rows(onf[c // 4], c, False), gs_in=core_rows(gs[c // 4], c, False),
                         hp_in=h2[c][128:]) for c in range(NCORES)])
    out = np.zeros((2, SEQ, D), f32)
    for c in range(NCORES):
        out[c // 4, (c % 4) * seg:(c % 4 + 1) * seg] = np.asarray(rE[c]["h2_out"])
    return out


GROUPS = [[0, 1, 2, 3], [4, 5, 6, 7]]


def build_fused(ntx=NTX, cap=CAP, stop=None):
    assert ntx % 4 == 0
    nt = ntx + 1
    nb = 4 * ntx + 1
    L = nb * 128
    nch = ntx // 4
    TX = ntx * 128
    nI = ntx + 1
    nslot = NE * cap
    nst = cap // 128
    nc = bass.Bass("TRN2", target_bir_lowering=False)

    def din(name, shape, dt=F32):
        return nc.dram_tensor(name, list(shape), dt, kind="ExternalInput").ap()

    def dint(name, shape, dt=BF16):
        return nc.dram_tensor(name, list(shape), dt).ap()

    x_in = din("x_in", [nt * 128, D])
    valid_in = din("valid_in", [nt * 128, 1])
    fnorm = din("fnorm", [1, D])
    wq_d, wk_d, wv_d = din("wq", [D, 256]), din("wk", [D, 256]), din("wv", [D, 256])
    wf_d = din("wf", [D, 4])
    gqc_d, gkc_d = din("gqc", [128, 1]), din("gkc", [128, 1])
    bf4r_d, bf4c_d = din("bf4r", [1, 4]), din("bf4c", [4, 1])
    wo1_d = din("wo1", [D, D])
    idx2_d = din("idx2", [128, 8], I32)
    idx3_d = din("idx3", [128, 8], I32)
    moe_d = []
    for l in range(2):
        if stop in ("T1", "H1p", "H1a") or (stop in ("T2", "H2") and l == 1):
            moe_d.append(None)
            continue
        moe_d.append(dict(mnorm=din("mnorm%d" % l, [1, D]), w_r=din("w_r%d" % l, [D, 36]), b_r=din("b_r%d" % l, [1, 36]),
                          w_up=din("w_up%d" % l, [NE, D, 2 * DE]), w_dn=din("w_dn%d" % l, [NE, DE, D])))
    hnorm = din("hnorm", [1, D])
    hwq_d, hwf_d, hwv_d = din("hwq", [2, D, 128]), din("hwf", [2, D, 128]), din("hwv", [2, D, 128])
    hwg_d = din("hwg", [D, D])
    hbfc_d, l0c_d, l1c_d = din("hbfc", [2, 128, 1]), din("l0c", [2, 128, 1]), din("l1c", [2, 128, 1])
    go_d = din("go", [1, 128])
    wo2_d = din("wo2", [D, D])
    out_d = nc.dram_tensor("out", [TX, D], F32, kind="ExternalOutput").ap()

    MA, MC = dint("MA", [128, 8 * 128]), dint("MC", [128, 8 * 128])
    SA, RA = dint("SA", [nch * 128, 8 * 512]), dint("RA", [nch * 512, 8 * 512])
    SC, RC = dint("SC", [nch * 128, 8 * 512]), dint("RC", [nch * 512, 8 * 512])
    qTs, kTs, vs = dint("qTs", [4, FD + 1, L]), dint("kTs", [4, FD, L]), dint("vs", [L, 256])
    SBb, RBb = dint("SBb", [8 * 128, TX]), dint("RBb", [8 * 512, TX])
    SBm, RBm = dint("SBm", [256, 128]), dint("RBm", [1024, 128])
    h1s, h2s = dint("h1s", [nt * 128, D], F32), dint("h2s", [nt * 128, D], F32)
    xpad, ypad = dint("xpad", [nslot, D]), dint("ypad", [nslot, D])
    GS = dint("GS", [nch * 128, 8 * 512])
    SD, RD = dint("SD", [8 * 128, TX]), dint("RD", [8 * 512, TX])
    d_MA, d_MC = (Dep(n) for n in ("MA", "MC"))
    d_SA = (Dep("SAw"), [Dep("SA%d" % j) for j in range(nch)])
    d_SC = (Dep("SCw"), [Dep("SC%d" % j) for j in range(nch)])
    d_SBc = [Dep("SBb%d" % j) for j in range(8)]
    d_SDc = [Dep("SD%d" % j) for j in range(8)]
    d_RA = [Dep("RA%d" % j) for j in range(nch)]
    d_RC = [Dep("RC%d" % j) for j in range(nch)]
    d_qTs, d_kTs, d_vs, d_SBb, d_RBb, d_SBm, d_RBm = (Dep(n) for n in ("qTs", "kTs", "vs", "SBb", "RBb", "SBm", "RBm"))
    d_h1s, d_h2s, d_xpad, d_ypad, d_GS, d_SD, d_RD = (Dep(n) for n in ("h1s", "h2s", "xpad", "ypad", "GS", "SD", "RD"))

    kb = KB(nc)
    identf, d_idf, ident, d_id = make_ident(kb)
    breg = nc.gpsimd.to_reg(nslot - 1)
    breg2 = nc.gpsimd.to_reg(8 * 512 - 1)

    def dump_and_finish(items):
        for name, ap, dep in items:
            o = nc.dram_tensor("dbg_" + name, list(ap.shape), ap.dtype, kind="ExternalOutput").ap()
            kb.dma("sp", o, ap, R=[dep])
        kb.finish()
        return nc

    def emit_hnT(src_t, d_src, gain_t, d_gain, scr, ssr, xnr, pstr, hcr, t, Mloc, d_Mloc, Ssend, d_S, Rrecv, d_R, hc_hook=None):
        b = t % 2
        sc_t, d_sc = scr[b]
        ss_t, d_ss = ssr[b]
        xn_t, d_xn = xnr[b]
        ps_t, d_pst = pstr[b]
        rms_rstd(kb, src_t[:], d_src, D, sc_t[:], d_sc, ss_t[:], d_ss)
        kb.op("dve", lambda e: e.scalar_tensor_tensor(out=xn_t[:], in0=src_t[:], scalar=ss_t[:, 0:1], in1=gain_t[:],
                                                       op0=ALU.mult, op1=ALU.mult), R=[d_src, d_ss, d_gain], W=[d_xn])
        for c in range(8):
            kb.op("pe", lambda e: e.transpose(out=ps_t[:, c, :], in_=xn_t[:, c * 128:(c + 1) * 128], identity=ident[:]),
                  R=[d_xn, d_id], W=[d_pst])
        if t == 0:
            hc_t, d_hc = hcr[0]
            kb.op("dve", lambda e: e.tensor_copy(out=hc_t[:, :, 0:128], in_=ps_t[:]), R=[d_pst], W=[d_hc])
            kb.dma("sp", Mloc.rearrange("p (c t) -> p c t", c=8), hc_t[:, :, 0:128], R=[d_hc], W=[d_Mloc])
            return
        j, s = (t - 1) // 4, (t - 1) % 4
        hc_t, d_hc = hcr[(j + 1) % 2]
        kb.op("dve", lambda e: e.tensor_copy(out=hc_t[:, :, s * 128:(s + 1) * 128], in_=ps_t[:]), R=[d_pst], W=[d_hc])
        if s == 3:
            kb.dma("sp", Ssend[j * 128:(j + 1) * 128, :], hc_t[:].rearrange("p c t -> p (c t)"), R=[d_hc],
                   W=[d_S[0], d_S[1][j]])
            kb.cc(Ssend[j * 128:(j + 1) * 128, :], Rrecv[j * 512:(j + 1) * 512, :], GROUPS, R=[d_S[1][j]], W=[d_R[j]])
            if hc_hook is not None:
                hc_hook(j, hc_t, d_hc)

    lfall = kb.sb("lfall", [128, nb, 4], F32)
    d_lfall = Dep("lfall")
    with kb.scope():
        g_t, d_g = load_bc(kb, "fnorm", fnorm, D)
        wq_t, d_wq = load_w(kb, "wq", wq_d, D, 256)
        wk_t, d_wk = load_w(kb, "wk", wk_d, D, 256)
        wv_t, d_wv = load_w(kb, "wv", wv_d, D, 256)
        wf_t, d_wf = load_w(kb, "wf", wf_d, D, 4)
        for d in (d_wq, d_wk, d_wv, d_wf):
            d.ro = True
        gqc = kb.sb("gqc", [128, 1], F32); d_gqc = Dep("gqc")
        gkc = kb.sb("gkc", [128, 1], F32); d_gkc = Dep("gkc")
        bf4c = kb.sb("bf4c", [4, 1], F32); d_bf4c = Dep("bf4c")
        kb.dma("sp", gqc[:], gqc_d, W=[d_gqc])
        kb.dma("sp", gkc[:], gkc_d, W=[d_gkc])
        kb.dma("sp", bf4c[:], bf4c_d, W=[d_bf4c])
        kb.op("dve", lambda e: e.tensor_scalar(out=bf4c[:], in0=bf4c[:], scalar1=-1.0, scalar2=None, op0=ALU.mult),
              R=[d_bf4c], W=[d_bf4c])
        kb.op("dve", lambda e: e.tensor_scalar(out=gqc[:], in0=gqc[:], scalar1=float(FD ** -0.5), scalar2=None,
                                                op0=ALU.mult), R=[d_gqc], W=[d_gqc])
        bf4r, d_bf4r = load_bc(kb, "bf4r", bf4r_d, 4)
        blkf = kb.sb("blkf", [128, 128], F32); d_blkf = Dep("blkf")
        blk = kb.sb("blk", [128, 128], BF16); d_blk = Dep("blk")
        ones1 = kb.sb("ones1", [128, 512], F32); d_ones1 = Dep("ones1")
        kb.op("pool", lambda e: e.memset(blkf[:], 0.0), W=[d_blkf])
        kb.op("pool", lambda e: e.memset(blkf[0:64, 0:64], 1.0), W=[d_blkf])
        kb.op("pool", lambda e: e.memset(blkf[64:128, 64:128], 1.0), W=[d_blkf])
        kb.op("dve", lambda e: e.tensor_copy(out=blk[:], in_=blkf[:]), R=[d_blkf], W=[d_blk])
        kb.op("pool", lambda e: e.memset(ones1[:], 1.0), W=[d_ones1])
        d_blk.ro = True
        d_ones1.ro = True
        xin = kb.ring("xin", [128, D], F32, 4)
        scr = kb.ring("scr", [128, D], F32, 2)
        ssr = kb.ring("ss", [128, 1], F32, 2)
        xnr = kb.ring("xn", [128, D], BF16, 2)
        hcr = kb.ring("hc", [128, 8, 512], BF16, 2)
        pstr = kb.ring("pst", [128, 8, 128], BF16, 2, psum=True)
        def t1_load(t):
            x_t, d_x = xin[t % 4]
            kb.dma("sp", x_t[:], x_in[t * 128:(t + 1) * 128, :], W=[d_x])
        for t in range(min(3, nt)):
            t1_load(t)
        for t in range(nt):
            if t + 3 < nt:
                t1_load(t + 3)
            x_t, d_x = xin[t % 4]
            emit_hnT(x_t, d_x, g_t, d_g, scr, ssr, xnr, pstr, hcr, t, MA, d_MA, SA, d_SA, RA, d_RA)

        if stop == "T1":
            kb.barrier()
            return dump_and_finish([("RA", RA, d_RA[nch - 1]), ("MA", MA, d_MA)])
        hgr = kb.ring("hg", [128, 8, 512], BF16, 2)
        sqr = kb.ring("sq", [128, 512], BF16, 2)
        rsr = kb.ring("rs", [128, 512], F32, 2)
        qnr = kb.ring("qn", [128, 512], BF16, 2)
        vsr = kb.ring("vsb", [128, 256], BF16, 2)
        lzr = kb.ring("lz", [128, 4], F32, 2)
        lrr = kb.ring("lr", [4, 512], F32, 2)
        drr = kb.ring("dr", [4, 512], BF16, 2)
        pqr = kb.ring("pq", [128, 512], F32, 2, psum=True)
        pssr = kb.ring("pss", [128, 512], F32, 1, psum=True)
        pvr = kb.ring("pv", [128, 512], F32, 2, psum=True)
        pfr = kb.ring("pf", [128, 512], F32, 1, psum=True)
        qi = 0
        vi = 0
        def h1_load(G):
            hg, d_hg = hgr[G % 2]
            if G == 0:
                kb.dma("sp", hg[:, :, 0:128], MA.rearrange("p (c t) -> p c t", c=8), R=[d_MA], W=[d_hg])
            else:
                j, rank = (G - 1) // 4, (G - 1) % 4
                r0 = j * 512 + rank * 128
                kb.dma("sp", hg[:].rearrange("p c t -> p (c t)"), RA[r0:r0 + 128, :], R=[d_RA[j]], W=[d_hg])
        h1_load(0)
        for G in range(1 + 4 * nch):
            hg, d_hg = hgr[G % 2]
            if G + 1 < 1 + 4 * nch:
                h1_load(G + 1)
            if G == 0:
                n, tok0 = 128, 0
            else:
                n, tok0 = 512, 128 + ((G - 1) % 4) * TX + ((G - 1) // 4) * 512
            for (w_t, d_w, gc, d_gc, dst, d_dst) in ((wq_t, d_wq, gqc, d_gqc, qTs, d_qTs), (wk_t, d_wk, gkc, d_gkc, kTs, d_kTs)):
                for pr in range(2):
                    pq, d_pq = pqr[qi % 2]
                    pss, d_pss = pssr[0]
                    sq, d_sq = sqr[qi % 2]
                    rs, d_rs = rsr[qi % 2]
                    qn, d_qn = qnr[qi % 2]
                    qi += 1
                    for c in range(8):
                        kb.op("pe", lambda e: e.matmul(pq[:, 0:n], lhsT=w_t[:, c, pr * 128:(pr + 1) * 128], rhs=hg[:, c, 0:n],
                                                       start=(c == 0), stop=(c == 7)), R=[d_w, d_hg], W=[d_pq])
                    kb.op("act", lambda e: e.activation(out=sq[:, 0:n], in_=pq[:, 0:n], func=AF.Square), R=[d_pq], W=[d_sq])
                    kb.op("pe", lambda e: e.matmul(pss[:, 0:n], lhsT=blk[:], rhs=sq[:, 0:n], start=True, stop=True),
                          R=[d_blk, d_sq], W=[d_pss])
                    kb.op("act", lambda e: e.activation(out=rs[:, 0:n], in_=pss[:, 0:n], func=AF.Ln, scale=1.0 / FD, bias=EPS),
                          R=[d_pss], W=[d_rs])
                    kb.op("act", lambda e: e.activation(out=rs[:, 0:n], in_=rs[:, 0:n], func=AF.Exp, scale=-0.5), R=[d_rs], W=[d_rs])
                    kb.op("dve", lambda e: e.scalar_tensor_tensor(out=qn[:, 0:n], in0=pq[:, 0:n], scalar=gc[:, 0:1], in1=rs[:, 0:n],
                                                                   op0=ALU.mult, op1=ALU.mult), R=[d_pq, d_gc, d_rs], W=[d_qn])
                    for hh in range(2):
                        kb.dma("sp", dst[2 * pr + hh, 0:FD, tok0:tok0 + n], qn[hh * 64:(hh + 1) * 64, 0:n], R=[d_qn], W=[d_dst])
            for s in range(n // 128):
                pv, d_pv = pvr[vi % 2]
                vsb, d_vsb = vsr[vi % 2]
                lz, d_lz = lzr[vi % 2]
                vi += 1
                for c in range(8):
                    kb.op("pe", lambda e: e.matmul(pv[:, 0:256], lhsT=hg[:, c, s * 128:(s + 1) * 128], rhs=wv_t[:, c, :],
                                                   start=(c == 0), stop=(c == 7)), R=[d_wv, d_hg], W=[d_pv])
                for c in range(8):
                    kb.op("pe", lambda e: e.matmul(pv[:, 256:260], lhsT=hg[:, c, s * 128:(s + 1) * 128], rhs=wf_t[:, c, :],
                                                   start=(c == 0), stop=(c == 7)), R=[d_wf, d_hg], W=[d_pv])
                kb.op("act", lambda e: e.copy(out=vsb[:], in_=pv[:, 0:256]), R=[d_pv], W=[d_vsb])
                kb.dma("sp", vs[tok0 + s * 128:tok0 + (s + 1) * 128, :], vsb[:], R=[d_vsb], W=[d_vs])
                blk_i = tok0 // 128 + s
                kb.op("dve", lambda e: e.tensor_tensor(out=lz[:], in0=pv[:, 256:260], in1=bf4r[:], op=ALU.add),
                      R=[d_pv, d_bf4r], W=[d_lz])
                kb.op("act", lambda e: e.activation(out=lz[:], in_=lz[:], func=AF.Exp, scale=-1.0), R=[d_lz], W=[d_lz])
                kb.op("act", lambda e: e.activation(out=lz[:], in_=lz[:], func=AF.Ln, bias=1.0), R=[d_lz], W=[d_lz])
                kb.op("dve", lambda e: e.tensor_scalar(out=lfall[:, blk_i, :], in0=lz[:], scalar1=-1.0, scalar2=None,
                                                        op0=ALU.mult), R=[d_lz], W=[d_lfall])
            pf, d_pf = pfr[0]
            lr, d_lr = lrr[G % 2]
            dr, d_dr = drr[G % 2]
            for c in range(8):
                kb.op("pe", lambda e: e.matmul(pf[0:4, 0:n], lhsT=wf_t[:, c, :], rhs=hg[:, c, 0:n], start=(c == 0), stop=(c == 7)),
                      R=[d_wf, d_hg], W=[d_pf])
            kb.op("act", lambda e: e.activation(out=lr[:, 0:n], in_=pf[0:4, 0:n], func=AF.Exp, scale=-1.0, bias=bf4c[:, 0:1]),
                  R=[d_pf, d_bf4c], W=[d_lr])
            kb.op("act", lambda e: e.activation(out=lr[:, 0:n], in_=lr[:, 0:n], func=AF.Ln, bias=1.0), R=[d_lr], W=[d_lr])
            kb.op("dve", lambda e: e.tensor_tensor_scan(out=lr[:, 0:n], data0=ones1[0:4, 0:n], data1=lr[:, 0:n], initial=0.0,
                                                         op0=ALU.mult, op1=ALU.subtract), R=[d_lr, d_ones1], W=[d_lr])
            kb.op("dve", lambda e: e.tensor_copy(out=dr[:, 0:n], in_=lr[:, 0:n]), R=[d_lr], W=[d_dr])
            kb.dma("sp", qTs[:, FD, tok0:tok0 + n], dr[:, 0:n], R=[d_dr], W=[d_qTs])

    if stop == "H1p":
        return dump_and_finish([("qTs", qTs, d_qTs), ("kTs", kTs, d_kTs), ("vs", vs, d_vs)])
    with kb.scope():
        trif = kb.sb("trif", [128, 128], F32); d_tri = Dep("trif")
        onesf = kb.sb("onesf", [128, 128], F32); d_ones = Dep("onesf")
        sel0 = kb.sb("sel0", [128, 128], F32); d_sel = Dep("sel0")
        maskb = kb.sb("maskb", [128, 128], BF16); d_mask = Dep("maskb")
        onerow = kb.sb("onerow", [128, nb], F32); d_or = Dep("onerow")
        kb.op("pool", lambda e: e.memset(onesf[:], 1.0), W=[d_ones])
        kb.op("pool", lambda e: e.memset(onerow[:], 1.0), W=[d_or])
        kb.op("pool", lambda e: e.affine_select(out=trif[:], in_=onesf[:], pattern=[[1, 128]], compare_op=ALU.is_ge,
                                                fill=0.0, base=0, channel_multiplier=-1), R=[d_ones], W=[d_tri])
        kb.op("pool", lambda e: e.affine_select(out=sel0[:], in_=onesf[:], pattern=[[0, 128]], compare_op=ALU.is_ge,
                                                fill=0.0, base=0, channel_multiplier=-1), R=[d_ones], W=[d_sel])
        kb.op("dve", lambda e: e.tensor_copy(out=maskb[:], in_=trif[:]), R=[d_tri], W=[d_mask])
        for d in (d_tri, d_ones, d_sel, d_mask, d_or):
            d.ro = True
        QA = kb.sb("QA", [FD + 1, L], BF16)
        tpc = ntx // 4
        qb_ = [0] + [128 + (c + 1) * tpc * 512 for c in range(3)] + [L]
        d_QAc = [Dep("QA%d" % c) for c in range(4)]

        def q_chunk(I):
            return 0 if I == 0 else (I - 1) // tpc

        def q_load(hx, c):
            kb.dma("sp", QA[:, qb_[c]:qb_[c + 1]], qTs[hx][:, qb_[c]:qb_[c + 1]], R=[d_qTs], W=[d_QAc[c]])
        KAr = kb.ring("KA", [FD + 1, L], BF16, 2)
        VAr = kb.ring("VA", [128, nb, 128], BF16, 2)
        lft = kb.sb("lft", [128, nb], F32); d_lft = Dep("lft")
        ct = kb.sb("ct", [128, nb], F32); d_ct = Dep("ct")
        tot = kb.sb("tot", [128, nb], F32); d_tot = Dep("tot")
        rall = kb.sb("rall", [128, nb], F32); d_rall = Dep("rall")
        biasr = kb.ring("bias", [128, nb], F32, 2)
        pr_ = kb.ring("P", [128, 512], BF16, 4)
        denr = kb.ring("den", [128, 512], F32, 2)
        rdr = kb.ring("rden", [64, 512], F32, 2)
        onr = kb.ring("onb", [64, 512], BF16, 2)
        psS = kb.ring("psS", [128, 512], F32, 4, psum=True)
        psO = kb.ring("psO", [128, 512], F32, 2, psum=True)
        psM = kb.ring("psM", [128, 512], F32, 2, psum=True)
        for (KA_, d_KA_), (VA_, d_VA_) in zip(KAr, VAr):
            kb.op("pool", lambda e: e.memset(KA_[FD:FD + 1, :], 1.0), W=[d_KA_])
            kb.op("pool", lambda e: e.memset(VA_[:, :, FD:128], 1.0), W=[d_VA_])

        def kv_load(hx):
            KA_, d_KA_ = KAr[hx % 2]
            VA_, d_VA_ = VAr[hx % 2]
            kb.dma("sp", KA_[0:FD, :], kTs[hx], R=[d_kTs], W=[d_KA_])
            kb.dma("sp", VA_[:, :, 0:FD], vs[:, hx * FD:(hx + 1) * FD].rearrange("(j p) d -> p j d", p=128), R=[d_vs], W=[d_VA_])
            kb.op("pool", lambda e: e.memset(VA_[0:112, 0, :], 0.0), W=[d_VA_])

        for c in range(4):
            q_load(0, c)
        kv_load(0)
        for h4 in range(4):
            pr4, hh4 = h4 // 2, h4 % 2
            KA, d_KA = KAr[h4 % 2]
            VA, d_VA = VAr[h4 % 2]
            kb.op("dve", lambda e: e.tensor_copy(out=lft[:], in_=lfall[:, :, h4]), R=[d_lfall], W=[d_lft])
            pm0, d_pm0 = psM[0]
            pm1, d_pm1 = psM[1]
            kb.op("pe", lambda e: e.matmul(pm0[:, 0:nb], lhsT=trif[:], rhs=lft[:], start=True, stop=True),
                  R=[d_tri, d_lft], W=[d_pm0])
            kb.op("pe", lambda e: e.matmul(pm1[:, 0:nb], lhsT=onesf[:], rhs=lft[:], start=True, stop=True),
                  R=[d_ones, d_lft], W=[d_pm1])
            kb.op("dve", lambda e: e.tensor_copy(out=tot[:], in_=pm1[:, 0:nb]), R=[d_pm1], W=[d_tot])
            kb.op("dve", lambda e: e.tensor_tensor_scan(out=ct[:], data0=onerow[:], data1=tot[:], initial=0.0,
                                                         op0=ALU.mult, op1=ALU.add), R=[d_tot, d_or], W=[d_ct])
            kb.op("dve", lambda e: e.tensor_tensor(out=ct[:], in0=ct[:], in1=tot[:], op=ALU.subtract),
                  R=[d_ct, d_tot], W=[d_ct])
            kb.op("dve", lambda e: e.tensor_tensor(out=ct[:], in0=ct[:], in1=pm0[:, 0:nb], op=ALU.add),
                  R=[d_ct, d_pm0], W=[d_ct])
            kb.op("pe", lambda e: e.matmul(pm1[:, 0:nb], lhsT=sel0[:], rhs=ct[:], start=True, stop=True),
                  R=[d_sel, d_ct], W=[d_pm1])
            kb.op("dve", lambda e: e.tensor_copy(out=rall[:], in_=pm1[:, 0:nb]), R=[d_pm1], W=[d_rall])
            steps = []
            for I in range(nI):
                j0 = 0 if I == 0 else 4 * I - 3
                nblk = 1 if I == 0 else 4
                nJ = j0 + nblk
                for J in range(nJ):
                    steps.append((I, J, j0, nblk, nJ))
            LA = 2
            for idx in range(len(steps) + LA):
                if idx < len(steps):
                    I, J, j0, nblk, nJ = steps[idx]
                    q0 = j0 * 128
                    bias_t, d_bias = biasr[I % 2]
                    if J == 0:
                        kb.op("dve", lambda e: e.tensor_scalar(out=bias_t[:, 0:nJ], in0=ct[:, 0:nJ], scalar1=-1.0,
                                                                scalar2=rall[:, j0:j0 + 1], op0=ALU.mult, op1=ALU.add),
                              R=[d_ct, d_rall], W=[d_bias])
                    m = max(0, J - j0)
                    c0 = m * 128
                    c1 = nblk * 128
                    ps, d_ps = psS[idx % 4]
                    p_t, d_p = pr_[idx % 4]
                    kb.op("pe", lambda e: e.matmul(ps[:, c0:c1], lhsT=KA[:, J * 128:(J + 1) * 128],
                                                   rhs=QA[:, q0 + c0:q0 + c1], start=True, stop=True),
                          R=[d_KA, d_QAc[q_chunk(I)]], W=[d_ps])
                    kb.op("act", lambda e: e.activation(out=p_t[:, c0:c1], in_=ps[:, c0:c1], func=AF.Exp,
                                                        bias=bias_t[:, J:J + 1], scale=1.0),
                          R=[d_ps, d_bias], W=[d_p])
                    if J >= j0:
                        kb.op("dve", lambda e: e.tensor_tensor(out=p_t[:, c0:c0 + 128], in0=p_t[:, c0:c0 + 128],
                                                                in1=maskb[:], op=ALU.mult), R=[d_p, d_mask], W=[d_p])
                    if J == 0 and I == nI // 2 and h4 + 1 < 4:
                        kv_load(h4 + 1)
                    if J == nJ - 1 and I > 0 and I % tpc == 0 and h4 + 1 < 4:
                        q_load(h4 + 1, I // tpc - 1)
                if idx >= LA:
                    I, J, j0, nblk, nJ = steps[idx - LA]
                    m = max(0, J - j0)
                    c0 = m * 128
                    c1 = nblk * 128
                    p_t, d_p = pr_[(idx - LA) % 4]
                    po, d_po = psO[I % 2]
                    kb.op("pe", lambda e: e.matmul(po[:, c0:c1], lhsT=VA[:, J, :], rhs=p_t[:, c0:c1],
                                                   start=(J == 0), stop=(J == nJ - 1)), R=[d_VA, d_p], W=[d_po])
                    if J == nJ - 1:
                        ncol = nblk * 128
                        den, d_den = denr[I % 2]
                        rd, d_rd = rdr[I % 2]
                        onb, d_onb = onr[I % 2]
                        kb.op("dve", lambda e: e.tensor_scalar(out=den[64:128, 0:ncol], in0=po[64:128, 0:ncol], scalar1=1e-30,
                                                                scalar2=None, op0=ALU.max), R=[d_po], W=[d_den])
                        kb.dma("sp", rd[:, 0:ncol], den[64:128, 0:ncol], R=[d_den], W=[d_rd])
                        kb.op("dve", lambda e: e.reciprocal(out=rd[:, 0:ncol], in_=rd[:, 0:ncol]), R=[d_rd], W=[d_rd])
                        kb.op("dve", lambda e: e.tensor_tensor(out=onb[:, 0:ncol], in0=po[0:64, 0:ncol], in1=rd[:, 0:ncol],
                                                                op=ALU.mult), R=[d_po, d_rd], W=[d_onb])
                        if I == 0:
                            kb.dma("sp", SBm[h4 * 64:(h4 + 1) * 64, :], onb[:, 0:128], R=[d_onb], W=[d_SBm])
                        else:
                            off = (I - 1) * 512
                            qq, col = off // TX, off % TX
                            r0 = (pr4 * 4 + qq) * 128 + hh4 * 64
                            cidx = pr4 * 4 + qq
                            kb.dma("sp", SBb[r0:r0 + 64, col:col + 512], onb[:, 0:512], R=[d_onb], W=[d_SBb, d_SBc[cidx]])
                            if hh4 == 1 and (off + 512) % TX == 0:
                                kb.cc(SBb[cidx * 128:(cidx + 1) * 128, :], RBb[cidx * 512:(cidx + 1) * 512, :], GROUPS,
                                      R=[d_SBc[cidx]], W=[d_RBb])
        kb.cc(SBm[:, :], RBm[:, :], GROUPS, R=[d_SBm], W=[d_RBm])

    if stop == "H1a":
        return dump_and_finish([("RBb", RBb, d_RBb), ("RBm", RBm, d_RBm)])
    usb = kb.sb("usb", [128, 128], BF16); d_us = Dep("usb")
    onesb = kb.sb("onesb", [128, 128], BF16); d_onesb = Dep("onesb")
    onesf2 = kb.sb("onesf2", [128, 128], F32); d_onesf2 = Dep("onesf2")
    tmpf = kb.sb("tmpf", [128, 128], F32); d_tmpf = Dep("tmpf")
    kb.op("pool", lambda e: e.memset(onesf2[:], 1.0), W=[d_onesf2])
    kb.op("pool", lambda e: e.affine_select(out=tmpf[:], in_=onesf2[:], pattern=[[1, 128]], compare_op=ALU.is_gt,
                                            fill=0.0, base=0, channel_multiplier=-1), R=[d_onesf2], W=[d_tmpf])
    kb.op("dve", lambda e: e.tensor_copy(out=usb[:], in_=tmpf[:]), R=[d_tmpf], W=[d_us])
    kb.op("dve", lambda e: e.tensor_copy(out=onesb[:], in_=onesf2[:]), R=[d_onesf2], W=[d_onesb])
    for d in (d_us, d_onesb, d_onesf2):
        d.ro = True
    dest_i = kb.sb("dest_i", [128, nt, 2], I32); d_dest = Dep("dest_i")
    gw = kb.sb("gw", [128, nt, 2], F32); d_gw = Dep("gw")
    cntb = kb.sb("cntb", [128, NE], F32); d_cntb = Dep("cntb")

    def tok_stage(l, tiles, has_meta, setup_lhsT, hp_ap, wo_d, pass3_setup, pass3_tile):
        md = moe_d[l]
        kb.op("pool", lambda e: e.iota(cntb[:], pattern=[[cap, NE]], base=0, channel_multiplier=0,
                                       allow_small_or_imprecise_dtypes=True), W=[d_cntb])
        with kb.scope():
            mn_t, d_mn = load_bc(kb, "mnorm", md["mnorm"], D)
            br_t, d_br = load_bc(kb, "b_r", md["b_r"], 36)
            wr_t, d_wr = load_w(kb, "w_r", md["w_r"], D, 36, dt=F32)
            d_wr.ro = True
            with kb.scope():
                wo_t, d_wo = load_w(kb, "w_out", wo_d, D, D)
                d_wo.ro = True
                get_lhsT = setup_lhsT()
                hp_r = kb.ring("hp", [128, D], F32, 4)
                h1_r = kb.ring("h1", [128, D], F32, 2)
                scr_r = kb.ring("scr", [128, D], F32, 2)
                ss_r = kb.ring("ss", [128, 1], F32, 2)
                hmf_r = kb.ring("hmf", [128, D], F32, 2)
                hmb_r = kb.ring("hmb", [128, D], BF16, 6)
                hmT_r = kb.ring("hmT", [128, 8, 128], F32, 2)
                sm_r = kb.ring("sm", [128, 256], F32, 4)
                ab_r = kb.ring("ab", [128, NE], BF16, 4)
                val_r = kb.ring("val", [128, 2], F32, 4)
                pstf_r = kb.ring("pstf", [128, 8, 128], F32, 1, psum=True)
                pmm_r = kb.ring("pmm", [128, 512], F32, 2, psum=True)
                prt_r = kb.ring("prt", [128, 512], F32, 4, psum=True)
                def p1_load(ti):
                    hp_t, d_hp = hp_r[ti % 4]
                    kb.dma("sp", hp_t[:], hp_ap(tiles[ti]), R=[d_h2s], W=[d_hp])
                    if hasattr(get_lhsT, "prefetch"):
                        get_lhsT.prefetch(tiles[ti])

                def front(ti):
                    t = tiles[ti]
                    b = ti % 2
                    r0, r1 = t * 128, (t + 1) * 128
                    hp_t, d_hp = hp_r[ti % 4]
                    if ti + 3 < len(tiles):
                        p1_load(ti + 3)
                    lhsT, d_lhsT = get_lhsT(t)
                    h1_t, d_h1 = h1_r[b]
                    for half in range(2):
                        p_t, d_p = pmm_r[half]
                        for c in range(8):
                            kb.op("pe", lambda e: e.matmul(p_t[:], lhsT=lhsT(c), rhs=wo_t[:, c, half * 512:(half + 1) * 512],
                                                           start=(c == 0), stop=(c == 7)), R=d_lhsT + [d_wo], W=[d_p])
                        kb.op("dve", lambda e: e.tensor_tensor(out=h1_t[:, half * 512:(half + 1) * 512], in0=p_t[:],
                                                                in1=hp_t[:, half * 512:(half + 1) * 512], op=ALU.add),
                              R=[d_p, d_hp], W=[d_h1])
                    kb.dma("sp", h1s[r0:r1, :], h1_t[:], R=[d_h1], W=[d_h1s])
                    sc_t, d_sc = scr_r[b]
                    ss_t, d_ss = ss_r[b]
                    hmf_t, d_hmf = hmf_r[b]
                    hmb_t, d_hmb = hmb_r[ti % 6]
                    rms_rstd(kb, h1_t[:], d_h1, D, sc_t[:], d_sc, ss_t[:], d_ss)
                    kb.op("dve", lambda e: e.scalar_tensor_tensor(out=hmf_t[:], in0=h1_t[:], scalar=ss_t[:, 0:1], in1=mn_t[:],
                                                                   op0=ALU.mult, op1=ALU.mult),
                          R=[d_h1, d_ss, d_mn], W=[d_hmf])
                    kb.op("pool", lambda e: e.tensor_copy(out=hmb_t[:], in_=hmf_t[:]), R=[d_hmf], W=[d_hmb])

                def front2(ti):
                    t = tiles[ti]
                    b = ti % 2
                    r0, r1 = t * 128, (t + 1) * 128
                    hmf_t, d_hmf = hmf_r[b]
                    hmb_t, d_hmb = hmb_r[ti % 6]
                    pf_t, d_pf = pstf_r[0]
                    hmT_t, d_hmT = hmT_r[b]
                    for c in range(8):
                        kb.op("pe", lambda e: e.transpose(out=pf_t[:, c, :], in_=hmf_t[:, c * 128:(c + 1) * 128],
                                                          identity=identf[:]), R=[d_hmf, d_idf], W=[d_pf])
                    kb.op("act", lambda e: e.copy(out=hmT_t[:], in_=pf_t[:]), R=[d_pf], W=[d_hmT])
                    pr_t, d_pr = prt_r[ti % 4]
                    for c in range(8):
                        kb.op("pe", lambda e: e.matmul(pr_t[:, 0:36], lhsT=hmT_t[:, c, :], rhs=wr_t[:, c, :],
                                                       start=(c == 0), stop=(c == 7)), R=[d_hmT, d_wr], W=[d_pr])
                    return dict(t=t, b=ti % 4, r0=r0, r1=r1, hmb_t=hmb_t, d_hmb=d_hmb, pr_t=pr_t, d_pr=d_pr)

                def route_ops(cx):
                    t, b, r0, r1 = cx["t"], cx["b"], cx["r0"], cx["r1"]
                    hmb_t, d_hmb, pr_t, d_pr = cx["hmb_t"], cx["d_hmb"], cx["pr_t"], cx["d_pr"]
                    ops = []
                    add = ops.append
                    sm, d_sm = sm_r[b]
                    lg = sm[:, 0:36]
                    gmax = sm[:, 36:37]
                    ngmax = sm[:, 37:38]
                    gsum = sm[:, 38:39]
                    ggate = sm[:, 39:40]
                    eg = sm[:, 40:44]
                    ohg = sm[:, 44:48]
                    esel = sm[:, 48:56]
                    top8 = sm[:, 56:64]
                    oh0 = sm[:, 64:72]
                    oh1 = sm[:, 72:80]
                    A0 = sm[:, 80:112]
                    A1 = sm[:, 112:144]
                    sbt = sm[:, 144:176]
                    tmp = sm[:, 176:208]
                    destf = sm[:, 208:210]
                    diff = sm[:, 210:211]
                    sg = sm[:, 211:212]
                    S = [d_sm]
                    dv = lambda fn, R=(), W=(): kb.op("dve", fn, R=list(R) + S, W=list(W) + S)
                    add(lambda: dv(lambda e: e.tensor_tensor(out=lg, in0=pr_t[:, 0:36], in1=br_t[:], op=ALU.add), R=[d_pr, d_br]))
                    add(lambda: dv(lambda e: e.tensor_reduce(out=gmax, in_=lg[:, 0:4], axis=AX.X, op=ALU.max)))
                    add(lambda: dv(lambda e: e.tensor_scalar(out=ngmax, in0=gmax, scalar1=-1.0, scalar2=None, op0=ALU.mult)))
                    add(lambda: kb.op("act", lambda e: e.activation(out=eg, in_=lg[:, 0:4], func=AF.Exp, bias=ngmax, scale=1.0,
                                                                    accum_out=gsum), R=S, W=S))
                    add(lambda: dv(lambda e: e.reciprocal(out=ggate, in_=gsum)))
                    add(lambda: dv(lambda e: e.tensor_scalar(out=ohg, in0=lg[:, 0:4], scalar1=gmax, scalar2=None, op0=ALU.is_equal)))
                    add(lambda: dv(lambda e: e.tensor_scalar(out=esel, in0=lg[:, 4:12], scalar1=ohg[:, 0:1], scalar2=None, op0=ALU.mult)))
                    for g in range(1, 4):
                        add(lambda g=g: dv(lambda e: e.scalar_tensor_tensor(out=esel, in0=lg[:, 4 + 8 * g:12 + 8 * g],
                                                                            scalar=ohg[:, g:g + 1], in1=esel, op0=ALU.mult, op1=ALU.add)))
                    add(lambda: dv(lambda e: e.max(out=top8, in_=esel)))
                    add(lambda: dv(lambda e: e.tensor_scalar(out=oh0, in0=esel, scalar1=top8[:, 0:1], scalar2=None, op0=ALU.is_equal)))
                    add(lambda: dv(lambda e: e.tensor_scalar(out=oh1, in0=esel, scalar1=top8[:, 1:2], scalar2=None, op0=ALU.is_equal)))
                    for (Ak, ohk) in ((A0, oh0), (A1, oh1)):
                        add(lambda Ak=Ak, ohk=ohk: dv(lambda e: e.tensor_tensor(
                            out=Ak.rearrange("p (g j) -> p g j", j=8), in0=ohg.unsqueeze(2).to_broadcast([128, 4, 8]),
                            in1=ohk.unsqueeze(1).to_broadcast([128, 4, 8]), op=ALU.mult)))
                    use_valid = has_meta and t == 0
                    val_t, d_val = val_r[b]
                    if use_valid:
                        add(lambda: kb.dma("sp", val_t[:, 0:1], valid_in[r0:r1, :], W=[d_val]))
                        add(lambda: kb.op("dve", lambda e: e.tensor_scalar(out=val_t[:, 1:2], in0=val_t[:, 0:1], scalar1=-BIGIDX,
                                                                            scalar2=BIGIDX, op0=ALU.mult, op1=ALU.add),
                                          R=[d_val], W=[d_val]))
                        for Ak in (A0, A1):
                            add(lambda Ak=Ak: dv(lambda e: e.tensor_scalar(out=Ak, in0=Ak, scalar1=val_t[:, 0:1], scalar2=None,
                                                                           op0=ALU.mult), R=[d_val]))
                    ab_t, d_ab = ab_r[b]
                    add(lambda: kb.op("dve", lambda e: e.tensor_tensor(out=ab_t[:], in0=A0, in1=A1, op=ALU.add), R=S, W=[d_ab]))
                    add(lambda: kb.op("pe", lambda e: e.matmul(pr_t[:, 64:96], lhsT=usb[:], rhs=ab_t[:], start=True, stop=True),
                                      R=[d_us, d_ab], W=[d_pr]))
                    add(lambda: kb.op("pe", lambda e: e.matmul(pr_t[:, 96:128], lhsT=onesb[:], rhs=ab_t[:], start=True, stop=True),
                                      R=[d_onesb, d_ab], W=[d_pr]))

                    def cnt_ops():
                        dv(lambda e: e.tensor_tensor(out=sbt, in0=pr_t[:, 64:96], in1=cntb[:], op=ALU.add), R=[d_pr, d_cntb])
                        kb.op("dve", lambda e: e.tensor_tensor(out=cntb[:], in0=cntb[:], in1=pr_t[:, 96:128], op=ALU.add),
                              R=[d_pr, d_cntb] + S, W=[d_cntb])
                    add(cnt_ops)
                    for k, Ak in enumerate((A0, A1)):
                        add(lambda Ak=Ak: dv(lambda e: e.tensor_tensor(out=tmp, in0=Ak, in1=sbt, op=ALU.mult)))
                        add(lambda k=k: dv(lambda e: e.tensor_reduce(out=destf[:, k:k + 1], in_=tmp, axis=AX.X, op=ALU.add)))
                    if use_valid:
                        add(lambda: dv(lambda e: e.tensor_scalar(out=destf, in0=destf, scalar1=val_t[:, 0:1], scalar2=val_t[:, 1:2],
                                                                 op0=ALU.mult, op1=ALU.add), R=[d_val]))
                    add(lambda: kb.op("dve", lambda e: e.tensor_copy(out=dest_i[:, t, :], in_=destf), R=S, W=[d_dest]))
                    add(lambda: dv(lambda e: e.tensor_tensor(out=diff, in0=top8[:, 0:1], in1=top8[:, 1:2], op=ALU.subtract)))
                    add(lambda: kb.op("act", lambda e: e.activation(out=sg, in_=diff, func=AF.Sigmoid), R=S, W=S))
                    add(lambda: kb.op("dve", lambda e: e.tensor_tensor(out=gw[:, t, 0:1], in0=sg, in1=ggate, op=ALU.mult), R=S, W=[d_gw]))
                    add(lambda: kb.op("dve", lambda e: e.tensor_tensor(out=gw[:, t, 1:2], in0=ggate, in1=gw[:, t, 0:1], op=ALU.subtract),
                                      R=S + [d_gw], W=[d_gw]))
                    for k in range(2):
                        add(lambda k=k: kb.idma(out=xpad[:, :], out_off=bass.IndirectOffsetOnAxis(ap=dest_i[:, t, k:k + 1], axis=0),
                                                in_=hmb_t[:], in_off=None, R=[d_hmb, d_dest], W=[d_xpad],
                                                bounds_check=breg, oob_is_err=False))
                    return ops

                p1_load(0)

                def emit_routes(pair):
                    lists = [route_ops(cxs[i]) for i in pair]
                    for k in range(max(len(l) for l in lists)):
                        for l in lists:
                            if k < len(l):
                                l[k]()

                cxs = {}
                ntl = len(tiles)
                for ti in range(1, min(3, ntl)):
                    p1_load(ti)
                front(0)
                pending = None
                for ti in range(ntl):
                    if ti + 1 < ntl:
                        front(ti + 1)
                    cxs[ti] = front2(ti)
                    if ti % 2 == 1 or ti == ntl - 1:
                        pair = (ti - 1, ti) if ti % 2 == 1 else (ti,)
                        if pending is not None:
                            emit_routes(pending)
                        pending = pair
                emit_routes(pending)
            with kb.scope():
                wup_r = kb.ring("wup", [128, 8, 2 * DE], BF16, 2)
                wdn_r = kb.ring("wdn", [128, 4, D], BF16, 2)
                wst_r = kb.ring("wst", [128, D], F32, 12)
                wdeps = [[Dep("w%d_%d" % (bb, cc)) for cc in range(12)] for bb in range(2)]
                xs_r = kb.ring("xs", [128, D], BF16, 2 * nst)
                xT_r = kb.ring("xT", [128, 8, cap], BF16, 2)
                sa_r = kb.ring("sa", [128, cap], F32, 2)
                aT_r = kb.ring("aT", [128, 4, cap], BF16, 2)
                yb_r = kb.ring("yb", [128, D], BF16, 2)
                pst_r = kb.ring("pst", [128, 8, 128], BF16, 2, psum=True)
                pau_r = kb.ring("pau", [128, 512], F32, 4, psum=True)
                py_r = kb.ring("py", [128, 512], F32, 2, psum=True)
                CE = ("act", "dve", "act", "dve", "act", "dve", "act", "dve", "act", "dve", "act", "dve")

                def w_dma(ex):
                    wuv = md["w_up"][ex].rearrange("(c p) n -> p c n", p=128)
                    wdv = md["w_dn"][ex].rearrange("(c p) n -> p c n", p=128)
                    for c in range(12):
                        stg, d_stg = wst_r[c]
                        kb.dma("sp", stg[:], wuv[:, c, :] if c < 8 else wdv[:, c - 8, :], W=[d_stg])

                def w_cast(ex, cs):
                    bb = ex % 2
                    for c in cs:
                        stg, d_stg = wst_r[c]
                        dst_ap = wup_r[bb][0][:, c, :] if c < 8 else wdn_r[bb][0][:, c - 8, :]
                        if CE[c] == "act":
                            kb.op("act", lambda e: e.copy(out=dst_ap, in_=stg[:]), R=[d_stg], W=[wdeps[bb][c]])
                        else:
                            kb.op("dve", lambda e: e.tensor_copy(out=dst_ap, in_=stg[:]), R=[d_stg], W=[wdeps[bb][c]])

                def x_dma(ex):
                    for st in range(nst):
                        xs_t, d_xs = xs_r[(ex % 2) * nst + st]
                        s0 = ex * cap + st * 128
                        kb.dma("sp", xs_t[:], xpad[s0:s0 + 128, :], R=[d_xpad], W=[d_xs])

                w_dma(0)
                x_dma(0)
                w_cast(0, range(12))
                yi = 0
                for ex in range(NE):
                    b = ex % 2
                    wup_t = wup_r[b][0]
                    wdn_t = wdn_r[b][0]
                    if ex + 1 < NE:
                        w_dma(ex + 1)
                        x_dma(ex + 1)
                    xT_t, d_xT = xT_r[b]
                    for st in range(nst):
                        xs_t, d_xs = xs_r[b * nst + st]
                        ps_t, d_pst = pst_r[st % 2]
                        for c in range(8):
                            kb.op("pe", lambda e: e.transpose(out=ps_t[:, c, :], in_=xs_t[:, c * 128:(c + 1) * 128],
                                                              identity=ident[:]), R=[d_xs, d_id], W=[d_pst])
                        kb.op("act" if st % 2 else "dve",
                              (lambda e: e.copy(out=xT_t[:, :, st * 128:(st + 1) * 128], in_=ps_t[:])) if st % 2 else
                              (lambda e: e.tensor_copy(out=xT_t[:, :, st * 128:(st + 1) * 128], in_=ps_t[:])),
                              R=[d_pst], W=[d_xT])
                    aT_t, d_aT = aT_r[b]
                    for fc in range(4):
                        pa, d_pa = pau_r[(2 * fc) % 4]
                        pu, d_pu = pau_r[(2 * fc + 1) % 4]
                        for c in range(8):
                            kb.op("pe", lambda e: e.matmul(pa[:, 0:cap], lhsT=wup_t[:, c, fc * 128:(fc + 1) * 128],
                                                           rhs=xT_t[:, c, :], start=(c == 0), stop=(c == 7)),
                                  R=[wdeps[b][c], d_xT], W=[d_pa])
                        for c in range(8):
                            kb.op("pe", lambda e: e.matmul(pu[:, 0:cap], lhsT=wup_t[:, c, DE + fc * 128:DE + (fc + 1) * 128],
                                                           rhs=xT_t[:, c, :], start=(c == 0), stop=(c == 7)),
                                  R=[wdeps[b][c], d_xT], W=[d_pu])
                        sa_t, d_sa = sa_r[fc % 2]
                        kb.op("act", lambda e: e.activation(out=sa_t[:], in_=pa[:, 0:cap], func=AF.Silu), R=[d_pa], W=[d_sa])
                        kb.op("dve", lambda e: e.tensor_tensor(out=aT_t[:, fc, :], in0=sa_t[:], in1=pu[:, 0:cap], op=ALU.mult),
                              R=[d_sa, d_pu], W=[d_aT])
                    if ex + 1 < NE:
                        w_cast(ex + 1, range(0, 8))
                    for st in range(nst):
                        yb_t, d_yb = yb_r[yi % 2]
                        yi += 1
                        for half in range(2):
                            py, d_py = py_r[half]
                            for fc in range(4):
                                kb.op("pe", lambda e: e.matmul(py[:], lhsT=aT_t[:, fc, st * 128:(st + 1) * 128],
                                                               rhs=wdn_t[:, fc, half * 512:(half + 1) * 512],
                                                               start=(fc == 0), stop=(fc == 3)),
                                      R=[d_aT, wdeps[b][8 + fc]], W=[d_py])
                            if half == 0:
                                kb.op("act", lambda e: e.copy(out=yb_t[:, 0:512], in_=py[:]), R=[d_py], W=[d_yb])
                            else:
                                kb.op("dve", lambda e: e.tensor_copy(out=yb_t[:, 512:1024], in_=py[:]), R=[d_py], W=[d_yb])
                        s0 = ex * cap + st * 128
                        kb.dma("sp", ypad[s0:s0 + 128, :], yb_t[:], R=[d_yb], W=[d_ypad])
                    if ex + 1 < NE:
                        w_cast(ex + 1, range(8, 12))
            with kb.scope():
                h1_r = kb.ring("h1", [128, D], F32, 4)
                y_r = kb.ring("y", [128, D], BF16, 8)
                h2_r = kb.ring("h2", [128, D], F32, 2)
                for (y_t, d_y) in y_r:
                    kb.op("pool", lambda e: e.memset(y_t[:], 0.0), W=[d_y])
                p3 = pass3_setup()
                def p3_load(ti):
                    tt = tiles[ti]
                    bb = ti % 4
                    h1_t, d_h1 = h1_r[bb]
                    kb.dma("sp", h1_t[:], h1s[tt * 128:(tt + 1) * 128, :], R=[d_h1s], W=[d_h1])
                    for k in range(2):
                        y_t, d_y = y_r[2 * bb + k]
                        kb.idma(out=y_t[:], out_off=None, in_=ypad[:, :],
                                in_off=bass.IndirectOffsetOnAxis(ap=dest_i[:, tt, k:k + 1], axis=0),
                                R=[d_ypad, d_dest], W=[d_y], bounds_check=breg, oob_is_err=False)
                for ti in range(min(3, len(tiles))):
                    p3_load(ti)
                for ti, t in enumerate(tiles):
                    b = ti % 2
                    r0, r1 = t * 128, (t + 1) * 128
                    h1_t, d_h1 = h1_r[ti % 4]
                    h2_t, d_h2 = h2_r[b]
                    y0_t, d_y0 = y_r[2 * (ti % 4)]
                    y1_t, d_y1 = y_r[2 * (ti % 4) + 1]
                    if ti + 3 < len(tiles):
                        p3_load(ti + 3)
                    kb.op("dve", lambda e: e.scalar_tensor_tensor(out=h2_t[:], in0=y0_t[:], scalar=gw[:, t, 0:1], in1=h1_t[:],
                                                                   op0=ALU.mult, op1=ALU.add), R=[d_y0, d_gw, d_h1], W=[d_h2])
                    kb.op("dve", lambda e: e.scalar_tensor_tensor(out=h2_t[:], in0=y1_t[:], scalar=gw[:, t, 1:2], in1=h2_t[:],
                                                                   op0=ALU.mult, op1=ALU.add), R=[d_y1, d_gw, d_h2], W=[d_h2])
                    pass3_tile(p3, t, h2_t, d_h2)

    def t2_setup_lhsT():
        idx2 = kb.sb("idx2", [128, 8], I32); d_idx2 = Dep("idx2")
        kb.dma("sp", idx2[:], idx2_d, W=[d_idx2])
        oTall = kb.sb("oTall", [128, 8, TX], BF16); d_oTall = Dep("oTall")
        oTm = kb.sb("oTm", [128, 8, 128], BF16); d_oTm = Dep("oTm")
        for c8 in range(8):
            kb.idma(out=oTall[:, c8, :], out_off=None, in_=RBb[:, :],
                    in_off=bass.IndirectOffsetOnAxis(ap=idx2[:, c8:c8 + 1], axis=0),
                    R=[d_RBb, d_idx2], W=[d_oTall], bounds_check=breg2, oob_is_err=False)
        kb.dma("sp", oTm[:], RBm.rearrange("(c p) t -> p c t", p=128), R=[d_RBm], W=[d_oTm])

        def get(t):
            if t == 0:
                return (lambda c: oTm[:, c, :]), [d_oTm]
            return (lambda c: oTall[:, c, (t - 1) * 128:t * 128]), [d_oTall]
        return get

    def t2_pass3_setup():
        p = {}
        p["wg"], p["d_wg"] = load_w(kb, "hwg", hwg_d, D, D)
        p["d_wg"].ro = True
        p["hn"], p["d_hn"] = load_bc(kb, "hnorm", hnorm, D)
        p["scr"] = kb.ring("scr", [128, D], F32, 2)
        p["ss"] = kb.ring("ss", [128, 1], F32, 2)
        p["xn"] = kb.ring("xn", [128, D], BF16, 2)
        p["hc"] = kb.ring("hc", [128, 8, 512], BF16, 2)
        p["gsb"] = kb.ring("gsb", [128, 8, 512], BF16, 2)
        p["pst"] = kb.ring("pst", [128, 8, 128], BF16, 2, psum=True)
        p["pg"] = kb.ring("pg", [128, 512], F32, 2, psum=True)
        return p

    def t2_pass3_tile(p, t, h2_t, d_h2):
        kb.dma("sp", h2s[t * 128:(t + 1) * 128, :], h2_t[:], R=[d_h2], W=[d_h2s])

        def hook(j, hc_t, d_hc):
            gsb, d_gsb = p["gsb"][j % 2]
            for g in range(8):
                pg, d_pg = p["pg"][g % 2]
                for c in range(8):
                    kb.op("pe", lambda e: e.matmul(pg[:], lhsT=p["wg"][:, c, g * 128:(g + 1) * 128], rhs=hc_t[:, c, :],
                                                   start=(c == 0), stop=(c == 7)), R=[p["d_wg"], d_hc], W=[d_pg])
                kb.op("act", lambda e: e.activation(out=gsb[:, g, :], in_=pg[:], func=AF.Silu), R=[d_pg], W=[d_gsb])
            kb.dma("sp", GS[j * 128:(j + 1) * 128, :], gsb[:].rearrange("p g t -> p (g t)"), R=[d_gsb], W=[d_GS])
        emit_hnT(h2_t, d_h2, p["hn"], p["d_hn"], p["scr"], p["ss"], p["xn"], p["pst"], p["hc"], t,
                 MC, d_MC, SC, d_SC, RC, d_RC, hc_hook=hook)

    tok_stage(0, list(range(nt)), True, t2_setup_lhsT, lambda t: x_in[t * 128:(t + 1) * 128, :], wo1_d,
              t2_pass3_setup, t2_pass3_tile)

    if stop == "T2":
        return dump_and_finish([("h2s", h2s, d_h2s), ("RC", RC, d_RC[nch - 1]), ("GS", GS, d_GS)])
    for i2 in range(2):
        with kb.scope():
            q_t = kb.sb("q", [128, L], BF16); d_q = Dep("q")
            lf_t = kb.sb("lf", [128, L], F32); d_lf = Dep("lf")
            v_t = kb.sb("v", [128, nb, 128], BF16); d_v = Dep("v")
            go_t, d_go = load_bc(kb, "go", go_d, 128)
            with kb.scope():
                hwq_t, d_hwq = load_w(kb, "hwq", hwq_d[i2], D, 128)
                hwf_t, d_hwf = load_w(kb, "hwf", hwf_d[i2], D, 128)
                hwv_t, d_hwv = load_w(kb, "hwv", hwv_d[i2], D, 128)
                for d in (d_hwq, d_hwf, d_hwv):
                    d.ro = True
                cols = kb.sb("cols", [128, 8], F32); d_cols = Dep("cols")
                kb.dma("sp", cols[:, 0:1], hbfc_d[i2], W=[d_cols])
                kb.dma("sp", cols[:, 1:2], l0c_d[i2], W=[d_cols])
                kb.dma("sp", cols[:, 2:3], l1c_d[i2], W=[d_cols])
                kb.op("dve", lambda e: e.tensor_tensor(out=cols[:, 3:4], in0=cols[:, 2:3], in1=cols[:, 1:2], op=ALU.subtract),
                      R=[d_cols], W=[d_cols])
                kb.op("act", lambda e: e.activation(out=cols[:, 4:5], in_=cols[:, 3:4], func=AF.Sigmoid), R=[d_cols], W=[d_cols])
                kb.op("act", lambda e: e.activation(out=cols[:, 5:6], in_=cols[:, 3:4], func=AF.Sigmoid, scale=-1.0),
                      R=[d_cols], W=[d_cols])
                d_cols.ro = True
                hgr = kb.ring("hg", [128, 8, 512], BF16, 2)
                sgr = kb.ring("sg", [128, 512], F32, 2)
                pqr = kb.ring("pq", [128, 512], F32, 2, psum=True)
                pfr = kb.ring("pf", [128, 512], F32, 2, psum=True)
                pvr = kb.ring("pv", [128, 512], F32, 2, psum=True)
                vi = 0
                def h2_load(G):
                    hg, d_hg = hgr[G % 2]
                    if G == 0:
                        kb.dma("sp", hg[:, :, 0:128], MC.rearrange("p (c t) -> p c t", c=8), R=[d_MC], W=[d_hg])
                    else:
                        rank, j = (G - 1) // nch, (G - 1) % nch
                        r0 = j * 512 + rank * 128
                        kb.dma("sp", hg[:].rearrange("p c t -> p (c t)"), RC[r0:r0 + 128, :], R=[d_RC[j]], W=[d_hg])
                h2_load(0)
                for G in range(1 + 4 * nch):
                    hg, d_hg = hgr[G % 2]
                    if G + 1 < 1 + 4 * nch:
                        h2_load(G + 1)
                    if G == 0:
                        n, tok0 = 128, 0
                    else:
                        n, tok0 = 512, 128 + (G - 1) * 512
                    pq, d_pq = pqr[G % 2]
                    pf, d_pf = pfr[G % 2]
                    sg, d_sg = sgr[G % 2]
                    for c in range(8):
                        kb.op("pe", lambda e: e.matmul(pq[:, 0:n], lhsT=hwq_t[:, c, :], rhs=hg[:, c, 0:n], start=(c == 0), stop=(c == 7)),
                              R=[d_hwq, d_hg], W=[d_pq])
                    kb.op("act", lambda e: e.activation(out=q_t[:, tok0:tok0 + n], in_=pq[:, 0:n], func=AF.Silu), R=[d_pq], W=[d_q])
                    for c in range(8):
                        kb.op("pe", lambda e: e.matmul(pf[:, 0:n], lhsT=hwf_t[:, c, :], rhs=hg[:, c, 0:n], start=(c == 0), stop=(c == 7)),
                              R=[d_hwf, d_hg], W=[d_pf])
                    kb.op("act", lambda e: e.activation(out=sg[:, 0:n], in_=pf[:, 0:n], func=AF.Sigmoid, bias=cols[:, 0:1], scale=1.0),
                          R=[d_pf, d_cols], W=[d_sg])
                    kb.op("dve", lambda e: e.tensor_scalar(out=sg[:, 0:n], in0=sg[:, 0:n], scalar1=cols[:, 5:6], scalar2=cols[:, 4:5],
                                                            op0=ALU.mult, op1=ALU.add), R=[d_sg, d_cols], W=[d_sg])
                    kb.op("act", lambda e: e.activation(out=lf_t[:, tok0:tok0 + n], in_=sg[:, 0:n], func=AF.Ln), R=[d_sg], W=[d_lf])
                    for s_ in range(n // 128):
                        pv, d_pv = pvr[vi % 2]
                        vi += 1
                        for c in range(8):
                            kb.op("pe", lambda e: e.matmul(pv[:, 0:128], lhsT=hg[:, c, s_ * 128:(s_ + 1) * 128], rhs=hwv_t[:, c, :],
                                                           start=(c == 0), stop=(c == 7)), R=[d_hwv, d_hg], W=[d_pv])
                        kb.op("dve", lambda e: e.tensor_copy(out=v_t[:, tok0 // 128 + s_, :], in_=pv[:, 0:128]), R=[d_pv], W=[d_v])
                kb.op("pool", lambda e: e.memset(lf_t[:, 0:128 - NMETA], 0.0), W=[d_lf])
            with kb.scope():
                onesf3 = kb.sb("onesf3", [128, 128], F32); d_onesf3 = Dep("onesf3")
                mf = kb.sb("mf", [128, 128], F32); d_mf = Dep("mf")
                mku = kb.sb("mku", [128, 128], U32); d_mk = Dep("mku")
                kb.op("pool", lambda e: e.memset(onesf3[:], 1.0), W=[d_onesf3])
                kb.op("pool", lambda e: e.affine_select(out=mf[:], in_=onesf3[:], pattern=[[1, 128]], compare_op=ALU.is_ge,
                                                        fill=0.0, base=0, channel_multiplier=-1), R=[d_onesf3], W=[d_mf])
                kb.op("dve", lambda e: e.tensor_copy(out=mku[:], in_=mf[:]), R=[d_mf], W=[d_mk])
                d_mk.ro = True
                d_onesf3.ro = True
                S_t = kb.sb("S", [128, 128], F32); d_S = Dep("S")
                Sb_r = kb.ring("Sb", [128, 128], BF16, 2)
                b_r = kb.ring("b", [128, 128], F32, 3)
                col_r = kb.ring("col", [128, 8], F32, 3)
                e1_r = kb.ring("e1", [128, 128], F32, 3)
                e2_r = kb.ring("e2", [128, 128], F32, 3)
                kk_r = kb.ring("kk", [128, 128], F32, 3)
                qd_r = kb.ring("qd", [128, 128], BF16, 3)
                kd_r = kb.ring("kd", [128, 128], BF16, 3)
                qb_r = kb.ring("qb", [128, 128], BF16, 3)
                ke_r = kb.ring("ke", [128, 128], BF16, 3)
                keT_r = kb.ring("keT", [128, 128], BF16, 2)
                scm_r = kb.ring("scm", [128, 128], BF16, 3)
                osq_r = kb.ring("osq", [128, 128], F32, 2)
                oss_r = kb.ring("oss", [128, 2], F32, 3)
                on_r = kb.ring("on", [128, 128], BF16, 3)
                stg_r = kb.ring("stg", [128, TX], BF16, 2)
                psc_b = [kb.ps("psc%d" % k, [128, 512], F32) for k in range(2)]
                po_b = [kb.ps("po%d" % k, [128, 512], F32) for k in range(2)]
                pu_b = [kb.ps("pu%d" % k, [128, 512], F32) for k in range(2)]
                pt_b = [kb.ps("pt%d" % k, [128, 8, 128], BF16) for k in range(2)]
                psc_r = [(psc_b[k % 2][:, 0:128], Dep("psc%d" % k)) for k in range(2)] * 2
                po_r = [(po_b[k % 2][:, 0:128], Dep("po%d" % k)) for k in range(2)] * 2
                pu_r = [(pu_b[k % 2][:, 0:128], Dep("pu%d" % k)) for k in range(2)] * 2
                ptk_r = [(pt_b[k % 2][:, 0, :], Dep("ptk%d" % k)) for k in range(2)] * 2
                pto_r = [(pt_b[k % 2][:, 1, :], Dep("pto%d" % k)) for k in range(2)] * 2
                for (t_, d_) in scm_r:
                    kb.op("pool", lambda e: e.memset(t_[:], 0.0), W=[d_])
                kb.op("pool", lambda e: e.memset(S_t[:], 0.0), W=[d_S])
                kb.op("pool", lambda e: e.memset(Sb_r[0][0][:], 0.0), W=[Sb_r[0][1]])

                def st_a(c):
                    cs = slice(c * 128, (c + 1) * 128)
                    b_t, d_b = b_r[c % 3]
                    col, d_col = col_r[c % 3]
                    e1, d_e1 = e1_r[c % 3]
                    e2, d_e2 = e2_r[c % 3]
                    kk, d_kk = kk_r[c % 3]
                    qd, d_qd = qd_r[c % 3]
                    kd, d_kd = kd_r[c % 3]
                    qb, d_qb = qb_r[c % 3]
                    ke, d_ke = ke_r[c % 3]
                    kb.op("dve", lambda e: e.tensor_tensor_scan(out=b_t[:], data0=onesf3[:], data1=lf_t[:, cs], initial=0.0,
                                                                 op0=ALU.mult, op1=ALU.add), R=[d_lf, d_onesf3], W=[d_b])
                    kb.op("dve", lambda e: e.tensor_copy(out=col[:, 0:1], in_=b_t[:, 63:64]), R=[d_b], W=[d_col])
                    kb.op("dve", lambda e: e.tensor_tensor(out=col[:, 1:2], in0=b_t[:, 127:128], in1=b_t[:, 63:64],
                                                            op=ALU.subtract), R=[d_b], W=[d_col])
                    kb.op("dve", lambda e: e.tensor_copy(out=col[:, 2:3], in_=b_t[:, 127:128]), R=[d_b], W=[d_col])
                    kb.op("dve", lambda e: e.tensor_scalar(out=col[:, 3:4], in0=b_t[:, 63:64], scalar1=-1.0, scalar2=None,
                                                            op0=ALU.mult), R=[d_b], W=[d_col])
                    kb.op("act", lambda e: e.activation(out=col[:, 4:7], in_=col[:, 0:3], func=AF.Exp), R=[d_col], W=[d_col])
                    kb.op("act", lambda e: e.activation(out=e1[:], in_=b_t[:], func=AF.Exp, bias=col[:, 3:4], scale=1.0),
                          R=[d_b, d_col], W=[d_e1])
                    kb.op("act", lambda e: e.activation(out=e2[:], in_=b_t[:], func=AF.Exp, bias=col[:, 0:1], scale=-1.0),
                          R=[d_b, d_col], W=[d_e2])
                    kb.op("act", lambda e: e.activation(out=kk[:], in_=lf_t[:, cs], func=AF.Exp), R=[d_lf], W=[d_kk])
                    kb.op("pool", lambda e: e.tensor_scalar(out=kk[:], in0=kk[:], scalar1=-1.0, scalar2=1.0, op0=ALU.mult,
                                                             op1=ALU.add), R=[d_kk], W=[d_kk])
                    kb.op("dve", lambda e: e.tensor_tensor(out=qd[:], in0=q_t[:, cs], in1=e1[:], op=ALU.mult),
                          R=[d_q, d_e1], W=[d_qd])
                    kb.op("dve", lambda e: e.tensor_tensor(out=kd[:], in0=kk[:], in1=e2[:], op=ALU.mult),
                          R=[d_kk, d_e2], W=[d_kd])
                    kb.op("dve", lambda e: e.scalar_tensor_tensor(out=qb[:], in0=q_t[:, cs], scalar=col[:, 4:5], in1=e1[:],
                                                                   op0=ALU.mult, op1=ALU.mult), R=[d_q, d_col, d_e1], W=[d_qb])
                    kb.op("dve", lambda e: e.scalar_tensor_tensor(out=ke[:], in0=kk[:], scalar=col[:, 5:6], in1=e2[:],
                                                                   op0=ALU.mult, op1=ALU.mult), R=[d_kk, d_col, d_e2], W=[d_ke])

                def st_f(c):
                    qd, d_qd = qd_r[c % 3]
                    kd, d_kd = kd_r[c % 3]
                    ke, d_ke = ke_r[c % 3]
                    keT, d_keT = keT_r[c % 2]
                    scm, d_scm = scm_r[c % 3]
                    psc, d_psc = psc_r[c % 4]
                    ptk, d_ptk = ptk_r[c % 4]
                    pu, d_pu = pu_r[c % 4]
                    kb.op("pe", lambda e: e.matmul(psc, lhsT=kd[:], rhs=qd[:], start=True, stop=True), R=[d_kd, d_qd], W=[d_psc])
                    kb.op("dve", lambda e: e.copy_predicated(out=scm[:], mask=mku[:], data=psc), R=[d_psc, d_mk], W=[d_scm])
                    kb.op("pe", lambda e: e.transpose(out=ptk, in_=ke[:], identity=ident[:]), R=[d_ke, d_id], W=[d_ptk])
                    kb.op("act", lambda e: e.copy(out=keT[:], in_=ptk), R=[d_ptk], W=[d_keT])
                    kb.op("pe", lambda e: e.matmul(pu, lhsT=keT[:], rhs=v_t[:, c, :], start=True, stop=True), R=[d_keT, d_v], W=[d_pu])

                def st_k(c):
                    col, d_col = col_r[c % 3]
                    qb, d_qb = qb_r[c % 3]
                    scm, d_scm = scm_r[c % 3]
                    po, d_po = po_r[c % 4]
                    pu, d_pu = pu_r[c % 4]
                    Sb, d_Sb = Sb_r[c % 2]
                    kb.op("pe", lambda e: e.matmul(po, lhsT=scm[:], rhs=v_t[:, c, :], start=True, stop=False), R=[d_scm, d_v], W=[d_po])
                    kb.op("pe", lambda e: e.matmul(po, lhsT=qb[:], rhs=Sb[:], start=False, stop=True), R=[d_qb, d_Sb], W=[d_po])
                    kb.op("dve", lambda e: e.scalar_tensor_tensor(out=S_t[:], in0=S_t[:], scalar=col[:, 6:7], in1=pu,
                                                                   op0=ALU.mult, op1=ALU.add), R=[d_S, d_col, d_pu], W=[d_S])
                    Sb2, d_Sb2 = Sb_r[(c + 1) % 2]
                    kb.op("pool", lambda e: e.tensor_copy(out=Sb2[:], in_=S_t[:]), R=[d_S], W=[d_Sb2])

                def st_n(c):
                    po, d_po = po_r[c % 4]
                    osq, d_osq = osq_r[c % 2]
                    oss, d_oss = oss_r[c % 3]
                    on_t, d_on = on_r[c % 3]
                    kb.op("act", lambda e: e.activation(out=osq[:], in_=po, func=AF.Square, accum_out=oss[:, 0:1]),
                          R=[d_po], W=[d_osq, d_oss])
                    kb.op("act", lambda e: e.activation(out=oss[:, 1:2], in_=oss[:, 0:1], func=AF.Ln, scale=1.0 / 128, bias=EPS),
                          R=[d_oss], W=[d_oss])
                    kb.op("act", lambda e: e.activation(out=oss[:, 1:2], in_=oss[:, 1:2], func=AF.Exp, scale=-0.5),
                          R=[d_oss], W=[d_oss])
                    kb.op("dve", lambda e: e.scalar_tensor_tensor(out=on_t[:], in0=po, scalar=oss[:, 1:2], in1=go_t[:],
                                                                   op0=ALU.mult, op1=ALU.mult), R=[d_po, d_oss, d_go], W=[d_on])

                def st_t(c):
                    on_t, d_on = on_r[c % 3]
                    pto, d_pto = pto_r[c % 4]
                    xb = c - 1
                    qq, tb = xb // ntx, xb % ntx
                    stg, d_stg = stg_r[qq % 2]
                    kb.op("pe", lambda e: e.transpose(out=pto, in_=on_t[:], identity=ident[:]), R=[d_on, d_id], W=[d_pto])
                    kb.op("act", lambda e: e.copy(out=stg[:, tb * 128:(tb + 1) * 128], in_=pto), R=[d_pto], W=[d_stg])
                    if tb == ntx - 1:
                        cidx = i2 * 4 + qq
                        kb.dma("sp", SD[cidx * 128:(cidx + 1) * 128, :], stg[:], R=[d_stg], W=[d_SD, d_SDc[cidx]])
                        kb.cc(SD[cidx * 128:(cidx + 1) * 128, :], RD[cidx * 512:(cidx + 1) * 512, :], GROUPS, R=[d_SDc[cidx]], W=[d_RD])

                st_a(0)
                if nb > 1:
                    st_a(1)
                st_f(0)
                for i in range(nb + 2):
                    if i + 2 < nb:
                        st_a(i + 2)
                    if i + 1 < nb:
                        st_f(i + 1)
                    if i < nb:
                        st_k(i)
                    if 1 <= i - 1 < nb:
                        st_n(i - 1)
                    if 1 <= i - 2 < nb:
                        st_t(i - 2)

    if stop == "H2":
        return dump_and_finish([("RD", RD, d_RD)])
    def t3_setup_lhsT():
        idx3 = kb.sb("idx3", [128, 8], I32); d_idx3 = Dep("idx3")
        kb.dma("sp", idx3[:], idx3_d, W=[d_idx3])
        onall = kb.sb("onall", [128, 8, TX], BF16); d_onall = Dep("onall")
        for c8 in range(8):
            kb.idma(out=onall[:, c8, :], out_off=None, in_=RD[:, :],
                    in_off=bass.IndirectOffsetOnAxis(ap=idx3[:, c8:c8 + 1], axis=0),
                    R=[d_RD, d_idx3], W=[d_onall], bounds_check=breg2, oob_is_err=False)
        gsr = kb.ring("gsT", [128, 8, 512], BF16, 2)
        ogr = kb.ring("og", [128, 8, 128], BF16, 2)

        def prefetch(t):
            if (t - 1) % 4 == 0:
                jj = (t - 1) // 4
                gs_t, d_gs = gsr[jj % 2]
                kb.dma("sp", gs_t[:].rearrange("p g t -> p (g t)"), GS[jj * 128:(jj + 1) * 128, :], R=[d_GS], W=[d_gs])

        def get(t):
            jj, ss_ = (t - 1) // 4, (t - 1) % 4
            gs_t, d_gs = gsr[jj % 2]
            og_t, d_og = ogr[t % 2]
            kb.op("dve", lambda e: e.tensor_tensor(out=og_t[:], in0=onall[:, :, (t - 1) * 128:t * 128],
                                                    in1=gs_t[:, :, ss_ * 128:(ss_ + 1) * 128], op=ALU.mult),
                  R=[d_onall, d_gs], W=[d_og])
            return (lambda c: og_t[:, c, :]), [d_og]
        get.prefetch = prefetch
        return get

    def t3_pass3_tile(p, t, h2_t, d_h2):
        kb.dma("sp", out_d[(t - 1) * 128:t * 128, :], h2_t[:], R=[d_h2])

    tok_stage(1, list(range(1, nt)), False, t3_setup_lhsT, lambda t: h2s[t * 128:(t + 1) * 128, :], wo2_d,
              lambda: None, t3_pass3_tile)
    kb.finish()
    return nc


def fused_in_maps(inp, ntx):
    f32 = np.float32
    TX = ntx * 128
    nt = ntx + 1
    x = inp["x"]
    metatile = np.zeros((128, D), f32)
    metatile[128 - NMETA:] = inp["meta_tokens"]
    valid = np.ones((nt * 128, 1), f32)
    valid[0:128 - NMETA] = 0.0
    fw = inp["fox_w_in"][0]
    hw = inp["hg_w_in"][0]
    common = {
        "valid_in": valid, "fnorm": inp["fox_norm"][0][None],
        "gqc": np.tile(inp["fox_q_norm"][0], 2)[:, None].astype(f32), "gkc": np.tile(inp["fox_k_norm"][0], 2)[:, None].astype(f32),
        "wo1": inp["fox_w_out"][0], "wo2": inp["hg_w_out"][0], "hnorm": inp["hg_norm"][0][None],
        "hwg": np.ascontiguousarray(hw[:, 3 * D:4 * D]), "go": inp["hg_o_norm"][0][None],
    }
    for l in range(2):
        common["mnorm%d" % l] = inp["moe_norm"][l][None]
        common["w_r%d" % l] = np.ascontiguousarray(np.concatenate([inp["moe_w_grp"][l], inp["moe_w_rt"][l]], 1))
        common["b_r%d" % l] = np.concatenate([inp["moe_b_grp"][l], inp["moe_b_rt"][l]])[None]
        common["w_up%d" % l] = inp["moe_w_up"][l]
        common["w_dn%d" % l] = inp["moe_w_down"][l]
    maps = []
    p = np.arange(128)
    for c in range(NCORES):
        b, r = c // 4, c % 4
        m = dict(common)
        m["x_in"] = np.concatenate([metatile, x[b, r * TX:(r + 1) * TX]], 0)
        hc = slice(4 * r * FD, (4 * r + 4) * FD)
        m["wq"] = np.ascontiguousarray(fw[:, 0:D][:, hc])
        m["wk"] = np.ascontiguousarray(fw[:, D:2 * D][:, hc])
        m["wv"] = np.ascontiguousarray(fw[:, 2 * D:3 * D][:, hc])
        m["wf"] = np.ascontiguousarray(fw[:, 3 * D + 4 * r:3 * D + 4 * r + 4])
        bf4 = inp["fox_b_f"][0][4 * r:4 * r + 4]
        m["bf4r"] = bf4[None].astype(f32)
        m["bf4c"] = bf4[:, None].astype(f32)
        idx2 = np.zeros((128, 8), np.int32)
        idx3 = np.zeros((128, 8), np.int32)
        for c8 in range(8):
            rank, sub = c8 // 2, c8 % 2
            idx2[:, c8] = (sub * 4 + r) * 512 + rank * 128 + p
            idx3[:, c8] = (sub * 4 + r) * 512 + rank * 128 + p
        m["idx2"] = idx2
        m["idx3"] = idx3
        hs = [slice((2 * r + i) * 128, (2 * r + i + 1) * 128) for i in range(2)]
        m["hwq"] = np.ascontiguousarray(np.stack([hw[:, 0:D][:, s] for s in hs]))
        m["hwf"] = np.ascontiguousarray(np.stack([hw[:, D:2 * D][:, s] for s in hs]))
        m["hwv"] = np.ascontiguousarray(np.stack([hw[:, 2 * D:3 * D][:, s] for s in hs]))
        m["hbfc"] = np.stack([inp["hg_b_f"][0][s][:, None] for s in hs]).astype(f32)
        m["l0c"] = np.stack([inp["hg_lb_logits"][0][s][:, None] for s in hs]).astype(f32)
        m["l1c"] = np.stack([inp["hg_lb_logits"][1][s][:, None] for s in hs]).astype(f32)
        maps.append(m)
    return maps


def kernel_fused(inp, ntx=NTX, cap=CAP, stop=None):
    inp = {k: np.asarray(v) for k, v in inp.items()}
    nc = _prog(("F", ntx, cap, stop), lambda: build_fused(ntx, cap, stop))
    maps = fused_in_maps(inp, ntx)
    if stop is not None:
        drop = []
        for l in range(2):
            if stop in ("T1", "H1p", "H1a") or (stop in ("T2", "H2") and l == 1):
                drop += [k + str(l) for k in ("mnorm", "w_r", "b_r", "w_up", "w_dn")]
        maps = [{k: v for k, v in m.items() if k not in drop} for m in maps]
    res = _run(nc, maps)
    if stop is not None:
        return res
    TX = ntx * 128
    out = np.zeros((2, 4 * TX, D), np.float32)
    for c in range(NCORES):
        out[c // 4, (c % 4) * TX:(c % 4 + 1) * TX] = np.asarray(res[c]["out"])
    return out


def kernel(**inp):
    return kernel_fused(inp, NTX, CAP)
```

```python
import contextlib
import numpy as np
import ml_dtypes
import concourse.bass as bass
import concourse.mybir as mybir
from concourse.bass_utils import run_bass_kernel_spmd

F32 = mybir.dt.float32
BF16 = mybir.dt.bfloat16
I32 = mybir.dt.int32
U32 = mybir.dt.uint32
AF = mybir.ActivationFunctionType
ALU = mybir.AluOpType
AX = mybir.AxisListType
NPBF = ml_dtypes.bfloat16

D = 1024
NCORES = 8
SEQ = 16384
NMETA = 16
BLK = 128
EPS = 1e-6
FH = 16
FD = 64
HH = 8
NE = 32
CAP = 384
DE = 512
NTX = 32
NT = NTX + 1
LP = SEQ + BLK
NB = LP // BLK


class Dep:
    __slots__ = ("w", "r", "name", "ro")

    def __init__(self, name=""):
        self.w = None
        self.r = []
        self.name = name
        self.ro = False


class _E:
    def __init__(self, name, eng, sem):
        self.name = name
        self.eng = eng
        self.sem = sem
        self.n = 0
        self.waited = {}
        self.pool = []
        self.pool_i = 0


class KB:
    def __init__(self, nc, dma_pool=(("sp", 16), ("pool", 16), ("act", 4)), same_eng_sync=True):
        self.nc = nc
        self.stacks = [contextlib.ExitStack()]
        self.same = same_eng_sync
        self.e = {}
        for name, eng in (("pe", nc.tensor), ("dve", nc.vector), ("act", nc.scalar),
                          ("pool", nc.gpsimd), ("sp", nc.sync)):
            sem = self.stacks[0].enter_context(nc.semaphore("c_" + name))
            self.e[name] = _E(name, eng, sem)
        for q, n in dma_pool:
            for i in range(n):
                sem = self.stacks[0].enter_context(nc.semaphore("d_%s%d" % (q, i)))
                self.e[q].pool.append([sem, 0])
        self.ninst = 0
        self.uid = 0

    def sb(self, name, shape, dt):
        self.uid += 1
        return self.stacks[-1].enter_context(self.nc.sbuf_tensor("%s_%d" % (name, self.uid), list(shape), dt))

    def ps(self, name, shape, dt):
        self.uid += 1
        return self.stacks[-1].enter_context(self.nc.psum_tensor("%s_%d" % (name, self.uid), list(shape), dt))

    def ring(self, name, shape, dt, n, psum=False):
        out = []
        for i in range(n):
            t = (self.ps if psum else self.sb)("%s%d" % (name, i), shape, dt)
            out.append((t, Dep("%s%d" % (name, i))))
        return out

    @contextlib.contextmanager
    def scope(self):
        self.stacks.append(contextlib.ExitStack())
        try:
            yield
        finally:
            self.barrier()
            self.stacks.pop().close()

    def _wait(self, E, ev, own_ok=False):
        if ev is None:
            return
        sem, val = ev
        if sem is E.sem and not own_ok:
            return
        k = id(sem)
        if E.waited.get(k, 0) >= val:
            return
        E.eng.wait_ge(sem, val)
        E.waited[k] = val

    def _deps(self, E, R, W):
        same = self.same and E.name != "pe"
        for d in R:
            self._wait(E, d.w, own_ok=same)
        for d in W:
            self._wait(E, d.w, own_ok=same)
            for ev in d.r:
                self._wait(E, ev, own_ok=False)

    def _record(self, ev, R, W):
        for d in R:
            if not d.ro:
                for i, (sem, val) in enumerate(d.r):
                    if sem is ev[0]:
                        if ev[1] > val:
                            d.r[i] = ev
                        break
                else:
                    d.r.append(ev)
        for d in W:
            d.w = ev
            d.r = []

    def op(self, en, fn, R=(), W=()):
        E = self.e[en]
        self._deps(E, R, W)
        ins = fn(E.eng)
        E.n += 1
        ins.then_inc(E.sem, 1)
        ev = (E.sem, E.n)
        self._record(ev, R, W)
        self.ninst += 1
        return ev

    def _dma_issue(self, E, issue, R, W):
        self._deps(E, R, W)
        slot = E.pool[E.pool_i % len(E.pool)]
        E.pool_i += 1
        sem, cur = slot
        if cur:
            self._wait(E, (sem, cur))
        ins = issue(E.eng)
        ins.then_inc(sem, 16)
        slot[1] = cur + 16
        ev = (sem, cur + 16)
        self._record(ev, R, W)
        self.ninst += 1
        return ev

    def dma(self, q, out, in_, R=(), W=(), **kw):
        return self._dma_issue(self.e[q], lambda e: e.dma_start(out=out, in_=in_, **kw), R, W)

    def idma(self, out, out_off, in_, in_off, R=(), W=(), **kw):
        return self._dma_issue(
            self.e["pool"],
            lambda e: e.indirect_dma_start(out=out, out_offset=out_off, in_=in_, in_offset=in_off, **kw),
            R, W)

    def cc(self, in_ap, out_ap, groups, R=(), W=()):
        E = self.e["pool"]
        self._deps(E, R, W)
        if not hasattr(self, "ccsem"):
            self.ccsem = self.stacks[0].enter_context(self.nc.semaphore("ccsem"))
            self.ccn = 0
        ins = E.eng.collective_compute("AllGather", ALU.bypass, replica_groups=groups, ins=[in_ap], outs=[out_ap], dma_qos="P2")
        ins.then_inc(self.ccsem, 1)
        self.ccn += 1
        ev = (self.ccsem, self.ccn)
        self._record(ev, R, W)
        self.ninst += 1
        return ev

    def barrier(self):
        evs = [(E.sem, E.n) for E in self.e.values() if E.n]
        for q in ("sp", "pool", "act"):
            for sem, cur in self.e[q].pool:
                if cur:
                    evs.append((sem, cur))
        if getattr(self, "ccn", 0):
            evs.append((self.ccsem, self.ccn))
        for E in self.e.values():
            for ev in evs:
                self._wait(E, ev, own_ok=False)

    def finish(self):
        self.barrier()
        self.e["sp"].eng.nop()
        while self.stacks:
            self.stacks.pop().close()


def make_ident(kb, n=128):
    identf = kb.sb("identf", [128, 128], F32)
    ident = kb.sb("ident", [128, 128], BF16)
    d1, d2 = Dep("identf"), Dep("ident")
    kb.op("pool", lambda e: e.memset(identf[:], 0.0), W=[d1])
    kb.op("pool", lambda e: e.affine_select(out=identf[:], in_=identf[:], pattern=[[-1, 128]],
                                            compare_op=ALU.not_equal, fill=1.0, base=0,
                                            channel_multiplier=1), R=[d1], W=[d1])
    kb.op("dve", lambda e: e.tensor_copy(out=ident[:], in_=identf[:]), R=[d1], W=[d2])
    d1.ro = True
    d2.ro = True
    return identf, d1, ident, d2


def load_bc(kb, name, src_ap, n, q="sp"):
    t = kb.sb(name, [128, n], F32)
    d = Dep(name)
    kb.dma(q, t[:], src_ap.to_broadcast([128, n]), W=[d])
    d.ro = True
    return t, d


def load_w(kb, name, w_ap, kin, nout, dt=BF16):
    kc = kin // 128
    t = kb.sb(name, [128, kc, nout], dt)
    d = Dep(name)
    wv = w_ap.rearrange("(c p) n -> p c n", p=128)
    for c in range(kc):
        kb.dma("pool" if dt != F32 else "sp", t[:, c, :], wv[:, c, :], W=[d])
    return t, d


def rms_rstd(kb, src, d_src, n, scr, d_scr, ss, d_ss):
    kb.op("act", lambda e: e.activation(out=scr, in_=src, func=AF.Square, accum_out=ss),
          R=[d_src], W=[d_scr, d_ss])
    kb.op("act", lambda e: e.activation(out=ss, in_=ss, func=AF.Ln, scale=1.0 / n, bias=EPS),
          R=[d_ss], W=[d_ss])
    kb.op("act", lambda e: e.activation(out=ss, in_=ss, func=AF.Exp, scale=-0.5), R=[d_ss], W=[d_ss])


def transpose_chunks(kb, src, d_src, nchunk, pst, d_pst, dst, d_dst, ident, d_id, copy_eng="dve"):
    for c in range(nchunk):
        kb.op("pe", lambda e: e.transpose(out=pst[:, c, :], in_=src[:, c * 128:(c + 1) * 128], identity=ident[:]),
              R=[d_src, d_id], W=[d_pst])
    if copy_eng == "act":
        kb.op("act", lambda e: e.copy(out=dst[:, 0:nchunk, :], in_=pst[:, 0:nchunk, :]), R=[d_pst], W=[d_dst])
    else:
        kb.op("dve", lambda e: e.tensor_copy(out=dst[:, 0:nchunk, :], in_=pst[:, 0:nchunk, :]), R=[d_pst], W=[d_dst])


def mm_acc(kb, ps_ap, d_ps, xT, d_xT, w, d_w, c0, c1, kc=8):
    for c in range(kc):
        kb.op("pe", lambda e: e.matmul(ps_ap, lhsT=xT[:, c, :], rhs=w[:, c, c0:c1], start=(c == 0), stop=(c == kc - 1)),
              R=[d_xT, d_w], W=[d_ps])


def build_A(nt):
    nc = bass.Bass("TRN2", target_bir_lowering=False)
    rows = nt * 128
    h_in = nc.dram_tensor("h", [rows, D], F32, kind="ExternalInput").ap()
    gain = nc.dram_tensor("gain", [1, D], F32, kind="ExternalInput").ap()
    w_in = nc.dram_tensor("w_in", [D, 3088], F32, kind="ExternalInput").ap()
    gq = nc.dram_tensor("gq", [1, D], F32, kind="ExternalInput").ap()
    gk = nc.dram_tensor("gk", [1, D], F32, kind="ExternalInput").ap()
    bfv = nc.dram_tensor("bf", [1, FH], F32, kind="ExternalInput").ap()
    qo = nc.dram_tensor("qo", [rows, D], BF16, kind="ExternalOutput").ap()
    ko = nc.dram_tensor("ko", [rows, D], BF16, kind="ExternalOutput").ap()
    vo = nc.dram_tensor("vo", [rows, D], BF16, kind="ExternalOutput").ap()
    lfo = nc.dram_tensor("lfo", [rows, FH], F32, kind="ExternalOutput").ap()
    d_out = Dep("out")
    kb = KB(nc)
    identf, d_idf, ident, d_id = make_ident(kb)
    g_t, d_g = load_bc(kb, "gain", gain, D)
    gq_t, d_gq = load_bc(kb, "gq", gq, D)
    gk_t, d_gk = load_bc(kb, "gk", gk, D)
    bf_t, d_bf = load_bc(kb, "bf", bfv, FH)
    w_t, d_w = load_w(kb, "w_in", w_in, D, 3088)
    d_w.ro = True
    xin = kb.ring("xin", [128, D], F32, 2)
    scr = kb.ring("scr", [128, D], F32, 2)
    ssr = kb.ring("ss", [128, 1], F32, 2)
    xnr = kb.ring("xn", [128, D], BF16, 2)
    xTr = kb.ring("xT", [128, 8, 128], BF16, 2)
    pstr = kb.ring("pst", [128, 8, 128], BF16, 2, psum=True)
    pmm = kb.ring("pmm", [128, 512], F32, 6, psum=True)
    qf = kb.ring("qf", [128, D], F32, 2)
    s16 = kb.ring("s16", [128, FH], F32, 2)
    qn = kb.ring("qn", [128, D], BF16, 2)
    kn = kb.ring("kn", [128, D], BF16, 2)
    vb = kb.ring("vb", [128, D], BF16, 2)
    lft = kb.ring("lft", [128, FH], F32, 2)
    pi = 0
    for t in range(nt):
        b = t % 2
        x_t, d_x = xin[b]
        sc_t, d_sc = scr[b]
        ss_t, d_ss = ssr[b]
        xn_t, d_xn = xnr[b]
        xT_t, d_xT = xTr[b]
        ps_t, d_pst = pstr[b]
        kb.dma("sp", x_t[:], h_in[t * 128:(t + 1) * 128, :], W=[d_x])
        rms_rstd(kb, x_t[:], d_x, D, sc_t[:], d_sc, ss_t[:], d_ss)
        kb.op("dve", lambda e: e.scalar_tensor_tensor(out=xn_t[:], in0=x_t[:], scalar=ss_t[:, 0:1], in1=g_t[:],
                                                       op0=ALU.mult, op1=ALU.mult),
              R=[d_x, d_ss, d_g], W=[d_xn])
        transpose_chunks(kb, xn_t, d_xn, 8, ps_t, d_pst, xT_t, d_xT, ident, d_id)
        for which, (g2_t, d_g2, o_ring, o_dram, scl) in enumerate(
                ((gq_t, d_gq, qn, qo, FD ** -0.5), (gk_t, d_gk, kn, ko, 1.0))):
            qf_t, d_qf = qf[which]
            for half in range(2):
                p_t, d_p = pmm[pi % 6]
                pi += 1
                c0 = which * 1024 + half * 512
                mm_acc(kb, p_t[:], d_p, xT_t, d_xT, w_t, d_w, c0, c0 + 512)
                kb.op("act", lambda e: e.copy(out=qf_t[:, half * 512:(half + 1) * 512], in_=p_t[:]),
                      R=[d_p], W=[d_qf])
            s_t, d_s = s16[which]
            kb.op("pool", lambda e: e.tensor_tensor(out=sc_t[:], in0=qf_t[:], in1=qf_t[:], op=ALU.mult),
                  R=[d_qf], W=[d_sc])
            kb.op("dve", lambda e: e.tensor_reduce(out=s_t[:], in_=sc_t[:].rearrange("p (h d) -> p h d", d=FD),
                                                    axis=AX.X, op=ALU.add), R=[d_sc], W=[d_s])
            kb.op("act", lambda e: e.activation(out=s_t[:], in_=s_t[:], func=AF.Sqrt, scale=1.0 / FD, bias=EPS),
                  R=[d_s], W=[d_s])
            kb.op("dve", lambda e: e.reciprocal(out=s_t[:], in_=s_t[:]), R=[d_s], W=[d_s])
            kb.op("dve", lambda e: e.tensor_tensor(
                out=sc_t[:].rearrange("p (h d) -> p h d", d=FD), in0=qf_t[:].rearrange("p (h d) -> p h d", d=FD),
                in1=s_t[:].unsqueeze(2).to_broadcast([128, FH, FD]), op=ALU.mult), R=[d_qf, d_s], W=[d_sc])
            o_t, d_o = o_ring[b]
            kb.op("dve", lambda e: e.scalar_tensor_tensor(out=o_t[:], in0=sc_t[:], scalar=float(scl), in1=g2_t[:],
                                                           op0=ALU.mult, op1=ALU.mult),
                  R=[d_sc, d_g2], W=[d_o])
            kb.dma("sp", o_dram[t * 128:(t + 1) * 128, :], o_t[:], R=[d_o])
        v_t, d_v = vb[b]
        for half in range(2):
            p_t, d_p = pmm[pi % 6]
            pi += 1
            c0 = 2048 + half * 512
            mm_acc(kb, p_t[:], d_p, xT_t, d_xT, w_t, d_w, c0, c0 + 512)
            kb.op("act", lambda e: e.copy(out=v_t[:, half * 512:(half + 1) * 512], in_=p_t[:]), R=[d_p], W=[d_v])
        kb.dma("sp", vo[t * 128:(t + 1) * 128, :], v_t[:], R=[d_v])
        p_t, d_p = pmm[pi % 6]
        pi += 1
        mm_acc(kb, p_t[:, 0:FH], d_p, xT_t, d_xT, w_t, d_w, 3072, 3088)
        l_t, d_l = lft[b]
        kb.op("dve", lambda e: e.tensor_tensor(out=l_t[:], in0=p_t[:, 0:FH], in1=bf_t[:], op=ALU.add),
              R=[d_p, d_bf], W=[d_l])
        kb.op("act", lambda e: e.activation(out=l_t[:], in_=l_t[:], func=AF.Exp, scale=-1.0), R=[d_l], W=[d_l])
        kb.op("act", lambda e: e.activation(out=l_t[:], in_=l_t[:], func=AF.Ln, bias=1.0), R=[d_l], W=[d_l])
        kb.op("dve", lambda e: e.tensor_scalar(out=l_t[:], in0=l_t[:], scalar1=-1.0, scalar2=None, op0=ALU.mult),
              R=[d_l], W=[d_l])
        kb.dma("sp", lfo[t * 128:(t + 1) * 128, :], l_t[:], R=[d_l])
    kb.finish()
    return nc


def build_B(nb, nbh):
    assert (nb - 1) % 4 == 0
    L = nb * 128
    nI = (nb - 1) // 4 + 1
    nc = bass.Bass("TRN2", target_bir_lowering=False)
    qT = nc.dram_tensor("qT", [nbh, FD, L], BF16, kind="ExternalInput").ap()
    kT = nc.dram_tensor("kT", [nbh, FD, L], BF16, kind="ExternalInput").ap()
    vv = nc.dram_tensor("v", [nbh, L, FD], BF16, kind="ExternalInput").ap()
    lfr = nc.dram_tensor("lfr", [nbh, L], F32, kind="ExternalInput").ap()
    lfT = nc.dram_tensor("lfT", [nbh, 128, nb], F32, kind="ExternalInput").ap()
    oT = nc.dram_tensor("oT", [nbh, FD + 1, L], F32, kind="ExternalOutput").ap()
    kb = KB(nc)
    trif = kb.sb("trif", [128, 128], F32); d_tri = Dep("trif")
    onesf = kb.sb("onesf", [128, 128], F32); d_ones = Dep("onesf")
    sel0 = kb.sb("sel0", [128, 128], F32); d_sel = Dep("sel0")
    maskb = kb.sb("maskb", [128, 128], BF16); d_mask = Dep("maskb")
    onerow = kb.sb("onerow", [128, nb], F32); d_or = Dep("onerow")
    kb.op("pool", lambda e: e.memset(onesf[:], 1.0), W=[d_ones])
    kb.op("pool", lambda e: e.memset(onerow[:], 1.0), W=[d_or])
    kb.op("pool", lambda e: e.affine_select(out=trif[:], in_=onesf[:], pattern=[[1, 128]], compare_op=ALU.is_ge,
                                            fill=0.0, base=0, channel_multiplier=-1), R=[d_ones], W=[d_tri])
    kb.op("pool", lambda e: e.affine_select(out=sel0[:], in_=onesf[:], pattern=[[0, 128]], compare_op=ALU.is_ge,
                                            fill=0.0, base=0, channel_multiplier=-1), R=[d_ones], W=[d_sel])
    kb.op("dve", lambda e: e.tensor_copy(out=maskb[:], in_=trif[:]), R=[d_tri], W=[d_mask])
    for d in (d_tri, d_ones, d_sel, d_mask, d_or):
        d.ro = True
    crow = kb.sb("crow", [nbh, L], F32); d_crow = Dep("crow")
    drow = kb.sb("drow", [nbh, L], BF16); d_drow = Dep("drow")
    kb.dma("sp", crow[:], lfr, W=[d_crow])
    kb.op("dve", lambda e: e.tensor_tensor_scan(out=crow[:], data0=onerow[0:nbh, 0:1].to_broadcast([nbh, L]),
                                                 data1=crow[:], initial=0.0, op0=ALU.mult, op1=ALU.add),
          R=[d_crow, d_or], W=[d_crow])
    kb.op("dve", lambda e: e.tensor_scalar(out=drow[:, 0:128], in0=crow[:, 0:128], scalar1=crow[:, 0:1], scalar2=None,
                                            op0=ALU.subtract), R=[d_crow], W=[d_drow])
    if nI > 1:
        kb.op("dve", lambda e: e.tensor_tensor(
            out=drow[:, 128:L].rearrange("p (i c) -> p i c", c=512),
            in0=crow[:, 128:L].rearrange("p (i c) -> p i c", c=512),
            in1=crow[:, 128:L].rearrange("p (i c) -> p i c", c=512)[:, :, 0:1].to_broadcast([nbh, nI - 1, 512]),
            op=ALU.subtract), R=[d_crow], W=[d_drow])
    QA = kb.sb("QA", [FD + 1, L], BF16); d_QA = Dep("QA")
    KA = kb.sb("KA", [FD + 1, L], BF16); d_KA = Dep("KA")
    VA = kb.sb("VA", [128, nb, FD + 1], BF16); d_VA = Dep("VA")
    lft = kb.sb("lft", [128, nb], F32); d_lft = Dep("lft")
    ct = kb.sb("ct", [128, nb], F32); d_ct = Dep("ct")
    tot = kb.sb("tot", [128, nb], F32); d_tot = Dep("tot")
    rall = kb.sb("rall", [128, nb], F32); d_rall = Dep("rall")
    biasr = kb.ring("bias", [128, nb], F32, 2)
    pr = kb.ring("P", [128, 512], BF16, 4)
    osb = kb.ring("osb", [FD + 1, 512], F32, 2)
    psS = kb.ring("psS", [128, 512], F32, 4, psum=True)
    psO = kb.ring("psO", [128, 512], F32, 2, psum=True)
    psM = kb.ring("psM", [128, 512], F32, 2, psum=True)
    si = 0
    oi = 0
    for bh in range(nbh):
        kb.dma("sp", QA[0:FD, :], qT[bh], W=[d_QA])
        kb.dma("sp", QA[FD:FD + 1, :], drow[bh:bh + 1, :], R=[d_drow], W=[d_QA])
        kb.dma("sp", KA[0:FD, :], kT[bh], W=[d_KA])
        kb.op("pool", lambda e: e.memset(KA[FD:FD + 1, :], 1.0), W=[d_KA])
        kb.dma("sp", VA[:, :, 0:FD], vv[bh].rearrange("(j p) d -> p j d", p=128), W=[d_VA])
        kb.op("pool", lambda e: e.memset(VA[:, :, FD:FD + 1], 1.0), W=[d_VA])
        kb.op("pool", lambda e: e.memset(VA[0:112, 0, :], 0.0), W=[d_VA])
        kb.dma("sp", lft[:], lfT[bh], W=[d_lft])
        pm0, d_pm0 = psM[0]
        pm1, d_pm1 = psM[1]
        kb.op("pe", lambda e: e.matmul(pm0[:, 0:nb], lhsT=trif[:], rhs=lft[:], start=True, stop=True),
              R=[d_tri, d_lft], W=[d_pm0])
        kb.op("pe", lambda e: e.matmul(pm1[:, 0:nb], lhsT=onesf[:], rhs=lft[:], start=True, stop=True),
              R=[d_ones, d_lft], W=[d_pm1])
        kb.op("dve", lambda e: e.tensor_copy(out=tot[:], in_=pm1[:, 0:nb]), R=[d_pm1], W=[d_tot])
        kb.op("dve", lambda e: e.tensor_tensor_scan(out=ct[:], data0=onerow[:], data1=tot[:], initial=0.0,
                                                     op0=ALU.mult, op1=ALU.add), R=[d_tot, d_or], W=[d_ct])
        kb.op("dve", lambda e: e.tensor_tensor(out=ct[:], in0=ct[:], in1=tot[:], op=ALU.subtract),
              R=[d_ct, d_tot], W=[d_ct])
        kb.op("dve", lambda e: e.tensor_tensor(out=ct[:], in0=ct[:], in1=pm0[:, 0:nb], op=ALU.add),
              R=[d_ct, d_pm0], W=[d_ct])
        kb.op("pe", lambda e: e.matmul(pm1[:, 0:nb], lhsT=sel0[:], rhs=ct[:], start=True, stop=True),
              R=[d_sel, d_ct], W=[d_pm1])
        kb.op("dve", lambda e: e.tensor_copy(out=rall[:], in_=pm1[:, 0:nb]), R=[d_pm1], W=[d_rall])
        steps = []
        for I in range(nI):
            j0 = 0 if I == 0 else 4 * I - 3
            nblk = 1 if I == 0 else 4
            nJ = j0 + nblk
            for J in range(nJ):
                steps.append((I, J, j0, nblk, nJ))
        LA = 2
        cur = {}
        for idx in range(len(steps) + LA):
            if idx < len(steps):
                I, J, j0, nblk, nJ = steps[idx]
                q0 = j0 * 128
                bias_t, d_bias = biasr[I % 2]
                if J == 0:
                    kb.op("dve", lambda e: e.tensor_scalar(out=bias_t[:, 0:nJ], in0=ct[:, 0:nJ], scalar1=-1.0,
                                                            scalar2=rall[:, j0:j0 + 1], op0=ALU.mult, op1=ALU.add),
                          R=[d_ct, d_rall], W=[d_bias])
                m = max(0, J - j0)
                c0 = m * 128
                c1 = nblk * 128
                ps, d_ps = psS[idx % 4]
                p_t, d_p = pr[idx % 4]
                kb.op("pe", lambda e: e.matmul(ps[:, c0:c1], lhsT=KA[:, J * 128:(J + 1) * 128],
                                               rhs=QA[:, q0 + c0:q0 + c1], start=True, stop=True),
                      R=[d_KA, d_QA], W=[d_ps])
                kb.op("act", lambda e: e.activation(out=p_t[:, c0:c1], in_=ps[:, c0:c1], func=AF.Exp,
                                                    bias=bias_t[:, J:J + 1], scale=1.0),
                      R=[d_ps, d_bias], W=[d_p])
                if J >= j0:
                    kb.op("dve", lambda e: e.tensor_tensor(out=p_t[:, c0:c0 + 128], in0=p_t[:, c0:c0 + 128],
                                                            in1=maskb[:], op=ALU.mult), R=[d_p, d_mask], W=[d_p])
            if idx >= LA:
                I, J, j0, nblk, nJ = steps[idx - LA]
                q0 = j0 * 128
                m = max(0, J - j0)
                c0 = m * 128
                c1 = nblk * 128
                p_t, d_p = pr[(idx - LA) % 4]
                po, d_po = psO[I % 2]
                kb.op("pe", lambda e: e.matmul(po[0:FD + 1, c0:c1], lhsT=VA[:, J, :], rhs=p_t[:, c0:c1],
                                               start=(J == 0), stop=(J == nJ - 1)),
                      R=[d_VA, d_p], W=[d_po])
                if J == nJ - 1:
                    o_t, d_o = osb[I % 2]
                    ncol = nblk * 128
                    kb.op("dve", lambda e: e.tensor_copy(out=o_t[:, 0:ncol], in_=po[0:FD + 1, 0:ncol]),
                          R=[d_po], W=[d_o])
                    kb.dma("sp", oT[bh, :, q0:q0 + ncol], o_t[:, 0:ncol], R=[d_o])
    kb.finish()
    return nc


BIGIDX = 1.0e6


def build_CE(stage, nt, has_meta, cap=CAP):
    nc = bass.Bass("TRN2", target_bir_lowering=False)
    rows = nt * 128
    nslot = NE * cap
    nst = cap // 128
    a_in = nc.dram_tensor("a_in", [rows, D], F32, kind="ExternalInput").ap()
    hp_in = nc.dram_tensor("hp_in", [rows, D], F32, kind="ExternalInput").ap()
    if stage == "C":
        den_in = nc.dram_tensor("den_in", [rows, FH], F32, kind="ExternalInput").ap()
    else:
        gs_in = nc.dram_tensor("gs_in", [rows, D], BF16, kind="ExternalInput").ap()
    valid_in = nc.dram_tensor("valid_in", [rows, 1], F32, kind="ExternalInput").ap()
    w_out = nc.dram_tensor("w_out", [D, D], F32, kind="ExternalInput").ap()
    mnorm = nc.dram_tensor("mnorm", [1, D], F32, kind="ExternalInput").ap()
    w_r = nc.dram_tensor("w_r", [D, 36], F32, kind="ExternalInput").ap()
    b_r = nc.dram_tensor("b_r", [1, 36], F32, kind="ExternalInput").ap()
    w_up = nc.dram_tensor("w_up", [NE, D, 2 * DE], F32, kind="ExternalInput").ap()
    w_dn = nc.dram_tensor("w_dn", [NE, DE, D], F32, kind="ExternalInput").ap()
    h2_out = nc.dram_tensor("h2_out", [rows, D], F32, kind="ExternalOutput").ap()
    if stage == "C":
        hnorm = nc.dram_tensor("hnorm", [1, D], F32, kind="ExternalInput").ap()
        hw_in = nc.dram_tensor("hw_in", [D, 4 * D], F32, kind="ExternalInput").ap()
        hbf = nc.dram_tensor("hbf", [1, D], F32, kind="ExternalInput").ap()
        lbl = nc.dram_tensor("lbl", [2, D], F32, kind="ExternalInput").ap()
        q1_out = nc.dram_tensor("q1_out", [rows, D], BF16, kind="ExternalOutput").ap()
        lf1_out = nc.dram_tensor("lf1_out", [rows, D], F32, kind="ExternalOutput").ap()
        v1_out = nc.dram_tensor("v1_out", [rows, D], BF16, kind="ExternalOutput").ap()
        gs_out = nc.dram_tensor("gs_out", [rows, D], BF16, kind="ExternalOutput").ap()
    xpad = nc.dram_tensor("xpad", [nslot, D], BF16, kind="Internal").ap()
    ypad = nc.dram_tensor("ypad", [nslot, D], BF16, kind="Internal").ap()
    h1s = nc.dram_tensor("h1s", [rows, D], F32, kind="Internal").ap()
    d_xpad, d_ypad, d_h1s = Dep("xpad"), Dep("ypad"), Dep("h1s")

    kb = KB(nc)
    identf, d_idf, ident, d_id = make_ident(kb)
    breg = nc.gpsimd.to_reg(nslot - 1)
    dest_i = kb.sb("dest_i", [128, nt, 2], I32); d_dest = Dep("dest_i")
    gw = kb.sb("gw", [128, nt, 2], F32); d_gw = Dep("gw")
    cntb = kb.sb("cntb", [128, NE], F32); d_cntb = Dep("cntb")
    usb = kb.sb("usb", [128, 128], BF16); d_us = Dep("usb")
    onesb = kb.sb("onesb", [128, 128], BF16); d_onesb = Dep("onesb")
    onesf = kb.sb("onesf", [128, 128], F32); d_onesf = Dep("onesf")
    tmpf = kb.sb("tmpf", [128, 128], F32); d_tmpf = Dep("tmpf")
    kb.op("pool", lambda e: e.memset(onesf[:], 1.0), W=[d_onesf])
    kb.op("pool", lambda e: e.affine_select(out=tmpf[:], in_=onesf[:], pattern=[[1, 128]], compare_op=ALU.is_gt,
                                            fill=0.0, base=0, channel_multiplier=-1), R=[d_onesf], W=[d_tmpf])
    kb.op("dve", lambda e: e.tensor_copy(out=usb[:], in_=tmpf[:]), R=[d_tmpf], W=[d_us])
    kb.op("dve", lambda e: e.tensor_copy(out=onesb[:], in_=onesf[:]), R=[d_onesf], W=[d_onesb])
    kb.op("pool", lambda e: e.iota(cntb[:], pattern=[[cap, NE]], base=0, channel_multiplier=0,
                                   allow_small_or_imprecise_dtypes=True), W=[d_cntb])
    for d in (d_us, d_onesb, d_onesf):
        d.ro = True
    mn_t, d_mn = load_bc(kb, "mnorm", mnorm, D)
    br_t, d_br = load_bc(kb, "b_r", b_r, 36)
    wr_t, d_wr = load_w(kb, "w_r", w_r, D, 36, dt=F32)
    d_wr.ro = True

    with kb.scope():
        wo_t, d_wo = load_w(kb, "w_out", w_out, D, D)
        d_wo.ro = True
        a_r = kb.ring("a", [128, D], F32, 2)
        hp_r = kb.ring("hp", [128, D], F32, 2)
        den_r = kb.ring("den", [128, FH], F32, 2)
        gs_r = kb.ring("gs", [128, D], BF16, 2)
        ob_r = kb.ring("ob", [128, D], BF16, 2)
        oT_r = kb.ring("oT", [128, 8, 128], BF16, 2)
        h1_r = kb.ring("h1", [128, D], F32, 2)
        scr_r = kb.ring("scr", [128, D], F32, 2)
        ss_r = kb.ring("ss", [128, 1], F32, 2)
        hmf_r = kb.ring("hmf", [128, D], F32, 2)
        hmb_r = kb.ring("hmb", [128, D], BF16, 2)
        hmT_r = kb.ring("hmT", [128, 8, 128], F32, 2)
        sm_r = kb.ring("sm", [128, 256], F32, 2)
        ab_r = kb.ring("ab", [128, NE], BF16, 2)
        val_r = kb.ring("val", [128, 2], F32, 2)
        pst_r = kb.ring("pst", [128, 8, 128], BF16, 2, psum=True)
        pstf_r = kb.ring("pstf", [128, 8, 128], F32, 1, psum=True)
        pmm_r = kb.ring("pmm", [128, 512], F32, 2, psum=True)
        prt_r = kb.ring("prt", [128, 512], F32, 2, psum=True)
        for t in range(nt):
            b = t % 2
            r0, r1 = t * 128, (t + 1) * 128
            a_t, d_a = a_r[b]
            hp_t, d_hp = hp_r[b]
            ob_t, d_ob = ob_r[b]
            kb.dma("sp", a_t[:], a_in[r0:r1, :], W=[d_a])
            kb.dma("sp", hp_t[:], hp_in[r0:r1, :], W=[d_hp])
            if stage == "C":
                den_t, d_den = den_r[b]
                kb.dma("sp", den_t[:], den_in[r0:r1, :], W=[d_den])
                kb.op("dve", lambda e: e.tensor_scalar(out=den_t[:], in0=den_t[:], scalar1=1e-30, scalar2=None,
                                                        op0=ALU.max), R=[d_den], W=[d_den])
                kb.op("dve", lambda e: e.reciprocal(out=den_t[:], in_=den_t[:]), R=[d_den], W=[d_den])
                kb.op("dve", lambda e: e.tensor_tensor(
                    out=ob_t[:].rearrange("p (h d) -> p h d", d=FD), in0=a_t[:].rearrange("p (h d) -> p h d", d=FD),
                    in1=den_t[:].unsqueeze(2).to_broadcast([128, FH, FD]), op=ALU.mult), R=[d_a, d_den], W=[d_ob])
            else:
                gs_t, d_gs = gs_r[b]
                kb.dma("sp", gs_t[:], gs_in[r0:r1, :], W=[d_gs])
                kb.op("dve", lambda e: e.tensor_tensor(out=ob_t[:], in0=a_t[:], in1=gs_t[:], op=ALU.mult),
                      R=[d_a, d_gs], W=[d_ob])
            oT_t, d_oT = oT_r[b]
            ps_t, d_pst = pst_r[b]
            transpose_chunks(kb, ob_t, d_ob, 8, ps_t, d_pst, oT_t, d_oT, ident, d_id, copy_eng="act")
            h1_t, d_h1 = h1_r[b]
            for half in range(2):
                p_t, d_p = pmm_r[half]
                mm_acc(kb, p_t[:], d_p, oT_t, d_oT, wo_t, d_wo, half * 512, half * 512 + 512)
                kb.op("dve", lambda e: e.tensor_tensor(out=h1_t[:, half * 512:(half + 1) * 512], in0=p_t[:],
                                                        in1=hp_t[:, half * 512:(half + 1) * 512], op=ALU.add),
                      R=[d_p, d_hp], W=[d_h1])
            kb.dma("sp", h1s[r0:r1, :], h1_t[:], R=[d_h1], W=[d_h1s])
            sc_t, d_sc = scr_r[b]
            ss_t, d_ss = ss_r[b]
            hmf_t, d_hmf = hmf_r[b]
            hmb_t, d_hmb = hmb_r[b]
            rms_rstd(kb, h1_t[:], d_h1, D, sc_t[:], d_sc, ss_t[:], d_ss)
            kb.op("dve", lambda e: e.scalar_tensor_tensor(out=hmf_t[:], in0=h1_t[:], scalar=ss_t[:, 0:1], in1=mn_t[:],
                                                           op0=ALU.mult, op1=ALU.mult),
                  R=[d_h1, d_ss, d_mn], W=[d_hmf])
            kb.op("pool", lambda e: e.tensor_copy(out=hmb_t[:], in_=hmf_t[:]), R=[d_hmf], W=[d_hmb])
            pf_t, d_pf = pstf_r[0]
            hmT_t, d_hmT = hmT_r[b]
            for c in range(8):
                kb.op("pe", lambda e: e.transpose(out=pf_t[:, c, :], in_=hmf_t[:, c * 128:(c + 1) * 128],
                                                  identity=identf[:]), R=[d_hmf, d_idf], W=[d_pf])
            kb.op("act", lambda e: e.copy(out=hmT_t[:], in_=pf_t[:]), R=[d_pf], W=[d_hmT])
            pr_t, d_pr = prt_r[b]
            for c in range(8):
                kb.op("pe", lambda e: e.matmul(pr_t[:, 0:36], lhsT=hmT_t[:, c, :], rhs=wr_t[:, c, :],
                                               start=(c == 0), stop=(c == 7)), R=[d_hmT, d_wr], W=[d_pr])
            sm, d_sm = sm_r[b]
            lg = sm[:, 0:36]
            gmax = sm[:, 36:37]
            ngmax = sm[:, 37:38]
            gsum = sm[:, 38:39]
            ggate = sm[:, 39:40]
            eg = sm[:, 40:44]
            ohg = sm[:, 44:48]
            esel = sm[:, 48:56]
            top8 = sm[:, 56:64]
            oh0 = sm[:, 64:72]
            oh1 = sm[:, 72:80]
            A0 = sm[:, 80:112]
            A1 = sm[:, 112:144]
            sbt = sm[:, 144:176]
            tmp = sm[:, 176:208]
            destf = sm[:, 208:210]
            diff = sm[:, 210:211]
            sg = sm[:, 211:212]
            S = [d_sm]
            dv = lambda fn, R=(), W=(): kb.op("dve", fn, R=list(R) + S, W=list(W) + S)
            dv(lambda e: e.tensor_tensor(out=lg, in0=pr_t[:, 0:36], in1=br_t[:], op=ALU.add), R=[d_pr, d_br])
            dv(lambda e: e.tensor_reduce(out=gmax, in_=lg[:, 0:4], axis=AX.X, op=ALU.max))
            dv(lambda e: e.tensor_scalar(out=ngmax, in0=gmax, scalar1=-1.0, scalar2=None, op0=ALU.mult))
            kb.op("act", lambda e: e.activation(out=eg, in_=lg[:, 0:4], func=AF.Exp, bias=ngmax, scale=1.0,
                                                accum_out=gsum), R=S, W=S)
            dv(lambda e: e.reciprocal(out=ggate, in_=gsum))
            dv(lambda e: e.tensor_scalar(out=ohg, in0=lg[:, 0:4], scalar1=gmax, scalar2=None, op0=ALU.is_equal))
            dv(lambda e: e.tensor_scalar(out=esel, in0=lg[:, 4:12], scalar1=ohg[:, 0:1], scalar2=None, op0=ALU.mult))
            for g in range(1, 4):
                dv(lambda e: e.scalar_tensor_tensor(out=esel, in0=lg[:, 4 + 8 * g:12 + 8 * g], scalar=ohg[:, g:g + 1],
                                                    in1=esel, op0=ALU.mult, op1=ALU.add))
            dv(lambda e: e.max(out=top8, in_=esel))
            dv(lambda e: e.tensor_scalar(out=oh0, in0=esel, scalar1=top8[:, 0:1], scalar2=None, op0=ALU.is_equal))
            dv(lambda e: e.tensor_scalar(out=oh1, in0=esel, scalar1=top8[:, 1:2], scalar2=None, op0=ALU.is_equal))
            for (Ak, ohk) in ((A0, oh0), (A1, oh1)):
                dv(lambda e: e.tensor_tensor(out=Ak.rearrange("p (g j) -> p g j", j=8),
                                             in0=ohg.unsqueeze(2).to_broadcast([128, 4, 8]),
                                             in1=ohk.unsqueeze(1).to_broadcast([128, 4, 8]), op=ALU.mult))
            use_valid = has_meta and t == 0
            if use_valid:
                val_t, d_val = val_r[b]
                kb.dma("sp", val_t[:, 0:1], valid_in[r0:r1, :], W=[d_val])
                kb.op("dve", lambda e: e.tensor_scalar(out=val_t[:, 1:2], in0=val_t[:, 0:1], scalar1=-BIGIDX,
                                                        scalar2=BIGIDX, op0=ALU.mult, op1=ALU.add),
                      R=[d_val], W=[d_val])
                for Ak in (A0, A1):
                    dv(lambda e: e.tensor_scalar(out=Ak, in0=Ak, scalar1=val_t[:, 0:1], scalar2=None, op0=ALU.mult),
                       R=[d_val])
            ab_t, d_ab = ab_r[b]
            kb.op("dve", lambda e: e.tensor_tensor(out=ab_t[:], in0=A0, in1=A1, op=ALU.add), R=S, W=[d_ab])
            kb.op("pe", lambda e: e.matmul(pr_t[:, 64:96], lhsT=usb[:], rhs=ab_t[:], start=True, stop=True),
                  R=[d_us, d_ab], W=[d_pr])
            kb.op("pe", lambda e: e.matmul(pr_t[:, 96:128], lhsT=onesb[:], rhs=ab_t[:], start=True, stop=True),
                  R=[d_onesb, d_ab], W=[d_pr])
            dv(lambda e: e.tensor_tensor(out=sbt, in0=pr_t[:, 64:96], in1=cntb[:], op=ALU.add), R=[d_pr, d_cntb])
            kb.op("dve", lambda e: e.tensor_tensor(out=cntb[:], in0=cntb[:], in1=pr_t[:, 96:128], op=ALU.add),
                  R=[d_pr, d_cntb] + S, W=[d_cntb])
            for k, Ak in enumerate((A0, A1)):
                dv(lambda e: e.tensor_tensor(out=tmp, in0=Ak, in1=sbt, op=ALU.mult))
                dv(lambda e: e.tensor_reduce(out=destf[:, k:k + 1], in_=tmp, axis=AX.X, op=ALU.add))
            if use_valid:
                dv(lambda e: e.tensor_scalar(out=destf, in0=destf, scalar1=val_t[:, 0:1], scalar2=val_t[:, 1:2],
                                             op0=ALU.mult, op1=ALU.add), R=[d_val])
            kb.op("dve", lambda e: e.tensor_copy(out=dest_i[:, t, :], in_=destf), R=S, W=[d_dest])
            dv(lambda e: e.tensor_tensor(out=diff, in0=top8[:, 0:1], in1=top8[:, 1:2], op=ALU.subtract))
            kb.op("act", lambda e: e.activation(out=sg, in_=diff, func=AF.Sigmoid), R=S, W=S)
            kb.op("dve", lambda e: e.tensor_tensor(out=gw[:, t, 0:1], in0=sg, in1=ggate, op=ALU.mult), R=S, W=[d_gw])
            kb.op("dve", lambda e: e.tensor_tensor(out=gw[:, t, 1:2], in0=ggate, in1=gw[:, t, 0:1], op=ALU.subtract),
                  R=S + [d_gw], W=[d_gw])
            for k in range(2):
                kb.idma(out=xpad[:, :], out_off=bass.IndirectOffsetOnAxis(ap=dest_i[:, t, k:k + 1], axis=0),
                        in_=hmb_t[:], in_off=None, R=[d_hmb, d_dest], W=[d_xpad],
                        bounds_check=breg, oob_is_err=False)

    with kb.scope():
        wup_r = kb.ring("wup", [128, 8, 2 * DE], BF16, 2)
        wdn_r = kb.ring("wdn", [128, 4, D], BF16, 2)
        xs_r = kb.ring("xs", [128, D], BF16, 3)
        xT_r = kb.ring("xT", [128, 8, cap], BF16, 2)
        sa_r = kb.ring("sa", [128, cap], F32, 2)
        aT_r = kb.ring("aT", [128, 4, cap], BF16, 2)
        yb_r = kb.ring("yb", [128, D], BF16, 2)
        pst_r = kb.ring("pst", [128, 8, 128], BF16, 2, psum=True)
        pau_r = kb.ring("pau", [128, 512], F32, 4, psum=True)
        py_r = kb.ring("py", [128, 512], F32, 2, psum=True)
        xi = 0
        yi = 0
        for ex in range(NE):
            b = ex % 2
            wup_t, d_wup = wup_r[b]
            wdn_t, d_wdn = wdn_r[b]
            wuv = w_up[ex].rearrange("(c p) n -> p c n", p=128)
            wdv = w_dn[ex].rearrange("(c p) n -> p c n", p=128)
            for c in range(8):
                kb.dma("pool", wup_t[:, c, :], wuv[:, c, :], W=[d_wup])
            for c in range(4):
                kb.dma("pool", wdn_t[:, c, :], wdv[:, c, :], W=[d_wdn])
            xT_t, d_xT = xT_r[b]
            for st in range(nst):
                xs_t, d_xs = xs_r[xi % 3]
                ps_t, d_pst = pst_r[xi % 2]
                xi += 1
                s0 = ex * cap + st * 128
                kb.dma("sp", xs_t[:], xpad[s0:s0 + 128, :], R=[d_xpad], W=[d_xs])
                for c in range(8):
                    kb.op("pe", lambda e: e.transpose(out=ps_t[:, c, :], in_=xs_t[:, c * 128:(c + 1) * 128],
                                                      identity=ident[:]), R=[d_xs, d_id], W=[d_pst])
                kb.op("act" if st % 2 else "dve",
                      (lambda e: e.copy(out=xT_t[:, :, st * 128:(st + 1) * 128], in_=ps_t[:])) if st % 2 else
                      (lambda e: e.tensor_copy(out=xT_t[:, :, st * 128:(st + 1) * 128], in_=ps_t[:])),
                      R=[d_pst], W=[d_xT])
            aT_t, d_aT = aT_r[b]
            for fc in range(4):
                pa, d_pa = pau_r[(2 * fc) % 4]
                pu, d_pu = pau_r[(2 * fc + 1) % 4]
                for c in range(8):
                    kb.op("pe", lambda e: e.matmul(pa[:, 0:cap], lhsT=wup_t[:, c, fc * 128:(fc + 1) * 128],
                                                   rhs=xT_t[:, c, :], start=(c == 0), stop=(c == 7)),
                          R=[d_wup, d_xT], W=[d_pa])
                for c in range(8):
                    kb.op("pe", lambda e: e.matmul(pu[:, 0:cap], lhsT=wup_t[:, c, DE + fc * 128:DE + (fc + 1) * 128],
                                                   rhs=xT_t[:, c, :], start=(c == 0), stop=(c == 7)),
                          R=[d_wup, d_xT], W=[d_pu])
                sa_t, d_sa = sa_r[fc % 2]
                kb.op("act", lambda e: e.activation(out=sa_t[:], in_=pa[:, 0:cap], func=AF.Silu), R=[d_pa], W=[d_sa])
                kb.op("dve", lambda e: e.tensor_tensor(out=aT_t[:, fc, :], in0=sa_t[:], in1=pu[:, 0:cap], op=ALU.mult),
                      R=[d_sa, d_pu], W=[d_aT])
            for st in range(nst):
                yb_t, d_yb = yb_r[yi % 2]
                yi += 1
                for half in range(2):
                    py, d_py = py_r[half]
                    for fc in range(4):
                        kb.op("pe", lambda e: e.matmul(py[:], lhsT=aT_t[:, fc, st * 128:(st + 1) * 128],
                                                       rhs=wdn_t[:, fc, half * 512:(half + 1) * 512],
                                                       start=(fc == 0), stop=(fc == 3)),
                              R=[d_aT, d_wdn], W=[d_py])
                    if half == 0:
                        kb.op("act", lambda e: e.copy(out=yb_t[:, 0:512], in_=py[:]), R=[d_py], W=[d_yb])
                    else:
                        kb.op("dve", lambda e: e.tensor_copy(out=yb_t[:, 512:1024], in_=py[:]), R=[d_py], W=[d_yb])
                s0 = ex * cap + st * 128
                kb.dma("sp", ypad[s0:s0 + 128, :], yb_t[:], R=[d_yb], W=[d_ypad])

    with kb.scope():
        h1_r = kb.ring("h1", [128, D], F32, 2)
        y_r = kb.ring("y", [128, D], BF16, 4)
        h2_r = kb.ring("h2", [128, D], F32, 2)
        for (y_t, d_y) in y_r:
            kb.op("pool", lambda e: e.memset(y_t[:], 0.0), W=[d_y])
        if stage == "C":
            hw_t, d_hw = load_w(kb, "hw_in", hw_in, D, 4 * D)
            d_hw.ro = True
            hn_t, d_hn = load_bc(kb, "hnorm", hnorm, D)
            hbf_t, d_hbf = load_bc(kb, "hbf", hbf, D)
            l0_t, d_l0 = load_bc(kb, "l0", lbl[0:1, :], D)
            l1_t, d_l1 = load_bc(kb, "l1", lbl[1:2, :], D)
            lb_t = kb.sb("lb", [128, D], F32); d_lb = Dep("lb")
            oml_t = kb.sb("oml", [128, D], F32); d_oml = Dep("oml")
            kb.op("dve", lambda e: e.tensor_tensor(out=lb_t[:], in0=l1_t[:], in1=l0_t[:], op=ALU.subtract),
                  R=[d_l0, d_l1], W=[d_lb])
            kb.op("act", lambda e: e.activation(out=oml_t[:], in_=lb_t[:], func=AF.Sigmoid, scale=-1.0),
                  R=[d_lb], W=[d_oml])
            kb.op("act", lambda e: e.activation(out=lb_t[:], in_=lb_t[:], func=AF.Sigmoid), R=[d_lb], W=[d_lb])
            d_lb.ro = True
            d_oml.ro = True
            scr_r = kb.ring("scr", [128, D], F32, 2)
            ss_r = kb.ring("ss", [128, 1], F32, 2)
            xn_r = kb.ring("xn", [128, D], BF16, 2)
            xT_r = kb.ring("xT", [128, 8, 128], BF16, 2)
            pst_r = kb.ring("pst", [128, 8, 128], BF16, 2, psum=True)
            pmm_r = kb.ring("pmm", [128, 512], F32, 4, psum=True)
            ob_r = kb.ring("obf", [128, D], BF16, 4)
            of_r = kb.ring("off", [128, D], F32, 2)
            zf_r = kb.ring("zf", [128, 512], F32, 2)
        pi = 0
        obi = 0
        for t in range(nt):
            b = t % 2
            r0, r1 = t * 128, (t + 1) * 128
            h1_t, d_h1 = h1_r[b]
            h2_t, d_h2 = h2_r[b]
            y0_t, d_y0 = y_r[2 * b]
            y1_t, d_y1 = y_r[2 * b + 1]
            kb.dma("sp", h1_t[:], h1s[r0:r1, :], R=[d_h1s], W=[d_h1])
            for k, (y_t, d_y) in enumerate(((y0_t, d_y0), (y1_t, d_y1))):
                kb.idma(out=y_t[:], out_off=None, in_=ypad[:, :],
                        in_off=bass.IndirectOffsetOnAxis(ap=dest_i[:, t, k:k + 1], axis=0),
                        R=[d_ypad, d_dest], W=[d_y], bounds_check=breg, oob_is_err=False)
            kb.op("dve", lambda e: e.scalar_tensor_tensor(out=h2_t[:], in0=y0_t[:], scalar=gw[:, t, 0:1], in1=h1_t[:],
                                                           op0=ALU.mult, op1=ALU.add), R=[d_y0, d_gw, d_h1], W=[d_h2])
            kb.op("dve", lambda e: e.scalar_tensor_tensor(out=h2_t[:], in0=y1_t[:], scalar=gw[:, t, 1:2], in1=h2_t[:],
                                                           op0=ALU.mult, op1=ALU.add), R=[d_y1, d_gw, d_h2], W=[d_h2])
            kb.dma("sp", h2_out[r0:r1, :], h2_t[:], R=[d_h2])
            if stage != "C":
                continue
            sc_t, d_sc = scr_r[b]
            ss_t, d_ss = ss_r[b]
            xn_t, d_xn = xn_r[b]
            xT_t, d_xT = xT_r[b]
            ps_t, d_pst = pst_r[b]
            rms_rstd(kb, h2_t[:], d_h2, D, sc_t[:], d_sc, ss_t[:], d_ss)
            kb.op("dve", lambda e: e.scalar_tensor_tensor(out=xn_t[:], in0=h2_t[:], scalar=ss_t[:, 0:1], in1=hn_t[:],
                                                           op0=ALU.mult, op1=ALU.mult), R=[d_h2, d_ss, d_hn], W=[d_xn])
            transpose_chunks(kb, xn_t, d_xn, 8, ps_t, d_pst, xT_t, d_xT, ident, d_id)
            for grp, (dram_o, kind) in enumerate(((q1_out, "silu"), (lf1_out, "f"), (v1_out, "copy"), (gs_out, "silu"))):
                if kind == "f":
                    o_t, d_o = of_r[b]
                else:
                    o_t, d_o = ob_r[obi % 4]
                    obi += 1
                for half in range(2):
                    p_t, d_p = pmm_r[pi % 4]
                    pi += 1
                    c0 = grp * 1024 + half * 512
                    hs = slice(half * 512, (half + 1) * 512)
                    mm_acc(kb, p_t[:], d_p, xT_t, d_xT, hw_t, d_hw, c0, c0 + 512)
                    if kind == "silu":
                        kb.op("act", lambda e: e.activation(out=o_t[:, hs], in_=p_t[:], func=AF.Silu), R=[d_p], W=[d_o])
                    elif kind == "copy":
                        kb.op("act", lambda e: e.copy(out=o_t[:, hs], in_=p_t[:]), R=[d_p], W=[d_o])
                    else:
                        z_t, d_z = zf_r[half]
                        kb.op("dve", lambda e: e.tensor_tensor(out=z_t[:], in0=p_t[:], in1=hbf_t[:, hs], op=ALU.add),
                              R=[d_p, d_hbf], W=[d_z])
                        kb.op("act", lambda e: e.activation(out=z_t[:], in_=z_t[:], func=AF.Sigmoid), R=[d_z], W=[d_z])
                        kb.op("dve", lambda e: e.tensor_tensor(out=z_t[:], in0=z_t[:], in1=oml_t[:, hs], op=ALU.mult),
                              R=[d_z, d_oml], W=[d_z])
                        kb.op("dve", lambda e: e.tensor_tensor(out=z_t[:], in0=z_t[:], in1=lb_t[:, hs], op=ALU.add),
                              R=[d_z, d_lb], W=[d_z])
                        kb.op("act", lambda e: e.activation(out=o_t[:, hs], in_=z_t[:], func=AF.Ln), R=[d_z], W=[d_o])
                kb.dma("sp", dram_o[r0:r1, :], o_t[:], R=[d_o])
    kb.finish()
    return nc


def build_D(nb, nbh):
    L = nb * 128
    nc = bass.Bass("TRN2", target_bir_lowering=False)
    qT = nc.dram_tensor("qT", [nbh, 128, L], BF16, kind="ExternalInput").ap()
    lfT = nc.dram_tensor("lfT", [nbh, 128, L], F32, kind="ExternalInput").ap()
    vv = nc.dram_tensor("v", [nbh, L, 128], BF16, kind="ExternalInput").ap()
    go = nc.dram_tensor("go", [1, 128], F32, kind="ExternalInput").ap()
    on = nc.dram_tensor("on", [nbh, L, 128], F32, kind="ExternalOutput").ap()
    kb = KB(nc)
    identf, d_idf, ident, d_id = make_ident(kb)
    go_t, d_go = load_bc(kb, "go", go, 128)
    onesf = kb.sb("onesf", [128, 128], F32); d_onesf = Dep("onesf")
    mf = kb.sb("mf", [128, 128], F32); d_mf = Dep("mf")
    mku = kb.sb("mku", [128, 128], U32); d_mk = Dep("mku")
    kb.op("pool", lambda e: e.memset(onesf[:], 1.0), W=[d_onesf])
    kb.op("pool", lambda e: e.affine_select(out=mf[:], in_=onesf[:], pattern=[[1, 128]], compare_op=ALU.is_ge,
                                            fill=0.0, base=0, channel_multiplier=-1), R=[d_onesf], W=[d_mf])
    kb.op("dve", lambda e: e.tensor_copy(out=mku[:], in_=mf[:]), R=[d_mf], W=[d_mk])
    d_mk.ro = True
    d_onesf.ro = True
    q_t = kb.sb("q", [128, L], BF16); d_q = Dep("q")
    lf_t = kb.sb("lf", [128, L], F32); d_lf = Dep("lf")
    v_t = kb.sb("v", [128, nb, 128], BF16); d_v = Dep("v")
    S_t = kb.sb("S", [128, 128], F32); d_S = Dep("S")
    Sb_r = kb.ring("Sb", [128, 128], BF16, 2)
    b_r = kb.ring("b", [128, 128], F32, 2)
    col_r = kb.ring("col", [128, 8], F32, 2)
    e1_r = kb.ring("e1", [128, 128], F32, 2)
    e2_r = kb.ring("e2", [128, 128], F32, 2)
    kk_r = kb.ring("kk", [128, 128], F32, 2)
    qd_r = kb.ring("qd", [128, 128], BF16, 2)
    kd_r = kb.ring("kd", [128, 128], BF16, 2)
    qb_r = kb.ring("qb", [128, 128], BF16, 2)
    ke_r = kb.ring("ke", [128, 128], BF16, 2)
    keT_r = kb.ring("keT", [128, 128], BF16, 2)
    scm_r = kb.ring("scm", [128, 128], BF16, 2)
    osq_r = kb.ring("osq", [128, 128], F32, 2)
    oss_r = kb.ring("oss", [128, 2], F32, 2)
    on_r = kb.ring("on", [128, 128], F32, 2)
    psc_r = kb.ring("psc", [128, 512], F32, 2, psum=True)
    po_r = kb.ring("po", [128, 512], F32, 2, psum=True)
    pt_r = kb.ring("pt", [128, 8, 128], BF16, 2, psum=True)
    pu_r = kb.ring("pu", [128, 512], F32, 2, psum=True)
    for (t_, d_) in scm_r:
        kb.op("pool", lambda e: e.memset(t_[:], 0.0), W=[d_])
    for bh in range(nbh):
        kb.dma("sp", q_t[:], qT[bh], W=[d_q])
        for hlf in range(4):
            c0 = (L // 4) * hlf
            kb.dma("sp", lf_t[:, c0:c0 + L // 4], lfT[bh][:, c0:c0 + L // 4], W=[d_lf])
        kb.dma("sp", v_t[:], vv[bh].rearrange("(j p) d -> p j d", p=128), W=[d_v])
        kb.op("pool", lambda e: e.memset(S_t[:], 0.0), W=[d_S])
        kb.op("pool", lambda e: e.memset(Sb_r[0][0][:], 0.0), W=[Sb_r[0][1]])
        for c in range(nb):
            r = c % 2
            cs = slice(c * 128, (c + 1) * 128)
            b_t, d_b = b_r[r]
            col, d_col = col_r[r]
            e1, d_e1 = e1_r[r]
            e2, d_e2 = e2_r[r]
            kk, d_kk = kk_r[r]
            qd, d_qd = qd_r[r]
            kd, d_kd = kd_r[r]
            qb, d_qb = qb_r[r]
            ke, d_ke = ke_r[r]
            keT, d_keT = keT_r[r]
            scm, d_scm = scm_r[r]
            kb.op("dve", lambda e: e.tensor_tensor_scan(out=b_t[:], data0=onesf[:], data1=lf_t[:, cs], initial=0.0,
                                                         op0=ALU.mult, op1=ALU.add), R=[d_lf, d_onesf], W=[d_b])
            kb.op("dve", lambda e: e.tensor_copy(out=col[:, 0:1], in_=b_t[:, 63:64]), R=[d_b], W=[d_col])
            kb.op("dve", lambda e: e.tensor_tensor(out=col[:, 1:2], in0=b_t[:, 127:128], in1=b_t[:, 63:64],
                                                    op=ALU.subtract), R=[d_b], W=[d_col])
            kb.op("dve", lambda e: e.tensor_copy(out=col[:, 2:3], in_=b_t[:, 127:128]), R=[d_b], W=[d_col])
            kb.op("dve", lambda e: e.tensor_scalar(out=col[:, 3:4], in0=b_t[:, 63:64], scalar1=-1.0, scalar2=None,
                                                    op0=ALU.mult), R=[d_b], W=[d_col])
            kb.op("act", lambda e: e.activation(out=col[:, 4:7], in_=col[:, 0:3], func=AF.Exp), R=[d_col], W=[d_col])
            kb.op("act", lambda e: e.activation(out=e1[:], in_=b_t[:], func=AF.Exp, bias=col[:, 3:4], scale=1.0),
                  R=[d_b, d_col], W=[d_e1])
            kb.op("act", lambda e: e.activation(out=e2[:], in_=b_t[:], func=AF.Exp, bias=col[:, 0:1], scale=-1.0),
                  R=[d_b, d_col], W=[d_e2])
            kb.op("act", lambda e: e.activation(out=kk[:], in_=lf_t[:, cs], func=AF.Exp), R=[d_lf], W=[d_kk])
            kb.op("pool", lambda e: e.tensor_scalar(out=kk[:], in0=kk[:], scalar1=-1.0, scalar2=1.0, op0=ALU.mult,
                                                     op1=ALU.add), R=[d_kk], W=[d_kk])
            kb.op("dve", lambda e: e.tensor_tensor(out=qd[:], in0=q_t[:, cs], in1=e1[:], op=ALU.mult),
                  R=[d_q, d_e1], W=[d_qd])
            kb.op("dve", lambda e: e.tensor_tensor(out=kd[:], in0=kk[:], in1=e2[:], op=ALU.mult),
                  R=[d_kk, d_e2], W=[d_kd])
            kb.op("dve", lambda e: e.scalar_tensor_tensor(out=qb[:], in0=q_t[:, cs], scalar=col[:, 4:5], in1=e1[:],
                                                           op0=ALU.mult, op1=ALU.mult), R=[d_q, d_col, d_e1], W=[d_qb])
            kb.op("dve", lambda e: e.scalar_tensor_tensor(out=ke[:], in0=kk[:], scalar=col[:, 5:6], in1=e2[:],
                                                           op0=ALU.mult, op1=ALU.mult), R=[d_kk, d_col, d_e2], W=[d_ke])
            psc, d_psc = psc_r[r]
            kb.op("pe", lambda e: e.matmul(psc[:, 0:128], lhsT=kd[:], rhs=qd[:], start=True, stop=True),
                  R=[d_kd, d_qd], W=[d_psc])
            kb.op("dve", lambda e: e.copy_predicated(out=scm[:], mask=mku[:], data=psc[:, 0:128]),
                  R=[d_psc, d_mk], W=[d_scm])
            po, d_po = po_r[r]
            Sb, d_Sb = Sb_r[c % 2]
            kb.op("pe", lambda e: e.matmul(po[:, 0:128], lhsT=scm[:], rhs=v_t[:, c, :], start=True, stop=False),
                  R=[d_scm, d_v], W=[d_po])
            kb.op("pe", lambda e: e.matmul(po[:, 0:128], lhsT=qb[:], rhs=Sb[:], start=False, stop=True),
                  R=[d_qb, d_Sb], W=[d_po])
            pt, d_pt = pt_r[r]
            kb.op("pe", lambda e: e.transpose(out=pt[:, 0, :], in_=ke[:], identity=ident[:]), R=[d_ke, d_id], W=[d_pt])
            kb.op("act", lambda e: e.copy(out=keT[:], in_=pt[:, 0, :]), R=[d_pt], W=[d_keT])
            pu, d_pu = pu_r[r]
            kb.op("pe", lambda e: e.matmul(pu[:, 0:128], lhsT=keT[:], rhs=v_t[:, c, :], start=True, stop=True),
                  R=[d_keT, d_v], W=[d_pu])
            kb.op("dve", lambda e: e.scalar_tensor_tensor(out=S_t[:], in0=S_t[:], scalar=col[:, 6:7], in1=pu[:, 0:128],
                                                           op0=ALU.mult, op1=ALU.add), R=[d_S, d_col, d_pu], W=[d_S])
            Sb2, d_Sb2 = Sb_r[(c + 1) % 2]
            kb.op("pool", lambda e: e.tensor_copy(out=Sb2[:], in_=S_t[:]), R=[d_S], W=[d_Sb2])
            osq, d_osq = osq_r[r]
            oss, d_oss = oss_r[r]
            on_t, d_on = on_r[r]
            kb.op("act", lambda e: e.activation(out=osq[:], in_=po[:, 0:128], func=AF.Square, accum_out=oss[:, 0:1]),
                  R=[d_po], W=[d_osq, d_oss])
            kb.op("act", lambda e: e.activation(out=oss[:, 1:2], in_=oss[:, 0:1], func=AF.Ln, scale=1.0 / 128, bias=EPS),
                  R=[d_oss], W=[d_oss])
            kb.op("act", lambda e: e.activation(out=oss[:, 1:2], in_=oss[:, 1:2], func=AF.Exp, scale=-0.5),
                  R=[d_oss], W=[d_oss])
            kb.op("dve", lambda e: e.scalar_tensor_tensor(out=on_t[:], in0=po[:, 0:128], scalar=oss[:, 1:2], in1=go_t[:],
                                                           op0=ALU.mult, op1=ALU.mult), R=[d_po, d_oss, d_go], W=[d_on])
            kb.dma("sp", on[bh, c * 128:(c + 1) * 128, :], on_t[:], R=[d_on])
    kb.finish()
    return nc


_CACHE = {}


def _prog(key, fn):
    if key not in _CACHE:
        _CACHE[key] = fn()
    return _CACHE[key]


def _run(nc, in_maps):
    res = run_bass_kernel_spmd(nc, in_maps, core_ids=list(range(NCORES)))
    return res.results


def kernel_unfused(**inp):
    inp = {k: np.asarray(v) for k, v in inp.items()}
    x = inp["x"]
    meta = inp["meta_tokens"]
    f32 = np.float32
    metatile = np.zeros((128, D), f32)
    metatile[128 - NMETA:] = meta
    seg = SEQ // 4

    def core_rows(full_b, c, with_meta=True):
        s = 128 + (c % 4) * seg
        body = full_b[s:s + seg]
        return np.concatenate([full_b[0:128], body], 0) if with_meta else body

    def assemble(per_core):
        out = []
        for b in range(2):
            parts = [per_core[4 * b][0:128]] + [per_core[4 * b + j][128:] for j in range(4)]
            out.append(np.concatenate(parts, 0))
        return out

    hA = [np.concatenate([metatile, x[c // 4, (c % 4) * seg:(c % 4 + 1) * seg]], 0) for c in range(NCORES)]
    valid = np.ones((NT * 128, 1), f32)
    valid[0:128 - NMETA] = 0.0

    ncA = _prog("A", lambda: build_A(NT))
    cA = {"gain": inp["fox_norm"][0][None], "w_in": inp["fox_w_in"][0],
          "gq": np.tile(inp["fox_q_norm"][0], FH)[None], "gk": np.tile(inp["fox_k_norm"][0], FH)[None],
          "bf": inp["fox_b_f"][0][None]}
    rA = _run(ncA, [dict(cA, h=hA[c]) for c in range(NCORES)])
    qf = assemble([np.asarray(r["qo"]) for r in rA])
    kf = assemble([np.asarray(r["ko"]) for r in rA])
    vf = assemble([np.asarray(r["vo"]) for r in rA])
    lff = assemble([np.asarray(r["lfo"]) for r in rA])

    ncB = _prog("B", lambda: build_B(NB, 4))
    imB = []
    for c in range(NCORES):
        b = c // 4
        hs = [(4 * c + i) % FH for i in range(4)]
        imB.append({
            "qT": np.ascontiguousarray(np.stack([qf[b][:, h * FD:(h + 1) * FD].T for h in hs])),
            "kT": np.ascontiguousarray(np.stack([kf[b][:, h * FD:(h + 1) * FD].T for h in hs])),
            "v": np.ascontiguousarray(np.stack([vf[b][:, h * FD:(h + 1) * FD] for h in hs])),
            "lfr": np.ascontiguousarray(np.stack([lff[b][:, h] for h in hs])),
            "lfT": np.ascontiguousarray(np.stack([lff[b][:, h].reshape(NB, 128).T for h in hs])),
        })
    rB = _run(ncB, imB)
    oun = [np.zeros((LP, D), f32) for _ in range(2)]
    den = [np.zeros((LP, FH), f32) for _ in range(2)]
    for c in range(NCORES):
        b = c // 4
        o = np.asarray(rB[c]["oT"])
        for i in range(4):
            h = (4 * c + i) % FH
            oun[b][:, h * FD:(h + 1) * FD] = o[i, 0:FD].T
            den[b][:, h] = o[i, FD]

    ncC = _prog("C", lambda: build_CE("C", NT, True))
    cC = {"valid_in": valid, "w_out": inp["fox_w_out"][0], "mnorm": inp["moe_norm"][0][None],
          "w_r": np.ascontiguousarray(np.concatenate([inp["moe_w_grp"][0], inp["moe_w_rt"][0]], 1)),
          "b_r": np.concatenate([inp["moe_b_grp"][0], inp["moe_b_rt"][0]])[None],
          "w_up": inp["moe_w_up"][0], "w_dn": inp["moe_w_down"][0],
          "hnorm": inp["hg_norm"][0][None], "hw_in": inp["hg_w_in"][0], "hbf": inp["hg_b_f"][0][None],
          "lbl": inp["hg_lb_logits"]}
    rC = _run(ncC, [dict(cC, a_in=core_rows(oun[c // 4], c), den_in=core_rows(den[c // 4], c), hp_in=hA[c])
                    for c in range(NCORES)])
    h2 = [np.asarray(r["h2_out"]) for r in rC]
    q1 = assemble([np.asarray(r["q1_out"]) for r in rC])
    lf1 = assemble([np.asarray(r["lf1_out"]) for r in rC])
    v1 = assemble([np.asarray(r["v1_out"]) for r in rC])
    gs = assemble([np.asarray(r["gs_out"]) for r in rC])
    npad = 128 - NMETA
    for b in range(2):
        q1[b][0:npad] = 0
        lf1[b][0:npad] = 0
        v1[b][0:npad] = 0

    ncD = _prog("D", lambda: build_D(NB, 2))
    imD = []
    for c in range(NCORES):
        prs = [2 * c, 2 * c + 1]
        imD.append({
            "qT": np.ascontiguousarray(np.stack([q1[p // HH][:, (p % HH) * 128:(p % HH + 1) * 128].T for p in prs])),
            "lfT": np.ascontiguousarray(np.stack([lf1[p // HH][:, (p % HH) * 128:(p % HH + 1) * 128].T for p in prs])),
            "v": np.ascontiguousarray(np.stack([v1[p // HH][:, (p % HH) * 128:(p % HH + 1) * 128] for p in prs])),
            "go": inp["hg_o_norm"][0][None],
        })
    rD = _run(ncD, imD)
    onf = [np.zeros((LP, D), f32) for _ in range(2)]
    for c in range(NCORES):
        o = np.asarray(rD[c]["on"])
        for i in range(2):
            p = 2 * c + i
            onf[p // HH][:, (p % HH) * 128:(p % HH + 1) * 128] = o[i]

    ncE = _prog("E", lambda: build_CE("E", NTX, False))
    cE = {"valid_in": np.ones((NTX * 128, 1), f32), "w_out": inp["hg_w_out"][0], "mnorm": inp["moe_norm"][1][None],
          "w_r": np.ascontiguousarray(np.concatenate([inp["moe_w_grp"][1], inp["moe_w_rt"][1]], 1)),
          "b_r": np.concatenate([inp["moe_b_grp"][1], inp["moe_b_rt"][1]])[None],
          "w_up": inp["moe_w_up"][1], "w_dn": inp["moe_w_down"][1]}
    rE = _run(ncE, [dict(cE, a_in=core_rows(onf[c // 4], c, False), gs_in=core_rows(gs[c // 4], c, False),
                         hp_in=h2[c][128:]) for c in range(NCORES)])
    out = np.zeros((2, SEQ, D), f32)
    for c in range(NCORES):
        out[c // 4, (c % 4) * seg:(c % 4 + 1) * seg] = np.asarray(rE[c]["h2_out"])
    return out


GROUPS = [[0, 1, 2, 3], [4, 5, 6, 7]]


def build_fused(ntx=NTX, cap=CAP, stop=None):
    assert ntx % 4 == 0
    nt = ntx + 1
    nb = 4 * ntx + 1
    L = nb * 128
    nch = ntx // 4
    TX = ntx * 128
    nI = ntx + 1
    nslot = NE * cap
    nst = cap // 128
    nc = bass.Bass("TRN2", target_bir_lowering=False)

    def din(name, shape, dt=F32):
        return nc.dram_tensor(name, list(shape), dt, kind="ExternalInput").ap()

    def dint(name, shape, dt=BF16):
        return nc.dram_tensor(name, list(shape), dt).ap()

    x_in = din("x_in", [nt * 128, D])
    valid_in = din("valid_in", [nt * 128, 1])
    fnorm = din("fnorm", [1, D])
    wq_d, wk_d, wv_d = din("wq", [D, 256]), din("wk", [D, 256]), din("wv", [D, 256])
    wf_d = din("wf", [D, 4])
    gqc_d, gkc_d = din("gqc", [128, 1]), din("gkc", [128, 1])
    bf4r_d, bf4c_d = din("bf4r", [1, 4]), din("bf4c", [4, 1])
    wo1_d = din("wo1", [D, D])
    idx2_d = din("idx2", [128, 8], I32)
    idx3_d = din("idx3", [128, 8], I32)
    moe_d = []
    for l in range(2):
        if stop in ("T1", "H1p", "H1a") or (stop in ("T2", "H2") and l == 1):
            moe_d.append(None)
            continue
        moe_d.append(dict(mnorm=din("mnorm%d" % l, [1, D]), w_r=din("w_r%d" % l, [D, 36]), b_r=din("b_r%d" % l, [1, 36]),
                          w_up=din("w_up%d" % l, [NE, D, 2 * DE]), w_dn=din("w_dn%d" % l, [NE, DE, D])))
    hnorm = din("hnorm", [1, D])
    hwq_d, hwf_d, hwv_d = din("hwq", [2, D, 128]), din("hwf", [2, D, 128]), din("hwv", [2, D, 128])
    hwg_d = din("hwg", [D, D])
    hbfc_d, l0c_d, l1c_d = din("hbfc", [2, 128, 1]), din("l0c", [2, 128, 1]), din("l1c", [2, 128, 1])
    go_d = din("go", [1, 128])
    wo2_d = din("wo2", [D, D])
    out_d = nc.dram_tensor("out", [TX, D], F32, kind="ExternalOutput").ap()

    MA, MC = dint("MA", [128, 8 * 128]), dint("MC", [128, 8 * 128])
    SA, RA = dint("SA", [nch * 128, 8 * 512]), dint("RA", [nch * 512, 8 * 512])
    SC, RC = dint("SC", [nch * 128, 8 * 512]), dint("RC", [nch * 512, 8 * 512])
    qTs, kTs, vs = dint("qTs", [4, FD + 1, L]), dint("kTs", [4, FD, L]), dint("vs", [L, 256])
    SBb, RBb = dint("SBb", [8 * 128, TX]), dint("RBb", [8 * 512, TX])
    SBm, RBm = dint("SBm", [256, 128]), dint("RBm", [1024, 128])
    h1s, h2s = dint("h1s", [nt * 128, D], F32), dint("h2s", [nt * 128, D], F32)
    xpad, ypad = dint("xpad", [nslot, D]), dint("ypad", [nslot, D])
    GS = dint("GS", [nch * 128, 8 * 512])
    SD, RD = dint("SD", [8 * 128, TX]), dint("RD", [8 * 512, TX])
    d_MA, d_MC = (Dep(n) for n in ("MA", "MC"))
    d_SA = (Dep("SAw"), [Dep("SA%d" % j) for j in range(nch)])
    d_SC = (Dep("SCw"), [Dep("SC%d" % j) for j in range(nch)])
    d_SBc = [Dep("SBb%d" % j) for j in range(8)]
    d_SDc = [Dep("SD%d" % j) for j in range(8)]
    d_RA = [Dep("RA%d" % j) for j in range(nch)]
    d_RC = [Dep("RC%d" % j) for j in range(nch)]
    d_qTs, d_kTs, d_vs, d_SBb, d_RBb, d_SBm, d_RBm = (Dep(n) for n in ("qTs", "kTs", "vs", "SBb", "RBb", "SBm", "RBm"))
    d_h1s, d_h2s, d_xpad, d_ypad, d_GS, d_SD, d_RD = (Dep(n) for n in ("h1s", "h2s", "xpad", "ypad", "GS", "SD", "RD"))

    kb = KB(nc)
    identf, d_idf, ident, d_id = make_ident(kb)
    breg = nc.gpsimd.to_reg(nslot - 1)
    breg2 = nc.gpsimd.to_reg(8 * 512 - 1)

    def dump_and_finish(items):
        for name, ap, dep in items:
            o = nc.dram_tensor("dbg_" + name, list(ap.shape), ap.dtype, kind="ExternalOutput").ap()
            kb.dma("sp", o, ap, R=[dep])
        kb.finish()
        return nc

    def emit_hnT(src_t, d_src, gain_t, d_gain, scr, ssr, xnr, pstr, hcr, t, Mloc, d_Mloc, Ssend, d_S, Rrecv, d_R, hc_hook=None):
        b = t % 2
        sc_t, d_sc = scr[b]
        ss_t, d_ss = ssr[b]
        xn_t, d_xn = xnr[b]
        ps_t, d_pst = pstr[b]
        rms_rstd(kb, src_t[:], d_src, D, sc_t[:], d_sc, ss_t[:], d_ss)
        kb.op("dve", lambda e: e.scalar_tensor_tensor(out=xn_t[:], in0=src_t[:], scalar=ss_t[:, 0:1], in1=gain_t[:],
                                                       op0=ALU.mult, op1=ALU.mult), R=[d_src, d_ss, d_gain], W=[d_xn])
        for c in range(8):
            kb.op("pe", lambda e: e.transpose(out=ps_t[:, c, :], in_=xn_t[:, c * 128:(c + 1) * 128], identity=ident[:]),
                  R=[d_xn, d_id], W=[d_pst])
        if t == 0:
            hc_t, d_hc = hcr[0]
            kb.op("dve", lambda e: e.tensor_copy(out=hc_t[:, :, 0:128], in_=ps_t[:]), R=[d_pst], W=[d_hc])
            kb.dma("sp", Mloc.rearrange("p (c t) -> p c t", c=8), hc_t[:, :, 0:128], R=[d_hc], W=[d_Mloc])
            return
        j, s = (t - 1) // 4, (t - 1) % 4
        hc_t, d_hc = hcr[(j + 1) % 2]
        kb.op("dve", lambda e: e.tensor_copy(out=hc_t[:, :, s * 128:(s + 1) * 128], in_=ps_t[:]), R=[d_pst], W=[d_hc])
        if s == 3:
            kb.dma("sp", Ssend[j * 128:(j + 1) * 128, :], hc_t[:].rearrange("p c t -> p (c t)"), R=[d_hc],
                   W=[d_S[0], d_S[1][j]])
            kb.cc(Ssend[j * 128:(j + 1) * 128, :], Rrecv[j * 512:(j + 1) * 512, :], GROUPS, R=[d_S[1][j]], W=[d_R[j]])
            if hc_hook is not None:
                hc_hook(j, hc_t, d_hc)

    lfall = kb.sb("lfall", [128, nb, 4], F32)
    d_lfall = Dep("lfall")
    with kb.scope():
        g_t, d_g = load_bc(kb, "fnorm", fnorm, D)
        wq_t, d_wq = load_w(kb, "wq", wq_d, D, 256)
        wk_t, d_wk = load_w(kb, "wk", wk_d, D, 256)
        wv_t, d_wv = load_w(kb, "wv", wv_d, D, 256)
        wf_t, d_wf = load_w(kb, "wf", wf_d, D, 4)
        for d in (d_wq, d_wk, d_wv, d_wf):
            d.ro = True
        gqc = kb.sb("gqc", [128, 1], F32); d_gqc = Dep("gqc")
        gkc = kb.sb("gkc", [128, 1], F32); d_gkc = Dep("gkc")
        bf4c = kb.sb("bf4c", [4, 1], F32); d_bf4c = Dep("bf4c")
        kb.dma("sp", gqc[:], gqc_d, W=[d_gqc])
        kb.dma("sp", gkc[:], gkc_d, W=[d_gkc])
        kb.dma("sp", bf4c[:], bf4c_d, W=[d_bf4c])
        kb.op("dve", lambda e: e.tensor_scalar(out=bf4c[:], in0=bf4c[:], scalar1=-1.0, scalar2=None, op0=ALU.mult),
              R=[d_bf4c], W=[d_bf4c])
        kb.op("dve", lambda e: e.tensor_scalar(out=gqc[:], in0=gqc[:], scalar1=float(FD ** -0.5), scalar2=None,
                                                op0=ALU.mult), R=[d_gqc], W=[d_gqc])
        bf4r, d_bf4r = load_bc(kb, "bf4r", bf4r_d, 4)
        blkf = kb.sb("blkf", [128, 128], F32); d_blkf = Dep("blkf")
        blk = kb.sb("blk", [128, 128], BF16); d_blk = Dep("blk")
        ones1 = kb.sb("ones1", [128, 512], F32); d_ones1 = Dep("ones1")
        kb.op("pool", lambda e: e.memset(blkf[:], 0.0), W=[d_blkf])
        kb.op("pool", lambda e: e.memset(blkf[0:64, 0:64], 1.0), W=[d_blkf])
        kb.op("pool", lambda e: e.memset(blkf[64:128, 64:128], 1.0), W=[d_blkf])
        kb.op("dve", lambda e: e.tensor_copy(out=blk[:], in_=blkf[:]), R=[d_blkf], W=[d_blk])
        kb.op("pool", lambda e: e.memset(ones1[:], 1.0), W=[d_ones1])
        d_blk.ro = True
        d_ones1.ro = True
        xin = kb.ring("xin", [128, D], F32, 4)
        scr = kb.ring("scr", [128, D], F32, 2)
        ssr = kb.ring("ss", [128, 1], F32, 2)
        xnr = kb.ring("xn", [128, D], BF16, 2)
        hcr = kb.ring("hc", [128, 8, 512], BF16, 2)
        pstr = kb.ring("pst", [128, 8, 128], BF16, 2, psum=True)
        def t1_load(t):
            x_t, d_x = xin[t % 4]
            kb.dma("sp", x_t[:], x_in[t * 128:(t + 1) * 128, :], W=[d_x])
        for t in range(min(3, nt)):
            t1_load(t)
        for t in range(nt):
            if t + 3 < nt:
                t1_load(t + 3)
            x_t, d_x = xin[t % 4]
            emit_hnT(x_t, d_x, g_t, d_g, scr, ssr, xnr, pstr, hcr, t, MA, d_MA, SA, d_SA, RA, d_RA)

        if stop == "T1":
            kb.barrier()
            return dump_and_finish([("RA", RA, d_RA[nch - 1]), ("MA", MA, d_MA)])
        hgr = kb.ring("hg", [128, 8, 512], BF16, 2)
        sqr = kb.ring("sq", [128, 512], BF16, 2)
        rsr = kb.ring("rs", [128, 512], F32, 2)
        qnr = kb.ring("qn", [128, 512], BF16, 2)
        vsr = kb.ring("vsb", [128, 256], BF16, 2)
        lzr = kb.ring("lz", [128, 4], F32, 2)
        lrr = kb.ring("lr", [4, 512], F32, 2)
        drr = kb.ring("dr", [4, 512], BF16, 2)
        pqr = kb.ring("pq", [128, 512], F32, 2, psum=True)
        pssr = kb.ring("pss", [128, 512], F32, 1, psum=True)
        pvr = kb.ring("pv", [128, 512], F32, 2, psum=True)
        pfr = kb.ring("pf", [128, 512], F32, 1, psum=True)
        qi = 0
        vi = 0
        def h1_load(G):
            hg, d_hg = hgr[G % 2]
            if G == 0:
                kb.dma("sp", hg[:, :, 0:128], MA.rearrange("p (c t) -> p c t", c=8), R=[d_MA], W=[d_hg])
            else:
                j, rank = (G - 1) // 4, (G - 1) % 4
                r0 = j * 512 + rank * 128
                kb.dma("sp", hg[:].rearrange("p c t -> p (c t)"), RA[r0:r0 + 128, :], R=[d_RA[j]], W=[d_hg])
        h1_load(0)
        for G in range(1 + 4 * nch):
            hg, d_hg = hgr[G % 2]
            if G + 1 < 1 + 4 * nch:
                h1_load(G + 1)
            if G == 0:
                n, tok0 = 128, 0
            else:
                n, tok0 = 512, 128 + ((G - 1) % 4) * TX + ((G - 1) // 4) * 512
            for (w_t, d_w, gc, d_gc, dst, d_dst) in ((wq_t, d_wq, gqc, d_gqc, qTs, d_qTs), (wk_t, d_wk, gkc, d_gkc, kTs, d_kTs)):
                for pr in range(2):
                    pq, d_pq = pqr[qi % 2]
                    pss, d_pss = pssr[0]
                    sq, d_sq = sqr[qi % 2]
                    rs, d_rs = rsr[qi % 2]
                    qn, d_qn = qnr[qi % 2]
                    qi += 1
                    for c in range(8):
                        kb.op("pe", lambda e: e.matmul(pq[:, 0:n], lhsT=w_t[:, c, pr * 128:(pr + 1) * 128], rhs=hg[:, c, 0:n],
                                                       start=(c == 0), stop=(c == 7)), R=[d_w, d_hg], W=[d_pq])
                    kb.op("act", lambda e: e.activation(out=sq[:, 0:n], in_=pq[:, 0:n], func=AF.Square), R=[d_pq], W=[d_sq])
                    kb.op("pe", lambda e: e.matmul(pss[:, 0:n], lhsT=blk[:], rhs=sq[:, 0:n], start=True, stop=True),
                          R=[d_blk, d_sq], W=[d_pss])
                    kb.op("act", lambda e: e.activation(out=rs[:, 0:n], in_=pss[:, 0:n], func=AF.Ln, scale=1.0 / FD, bias=EPS),
                          R=[d_pss], W=[d_rs])
                    kb.op("act", lambda e: e.activation(out=rs[:, 0:n], in_=rs[:, 0:n], func=AF.Exp, scale=-0.5), R=[d_rs], W=[d_rs])
                    kb.op("dve", lambda e: e.scalar_tensor_tensor(out=qn[:, 0:n], in0=pq[:, 0:n], scalar=gc[:, 0:1], in1=rs[:, 0:n],
                                                                   op0=ALU.mult, op1=ALU.mult), R=[d_pq, d_gc, d_rs], W=[d_qn])
                    for hh in range(2):
                        kb.dma("sp", dst[2 * pr + hh, 0:FD, tok0:tok0 + n], qn[hh * 64:(hh + 1) * 64, 0:n], R=[d_qn], W=[d_dst])
            for s in range(n // 128):
                pv, d_pv = pvr[vi % 2]
                vsb, d_vsb = vsr[vi % 2]
                lz, d_lz = lzr[vi % 2]
                vi += 1
                for c in range(8):
                    kb.op("pe", lambda e: e.matmul(pv[:, 0:256], lhsT=hg[:, c, s * 128:(s + 1) * 128], rhs=wv_t[:, c, :],
                                                   start=(c == 0), stop=(c == 7)), R=[d_wv, d_hg], W=[d_pv])
                for c in range(8):
                    kb.op("pe", lambda e: e.matmul(pv[:, 256:260], lhsT=hg[:, c, s * 128:(s + 1) * 128], rhs=wf_t[:, c, :],
                                                   start=(c == 0), stop=(c == 7)), R=[d_wf, d_hg], W=[d_pv])
                kb.op("act", lambda e: e.copy(out=vsb[:], in_=pv[:, 0:256]), R=[d_pv], W=[d_vsb])
                kb.dma("sp", vs[tok0 + s * 128:tok0 + (s + 1) * 128, :], vsb[:], R=[d_vsb], W=[d_vs])
                blk_i = tok0 // 128 + s
                kb.op("dve", lambda e: e.tensor_tensor(out=lz[:], in0=pv[:, 256:260], in1=bf4r[:], op=ALU.add),
                      R=[d_pv, d_bf4r], W=[d_lz])
                kb.op("act", lambda e: e.activation(out=lz[:], in_=lz[:], func=AF.Exp, scale=-1.0), R=[d_lz], W=[d_lz])
                kb.op("act", lambda e: e.activation(out=lz[:], in_=lz[:], func=AF.Ln, bias=1.0), R=[d_lz], W=[d_lz])
                kb.op("dve", lambda e: e.tensor_scalar(out=lfall[:, blk_i, :], in0=lz[:], scalar1=-1.0, scalar2=None,
                                                        op0=ALU.mult), R=[d_lz], W=[d_lfall])
            pf, d_pf = pfr[0]
            lr, d_lr = lrr[G % 2]
            dr, d_dr = drr[G % 2]
            for c in range(8):
                kb.op("pe", lambda e: e.matmul(pf[0:4, 0:n], lhsT=wf_t[:, c, :], rhs=hg[:, c, 0:n], start=(c == 0), stop=(c == 7)),
                      R=[d_wf, d_hg], W=[d_pf])
            kb.op("act", lambda e: e.activation(out=lr[:, 0:n], in_=pf[0:4, 0:n], func=AF.Exp, scale=-1.0, bias=bf4c[:, 0:1]),
                  R=[d_pf, d_bf4c], W=[d_lr])
            kb.op("act", lambda e: e.activation(out=lr[:, 0:n], in_=lr[:, 0:n], func=AF.Ln, bias=1.0), R=[d_lr], W=[d_lr])
            kb.op("dve", lambda e: e.tensor_tensor_scan(out=lr[:, 0:n], data0=ones1[0:4, 0:n], data1=lr[:, 0:n], initial=0.0,
                                                         op0=ALU.mult, op1=ALU.subtract), R=[d_lr, d_ones1], W=[d_lr])
            kb.op("dve", lambda e: e.tensor_copy(out=dr[:, 0:n], in_=lr[:, 0:n]), R=[d_lr], W=[d_dr])
            kb.dma("sp", qTs[:, FD, tok0:tok0 + n], dr[:, 0:n], R=[d_dr], W=[d_qTs])

    if stop == "H1p":
        return dump_and_finish([("qTs", qTs, d_qTs), ("kTs", kTs, d_kTs), ("vs", vs, d_vs)])
    with kb.scope():
        trif = kb.sb("trif", [128, 128], F32); d_tri = Dep("trif")
        onesf = kb.sb("onesf", [128, 128], F32); d_ones = Dep("onesf")
        sel0 = kb.sb("sel0", [128, 128], F32); d_sel = Dep("sel0")
        maskb = kb.sb("maskb", [128, 128], BF16); d_mask = Dep("maskb")
        onerow = kb.sb("onerow", [128, nb], F32); d_or = Dep("onerow")
        kb.op("pool", lambda e: e.memset(onesf[:], 1.0), W=[d_ones])
        kb.op("pool", lambda e: e.memset(onerow[:], 1.0), W=[d_or])
        kb.op("pool", lambda e: e.affine_select(out=trif[:], in_=onesf[:], pattern=[[1, 128]], compare_op=ALU.is_ge,
                                                fill=0.0, base=0, channel_multiplier=-1), R=[d_ones], W=[d_tri])
        kb.op("pool", lambda e: e.affine_select(out=sel0[:], in_=onesf[:], pattern=[[0, 128]], compare_op=ALU.is_ge,
                                                fill=0.0, base=0, channel_multiplier=-1), R=[d_ones], W=[d_sel])
        kb.op("dve", lambda e: e.tensor_copy(out=maskb[:], in_=trif[:]), R=[d_tri], W=[d_mask])
        for d in (d_tri, d_ones, d_sel, d_mask, d_or):
            d.ro = True
        QA = kb.sb("QA", [FD + 1, L], BF16)
        tpc = ntx // 4
        qb_ = [0] + [128 + (c + 1) * tpc * 512 for c in range(3)] + [L]
        d_QAc = [Dep("QA%d" % c) for c in range(4)]

        def q_chunk(I):
            return 0 if I == 0 else (I - 1) // tpc

        def q_load(hx, c):
            kb.dma("sp", QA[:, qb_[c]:qb_[c + 1]], qTs[hx][:, qb_[c]:qb_[c + 1]], R=[d_qTs], W=[d_QAc[c]])
        KAr = kb.ring("KA", [FD + 1, L], BF16, 2)
        VAr = kb.ring("VA", [128, nb, 128], BF16, 2)
        lft = kb.sb("lft", [128, nb], F32); d_lft = Dep("lft")
        ct = kb.sb("ct", [128, nb], F32); d_ct = Dep("ct")
        tot = kb.sb("tot", [128, nb], F32); d_tot = Dep("tot")
        rall = kb.sb("rall", [128, nb], F32); d_rall = Dep("rall")
        biasr = kb.ring("bias", [128, nb], F32, 2)
        pr_ = kb.ring("P", [128, 512], BF16, 4)
        denr = kb.ring("den", [128, 512], F32, 2)
        rdr = kb.ring("rden", [64, 512], F32, 2)
        onr = kb.ring("onb", [64, 512], BF16, 2)
        psS = kb.ring("psS", [128, 512], F32, 4, psum=True)
        psO = kb.ring("psO", [128, 512], F32, 2, psum=True)
        psM = kb.ring("psM", [128, 512], F32, 2, psum=True)
        for (KA_, d_KA_), (VA_, d_VA_) in zip(KAr, VAr):
            kb.op("pool", lambda e: e.memset(KA_[FD:FD + 1, :], 1.0), W=[d_KA_])
            kb.op("pool", lambda e: e.memset(VA_[:, :, FD:128], 1.0), W=[d_VA_])

        def kv_load(hx):
            KA_, d_KA_ = KAr[hx % 2]
            VA_, d_VA_ = VAr[hx % 2]
            kb.dma("sp", KA_[0:FD, :], kTs[hx], R=[d_kTs], W=[d_KA_])
            kb.dma("sp", VA_[:, :, 0:FD], vs[:, hx * FD:(hx + 1) * FD].rearrange("(j p) d -> p j d", p=128), R=[d_vs], W=[d_VA_])
            kb.op("pool", lambda e: e.memset(VA_[0:112, 0, :], 0.0), W=[d_VA_])

        for c in range(4):
            q_load(0, c)
        kv_load(0)
        for h4 in range(4):
            pr4, hh4 = h4 // 2, h4 % 2
            KA, d_KA = KAr[h4 % 2]
            VA, d_VA = VAr[h4 % 2]
            kb.op("dve", lambda e: e.tensor_copy(out=lft[:], in_=lfall[:, :, h4]), R=[d_lfall], W=[d_lft])
            pm0, d_pm0 = psM[0]
            pm1, d_pm1 = psM[1]
            kb.op("pe", lambda e: e.matmul(pm0[:, 0:nb], lhsT=trif[:], rhs=lft[:], start=True, stop=True),
                  R=[d_tri, d_lft], W=[d_pm0])
            kb.op("pe", lambda e: e.matmul(pm1[:, 0:nb], lhsT=onesf[:], rhs=lft[:], start=True, stop=True),
                  R=[d_ones, d_lft], W=[d_pm1])
            kb.op("dve", lambda e: e.tensor_copy(out=tot[:], in_=pm1[:, 0:nb]), R=[d_pm1], W=[d_tot])
            kb.op("dve", lambda e: e.tensor_tensor_scan(out=ct[:], data0=onerow[:], data1=tot[:], initial=0.0,
                                                         op0=ALU.mult, op1=ALU.add), R=[d_tot, d_or], W=[d_ct])
            kb.op("dve", lambda e: e.tensor_tensor(out=ct[:], in0=ct[:], in1=tot[:], op=ALU.subtract),
                  R=[d_ct, d_tot], W=[d_ct])
            kb.op("dve", lambda e: e.tensor_tensor(out=ct[:], in0=ct[:], in1=pm0[:, 0:nb], op=ALU.add),
                  R=[d_ct, d_pm0], W=[d_ct])
            kb.op("pe", lambda e: e.matmul(pm1[:, 0:nb], lhsT=sel0[:], rhs=ct[:], start=True, stop=True),
                  R=[d_sel, d_ct], W=[d_pm1])
            kb.op("dve", lambda e: e.tensor_copy(out=rall[:], in_=pm1[:, 0:nb]), R=[d_pm1], W=[d_rall])
            steps = []
            for I in range(nI):
                j0 = 0 if I == 0 else 4 * I - 3
                nblk = 1 if I == 0 else 4
                nJ = j0 + nblk
                for J in range(nJ):
                    steps.append((I, J, j0, nblk, nJ))
            LA = 3
            for idx in range(len(steps) + LA):
                if idx < len(steps):
                    I, J, j0, nblk, nJ = steps[idx]
                    q0 = j0 * 128
                    bias_t, d_bias = biasr[I % 2]
                    if J == 0:
                        kb.op("dve", lambda e: e.tensor_scalar(out=bias_t[:, 0:nJ], in0=ct[:, 0:nJ], scalar1=-1.0,
                                                                scalar2=rall[:, j0:j0 + 1], op0=ALU.mult, op1=ALU.add),
                              R=[d_ct, d_rall], W=[d_bias])
                    m = max(0, J - j0)
                    c0 = m * 128
                    c1 = nblk * 128
                    ps, d_ps = psS[idx % 4]
                    p_t, d_p = pr_[idx % 4]
                    kb.op("pe", lambda e: e.matmul(ps[:, c0:c1], lhsT=KA[:, J * 128:(J + 1) * 128],
                                                   rhs=QA[:, q0 + c0:q0 + c1], start=True, stop=True),
                          R=[d_KA, d_QAc[q_chunk(I)]], W=[d_ps])
                    kb.op("act", lambda e: e.activation(out=p_t[:, c0:c1], in_=ps[:, c0:c1], func=AF.Exp,
                                                        bias=bias_t[:, J:J + 1], scale=1.0),
                          R=[d_ps, d_bias], W=[d_p])
                    if J >= j0:
                        kb.op("dve", lambda e: e.tensor_tensor(out=p_t[:, c0:c0 + 128], in0=p_t[:, c0:c0 + 128],
                                                                in1=maskb[:], op=ALU.mult), R=[d_p, d_mask], W=[d_p])
                    if J == 0 and I == nI // 2 and h4 + 1 < 4:
                        kv_load(h4 + 1)
                    if J == nJ - 1 and I > 0 and I % tpc == 0 and h4 + 1 < 4:
                        q_load(h4 + 1, I // tpc - 1)
                if idx >= LA:
                    I, J, j0, nblk, nJ = steps[idx - LA]
                    m = max(0, J - j0)
                    c0 = m * 128
                    c1 = nblk * 128
                    p_t, d_p = pr_[(idx - LA) % 4]
                    po, d_po = psO[I % 2]
                    kb.op("pe", lambda e: e.matmul(po[:, c0:c1], lhsT=VA[:, J, :], rhs=p_t[:, c0:c1],
                                                   start=(J == 0), stop=(J == nJ - 1)), R=[d_VA, d_p], W=[d_po])
                    if J == nJ - 1:
                        ncol = nblk * 128
                        den, d_den = denr[I % 2]
                        rd, d_rd = rdr[I % 2]
                        onb, d_onb = onr[I % 2]
                        kb.op("dve", lambda e: e.tensor_scalar(out=den[64:128, 0:ncol], in0=po[64:128, 0:ncol], scalar1=1e-30,
                                                                scalar2=None, op0=ALU.max), R=[d_po], W=[d_den])
                        kb.dma("sp", rd[:, 0:ncol], den[64:128, 0:ncol], R=[d_den], W=[d_rd])
                        kb.op("dve", lambda e: e.reciprocal(out=rd[:, 0:ncol], in_=rd[:, 0:ncol]), R=[d_rd], W=[d_rd])
                        kb.op("dve", lambda e: e.tensor_tensor(out=onb[:, 0:ncol], in0=po[0:64, 0:ncol], in1=rd[:, 0:ncol],
                                                                op=ALU.mult), R=[d_po, d_rd], W=[d_onb])
                        if I == 0:
                            kb.dma("sp", SBm[h4 * 64:(h4 + 1) * 64, :], onb[:, 0:128], R=[d_onb], W=[d_SBm])
                        else:
                            off = (I - 1) * 512
                            qq, col = off // TX, off % TX
                            r0 = (pr4 * 4 + qq) * 128 + hh4 * 64
                            cidx = pr4 * 4 + qq
                            kb.dma("sp", SBb[r0:r0 + 64, col:col + 512], onb[:, 0:512], R=[d_onb], W=[d_SBb, d_SBc[cidx]])
                            if hh4 == 1 and (off + 512) % TX == 0:
                                kb.cc(SBb[cidx * 128:(cidx + 1) * 128, :], RBb[cidx * 512:(cidx + 1) * 512, :], GROUPS,
                                      R=[d_SBc[cidx]], W=[d_RBb])
        kb.cc(SBm[:, :], RBm[:, :], GROUPS, R=[d_SBm], W=[d_RBm])

    if stop == "H1a":
        return dump_and_finish([("RBb", RBb, d_RBb), ("RBm", RBm, d_RBm)])
    usb = kb.sb("usb", [128, 128], BF16); d_us = Dep("usb")
    onesb = kb.sb("onesb", [128, 128], BF16); d_onesb = Dep("onesb")
    onesf2 = kb.sb("onesf2", [128, 128], F32); d_onesf2 = Dep("onesf2")
    tmpf = kb.sb("tmpf", [128, 128], F32); d_tmpf = Dep("tmpf")
    kb.op("pool", lambda e: e.memset(onesf2[:], 1.0), W=[d_onesf2])
    kb.op("pool", lambda e: e.affine_select(out=tmpf[:], in_=onesf2[:], pattern=[[1, 128]], compare_op=ALU.is_gt,
                                            fill=0.0, base=0, channel_multiplier=-1), R=[d_onesf2], W=[d_tmpf])
    kb.op("dve", lambda e: e.tensor_copy(out=usb[:], in_=tmpf[:]), R=[d_tmpf], W=[d_us])
    kb.op("dve", lambda e: e.tensor_copy(out=onesb[:], in_=onesf2[:]), R=[d_onesf2], W=[d_onesb])
    for d in (d_us, d_onesb, d_onesf2):
        d.ro = True
    dest_i = kb.sb("dest_i", [128, nt, 2], I32); d_dest = Dep("dest_i")
    gw = kb.sb("gw", [128, nt, 2], F32); d_gw = Dep("gw")
    cntb = kb.sb("cntb", [128, NE], F32); d_cntb = Dep("cntb")

    def tok_stage(l, tiles, has_meta, setup_lhsT, hp_ap, wo_d, pass3_setup, pass3_tile):
        md = moe_d[l]
        kb.op("pool", lambda e: e.iota(cntb[:], pattern=[[cap, NE]], base=0, channel_multiplier=0,
                                       allow_small_or_imprecise_dtypes=True), W=[d_cntb])
        with kb.scope():
            mn_t, d_mn = load_bc(kb, "mnorm", md["mnorm"], D)
            br_t, d_br = load_bc(kb, "b_r", md["b_r"], 36)
            wr_t, d_wr = load_w(kb, "w_r", md["w_r"], D, 36, dt=F32)
            d_wr.ro = True
            with kb.scope():
                wo_t, d_wo = load_w(kb, "w_out", wo_d, D, D)
                d_wo.ro = True
                get_lhsT = setup_lhsT()
                hp_r = kb.ring("hp", [128, D], F32, 4)
                h1_r = kb.ring("h1", [128, D], F32, 2)
                scr_r = kb.ring("scr", [128, D], F32, 2)
                ss_r = kb.ring("ss", [128, 1], F32, 2)
                hmf_r = kb.ring("hmf", [128, D], F32, 2)
                hmb_r = kb.ring("hmb", [128, D], BF16, 6)
                hmT_r = kb.ring("hmT", [128, 8, 128], F32, 2)
                sm_r = kb.ring("sm", [128, 256], F32, 4)
                ab_r = kb.ring("ab", [128, NE], BF16, 4)
                val_r = kb.ring("val", [128, 2], F32, 4)
                pstf_r = kb.ring("pstf", [128, 8, 128], F32, 1, psum=True)
                pmm_r = kb.ring("pmm", [128, 512], F32, 2, psum=True)
                prt_r = kb.ring("prt", [128, 512], F32, 4, psum=True)
                def p1_load(ti):
                    hp_t, d_hp = hp_r[ti % 4]
                    kb.dma("sp", hp_t[:], hp_ap(tiles[ti]), R=[d_h2s], W=[d_hp])
                    if hasattr(get_lhsT, "prefetch"):
                        get_lhsT.prefetch(tiles[ti])

                def front(ti):
                    t = tiles[ti]
                    b = ti % 2
                    r0, r1 = t * 128, (t + 1) * 128
                    hp_t, d_hp = hp_r[ti % 4]
                    if ti + 3 < len(tiles):
                        p1_load(ti + 3)
                    lhsT, d_lhsT = get_lhsT(t)
                    h1_t, d_h1 = h1_r[b]
                    for half in range(2):
                        p_t, d_p = pmm_r[half]
                        for c in range(8):
                            kb.op("pe", lambda e: e.matmul(p_t[:], lhsT=lhsT(c), rhs=wo_t[:, c, half * 512:(half + 1) * 512],
                                                           start=(c == 0), stop=(c == 7)), R=d_lhsT + [d_wo], W=[d_p])
                        kb.op("dve", lambda e: e.tensor_tensor(out=h1_t[:, half * 512:(half + 1) * 512], in0=p_t[:],
                                                                in1=hp_t[:, half * 512:(half + 1) * 512], op=ALU.add),
                              R=[d_p, d_hp], W=[d_h1])
                    kb.dma("sp", h1s[r0:r1, :], h1_t[:], R=[d_h1], W=[d_h1s])
                    sc_t, d_sc = scr_r[b]
                    ss_t, d_ss = ss_r[b]
                    hmf_t, d_hmf = hmf_r[b]
                    hmb_t, d_hmb = hmb_r[ti % 6]
                    rms_rstd(kb, h1_t[:], d_h1, D, sc_t[:], d_sc, ss_t[:], d_ss)
                    kb.op("dve", lambda e: e.scalar_tensor_tensor(out=hmf_t[:], in0=h1_t[:], scalar=ss_t[:, 0:1], in1=mn_t[:],
                                                                   op0=ALU.mult, op1=ALU.mult),
                          R=[d_h1, d_ss, d_mn], W=[d_hmf])
                    kb.op("pool", lambda e: e.tensor_copy(out=hmb_t[:], in_=hmf_t[:]), R=[d_hmf], W=[d_hmb])

                def front2(ti):
                    t = tiles[ti]
                    b = ti % 2
                    r0, r1 = t * 128, (t + 1) * 128
                    hmf_t, d_hmf = hmf_r[b]
                    hmb_t, d_hmb = hmb_r[ti % 6]
                    pf_t, d_pf = pstf_r[0]
                    hmT_t, d_hmT = hmT_r[b]
                    for c in range(8):
                        kb.op("pe", lambda e: e.transpose(out=pf_t[:, c, :], in_=hmf_t[:, c * 128:(c + 1) * 128],
                                                          identity=identf[:]), R=[d_hmf, d_idf], W=[d_pf])
                    kb.op("act", lambda e: e.copy(out=hmT_t[:], in_=pf_t[:]), R=[d_pf], W=[d_hmT])
                    pr_t, d_pr = prt_r[ti % 4]
                    for c in range(8):
                        kb.op("pe", lambda e: e.matmul(pr_t[:, 0:36], lhsT=hmT_t[:, c, :], rhs=wr_t[:, c, :],
                                                       start=(c == 0), stop=(c == 7)), R=[d_hmT, d_wr], W=[d_pr])
                    return dict(t=t, b=ti % 4, r0=r0, r1=r1, hmb_t=hmb_t, d_hmb=d_hmb, pr_t=pr_t, d_pr=d_pr)

                def route_ops(cx):
                    t, b, r0, r1 = cx["t"], cx["b"], cx["r0"], cx["r1"]
                    hmb_t, d_hmb, pr_t, d_pr = cx["hmb_t"], cx["d_hmb"], cx["pr_t"], cx["d_pr"]
                    ops = []
                    add = ops.append
                    sm, d_sm = sm_r[b]
                    lg = sm[:, 0:36]
                    gmax = sm[:, 36:37]
                    ngmax = sm[:, 37:38]
                    gsum = sm[:, 38:39]
                    ggate = sm[:, 39:40]
                    eg = sm[:, 40:44]
                    ohg = sm[:, 44:48]
                    esel = sm[:, 48:56]
                    top8 = sm[:, 56:64]
                    oh0 = sm[:, 64:72]
                    oh1 = sm[:, 72:80]
                    A0 = sm[:, 80:112]
                    A1 = sm[:, 112:144]
                    sbt = sm[:, 144:176]
                    tmp = sm[:, 176:208]
                    destf = sm[:, 208:210]
                    diff = sm[:, 210:211]
                    sg = sm[:, 211:212]
                    S = [d_sm]
                    dv = lambda fn, R=(), W=(): kb.op("dve", fn, R=list(R) + S, W=list(W) + S)
                    add(lambda: dv(lambda e: e.tensor_tensor(out=lg, in0=pr_t[:, 0:36], in1=br_t[:], op=ALU.add), R=[d_pr, d_br]))
                    add(lambda: dv(lambda e: e.tensor_reduce(out=gmax, in_=lg[:, 0:4], axis=AX.X, op=ALU.max)))
                    add(lambda: dv(lambda e: e.tensor_scalar(out=ngmax, in0=gmax, scalar1=-1.0, scalar2=None, op0=ALU.mult)))
                    add(lambda: kb.op("act", lambda e: e.activation(out=eg, in_=lg[:, 0:4], func=AF.Exp, bias=ngmax, scale=1.0,
                                                                    accum_out=gsum), R=S, W=S))
                    add(lambda: dv(lambda e: e.reciprocal(out=ggate, in_=gsum)))
                    add(lambda: dv(lambda e: e.tensor_scalar(out=ohg, in0=lg[:, 0:4], scalar1=gmax, scalar2=None, op0=ALU.is_equal)))
                    add(lambda: dv(lambda e: e.tensor_scalar(out=esel, in0=lg[:, 4:12], scalar1=ohg[:, 0:1], scalar2=None, op0=ALU.mult)))
                    for g in range(1, 4):
                        add(lambda g=g: dv(lambda e: e.scalar_tensor_tensor(out=esel, in0=lg[:, 4 + 8 * g:12 + 8 * g],
                                                                            scalar=ohg[:, g:g + 1], in1=esel, op0=ALU.mult, op1=ALU.add)))
                    add(lambda: dv(lambda e: e.max(out=top8, in_=esel)))
                    add(lambda: dv(lambda e: e.tensor_scalar(out=oh0, in0=esel, scalar1=top8[:, 0:1], scalar2=None, op0=ALU.is_equal)))
                    add(lambda: dv(lambda e: e.tensor_scalar(out=oh1, in0=esel, scalar1=top8[:, 1:2], scalar2=None, op0=ALU.is_equal)))
                    for (Ak, ohk) in ((A0, oh0), (A1, oh1)):
                        add(lambda Ak=Ak, ohk=ohk: dv(lambda e: e.tensor_tensor(
                            out=Ak.rearrange("p (g j) -> p g j", j=8), in0=ohg.unsqueeze(2).to_broadcast([128, 4, 8]),
                            in1=ohk.unsqueeze(1).to_broadcast([128, 4, 8]), op=ALU.mult)))
                    use_valid = has_meta and t == 0
                    val_t, d_val = val_r[b]
                    if use_valid:
                        add(lambda: kb.dma("sp", val_t[:, 0:1], valid_in[r0:r1, :], W=[d_val]))
                        add(lambda: kb.op("dve", lambda e: e.tensor_scalar(out=val_t[:, 1:2], in0=val_t[:, 0:1], scalar1=-BIGIDX,
                                                                            scalar2=BIGIDX, op0=ALU.mult, op1=ALU.add),
                                          R=[d_val], W=[d_val]))
                        for Ak in (A0, A1):
                            add(lambda Ak=Ak: dv(lambda e: e.tensor_scalar(out=Ak, in0=Ak, scalar1=val_t[:, 0:1], scalar2=None,
                                                                           op0=ALU.mult), R=[d_val]))
                    ab_t, d_ab = ab_r[b]
                    add(lambda: kb.op("dve", lambda e: e.tensor_tensor(out=ab_t[:], in0=A0, in1=A1, op=ALU.add), R=S, W=[d_ab]))
                    add(lambda: kb.op("pe", lambda e: e.matmul(pr_t[:, 64:96], lhsT=usb[:], rhs=ab_t[:], start=True, stop=True),
                                      R=[d_us, d_ab], W=[d_pr]))
                    add(lambda: kb.op("pe", lambda e: e.matmul(pr_t[:, 96:128], lhsT=onesb[:], rhs=ab_t[:], start=True, stop=True),
                                      R=[d_onesb, d_ab], W=[d_pr]))

                    def cnt_ops():
                        dv(lambda e: e.tensor_tensor(out=sbt, in0=pr_t[:, 64:96], in1=cntb[:], op=ALU.add), R=[d_pr, d_cntb])
                        kb.op("dve", lambda e: e.tensor_tensor(out=cntb[:], in0=cntb[:], in1=pr_t[:, 96:128], op=ALU.add),
                              R=[d_pr, d_cntb] + S, W=[d_cntb])
                    add(cnt_ops)
                    for k, Ak in enumerate((A0, A1)):
                        add(lambda Ak=Ak: dv(lambda e: e.tensor_tensor(out=tmp, in0=Ak, in1=sbt, op=ALU.mult)))
                        add(lambda k=k: dv(lambda e: e.tensor_reduce(out=destf[:, k:k + 1], in_=tmp, axis=AX.X, op=ALU.add)))
                    if use_valid:
                        add(lambda: dv(lambda e: e.tensor_scalar(out=destf, in0=destf, scalar1=val_t[:, 0:1], scalar2=val_t[:, 1:2],
                                                                 op0=ALU.mult, op1=ALU.add), R=[d_val]))
                    add(lambda: kb.op("dve", lambda e: e.tensor_copy(out=dest_i[:, t, :], in_=destf), R=S, W=[d_dest]))
                    add(lambda: dv(lambda e: e.tensor_tensor(out=diff, in0=top8[:, 0:1], in1=top8[:, 1:2], op=ALU.subtract)))
                    add(lambda: kb.op("act", lambda e: e.activation(out=sg, in_=diff, func=AF.Sigmoid), R=S, W=S))
                    add(lambda: kb.op("dve", lambda e: e.tensor_tensor(out=gw[:, t, 0:1], in0=sg, in1=ggate, op=ALU.mult), R=S, W=[d_gw]))
                    add(lambda: kb.op("dve", lambda e: e.tensor_tensor(out=gw[:, t, 1:2], in0=ggate, in1=gw[:, t, 0:1], op=ALU.subtract),
                                      R=S + [d_gw], W=[d_gw]))
                    for k in range(2):
                        add(lambda k=k: kb.idma(out=xpad[:, :], out_off=bass.IndirectOffsetOnAxis(ap=dest_i[:, t, k:k + 1], axis=0),
                                                in_=hmb_t[:], in_off=None, R=[d_hmb, d_dest], W=[d_xpad],
                                                bounds_check=breg, oob_is_err=False))
                    return ops

                p1_load(0)

                def emit_routes(pair):
                    lists = [route_ops(cxs[i]) for i in pair]
                    for k in range(max(len(l) for l in lists)):
                        for l in lists:
                            if k < len(l):
                                l[k]()

                cxs = {}
                ntl = len(tiles)
                for ti in range(1, min(3, ntl)):
                    p1_load(ti)
                front(0)
                pending = None
                for ti in range(ntl):
                    if ti + 1 < ntl:
                        front(ti + 1)
                    cxs[ti] = front2(ti)
                    if ti % 2 == 1 or ti == ntl - 1:
                        pair = (ti - 1, ti) if ti % 2 == 1 else (ti,)
                        if pending is not None:
                            emit_routes(pending)
                        pending = pair
                emit_routes(pending)
            with kb.scope():
                wup_r = kb.ring("wup", [128, 8, 2 * DE], BF16, 2)
                wdn_r = kb.ring("wdn", [128, 4, D], BF16, 2)
                wst_r = kb.ring("wst", [128, D], F32, 12)
                wdeps = [[Dep("w%d_%d" % (bb, cc)) for cc in range(12)] for bb in range(2)]
                xs_r = kb.ring("xs", [128, D], BF16, 2 * nst)
                xT_r = kb.ring("xT", [128, 8, cap], BF16, 2)
                sa_r = kb.ring("sa", [128, cap], F32, 2)
                aT_r = kb.ring("aT", [128, 4, cap], BF16, 2)
                yb_r = kb.ring("yb", [128, D], BF16, 2)
                pst_r = kb.ring("pst", [128, 8, 128], BF16, 2, psum=True)
                pau_r = kb.ring("pau", [128, 512], F32, 4, psum=True)
                py_r = kb.ring("py", [128, 512], F32, 2, psum=True)
                CE = ("act", "dve", "act", "dve", "act", "dve", "act", "dve", "act", "dve", "act", "dve")

                def w_dma(ex):
                    wuv = md["w_up"][ex].rearrange("(c p) n -> p c n", p=128)
                    wdv = md["w_dn"][ex].rearrange("(c p) n -> p c n", p=128)
                    for c in range(12):
                        stg, d_stg = wst_r[c]
                        kb.dma("sp", stg[:], wuv[:, c, :] if c < 8 else wdv[:, c - 8, :], W=[d_stg])

                def w_cast(ex, cs):
                    bb = ex % 2
                    for c in cs:
                        stg, d_stg = wst_r[c]
                        dst_ap = wup_r[bb][0][:, c, :] if c < 8 else wdn_r[bb][0][:, c - 8, :]
                        if CE[c] == "act":
                            kb.op("act", lambda e: e.copy(out=dst_ap, in_=stg[:]), R=[d_stg], W=[wdeps[bb][c]])
                        else:
                            kb.op("dve", lambda e: e.tensor_copy(out=dst_ap, in_=stg[:]), R=[d_stg], W=[wdeps[bb][c]])

                def x_dma(ex):
                    for st in range(nst):
                        xs_t, d_xs = xs_r[(ex % 2) * nst + st]
                        s0 = ex * cap + st * 128
                        kb.dma("sp", xs_t[:], xpad[s0:s0 + 128, :], R=[d_xpad], W=[d_xs])

                w_dma(0)
                x_dma(0)
                w_cast(0, range(12))
                yi = 0
                for ex in range(NE):
                    b = ex % 2
                    wup_t = wup_r[b][0]
                    wdn_t = wdn_r[b][0]
                    if ex + 1 < NE:
                        w_dma(ex + 1)
                        x_dma(ex + 1)
                    xT_t, d_xT = xT_r[b]
                    for st in range(nst):
                        xs_t, d_xs = xs_r[b * nst + st]
                        ps_t, d_pst = pst_r[st % 2]
                        for c in range(8):
                            kb.op("pe", lambda e: e.transpose(out=ps_t[:, c, :], in_=xs_t[:, c * 128:(c + 1) * 128],
                                                              identity=ident[:]), R=[d_xs, d_id], W=[d_pst])
                        kb.op("act" if st % 2 else "dve",
                              (lambda e: e.copy(out=xT_t[:, :, st * 128:(st + 1) * 128], in_=ps_t[:])) if st % 2 else
                              (lambda e: e.tensor_copy(out=xT_t[:, :, st * 128:(st + 1) * 128], in_=ps_t[:])),
                              R=[d_pst], W=[d_xT])
                    aT_t, d_aT = aT_r[b]
                    for fc in range(4):
                        pa, d_pa = pau_r[(2 * fc) % 4]
                        pu, d_pu = pau_r[(2 * fc + 1) % 4]
                        for c in range(8):
                            kb.op("pe", lambda e: e.matmul(pa[:, 0:cap], lhsT=wup_t[:, c, fc * 128:(fc + 1) * 128],
                                                           rhs=xT_t[:, c, :], start=(c == 0), stop=(c == 7)),
                                  R=[wdeps[b][c], d_xT], W=[d_pa])
                        for c in range(8):
                            kb.op("pe", lambda e: e.matmul(pu[:, 0:cap], lhsT=wup_t[:, c, DE + fc * 128:DE + (fc + 1) * 128],
                                                           rhs=xT_t[:, c, :], start=(c == 0), stop=(c == 7)),
                                  R=[wdeps[b][c], d_xT], W=[d_pu])
                        sa_t, d_sa = sa_r[fc % 2]
                        kb.op("act", lambda e: e.activation(out=sa_t[:], in_=pa[:, 0:cap], func=AF.Silu), R=[d_pa], W=[d_sa])
                        kb.op("dve", lambda e: e.tensor_tensor(out=aT_t[:, fc, :], in0=sa_t[:], in1=pu[:, 0:cap], op=ALU.mult),
                              R=[d_sa, d_pu], W=[d_aT])
                    if ex + 1 < NE:
                        w_cast(ex + 1, range(0, 8))
                    for st in range(nst):
                        yb_t, d_yb = yb_r[yi % 2]
                        yi += 1
                        for half in range(2):
                            py, d_py = py_r[half]
                            for fc in range(4):
                                kb.op("pe", lambda e: e.matmul(py[:], lhsT=aT_t[:, fc, st * 128:(st + 1) * 128],
                                                               rhs=wdn_t[:, fc, half * 512:(half + 1) * 512],
                                                               start=(fc == 0), stop=(fc == 3)),
                                      R=[d_aT, wdeps[b][8 + fc]], W=[d_py])
                            if half == 0:
                                kb.op("act", lambda e: e.copy(out=yb_t[:, 0:512], in_=py[:]), R=[d_py], W=[d_yb])
                            else:
                                kb.op("dve", lambda e: e.tensor_copy(out=yb_t[:, 512:1024], in_=py[:]), R=[d_py], W=[d_yb])
                        s0 = ex * cap + st * 128
                        kb.dma("sp", ypad[s0:s0 + 128, :], yb_t[:], R=[d_yb], W=[d_ypad])
                    if ex + 1 < NE:
                        w_cast(ex + 1, range(8, 12))
            with kb.scope():
                h1_r = kb.ring("h1", [128, D], F32, 4)
                y_r = kb.ring("y", [128, D], BF16, 8)
                h2_r = kb.ring("h2", [128, D], F32, 2)
                for (y_t, d_y) in y_r:
                    kb.op("pool", lambda e: e.memset(y_t[:], 0.0), W=[d_y])
                p3 = pass3_setup()
                def p3_load(ti):
                    tt = tiles[ti]
                    bb = ti % 4
                    h1_t, d_h1 = h1_r[bb]
                    kb.dma("sp", h1_t[:], h1s[tt * 128:(tt + 1) * 128, :], R=[d_h1s], W=[d_h1])
                    for k in range(2):
                        y_t, d_y = y_r[2 * bb + k]
                        kb.idma(out=y_t[:], out_off=None, in_=ypad[:, :],
                                in_off=bass.IndirectOffsetOnAxis(ap=dest_i[:, tt, k:k + 1], axis=0),
                                R=[d_ypad, d_dest], W=[d_y], bounds_check=breg, oob_is_err=False)
                for ti in range(min(3, len(tiles))):
                    p3_load(ti)
                for ti, t in enumerate(tiles):
                    b = ti % 2
                    r0, r1 = t * 128, (t + 1) * 128
                    h1_t, d_h1 = h1_r[ti % 4]
                    h2_t, d_h2 = h2_r[b]
                    y0_t, d_y0 = y_r[2 * (ti % 4)]
                    y1_t, d_y1 = y_r[2 * (ti % 4) + 1]
                    if ti + 3 < len(tiles):
                        p3_load(ti + 3)
                    kb.op("dve", lambda e: e.scalar_tensor_tensor(out=h2_t[:], in0=y0_t[:], scalar=gw[:, t, 0:1], in1=h1_t[:],
                                                                   op0=ALU.mult, op1=ALU.add), R=[d_y0, d_gw, d_h1], W=[d_h2])
                    kb.op("dve", lambda e: e.scalar_tensor_tensor(out=h2_t[:], in0=y1_t[:], scalar=gw[:, t, 1:2], in1=h2_t[:],
                                                                   op0=ALU.mult, op1=ALU.add), R=[d_y1, d_gw, d_h2], W=[d_h2])
                    pass3_tile(p3, t, h2_t, d_h2)

    def t2_setup_lhsT():
        idx2 = kb.sb("idx2", [128, 8], I32); d_idx2 = Dep("idx2")
        kb.dma("sp", idx2[:], idx2_d, W=[d_idx2])
        oTall = kb.sb("oTall", [128, 8, TX], BF16); d_oTall = Dep("oTall")
        oTm = kb.sb("oTm", [128, 8, 128], BF16); d_oTm = Dep("oTm")
        for c8 in range(8):
            kb.idma(out=oTall[:, c8, :], out_off=None, in_=RBb[:, :],
                    in_off=bass.IndirectOffsetOnAxis(ap=idx2[:, c8:c8 + 1], axis=0),
                    R=[d_RBb, d_idx2], W=[d_oTall], bounds_check=breg2, oob_is_err=False)
        kb.dma("sp", oTm[:], RBm.rearrange("(c p) t -> p c t", p=128), R=[d_RBm], W=[d_oTm])

        def get(t):
            if t == 0:
                return (lambda c: oTm[:, c, :]), [d_oTm]
            return (lambda c: oTall[:, c, (t - 1) * 128:t * 128]), [d_oTall]
        return get

    def t2_pass3_setup():
        p = {}
        p["wg"], p["d_wg"] = load_w(kb, "hwg", hwg_d, D, D)
        p["d_wg"].ro = True
        p["hn"], p["d_hn"] = load_bc(kb, "hnorm", hnorm, D)
        p["scr"] = kb.ring("scr", [128, D], F32, 2)
        p["ss"] = kb.ring("ss", [128, 1], F32, 2)
        p["xn"] = kb.ring("xn", [128, D], BF16, 2)
        p["hc"] = kb.ring("hc", [128, 8, 512], BF16, 2)
        p["gsb"] = kb.ring("gsb", [128, 8, 512], BF16, 2)
        p["pst"] = kb.ring("pst", [128, 8, 128], BF16, 2, psum=True)
        p["pg"] = kb.ring("pg", [128, 512], F32, 2, psum=True)
        return p

    def t2_pass3_tile(p, t, h2_t, d_h2):
        kb.dma("sp", h2s[t * 128:(t + 1) * 128, :], h2_t[:], R=[d_h2], W=[d_h2s])

        def hook(j, hc_t, d_hc):
            gsb, d_gsb = p["gsb"][j % 2]
            for g in range(8):
                pg, d_pg = p["pg"][g % 2]
                for c in range(8):
                    kb.op("pe", lambda e: e.matmul(pg[:], lhsT=p["wg"][:, c, g * 128:(g + 1) * 128], rhs=hc_t[:, c, :],
                                                   start=(c == 0), stop=(c == 7)), R=[p["d_wg"], d_hc], W=[d_pg])
                kb.op("act", lambda e: e.activation(out=gsb[:, g, :], in_=pg[:], func=AF.Silu), R=[d_pg], W=[d_gsb])
            kb.dma("sp", GS[j * 128:(j + 1) * 128, :], gsb[:].rearrange("p g t -> p (g t)"), R=[d_gsb], W=[d_GS])
        emit_hnT(h2_t, d_h2, p["hn"], p["d_hn"], p["scr"], p["ss"], p["xn"], p["pst"], p["hc"], t,
                 MC, d_MC, SC, d_SC, RC, d_RC, hc_hook=hook)

    tok_stage(0, list(range(nt)), True, t2_setup_lhsT, lambda t: x_in[t * 128:(t + 1) * 128, :], wo1_d,
              t2_pass3_setup, t2_pass3_tile)

    if stop == "T2":
        return dump_and_finish([("h2s", h2s, d_h2s), ("RC", RC, d_RC[nch - 1]), ("GS", GS, d_GS)])
    for i2 in range(2):
        with kb.scope():
            q_t = kb.sb("q", [128, L], BF16); d_q = Dep("q")
            lf_t = kb.sb("lf", [128, L], F32); d_lf = Dep("lf")
            v_t = kb.sb("v", [128, nb, 128], BF16); d_v = Dep("v")
            go_t, d_go = load_bc(kb, "go", go_d, 128)
            with kb.scope():
                hwq_t, d_hwq = load_w(kb, "hwq", hwq_d[i2], D, 128)
                hwf_t, d_hwf = load_w(kb, "hwf", hwf_d[i2], D, 128)
                hwv_t, d_hwv = load_w(kb, "hwv", hwv_d[i2], D, 128)
                for d in (d_hwq, d_hwf, d_hwv):
                    d.ro = True
                cols = kb.sb("cols", [128, 8], F32); d_cols = Dep("cols")
                kb.dma("sp", cols[:, 0:1], hbfc_d[i2], W=[d_cols])
                kb.dma("sp", cols[:, 1:2], l0c_d[i2], W=[d_cols])
                kb.dma("sp", cols[:, 2:3], l1c_d[i2], W=[d_cols])
                kb.op("dve", lambda e: e.tensor_tensor(out=cols[:, 3:4], in0=cols[:, 2:3], in1=cols[:, 1:2], op=ALU.subtract),
                      R=[d_cols], W=[d_cols])
                kb.op("act", lambda e: e.activation(out=cols[:, 4:5], in_=cols[:, 3:4], func=AF.Sigmoid), R=[d_cols], W=[d_cols])
                kb.op("act", lambda e: e.activation(out=cols[:, 5:6], in_=cols[:, 3:4], func=AF.Sigmoid, scale=-1.0),
                      R=[d_cols], W=[d_cols])
                d_cols.ro = True
                hgr = kb.ring("hg", [128, 8, 512], BF16, 2)
                sgr = kb.ring("sg", [128, 512], F32, 2)
                pqr = kb.ring("pq", [128, 512], F32, 2, psum=True)
                pfr = kb.ring("pf", [128, 512], F32, 2, psum=True)
                pvr = kb.ring("pv", [128, 512], F32, 2, psum=True)
                vi = 0
                def h2_load(G):
                    hg, d_hg = hgr[G % 2]
                    if G == 0:
                        kb.dma("sp", hg[:, :, 0:128], MC.rearrange("p (c t) -> p c t", c=8), R=[d_MC], W=[d_hg])
                    else:
                        rank, j = (G - 1) // nch, (G - 1) % nch
                        r0 = j * 512 + rank * 128
                        kb.dma("sp", hg[:].rearrange("p c t -> p (c t)"), RC[r0:r0 + 128, :], R=[d_RC[j]], W=[d_hg])
                h2_load(0)
                for G in range(1 + 4 * nch):
                    hg, d_hg = hgr[G % 2]
                    if G + 1 < 1 + 4 * nch:
                        h2_load(G + 1)
                    if G == 0:
                        n, tok0 = 128, 0
                    else:
                        n, tok0 = 512, 128 + (G - 1) * 512
                    pq, d_pq = pqr[G % 2]
                    pf, d_pf = pfr[G % 2]
                    sg, d_sg = sgr[G % 2]
                    for c in range(8):
                        kb.op("pe", lambda e: e.matmul(pq[:, 0:n], lhsT=hwq_t[:, c, :], rhs=hg[:, c, 0:n], start=(c == 0), stop=(c == 7)),
                              R=[d_hwq, d_hg], W=[d_pq])
                    kb.op("act", lambda e: e.activation(out=q_t[:, tok0:tok0 + n], in_=pq[:, 0:n], func=AF.Silu), R=[d_pq], W=[d_q])
                    for c in range(8):
                        kb.op("pe", lambda e: e.matmul(pf[:, 0:n], lhsT=hwf_t[:, c, :], rhs=hg[:, c, 0:n], start=(c == 0), stop=(c == 7)),
                              R=[d_hwf, d_hg], W=[d_pf])
                    kb.op("act", lambda e: e.activation(out=sg[:, 0:n], in_=pf[:, 0:n], func=AF.Sigmoid, bias=cols[:, 0:1], scale=1.0),
                          R=[d_pf, d_cols], W=[d_sg])
                    kb.op("dve", lambda e: e.tensor_scalar(out=sg[:, 0:n], in0=sg[:, 0:n], scalar1=cols[:, 5:6], scalar2=cols[:, 4:5],
                                                            op0=ALU.mult, op1=ALU.add), R=[d_sg, d_cols], W=[d_sg])
                    kb.op("act", lambda e: e.activation(out=lf_t[:, tok0:tok0 + n], in_=sg[:, 0:n], func=AF.Ln), R=[d_sg], W=[d_lf])
                    for s_ in range(n // 128):
                        pv, d_pv = pvr[vi % 2]
                        vi += 1
                        for c in range(8):
                            kb.op("pe", lambda e: e.matmul(pv[:, 0:128], lhsT=hg[:, c, s_ * 128:(s_ + 1) * 128], rhs=hwv_t[:, c, :],
                                                           start=(c == 0), stop=(c == 7)), R=[d_hwv, d_hg], W=[d_pv])
                        kb.op("dve", lambda e: e.tensor_copy(out=v_t[:, tok0 // 128 + s_, :], in_=pv[:, 0:128]), R=[d_pv], W=[d_v])
                kb.op("pool", lambda e: e.memset(lf_t[:, 0:128 - NMETA], 0.0), W=[d_lf])
            with kb.scope():
                onesf3 = kb.sb("onesf3", [128, 128], F32); d_onesf3 = Dep("onesf3")
                mf = kb.sb("mf", [128, 128], F32); d_mf = Dep("mf")
                mku = kb.sb("mku", [128, 128], U32); d_mk = Dep("mku")
                kb.op("pool", lambda e: e.memset(onesf3[:], 1.0), W=[d_onesf3])
                kb.op("pool", lambda e: e.affine_select(out=mf[:], in_=onesf3[:], pattern=[[1, 128]], compare_op=ALU.is_ge,
                                                        fill=0.0, base=0, channel_multiplier=-1), R=[d_onesf3], W=[d_mf])
                kb.op("dve", lambda e: e.tensor_copy(out=mku[:], in_=mf[:]), R=[d_mf], W=[d_mk])
                d_mk.ro = True
                d_onesf3.ro = True
                S_t = kb.sb("S", [128, 128], F32); d_S = Dep("S")
                Sb_r = kb.ring("Sb", [128, 128], BF16, 2)
                b_r = kb.ring("b", [128, 128], F32, 3)
                col_r = kb.ring("col", [128, 8], F32, 3)
                e1_r = kb.ring("e1", [128, 128], F32, 3)
                e2_r = kb.ring("e2", [128, 128], F32, 3)
                kk_r = kb.ring("kk", [128, 128], F32, 3)
                qd_r = kb.ring("qd", [128, 128], BF16, 3)
                kd_r = kb.ring("kd", [128, 128], BF16, 3)
                qb_r = kb.ring("qb", [128, 128], BF16, 3)
                ke_r = kb.ring("ke", [128, 128], BF16, 3)
                keT_r = kb.ring("keT", [128, 128], BF16, 2)
                scm_r = kb.ring("scm", [128, 128], BF16, 3)
                osq_r = kb.ring("osq", [128, 128], F32, 2)
                oss_r = kb.ring("oss", [128, 2], F32, 3)
                on_r = kb.ring("on", [128, 128], BF16, 3)
                stg_r = kb.ring("stg", [128, TX], BF16, 2)
                psc_b = [kb.ps("psc%d" % k, [128, 512], F32) for k in range(2)]
                po_b = [kb.ps("po%d" % k, [128, 512], F32) for k in range(2)]
                pu_b = [kb.ps("pu%d" % k, [128, 512], F32) for k in range(2)]
                pt_b = [kb.ps("pt%d" % k, [128, 8, 128], BF16) for k in range(2)]
                psc_r = [(psc_b[k % 2][:, 0:128], Dep("psc%d" % k)) for k in range(2)] * 2
                po_r = [(po_b[k % 2][:, 0:128], Dep("po%d" % k)) for k in range(2)] * 2
                pu_r = [(pu_b[k % 2][:, 0:128], Dep("pu%d" % k)) for k in range(2)] * 2
                ptk_r = [(pt_b[k % 2][:, 0, :], Dep("ptk%d" % k)) for k in range(2)] * 2
                pto_r = [(pt_b[k % 2][:, 1, :], Dep("pto%d" % k)) for k in range(2)] * 2
                for (t_, d_) in scm_r:
                    kb.op("pool", lambda e: e.memset(t_[:], 0.0), W=[d_])
                kb.op("pool", lambda e: e.memset(S_t[:], 0.0), W=[d_S])
                kb.op("pool", lambda e: e.memset(Sb_r[0][0][:], 0.0), W=[Sb_r[0][1]])

                def st_a(c):
                    cs = slice(c * 128, (c + 1) * 128)
                    b_t, d_b = b_r[c % 3]
                    col, d_col = col_r[c % 3]
                    e1, d_e1 = e1_r[c % 3]
                    e2, d_e2 = e2_r[c % 3]
                    kk, d_kk = kk_r[c % 3]
                    qd, d_qd = qd_r[c % 3]
                    kd, d_kd = kd_r[c % 3]
                    qb, d_qb = qb_r[c % 3]
                    ke, d_ke = ke_r[c % 3]
                    kb.op("dve", lambda e: e.tensor_tensor_scan(out=b_t[:], data0=onesf3[:], data1=lf_t[:, cs], initial=0.0,
                                                                 op0=ALU.mult, op1=ALU.add), R=[d_lf, d_onesf3], W=[d_b])
                    kb.op("dve", lambda e: e.tensor_copy(out=col[:, 0:1], in_=b_t[:, 63:64]), R=[d_b], W=[d_col])
                    kb.op("dve", lambda e: e.tensor_tensor(out=col[:, 1:2], in0=b_t[:, 127:128], in1=b_t[:, 63:64],
                                                            op=ALU.subtract), R=[d_b], W=[d_col])
                    kb.op("dve", lambda e: e.tensor_copy(out=col[:, 2:3], in_=b_t[:, 127:128]), R=[d_b], W=[d_col])
                    kb.op("dve", lambda e: e.tensor_scalar(out=col[:, 3:4], in0=b_t[:, 63:64], scalar1=-1.0, scalar2=None,
                                                            op0=ALU.mult), R=[d_b], W=[d_col])
                    kb.op("act", lambda e: e.activation(out=col[:, 4:7], in_=col[:, 0:3], func=AF.Exp), R=[d_col], W=[d_col])
                    kb.op("act", lambda e: e.activation(out=e1[:], in_=b_t[:], func=AF.Exp, bias=col[:, 3:4], scale=1.0),
                          R=[d_b, d_col], W=[d_e1])
                    kb.op("act", lambda e: e.activation(out=e2[:], in_=b_t[:], func=AF.Exp, bias=col[:, 0:1], scale=-1.0),
                          R=[d_b, d_col], W=[d_e2])
                    kb.op("act", lambda e: e.activation(out=kk[:], in_=lf_t[:, cs], func=AF.Exp), R=[d_lf], W=[d_kk])
                    kb.op("pool", lambda e: e.tensor_scalar(out=kk[:], in0=kk[:], scalar1=-1.0, scalar2=1.0, op0=ALU.mult,
                                                             op1=ALU.add), R=[d_kk], W=[d_kk])
                    kb.op("dve", lambda e: e.tensor_tensor(out=qd[:], in0=q_t[:, cs], in1=e1[:], op=ALU.mult),
                          R=[d_q, d_e1], W=[d_qd])
                    kb.op("dve", lambda e: e.tensor_tensor(out=kd[:], in0=kk[:], in1=e2[:], op=ALU.mult),
                          R=[d_kk, d_e2], W=[d_kd])
                    kb.op("dve", lambda e: e.scalar_tensor_tensor(out=qb[:], in0=q_t[:, cs], scalar=col[:, 4:5], in1=e1[:],
                                                                   op0=ALU.mult, op1=ALU.mult), R=[d_q, d_col, d_e1], W=[d_qb])
                    kb.op("dve", lambda e: e.scalar_tensor_tensor(out=ke[:], in0=kk[:], scalar=col[:, 5:6], in1=e2[:],
                                                                   op0=ALU.mult, op1=ALU.mult), R=[d_kk, d_col, d_e2], W=[d_ke])

                def st_f(c):
                    qd, d_qd = qd_r[c % 3]
                    kd, d_kd = kd_r[c % 3]
                    ke, d_ke = ke_r[c % 3]
                    keT, d_keT = keT_r[c % 2]
                    scm, d_scm = scm_r[c % 3]
                    psc, d_psc = psc_r[c % 4]
                    ptk, d_ptk = ptk_r[c % 4]
                    pu, d_pu = pu_r[c % 4]
                    kb.op("pe", lambda e: e.matmul(psc, lhsT=kd[:], rhs=qd[:], start=True, stop=True), R=[d_kd, d_qd], W=[d_psc])
                    kb.op("dve", lambda e: e.copy_predicated(out=scm[:], mask=mku[:], data=psc), R=[d_psc, d_mk], W=[d_scm])
                    kb.op("pe", lambda e: e.transpose(out=ptk, in_=ke[:], identity=ident[:]), R=[d_ke, d_id], W=[d_ptk])
                    kb.op("act", lambda e: e.copy(out=keT[:], in_=ptk), R=[d_ptk], W=[d_keT])
                    kb.op("pe", lambda e: e.matmul(pu, lhsT=keT[:], rhs=v_t[:, c, :], start=True, stop=True), R=[d_keT, d_v], W=[d_pu])

                def st_k(c):
                    col, d_col = col_r[c % 3]
                    qb, d_qb = qb_r[c % 3]
                    scm, d_scm = scm_r[c % 3]
                    po, d_po = po_r[c % 4]
                    pu, d_pu = pu_r[c % 4]
                    Sb, d_Sb = Sb_r[c % 2]
                    kb.op("pe", lambda e: e.matmul(po, lhsT=scm[:], rhs=v_t[:, c, :], start=True, stop=False), R=[d_scm, d_v], W=[d_po])
                    kb.op("pe", lambda e: e.matmul(po, lhsT=qb[:], rhs=Sb[:], start=False, stop=True), R=[d_qb, d_Sb], W=[d_po])
                    kb.op("dve", lambda e: e.scalar_tensor_tensor(out=S_t[:], in0=S_t[:], scalar=col[:, 6:7], in1=pu,
                                                                   op0=ALU.mult, op1=ALU.add), R=[d_S, d_col, d_pu], W=[d_S])
                    Sb2, d_Sb2 = Sb_r[(c + 1) % 2]
                    kb.op("pool", lambda e: e.tensor_copy(out=Sb2[:], in_=S_t[:]), R=[d_S], W=[d_Sb2])

                def st_n(c):
                    po, d_po = po_r[c % 4]
                    osq, d_osq = osq_r[c % 2]
                    oss, d_oss = oss_r[c % 3]
                    on_t, d_on = on_r[c % 3]
                    kb.op("act", lambda e: e.activation(out=osq[:], in_=po, func=AF.Square, accum_out=oss[:, 0:1]),
                          R=[d_po], W=[d_osq, d_oss])
                    kb.op("act", lambda e: e.activation(out=oss[:, 1:2], in_=oss[:, 0:1], func=AF.Ln, scale=1.0 / 128, bias=EPS),
                          R=[d_oss], W=[d_oss])
                    kb.op("act", lambda e: e.activation(out=oss[:, 1:2], in_=oss[:, 1:2], func=AF.Exp, scale=-0.5),
                          R=[d_oss], W=[d_oss])
                    kb.op("dve", lambda e: e.scalar_tensor_tensor(out=on_t[:], in0=po, scalar=oss[:, 1:2], in1=go_t[:],
                                                                   op0=ALU.mult, op1=ALU.mult), R=[d_po, d_oss, d_go], W=[d_on])

                def st_t(c):
                    on_t, d_on = on_r[c % 3]
                    pto, d_pto = pto_r[c % 4]
                    xb = c - 1
                    qq, tb = xb // ntx, xb % ntx
                    stg, d_stg = stg_r[qq % 2]
                    kb.op("pe", lambda e: e.transpose(out=pto, in_=on_t[:], identity=ident[:]), R=[d_on, d_id], W=[d_pto])
                    kb.op("act", lambda e: e.copy(out=stg[:, tb * 128:(tb + 1) * 128], in_=pto), R=[d_pto], W=[d_stg])
                    if tb == ntx - 1:
                        cidx = i2 * 4 + qq
                        kb.dma("sp", SD[cidx * 128:(cidx + 1) * 128, :], stg[:], R=[d_stg], W=[d_SD, d_SDc[cidx]])
                        kb.cc(SD[cidx * 128:(cidx + 1) * 128, :], RD[cidx * 512:(cidx + 1) * 512, :], GROUPS, R=[d_SDc[cidx]], W=[d_RD])

                st_a(0)
                if nb > 1:
                    st_a(1)
                st_f(0)
                for i in range(nb + 2):
                    if i + 2 < nb:
                        st_a(i + 2)
                    if i + 1 < nb:
                        st_f(i + 1)
                    if i < nb:
                        st_k(i)
                    if 1 <= i - 1 < nb:
                        st_n(i - 1)
                    if 1 <= i - 2 < nb:
                        st_t(i - 2)

    if stop == "H2":
        return dump_and_finish([("RD", RD, d_RD)])
    def t3_setup_lhsT():
        idx3 = kb.sb("idx3", [128, 8], I32); d_idx3 = Dep("idx3")
        kb.dma("sp", idx3[:], idx3_d, W=[d_idx3])
        onall = kb.sb("onall", [128, 8, TX], BF16); d_onall = Dep("onall")
        for c8 in range(8):
            kb.idma(out=onall[:, c8, :], out_off=None, in_=RD[:, :],
                    in_off=bass.IndirectOffsetOnAxis(ap=idx3[:, c8:c8 + 1], axis=0),
                    R=[d_RD, d_idx3], W=[d_onall], bounds_check=breg2, oob_is_err=False)
        gsr = kb.ring("gsT", [128, 8, 512], BF16, 2)
        ogr = kb.ring("og", [128, 8, 128], BF16, 2)

        def prefetch(t):
            if (t - 1) % 4 == 0:
                jj = (t - 1) // 4
                gs_t, d_gs = gsr[jj % 2]
                kb.dma("sp", gs_t[:].rearrange("p g t -> p (g t)"), GS[jj * 128:(jj + 1) * 128, :], R=[d_GS], W=[d_gs])

        def get(t):
            jj, ss_ = (t - 1) // 4, (t - 1) % 4
            gs_t, d_gs = gsr[jj % 2]
            og_t, d_og = ogr[t % 2]
            kb.op("dve", lambda e: e.tensor_tensor(out=og_t[:], in0=onall[:, :, (t - 1) * 128:t * 128],
                                                    in1=gs_t[:, :, ss_ * 128:(ss_ + 1) * 128], op=ALU.mult),
                  R=[d_onall, d_gs], W=[d_og])
            return (lambda c: og_t[:, c, :]), [d_og]
        get.prefetch = prefetch
        return get

    def t3_pass3_tile(p, t, h2_t, d_h2):
        kb.dma("sp", out_d[(t - 1) * 128:t * 128, :], h2_t[:], R=[d_h2])

    tok_stage(1, list(range(1, nt)), False, t3_setup_lhsT, lambda t: h2s[t * 128:(t + 1) * 128, :], wo2_d,
              lambda: None, t3_pass3_tile)
    kb.finish()
    return nc


def fused_in_maps(inp, ntx):
    f32 = np.float32
    TX = ntx * 128
    nt = ntx + 1
    x = inp["x"]
    metatile = np.zeros((128, D), f32)
    metatile[128 - NMETA:] = inp["meta_tokens"]
    valid = np.ones((nt * 128, 1), f32)
    valid[0:128 - NMETA] = 0.0
    fw = inp["fox_w_in"][0]
    hw = inp["hg_w_in"][0]
    common = {
        "valid_in": valid, "fnorm": inp["fox_norm"][0][None],
        "gqc": np.tile(inp["fox_q_norm"][0], 2)[:, None].astype(f32), "gkc": np.tile(inp["fox_k_norm"][0], 2)[:, None].astype(f32),
        "wo1": inp["fox_w_out"][0], "wo2": inp["hg_w_out"][0], "hnorm": inp["hg_norm"][0][None],
        "hwg": np.ascontiguousarray(hw[:, 3 * D:4 * D]), "go": inp["hg_o_norm"][0][None],
    }
    for l in range(2):
        common["mnorm%d" % l] = inp["moe_norm"][l][None]
        common["w_r%d" % l] = np.ascontiguousarray(np.concatenate([inp["moe_w_grp"][l], inp["moe_w_rt"][l]], 1))
        common["b_r%d" % l] = np.concatenate([inp["moe_b_grp"][l], inp["moe_b_rt"][l]])[None]
        common["w_up%d" % l] = inp["moe_w_up"][l]
        common["w_dn%d" % l] = inp["moe_w_down"][l]
    maps = []
    p = np.arange(128)
    for c in range(NCORES):
        b, r = c // 4, c % 4
        m = dict(common)
        m["x_in"] = np.concatenate([metatile, x[b, r * TX:(r + 1) * TX]], 0)
        hc = slice(4 * r * FD, (4 * r + 4) * FD)
        m["wq"] = np.ascontiguousarray(fw[:, 0:D][:, hc])
        m["wk"] = np.ascontiguousarray(fw[:, D:2 * D][:, hc])
        m["wv"] = np.ascontiguousarray(fw[:, 2 * D:3 * D][:, hc])
        m["wf"] = np.ascontiguousarray(fw[:, 3 * D + 4 * r:3 * D + 4 * r + 4])
        bf4 = inp["fox_b_f"][0][4 * r:4 * r + 4]
        m["bf4r"] = bf4[None].astype(f32)
        m["bf4c"] = bf4[:, None].astype(f32)
        idx2 = np.zeros((128, 8), np.int32)
        idx3 = np.zeros((128, 8), np.int32)
        for c8 in range(8):
            rank, sub = c8 // 2, c8 % 2
            idx2[:, c8] = (sub * 4 + r) * 512 + rank * 128 + p
            idx3[:, c8] = (sub * 4 + r) * 512 + rank * 128 + p
        m["idx2"] = idx2
        m["idx3"] = idx3
        hs = [slice((2 * r + i) * 128, (2 * r + i + 1) * 128) for i in range(2)]
        m["hwq"] = np.ascontiguousarray(np.stack([hw[:, 0:D][:, s] for s in hs]))
        m["hwf"] = np.ascontiguousarray(np.stack([hw[:, D:2 * D][:, s] for s in hs]))
        m["hwv"] = np.ascontiguousarray(np.stack([hw[:, 2 * D:3 * D][:, s] for s in hs]))
        m["hbfc"] = np.stack([inp["hg_b_f"][0][s][:, None] for s in hs]).astype(f32)
        m["l0c"] = np.stack([inp["hg_lb_logits"][0][s][:, None] for s in hs]).astype(f32)
        m["l1c"] = np.stack([inp["hg_lb_logits"][1][s][:, None] for s in hs]).astype(f32)
        maps.append(m)
    return maps


def kernel_fused(inp, ntx=NTX, cap=CAP, stop=None):
    inp = {k: np.asarray(v) for k, v in inp.items()}
    nc = _prog(("F", ntx, cap, stop), lambda: build_fused(ntx, cap, stop))
    maps = fused_in_maps(inp, ntx)
    if stop is not None:
        drop = []
        for l in range(2):
            if stop in ("T1", "H1p", "H1a") or (stop in ("T2", "H2") and l == 1):
                drop += [k + str(l) for k in ("mnorm", "w_r", "b_r", "w_up", "w_dn")]
        maps = [{k: v for k, v in m.items() if k not in drop} for m in maps]
    res = _run(nc, maps)
    if stop is not None:
        return res
    TX = ntx * 128
    out = np.zeros((2, 4 * TX, D), np.float32)
    for c in range(NCORES):
        out[c // 4, (c % 4) * TX:(c % 4 + 1) * TX] = np.asarray(res[c]["out"])
    return out


def kernel(**inp):
    return kernel_fused(inp, NTX, CAP)
```

```python
import contextlib
import numpy as np
import ml_dtypes
import concourse.bass as bass
import concourse.mybir as mybir
from concourse.bass_utils import run_bass_kernel_spmd

F32 = mybir.dt.float32
BF16 = mybir.dt.bfloat16
I32 = mybir.dt.int32
U32 = mybir.dt.uint32
AF = mybir.ActivationFunctionType
ALU = mybir.AluOpType
AX = mybir.AxisListType
NPBF = ml_dtypes.bfloat16

D = 1024
NCORES = 8
SEQ = 16384
NMETA = 16
BLK = 128
EPS = 1e-6
FH = 16
FD = 64
HH = 8
NE = 32
CAP = 384
DE = 512
NTX = 32
NT = NTX + 1
LP = SEQ + BLK
NB = LP // BLK


class Dep:
    __slots__ = ("w", "r", "name", "ro")

    def __init__(self, name=""):
        self.w = None
        self.r = []
        self.name = name
        self.ro = False


class _E:
    def __init__(self, name, eng, sem):
        self.name = name
        self.eng = eng
        self.sem = sem
        self.n = 0
        self.waited = {}
        self.pool = []
        self.pool_i = 0


class KB:
    def __init__(self, nc, dma_pool=(("sp", 16), ("pool", 16), ("act", 4)), same_eng_sync=True):
        self.nc = nc
        self.stacks = [contextlib.ExitStack()]
        self.same = same_eng_sync
        self.e = {}
        for name, eng in (("pe", nc.tensor), ("dve", nc.vector), ("act", nc.scalar),
                          ("pool", nc.gpsimd), ("sp", nc.sync)):
            sem = self.stacks[0].enter_context(nc.semaphore("c_" + name))
            self.e[name] = _E(name, eng, sem)
        for q, n in dma_pool:
            for i in range(n):
                sem = self.stacks[0].enter_context(nc.semaphore("d_%s%d" % (q, i)))
                self.e[q].pool.append([sem, 0])
        self.ninst = 0
        self.uid = 0

    def sb(self, name, shape, dt):
        self.uid += 1
        return self.stacks[-1].enter_context(self.nc.sbuf_tensor("%s_%d" % (name, self.uid), list(shape), dt))

    def ps(self, name, shape, dt):
        self.uid += 1
        return self.stacks[-1].enter_context(self.nc.psum_tensor("%s_%d" % (name, self.uid), list(shape), dt))

    def ring(self, name, shape, dt, n, psum=False):
        out = []
        for i in range(n):
            t = (self.ps if psum else self.sb)("%s%d" % (name, i), shape, dt)
            out.append((t, Dep("%s%d" % (name, i))))
        return out

    @contextlib.contextmanager
    def scope(self):
        self.stacks.append(contextlib.ExitStack())
        try:
            yield
        finally:
            self.barrier()
            self.stacks.pop().close()

    def _wait(self, E, ev, own_ok=False):
        if ev is None:
            return
        sem, val = ev
        if sem is E.sem and not own_ok:
            return
        k = id(sem)
        if E.waited.get(k, 0) >= val:
            return
        E.eng.wait_ge(sem, val)
        E.waited[k] = val

    def _deps(self, E, R, W):
        same = self.same and E.name != "pe"
        for d in R:
            self._wait(E, d.w, own_ok=same)
        for d in W:
            self._wait(E, d.w, own_ok=same)
            for ev in d.r:
                self._wait(E, ev, own_ok=False)

    def _record(self, ev, R, W):
        for d in R:
            if not d.ro:
                for i, (sem, val) in enumerate(d.r):
                    if sem is ev[0]:
                        if ev[1] > val:
                            d.r[i] = ev
                        break
                else:
                    d.r.append(ev)
        for d in W:
            d.w = ev
            d.r = []

    def op(self, en, fn, R=(), W=()):
        E = self.e[en]
        self._deps(E, R, W)
        ins = fn(E.eng)
        E.n += 1
        ins.then_inc(E.sem, 1)
        ev = (E.sem, E.n)
        self._record(ev, R, W)
        self.ninst += 1
        return ev

    def _dma_issue(self, E, issue, R, W):
        self._deps(E, R, W)
        slot = E.pool[E.pool_i % len(E.pool)]
        E.pool_i += 1
        sem, cur = slot
        if cur:
            self._wait(E, (sem, cur))
        ins = issue(E.eng)
        ins.then_inc(sem, 16)
        slot[1] = cur + 16
        ev = (sem, cur + 16)
        self._record(ev, R, W)
        self.ninst += 1
        return ev

    def dma(self, q, out, in_, R=(), W=(), **kw):
        return self._dma_issue(self.e[q], lambda e: e.dma_start(out=out, in_=in_, **kw), R, W)

    def idma(self, out, out_off, in_, in_off, R=(), W=(), **kw):
        return self._dma_issue(
            self.e["pool"],
            lambda e: e.indirect_dma_start(out=out, out_offset=out_off, in_=in_, in_offset=in_off, **kw),
            R, W)

    def cc(self, in_ap, out_ap, groups, R=(), W=()):
        E = self.e["pool"]
        self._deps(E, R, W)
        if not hasattr(self, "ccsem"):
            self.ccsem = self.stacks[0].enter_context(self.nc.semaphore("ccsem"))
            self.ccn = 0
        ins = E.eng.collective_compute("AllGather", ALU.bypass, replica_groups=groups, ins=[in_ap], outs=[out_ap], dma_qos="P2")
        ins.then_inc(self.ccsem, 1)
        self.ccn += 1
        ev = (self.ccsem, self.ccn)
        self._record(ev, R, W)
        self.ninst += 1
        return ev

    def barrier(self):
        evs = [(E.sem, E.n) for E in self.e.values() if E.n]
        for q in ("sp", "pool", "act"):
            for sem, cur in self.e[q].pool:
                if cur:
                    evs.append((sem, cur))
        if getattr(self, "ccn", 0):
            evs.append((self.ccsem, self.ccn))
        for E in self.e.values():
            for ev in evs:
                self._wait(E, ev, own_ok=False)

    def finish(self):
        self.barrier()
        self.e["sp"].eng.nop()
        while self.stacks:
            self.stacks.pop().close()


def make_ident(kb, n=128):
    identf = kb.sb("identf", [128, 128], F32)
    ident = kb.sb("ident", [128, 128], BF16)
    d1, d2 = Dep("identf"), Dep("ident")
    kb.op("pool", lambda e: e.memset(identf[:], 0.0), W=[d1])
    kb.op("pool", lambda e: e.affine_select(out=identf[:], in_=identf[:], pattern=[[-1, 128]],
                                            compare_op=ALU.not_equal, fill=1.0, base=0,
                                            channel_multiplier=1), R=[d1], W=[d1])
    kb.op("dve", lambda e: e.tensor_copy(out=ident[:], in_=identf[:]), R=[d1], W=[d2])
    d1.ro = True
    d2.ro = True
    return identf, d1, ident, d2


def load_bc(kb, name, src_ap, n, q="sp"):
    t = kb.sb(name, [128, n], F32)
    d = Dep(name)
    kb.dma(q, t[:], src_ap.to_broadcast([128, n]), W=[d])
    d.ro = True
    return t, d


def load_w(kb, name, w_ap, kin, nout, dt=BF16):
    kc = kin // 128
    t = kb.sb(name, [128, kc, nout], dt)
    d = Dep(name)
    wv = w_ap.rearrange("(c p) n -> p c n", p=128)
    for c in range(kc):
        kb.dma("pool" if dt != F32 else "sp", t[:, c, :], wv[:, c, :], W=[d])
    return t, d


def rms_rstd(kb, src, d_src, n, scr, d_scr, ss, d_ss):
    kb.op("act", lambda e: e.activation(out=scr, in_=src, func=AF.Square, accum_out=ss),
          R=[d_src], W=[d_scr, d_ss])
    kb.op("act", lambda e: e.activation(out=ss, in_=ss, func=AF.Ln, scale=1.0 / n, bias=EPS),
          R=[d_ss], W=[d_ss])
    kb.op("act", lambda e: e.activation(out=ss, in_=ss, func=AF.Exp, scale=-0.5), R=[d_ss], W=[d_ss])


def transpose_chunks(kb, src, d_src, nchunk, pst, d_pst, dst, d_dst, ident, d_id, copy_eng="dve"):
    for c in range(nchunk):
        kb.op("pe", lambda e: e.transpose(out=pst[:, c, :], in_=src[:, c * 128:(c + 1) * 128], identity=ident[:]),
              R=[d_src, d_id], W=[d_pst])
    if copy_eng == "act":
        kb.op("act", lambda e: e.copy(out=dst[:, 0:nchunk, :], in_=pst[:, 0:nchunk, :]), R=[d_pst], W=[d_dst])
    else:
        kb.op("dve", lambda e: e.tensor_copy(out=dst[:, 0:nchunk, :], in_=pst[:, 0:nchunk, :]), R=[d_pst], W=[d_dst])


def mm_acc(kb, ps_ap, d_ps, xT, d_xT, w, d_w, c0, c1, kc=8):
    for c in range(kc):
        kb.op("pe", lambda e: e.matmul(ps_ap, lhsT=xT[:, c, :], rhs=w[:, c, c0:c1], start=(c == 0), stop=(c == kc - 1)),
              R=[d_xT, d_w], W=[d_ps])


def build_A(nt):
    nc = bass.Bass("TRN2", target_bir_lowering=False)
    rows = nt * 128
    h_in = nc.dram_tensor("h", [rows, D], F32, kind="ExternalInput").ap()
    gain = nc.dram_tensor("gain", [1, D], F32, kind="ExternalInput").ap()
    w_in = nc.dram_tensor("w_in", [D, 3088], F32, kind="ExternalInput").ap()
    gq = nc.dram_tensor("gq", [1, D], F32, kind="ExternalInput").ap()
    gk = nc.dram_tensor("gk", [1, D], F32, kind="ExternalInput").ap()
    bfv = nc.dram_tensor("bf", [1, FH], F32, kind="ExternalInput").ap()
    qo = nc.dram_tensor("qo", [rows, D], BF16, kind="ExternalOutput").ap()
    ko = nc.dram_tensor("ko", [rows, D], BF16, kind="ExternalOutput").ap()
    vo = nc.dram_tensor("vo", [rows, D], BF16, kind="ExternalOutput").ap()
    lfo = nc.dram_tensor("lfo", [rows, FH], F32, kind="ExternalOutput").ap()
    d_out = Dep("out")
    kb = KB(nc)
    identf, d_idf, ident, d_id = make_ident(kb)
    g_t, d_g = load_bc(kb, "gain", gain, D)
    gq_t, d_gq = load_bc(kb, "gq", gq, D)
    gk_t, d_gk = load_bc(kb, "gk", gk, D)
    bf_t, d_bf = load_bc(kb, "bf", bfv, FH)
    w_t, d_w = load_w(kb, "w_in", w_in, D, 3088)
    d_w.ro = True
    xin = kb.ring("xin", [128, D], F32, 2)
    scr = kb.ring("scr", [128, D], F32, 2)
    ssr = kb.ring("ss", [128, 1], F32, 2)
    xnr = kb.ring("xn", [128, D], BF16, 2)
    xTr = kb.ring("xT", [128, 8, 128], BF16, 2)
    pstr = kb.ring("pst", [128, 8, 128], BF16, 2, psum=True)
    pmm = kb.ring("pmm", [128, 512], F32, 6, psum=True)
    qf = kb.ring("qf", [128, D], F32, 2)
    s16 = kb.ring("s16", [128, FH], F32, 2)
    qn = kb.ring("qn", [128, D], BF16, 2)
    kn = kb.ring("kn", [128, D], BF16, 2)
    vb = kb.ring("vb", [128, D], BF16, 2)
    lft = kb.ring("lft", [128, FH], F32, 2)
    pi = 0
    for t in range(nt):
        b = t % 2
        x_t, d_x = xin[b]
        sc_t, d_sc = scr[b]
        ss_t, d_ss = ssr[b]
        xn_t, d_xn = xnr[b]
        xT_t, d_xT = xTr[b]
        ps_t, d_pst = pstr[b]
        kb.dma("sp", x_t[:], h_in[t * 128:(t + 1) * 128, :], W=[d_x])
        rms_rstd(kb, x_t[:], d_x, D, sc_t[:], d_sc, ss_t[:], d_ss)
        kb.op("dve", lambda e: e.scalar_tensor_tensor(out=xn_t[:], in0=x_t[:], scalar=ss_t[:, 0:1], in1=g_t[:],
                                                       op0=ALU.mult, op1=ALU.mult),
              R=[d_x, d_ss, d_g], W=[d_xn])
        transpose_chunks(kb, xn_t, d_xn, 8, ps_t, d_pst, xT_t, d_xT, ident, d_id)
        for which, (g2_t, d_g2, o_ring, o_dram, scl) in enumerate(
                ((gq_t, d_gq, qn, qo, FD ** -0.5), (gk_t, d_gk, kn, ko, 1.0))):
            qf_t, d_qf = qf[which]
            for half in range(2):
                p_t, d_p = pmm[pi % 6]
                pi += 1
                c0 = which * 1024 + half * 512
                mm_acc(kb, p_t[:], d_p, xT_t, d_xT, w_t, d_w, c0, c0 + 512)
                kb.op("act", lambda e: e.copy(out=qf_t[:, half * 512:(half + 1) * 512], in_=p_t[:]),
                      R=[d_p], W=[d_qf])
            s_t, d_s = s16[which]
            kb.op("pool", lambda e: e.tensor_tensor(out=sc_t[:], in0=qf_t[:], in1=qf_t[:], op=ALU.mult),
                  R=[d_qf], W=[d_sc])
            kb.op("dve", lambda e: e.tensor_reduce(out=s_t[:], in_=sc_t[:].rearrange("p (h d) -> p h d", d=FD),
                                                    axis=AX.X, op=ALU.add), R=[d_sc], W=[d_s])
            kb.op("act", lambda e: e.activation(out=s_t[:], in_=s_t[:], func=AF.Sqrt, scale=1.0 / FD, bias=EPS),
                  R=[d_s], W=[d_s])
            kb.op("dve", lambda e: e.reciprocal(out=s_t[:], in_=s_t[:]), R=[d_s], W=[d_s])
            kb.op("dve", lambda e: e.tensor_tensor(
                out=sc_t[:].rearrange("p (h d) -> p h d", d=FD), in0=qf_t[:].rearrange("p (h d) -> p h d", d=FD),
                in1=s_t[:].unsqueeze(2).to_broadcast([128, FH, FD]), op=ALU.mult), R=[d_qf, d_s], W=[d_sc])
            o_t, d_o = o_ring[b]
            kb.op("dve", lambda e: e.scalar_tensor_tensor(out=o_t[:], in0=sc_t[:], scalar=float(scl), in1=g2_t[:],
                                                           op0=ALU.mult, op1=ALU.mult),
                  R=[d_sc, d_g2], W=[d_o])
            kb.dma("sp", o_dram[t * 128:(t + 1) * 128, :], o_t[:], R=[d_o])
        v_t, d_v = vb[b]
        for half in range(2):
            p_t, d_p = pmm[pi % 6]
            pi += 1
            c0 = 2048 + half * 512
            mm_acc(kb, p_t[:], d_p, xT_t, d_xT, w_t, d_w, c0, c0 + 512)
            kb.op("act", lambda e: e.copy(out=v_t[:, half * 512:(half + 1) * 512], in_=p_t[:]), R=[d_p], W=[d_v])
        kb.dma("sp", vo[t * 128:(t + 1) * 128, :], v_t[:], R=[d_v])
        p_t, d_p = pmm[pi % 6]
        pi += 1
        mm_acc(kb, p_t[:, 0:FH], d_p, xT_t, d_xT, w_t, d_w, 3072, 3088)
        l_t, d_l = lft[b]
        kb.op("dve", lambda e: e.tensor_tensor(out=l_t[:], in0=p_t[:, 0:FH], in1=bf_t[:], op=ALU.add),
              R=[d_p, d_bf], W=[d_l])
        kb.op("act", lambda e: e.activation(out=l_t[:], in_=l_t[:], func=AF.Exp, scale=-1.0), R=[d_l], W=[d_l])
        kb.op("act", lambda e: e.activation(out=l_t[:], in_=l_t[:], func=AF.Ln, bias=1.0), R=[d_l], W=[d_l])
        kb.op("dve", lambda e: e.tensor_scalar(out=l_t[:], in0=l_t[:], scalar1=-1.0, scalar2=None, op0=ALU.mult),
              R=[d_l], W=[d_l])
        kb.dma("sp", lfo[t * 128:(t + 1) * 128, :], l_t[:], R=[d_l])
    kb.finish()
    return nc


def build_B(nb, nbh):
    assert (nb - 1) % 4 == 0
    L = nb * 128
    nI = (nb - 1) // 4 + 1
    nc = bass.Bass("TRN2", target_bir_lowering=False)
    qT = nc.dram_tensor("qT", [nbh, FD, L], BF16, kind="ExternalInput").ap()
    kT = nc.dram_tensor("kT", [nbh, FD, L], BF16, kind="ExternalInput").ap()
    vv = nc.dram_tensor("v", [nbh, L, FD], BF16, kind="ExternalInput").ap()
    lfr = nc.dram_tensor("lfr", [nbh, L], F32, kind="ExternalInput").ap()
    lfT = nc.dram_tensor("lfT", [nbh, 128, nb], F32, kind="ExternalInput").ap()
    oT = nc.dram_tensor("oT", [nbh, FD + 1, L], F32, kind="ExternalOutput").ap()
    kb = KB(nc)
    trif = kb.sb("trif", [128, 128], F32); d_tri = Dep("trif")
    onesf = kb.sb("onesf", [128, 128], F32); d_ones = Dep("onesf")
    sel0 = kb.sb("sel0", [128, 128], F32); d_sel = Dep("sel0")
    maskb = kb.sb("maskb", [128, 128], BF16); d_mask = Dep("maskb")
    onerow = kb.sb("onerow", [128, nb], F32); d_or = Dep("onerow")
    kb.op("pool", lambda e: e.memset(onesf[:], 1.0), W=[d_ones])
    kb.op("pool", lambda e: e.memset(onerow[:], 1.0), W=[d_or])
    kb.op("pool", lambda e: e.affine_select(out=trif[:], in_=onesf[:], pattern=[[1, 128]], compare_op=ALU.is_ge,
                                            fill=0.0, base=0, channel_multiplier=-1), R=[d_ones], W=[d_tri])
    kb.op("pool", lambda e: e.affine_select(out=sel0[:], in_=onesf[:], pattern=[[0, 128]], compare_op=ALU.is_ge,
                                            fill=0.0, base=0, channel_multiplier=-1), R=[d_ones], W=[d_sel])
    kb.op("dve", lambda e: e.tensor_copy(out=maskb[:], in_=trif[:]), R=[d_tri], W=[d_mask])
    for d in (d_tri, d_ones, d_sel, d_mask, d_or):
        d.ro = True
    crow = kb.sb("crow", [nbh, L], F32); d_crow = Dep("crow")
    drow = kb.sb("drow", [nbh, L], BF16); d_drow = Dep("drow")
    kb.dma("sp", crow[:], lfr, W=[d_crow])
    kb.op("dve", lambda e: e.tensor_tensor_scan(out=crow[:], data0=onerow[0:nbh, 0:1].to_broadcast([nbh, L]),
                                                 data1=crow[:], initial=0.0, op0=ALU.mult, op1=ALU.add),
          R=[d_crow, d_or], W=[d_crow])
    kb.op("dve", lambda e: e.tensor_scalar(out=drow[:, 0:128], in0=crow[:, 0:128], scalar1=crow[:, 0:1], scalar2=None,
                                            op0=ALU.subtract), R=[d_crow], W=[d_drow])
    if nI > 1:
        kb.op("dve", lambda e: e.tensor_tensor(
            out=drow[:, 128:L].rearrange("p (i c) -> p i c", c=512),
            in0=crow[:, 128:L].rearrange("p (i c) -> p i c", c=512),
            in1=crow[:, 128:L].rearrange("p (i c) -> p i c", c=512)[:, :, 0:1].to_broadcast([nbh, nI - 1, 512]),
            op=ALU.subtract), R=[d_crow], W=[d_drow])
    QA = kb.sb("QA", [FD + 1, L], BF16); d_QA = Dep("QA")
    KA = kb.sb("KA", [FD + 1, L], BF16); d_KA = Dep("KA")
    VA = kb.sb("VA", [128, nb, FD + 1], BF16); d_VA = Dep("VA")
    lft = kb.sb("lft", [128, nb], F32); d_lft = Dep("lft")
    ct = kb.sb("ct", [128, nb], F32); d_ct = Dep("ct")
    tot = kb.sb("tot", [128, nb], F32); d_tot = Dep("tot")
    rall = kb.sb("rall", [128, nb], F32); d_rall = Dep("rall")
    biasr = kb.ring("bias", [128, nb], F32, 2)
    pr = kb.ring("P", [128, 512], BF16, 4)
    osb = kb.ring("osb", [FD + 1, 512], F32, 2)
    psS = kb.ring("psS", [128, 512], F32, 4, psum=True)
    psO = kb.ring("psO", [128, 512], F32, 2, psum=True)
    psM = kb.ring("psM", [128, 512], F32, 2, psum=True)
    si = 0
    oi = 0
    for bh in range(nbh):
        kb.dma("sp", QA[0:FD, :], qT[bh], W=[d_QA])
        kb.dma("sp", QA[FD:FD + 1, :], drow[bh:bh + 1, :], R=[d_drow], W=[d_QA])
        kb.dma("sp", KA[0:FD, :], kT[bh], W=[d_KA])
        kb.op("pool", lambda e: e.memset(KA[FD:FD + 1, :], 1.0), W=[d_KA])
        kb.dma("sp", VA[:, :, 0:FD], vv[bh].rearrange("(j p) d -> p j d", p=128), W=[d_VA])
        kb.op("pool", lambda e: e.memset(VA[:, :, FD:FD + 1], 1.0), W=[d_VA])
        kb.op("pool", lambda e: e.memset(VA[0:112, 0, :], 0.0), W=[d_VA])
        kb.dma("sp", lft[:], lfT[bh], W=[d_lft])
        pm0, d_pm0 = psM[0]
        pm1, d_pm1 = psM[1]
        kb.op("pe", lambda e: e.matmul(pm0[:, 0:nb], lhsT=trif[:], rhs=lft[:], start=True, stop=True),
              R=[d_tri, d_lft], W=[d_pm0])
        kb.op("pe", lambda e: e.matmul(pm1[:, 0:nb], lhsT=onesf[:], rhs=lft[:], start=True, stop=True),
              R=[d_ones, d_lft], W=[d_pm1])
        kb.op("dve", lambda e: e.tensor_copy(out=tot[:], in_=pm1[:, 0:nb]), R=[d_pm1], W=[d_tot])
        kb.op("dve", lambda e: e.tensor_tensor_scan(out=ct[:], data0=onerow[:], data1=tot[:], initial=0.0,
                                                     op0=ALU.mult, op1=ALU.add), R=[d_tot, d_or], W=[d_ct])
        kb.op("dve", lambda e: e.tensor_tensor(out=ct[:], in0=ct[:], in1=tot[:], op=ALU.subtract),
              R=[d_ct, d_tot], W=[d_ct])
        kb.op("dve", lambda e: e.tensor_tensor(out=ct[:], in0=ct[:], in1=pm0[:, 0:nb], op=ALU.add),
              R=[d_ct, d_pm0], W=[d_ct])
        kb.op("pe", lambda e: e.matmul(pm1[:, 0:nb], lhsT=sel0[:], rhs=ct[:], start=True, stop=True),
              R=[d_sel, d_ct], W=[d_pm1])
        kb.op("dve", lambda e: e.tensor_copy(out=rall[:], in_=pm1[:, 0:nb]), R=[d_pm1], W=[d_rall])
        steps = []
        for I in range(nI):
            j0 = 0 if I == 0 else 4 * I - 3
            nblk = 1 if I == 0 else 4
            nJ = j0 + nblk
            for J in range(nJ):
                steps.append((I, J, j0, nblk, nJ))
        LA = 2
        cur = {}
        for idx in range(len(steps) + LA):
            if idx < len(steps):
                I, J, j0, nblk, nJ = steps[idx]
                q0 = j0 * 128
                bias_t, d_bias = biasr[I % 2]
                if J == 0:
                    kb.op("dve", lambda e: e.tensor_scalar(out=bias_t[:, 0:nJ], in0=ct[:, 0:nJ], scalar1=-1.0,
                                                            scalar2=rall[:, j0:j0 + 1], op0=ALU.mult, op1=ALU.add),
                          R=[d_ct, d_rall], W=[d_bias])
                m = max(0, J - j0)
                c0 = m * 128
                c1 = nblk * 128
                ps, d_ps = psS[idx % 4]
                p_t, d_p = pr[idx % 4]
                kb.op("pe", lambda e: e.matmul(ps[:, c0:c1], lhsT=KA[:, J * 128:(J + 1) * 128],
                                               rhs=QA[:, q0 + c0:q0 + c1], start=True, stop=True),
                      R=[d_KA, d_QA], W=[d_ps])
                kb.op("act", lambda e: e.activation(out=p_t[:, c0:c1], in_=ps[:, c0:c1], func=AF.Exp,
                                                    bias=bias_t[:, J:J + 1], scale=1.0),
                      R=[d_ps, d_bias], W=[d_p])
                if J >= j0:
                    kb.op("dve", lambda e: e.tensor_tensor(out=p_t[:, c0:c0 + 128], in0=p_t[:, c0:c0 + 128],
                                                            in1=maskb[:], op=ALU.mult), R=[d_p, d_mask], W=[d_p])
            if idx >= LA:
                I, J, j0, nblk, nJ = steps[idx - LA]
                q0 = j0 * 128
                m = max(0, J - j0)
                c0 = m * 128
                c1 = nblk * 128
                p_t, d_p = pr[(idx - LA) % 4]
                po, d_po = psO[I % 2]
                kb.op("pe", lambda e: e.matmul(po[0:FD + 1, c0:c1], lhsT=VA[:, J, :], rhs=p_t[:, c0:c1],
                                               start=(J == 0), stop=(J == nJ - 1)),
                      R=[d_VA, d_p], W=[d_po])
                if J == nJ - 1:
                    o_t, d_o = osb[I % 2]
                    ncol = nblk * 128
                    kb.op("dve", lambda e: e.tensor_copy(out=o_t[:, 0:ncol], in_=po[0:FD + 1, 0:ncol]),
                          R=[d_po], W=[d_o])
                    kb.dma("sp", oT[bh, :, q0:q0 + ncol], o_t[:, 0:ncol], R=[d_o])
    kb.finish()
    return nc


BIGIDX = 1.0e6


def build_CE(stage, nt, has_meta, cap=CAP):
    nc = bass.Bass("TRN2", target_bir_lowering=False)
    rows = nt * 128
    nslot = NE * cap
    nst = cap // 128
    a_in = nc.dram_tensor("a_in", [rows, D], F32, kind="ExternalInput").ap()
    hp_in = nc.dram_tensor("hp_in", [rows, D], F32, kind="ExternalInput").ap()
    if stage == "C":
        den_in = nc.dram_tensor("den_in", [rows, FH], F32, kind="ExternalInput").ap()
    else:
        gs_in = nc.dram_tensor("gs_in", [rows, D], BF16, kind="ExternalInput").ap()
    valid_in = nc.dram_tensor("valid_in", [rows, 1], F32, kind="ExternalInput").ap()
    w_out = nc.dram_tensor("w_out", [D, D], F32, kind="ExternalInput").ap()
    mnorm = nc.dram_tensor("mnorm", [1, D], F32, kind="ExternalInput").ap()
    w_r = nc.dram_tensor("w_r", [D, 36], F32, kind="ExternalInput").ap()
    b_r = nc.dram_tensor("b_r", [1, 36], F32, kind="ExternalInput").ap()
    w_up = nc.dram_tensor("w_up", [NE, D, 2 * DE], F32, kind="ExternalInput").ap()
    w_dn = nc.dram_tensor("w_dn", [NE, DE, D], F32, kind="ExternalInput").ap()
    h2_out = nc.dram_tensor("h2_out", [rows, D], F32, kind="ExternalOutput").ap()
    if stage == "C":
        hnorm = nc.dram_tensor("hnorm", [1, D], F32, kind="ExternalInput").ap()
        hw_in = nc.dram_tensor("hw_in", [D, 4 * D], F32, kind="ExternalInput").ap()
        hbf = nc.dram_tensor("hbf", [1, D], F32, kind="ExternalInput").ap()
        lbl = nc.dram_tensor("lbl", [2, D], F32, kind="ExternalInput").ap()
        q1_out = nc.dram_tensor("q1_out", [rows, D], BF16, kind="ExternalOutput").ap()
        lf1_out = nc.dram_tensor("lf1_out", [rows, D], F32, kind="ExternalOutput").ap()
        v1_out = nc.dram_tensor("v1_out", [rows, D], BF16, kind="ExternalOutput").ap()
        gs_out = nc.dram_tensor("gs_out", [rows, D], BF16, kind="ExternalOutput").ap()
    xpad = nc.dram_tensor("xpad", [nslot, D], BF16, kind="Internal").ap()
    ypad = nc.dram_tensor("ypad", [nslot, D], BF16, kind="Internal").ap()
    h1s = nc.dram_tensor("h1s", [rows, D], F32, kind="Internal").ap()
    d_xpad, d_ypad, d_h1s = Dep("xpad"), Dep("ypad"), Dep("h1s")

    kb = KB(nc)
    identf, d_idf, ident, d_id = make_ident(kb)
    breg = nc.gpsimd.to_reg(nslot - 1)
    dest_i = kb.sb("dest_i", [128, nt, 2], I32); d_dest = Dep("dest_i")
    gw = kb.sb("gw", [128, nt, 2], F32); d_gw = Dep("gw")
    cntb = kb.sb("cntb", [128, NE], F32); d_cntb = Dep("cntb")
    usb = kb.sb("usb", [128, 128], BF16); d_us = Dep("usb")
    onesb = kb.sb("onesb", [128, 128], BF16); d_onesb = Dep("onesb")
    onesf = kb.sb("onesf", [128, 128], F32); d_onesf = Dep("onesf")
    tmpf = kb.sb("tmpf", [128, 128], F32); d_tmpf = Dep("tmpf")
    kb.op("pool", lambda e: e.memset(onesf[:], 1.0), W=[d_onesf])
    kb.op("pool", lambda e: e.affine_select(out=tmpf[:], in_=onesf[:], pattern=[[1, 128]], compare_op=ALU.is_gt,
                                            fill=0.0, base=0, channel_multiplier=-1), R=[d_onesf], W=[d_tmpf])
    kb.op("dve", lambda e: e.tensor_copy(out=usb[:], in_=tmpf[:]), R=[d_tmpf], W=[d_us])
    kb.op("dve", lambda e: e.tensor_copy(out=onesb[:], in_=onesf[:]), R=[d_onesf], W=[d_onesb])
    kb.op("pool", lambda e: e.iota(cntb[:], pattern=[[cap, NE]], base=0, channel_multiplier=0,
                                   allow_small_or_imprecise_dtypes=True), W=[d_cntb])
    for d in (d_us, d_onesb, d_onesf):
        d.ro = True
    mn_t, d_mn = load_bc(kb, "mnorm", mnorm, D)
    br_t, d_br = load_bc(kb, "b_r", b_r, 36)
    wr_t, d_wr = load_w(kb, "w_r", w_r, D, 36, dt=F32)
    d_wr.ro = True

    with kb.scope():
        wo_t, d_wo = load_w(kb, "w_out", w_out, D, D)
        d_wo.ro = True
        a_r = kb.ring("a", [128, D], F32, 2)
        hp_r = kb.ring("hp", [128, D], F32, 2)
        den_r = kb.ring("den", [128, FH], F32, 2)
        gs_r = kb.ring("gs", [128, D], BF16, 2)
        ob_r = kb.ring("ob", [128, D], BF16, 2)
        oT_r = kb.ring("oT", [128, 8, 128], BF16, 2)
        h1_r = kb.ring("h1", [128, D], F32, 2)
        scr_r = kb.ring("scr", [128, D], F32, 2)
        ss_r = kb.ring("ss", [128, 1], F32, 2)
        hmf_r = kb.ring("hmf", [128, D], F32, 2)
        hmb_r = kb.ring("hmb", [128, D], BF16, 2)
        hmT_r = kb.ring("hmT", [128, 8, 128], F32, 2)
        sm_r = kb.ring("sm", [128, 256], F32, 2)
        ab_r = kb.ring("ab", [128, NE], BF16, 2)
        val_r = kb.ring("val", [128, 2], F32, 2)
        pst_r = kb.ring("pst", [128, 8, 128], BF16, 2, psum=True)
        pstf_r = kb.ring("pstf", [128, 8, 128], F32, 1, psum=True)
        pmm_r = kb.ring("pmm", [128, 512], F32, 2, psum=True)
        prt_r = kb.ring("prt", [128, 512], F32, 2, psum=True)
        for t in range(nt):
            b = t % 2
            r0, r1 = t * 128, (t + 1) * 128
            a_t, d_a = a_r[b]
            hp_t, d_hp = hp_r[b]
            ob_t, d_ob = ob_r[b]
            kb.dma("sp", a_t[:], a_in[r0:r1, :], W=[d_a])
            kb.dma("sp", hp_t[:], hp_in[r0:r1, :], W=[d_hp])
            if stage == "C":
                den_t, d_den = den_r[b]
                kb.dma("sp", den_t[:], den_in[r0:r1, :], W=[d_den])
                kb.op("dve", lambda e: e.tensor_scalar(out=den_t[:], in0=den_t[:], scalar1=1e-30, scalar2=None,
                                                        op0=ALU.max), R=[d_den], W=[d_den])
                kb.op("dve", lambda e: e.reciprocal(out=den_t[:], in_=den_t[:]), R=[d_den], W=[d_den])
                kb.op("dve", lambda e: e.tensor_tensor(
                    out=ob_t[:].rearrange("p (h d) -> p h d", d=FD), in0=a_t[:].rearrange("p (h d) -> p h d", d=FD),
                    in1=den_t[:].unsqueeze(2).to_broadcast([128, FH, FD]), op=ALU.mult), R=[d_a, d_den], W=[d_ob])
            else:
                gs_t, d_gs = gs_r[b]
                kb.dma("sp", gs_t[:], gs_in[r0:r1, :], W=[d_gs])
                kb.op("dve", lambda e: e.tensor_tensor(out=ob_t[:], in0=a_t[:], in1=gs_t[:], op=ALU.mult),
                      R=[d_a, d_gs], W=[d_ob])
            oT_t, d_oT = oT_r[b]
            ps_t, d_pst = pst_r[b]
            transpose_chunks(kb, ob_t, d_ob, 8, ps_t, d_pst, oT_t, d_oT, ident, d_id, copy_eng="act")
            h1_t, d_h1 = h1_r[b]
            for half in range(2):
                p_t, d_p = pmm_r[half]
                mm_acc(kb, p_t[:], d_p, oT_t, d_oT, wo_t, d_wo, half * 512, half * 512 + 512)
                kb.op("dve", lambda e: e.tensor_tensor(out=h1_t[:, half * 512:(half + 1) * 512], in0=p_t[:],
                                                        in1=hp_t[:, half * 512:(half + 1) * 512], op=ALU.add),
                      R=[d_p, d_hp], W=[d_h1])
            kb.dma("sp", h1s[r0:r1, :], h1_t[:], R=[d_h1], W=[d_h1s])
            sc_t, d_sc = scr_r[b]
            ss_t, d_ss = ss_r[b]
            hmf_t, d_hmf = hmf_r[b]
            hmb_t, d_hmb = hmb_r[b]
            rms_rstd(kb, h1_t[:], d_h1, D, sc_t[:], d_sc, ss_t[:], d_ss)
            kb.op("dve", lambda e: e.scalar_tensor_tensor(out=hmf_t[:], in0=h1_t[:], scalar=ss_t[:, 0:1], in1=mn_t[:],
                                                           op0=ALU.mult, op1=ALU.mult),
                  R=[d_h1, d_ss, d_mn], W=[d_hmf])
            kb.op("pool", lambda e: e.tensor_copy(out=hmb_t[:], in_=hmf_t[:]), R=[d_hmf], W=[d_hmb])
            pf_t, d_pf = pstf_r[0]
            hmT_t, d_hmT = hmT_r[b]
            for c in range(8):
                kb.op("pe", lambda e: e.transpose(out=pf_t[:, c, :], in_=hmf_t[:, c * 128:(c + 1) * 128],
                                                  identity=identf[:]), R=[d_hmf, d_idf], W=[d_pf])
            kb.op("act", lambda e: e.copy(out=hmT_t[:], in_=pf_t[:]), R=[d_pf], W=[d_hmT])
            pr_t, d_pr = prt_r[b]
            for c in range(8):
                kb.op("pe", lambda e: e.matmul(pr_t[:, 0:36], lhsT=hmT_t[:, c, :], rhs=wr_t[:, c, :],
                                               start=(c == 0), stop=(c == 7)), R=[d_hmT, d_wr], W=[d_pr])
            sm, d_sm = sm_r[b]
            lg = sm[:, 0:36]
            gmax = sm[:, 36:37]
            ngmax = sm[:, 37:38]
            gsum = sm[:, 38:39]
            ggate = sm[:, 39:40]
            eg = sm[:, 40:44]
            ohg = sm[:, 44:48]
            esel = sm[:, 48:56]
            top8 = sm[:, 56:64]
            oh0 = sm[:, 64:72]
            oh1 = sm[:, 72:80]
            A0 = sm[:, 80:112]
            A1 = sm[:, 112:144]
            sbt = sm[:, 144:176]
            tmp = sm[:, 176:208]
            destf = sm[:, 208:210]
            diff = sm[:, 210:211]
            sg = sm[:, 211:212]
            S = [d_sm]
            dv = lambda fn, R=(), W=(): kb.op("dve", fn, R=list(R) + S, W=list(W) + S)
            dv(lambda e: e.tensor_tensor(out=lg, in0=pr_t[:, 0:36], in1=br_t[:], op=ALU.add), R=[d_pr, d_br])
            dv(lambda e: e.tensor_reduce(out=gmax, in_=lg[:, 0:4], axis=AX.X, op=ALU.max))
            dv(lambda e: e.tensor_scalar(out=ngmax, in0=gmax, scalar1=-1.0, scalar2=None, op0=ALU.mult))
            kb.op("act", lambda e: e.activation(out=eg, in_=lg[:, 0:4], func=AF.Exp, bias=ngmax, scale=1.0,
                                                accum_out=gsum), R=S, W=S)
            dv(lambda e: e.reciprocal(out=ggate, in_=gsum))
            dv(lambda e: e.tensor_scalar(out=ohg, in0=lg[:, 0:4], scalar1=gmax, scalar2=None, op0=ALU.is_equal))
            dv(lambda e: e.tensor_scalar(out=esel, in0=lg[:, 4:12], scalar1=ohg[:, 0:1], scalar2=None, op0=ALU.mult))
            for g in range(1, 4):
                dv(lambda e: e.scalar_tensor_tensor(out=esel, in0=lg[:, 4 + 8 * g:12 + 8 * g], scalar=ohg[:, g:g + 1],
                                                    in1=esel, op0=ALU.mult, op1=ALU.add))
            dv(lambda e: e.max(out=top8, in_=esel))
            dv(lambda e: e.tensor_scalar(out=oh0, in0=esel, scalar1=top8[:, 0:1], scalar2=None, op0=ALU.is_equal))
            dv(lambda e: e.tensor_scalar(out=oh1, in0=esel, scalar1=top8[:, 1:2], scalar2=None, op0=ALU.is_equal))
            for (Ak, ohk) in ((A0, oh0), (A1, oh1)):
                dv(lambda e: e.tensor_tensor(out=Ak.rearrange("p (g j) -> p g j", j=8),
                                             in0=ohg.unsqueeze(2).to_broadcast([128, 4, 8]),
                                             in1=ohk.unsqueeze(1).to_broadcast([128, 4, 8]), op=ALU.mult))
            use_valid = has_meta and t == 0
            if use_valid:
                val_t, d_val = val_r[b]
                kb.dma("sp", val_t[:, 0:1], valid_in[r0:r1, :], W=[d_val])
                kb.op("dve", lambda e: e.tensor_scalar(out=val_t[:, 1:2], in0=val_t[:, 0:1], scalar1=-BIGIDX,
                                                        scalar2=BIGIDX, op0=ALU.mult, op1=ALU.add),
                      R=[d_val], W=[d_val])
                for Ak in (A0, A1):
                    dv(lambda e: e.tensor_scalar(out=Ak, in0=Ak, scalar1=val_t[:, 0:1], scalar2=None, op0=ALU.mult),
                       R=[d_val])
            ab_t, d_ab = ab_r[b]
            kb.op("dve", lambda e: e.tensor_tensor(out=ab_t[:], in0=A0, in1=A1, op=ALU.add), R=S, W=[d_ab])
            kb.op("pe", lambda e: e.matmul(pr_t[:, 64:96], lhsT=usb[:], rhs=ab_t[:], start=True, stop=True),
                  R=[d_us, d_ab], W=[d_pr])
            kb.op("pe", lambda e: e.matmul(pr_t[:, 96:128], lhsT=onesb[:], rhs=ab_t[:], start=True, stop=True),
                  R=[d_onesb, d_ab], W=[d_pr])
            dv(lambda e: e.tensor_tensor(out=sbt, in0=pr_t[:, 64:96], in1=cntb[:], op=ALU.add), R=[d_pr, d_cntb])
            kb.op("dve", lambda e: e.tensor_tensor(out=cntb[:], in0=cntb[:], in1=pr_t[:, 96:128], op=ALU.add),
                  R=[d_pr, d_cntb] + S, W=[d_cntb])
            for k, Ak in enumerate((A0, A1)):
                dv(lambda e: e.tensor_tensor(out=tmp, in0=Ak, in1=sbt, op=ALU.mult))
                dv(lambda e: e.tensor_reduce(out=destf[:, k:k + 1], in_=tmp, axis=AX.X, op=ALU.add))
            if use_valid:
                dv(lambda e: e.tensor_scalar(out=destf, in0=destf, scalar1=val_t[:, 0:1], scalar2=val_t[:, 1:2],
                                             op0=ALU.mult, op1=ALU.add), R=[d_val])
            kb.op("dve", lambda e: e.tensor_copy(out=dest_i[:, t, :], in_=destf), R=S, W=[d_dest])
            dv(lambda e: e.tensor_tensor(out=diff, in0=top8[:, 0:1], in1=top8[:, 1:2], op=ALU.subtract))
            kb.op("act", lambda e: e.activation(out=sg, in_=diff, func=AF.Sigmoid), R=S, W=S)
            kb.op("dve", lambda e: e.tensor_tensor(out=gw[:, t, 0:1], in0=sg, in1=ggate, op=ALU.mult), R=S, W=[d_gw])
            kb.op("dve", lambda e: e.tensor_tensor(out=gw[:, t, 1:2], in0=ggate, in1=gw[:, t, 0:1], op=ALU.subtract),
                  R=S + [d_gw], W=[d_gw])
            for k in range(2):
                kb.idma(out=xpad[:, :], out_off=bass.IndirectOffsetOnAxis(ap=dest_i[:, t, k:k + 1], axis=0),
                        in_=hmb_t[:], in_off=None, R=[d_hmb, d_dest], W=[d_xpad],
                        bounds_check=breg, oob_is_err=False)

    with kb.scope():
        wup_r = kb.ring("wup", [128, 8, 2 * DE], BF16, 2)
        wdn_r = kb.ring("wdn", [128, 4, D], BF16, 2)
        xs_r = kb.ring("xs", [128, D], BF16, 3)
        xT_r = kb.ring("xT", [128, 8, cap], BF16, 2)
        sa_r = kb.ring("sa", [128, cap], F32, 2)
        aT_r = kb.ring("aT", [128, 4, cap], BF16, 2)
        yb_r = kb.ring("yb", [128, D], BF16, 2)
        pst_r = kb.ring("pst", [128, 8, 128], BF16, 2, psum=True)
        pau_r = kb.ring("pau", [128, 512], F32, 4, psum=True)
        py_r = kb.ring("py", [128, 512], F32, 2, psum=True)
        xi = 0
        yi = 0
        for ex in range(NE):
            b = ex % 2
            wup_t, d_wup = wup_r[b]
            wdn_t, d_wdn = wdn_r[b]
            wuv = w_up[ex].rearrange("(c p) n -> p c n", p=128)
            wdv = w_dn[ex].rearrange("(c p) n -> p c n", p=128)
            for c in range(8):
                kb.dma("pool", wup_t[:, c, :], wuv[:, c, :], W=[d_wup])
            for c in range(4):
                kb.dma("pool", wdn_t[:, c, :], wdv[:, c, :], W=[d_wdn])
            xT_t, d_xT = xT_r[b]
            for st in range(nst):
                xs_t, d_xs = xs_r[xi % 3]
                ps_t, d_pst = pst_r[xi % 2]
                xi += 1
                s0 = ex * cap + st * 128
                kb.dma("sp", xs_t[:], xpad[s0:s0 + 128, :], R=[d_xpad], W=[d_xs])
                for c in range(8):
                    kb.op("pe", lambda e: e.transpose(out=ps_t[:, c, :], in_=xs_t[:, c * 128:(c + 1) * 128],
                                                      identity=ident[:]), R=[d_xs, d_id], W=[d_pst])
                kb.op("act" if st % 2 else "dve",
                      (lambda e: e.copy(out=xT_t[:, :, st * 128:(st + 1) * 128], in_=ps_t[:])) if st % 2 else
                      (lambda e: e.tensor_copy(out=xT_t[:, :, st * 128:(st + 1) * 128], in_=ps_t[:])),
                      R=[d_pst], W=[d_xT])
            aT_t, d_aT = aT_r[b]
            for fc in range(4):
                pa, d_pa = pau_r[(2 * fc) % 4]
                pu, d_pu = pau_r[(2 * fc + 1) % 4]
                for c in range(8):
                    kb.op("pe", lambda e: e.matmul(pa[:, 0:cap], lhsT=wup_t[:, c, fc * 128:(fc + 1) * 128],
                                                   rhs=xT_t[:, c, :], start=(c == 0), stop=(c == 7)),
                          R=[d_wup, d_xT], W=[d_pa])
                for c in range(8):
                    kb.op("pe", lambda e: e.matmul(pu[:, 0:cap], lhsT=wup_t[:, c, DE + fc * 128:DE + (fc + 1) * 128],
                                                   rhs=xT_t[:, c, :], start=(c == 0), stop=(c == 7)),
                          R=[d_wup, d_xT], W=[d_pu])
                sa_t, d_sa = sa_r[fc % 2]
                kb.op("act", lambda e: e.activation(out=sa_t[:], in_=pa[:, 0:cap], func=AF.Silu), R=[d_pa], W=[d_sa])
                kb.op("dve", lambda e: e.tensor_tensor(out=aT_t[:, fc, :], in0=sa_t[:], in1=pu[:, 0:cap], op=ALU.mult),
                      R=[d_sa, d_pu], W=[d_aT])
            for st in range(nst):
                yb_t, d_yb = yb_r[yi % 2]
                yi += 1
                for half in range(2):
                    py, d_py = py_r[half]
                    for fc in range(4):
                        kb.op("pe", lambda e: e.matmul(py[:], lhsT=aT_t[:, fc, st * 128:(st + 1) * 128],
                                                       rhs=wdn_t[:, fc, half * 512:(half + 1) * 512],
                                                       start=(fc == 0), stop=(fc == 3)),
                              R=[d_aT, d_wdn], W=[d_py])
                    if half == 0:
                        kb.op("act", lambda e: e.copy(out=yb_t[:, 0:512], in_=py[:]), R=[d_py], W=[d_yb])
                    else:
                        kb.op("dve", lambda e: e.tensor_copy(out=yb_t[:, 512:1024], in_=py[:]), R=[d_py], W=[d_yb])
                s0 = ex * cap + st * 128
                kb.dma("sp", ypad[s0:s0 + 128, :], yb_t[:], R=[d_yb], W=[d_ypad])

    with kb.scope():
        h1_r = kb.ring("h1", [128, D], F32, 2)
        y_r = kb.ring("y", [128, D], BF16, 4)
        h2_r = kb.ring("h2", [128, D], F32, 2)
        for (y_t, d_y) in y_r:
            kb.op("pool", lambda e: e.memset(y_t[:], 0.0), W=[d_y])
        if stage == "C":
            hw_t, d_hw = load_w(kb, "hw_in", hw_in, D, 4 * D)
            d_hw.ro = True
            hn_t, d_hn = load_bc(kb, "hnorm", hnorm, D)
            hbf_t, d_hbf = load_bc(kb, "hbf", hbf, D)
            l0_t, d_l0 = load_bc(kb, "l0", lbl[0:1, :], D)
            l1_t, d_l1 = load_bc(kb, "l1", lbl[1:2, :], D)
            lb_t = kb.sb("lb", [128, D], F32); d_lb = Dep("lb")
            oml_t = kb.sb("oml", [128, D], F32); d_oml = Dep("oml")
            kb.op("dve", lambda e: e.tensor_tensor(out=lb_t[:], in0=l1_t[:], in1=l0_t[:], op=ALU.subtract),
                  R=[d_l0, d_l1], W=[d_lb])
            kb.op("act", lambda e: e.activation(out=oml_t[:], in_=lb_t[:], func=AF.Sigmoid, scale=-1.0),
                  R=[d_lb], W=[d_oml])
            kb.op("act", lambda e: e.activation(out=lb_t[:], in_=lb_t[:], func=AF.Sigmoid), R=[d_lb], W=[d_lb])
            d_lb.ro = True
            d_oml.ro = True
            scr_r = kb.ring("scr", [128, D], F32, 2)
            ss_r = kb.ring("ss", [128, 1], F32, 2)
            xn_r = kb.ring("xn", [128, D], BF16, 2)
            xT_r = kb.ring("xT", [128, 8, 128], BF16, 2)
            pst_r = kb.ring("pst", [128, 8, 128], BF16, 2, psum=True)
            pmm_r = kb.ring("pmm", [128, 512], F32, 4, psum=True)
            ob_r = kb.ring("obf", [128, D], BF16, 4)
            of_r = kb.ring("off", [128, D], F32, 2)
            zf_r = kb.ring("zf", [128, 512], F32, 2)
        pi = 0
        obi = 0
        for t in range(nt):
            b = t % 2
            r0, r1 = t * 128, (t + 1) * 128
            h1_t, d_h1 = h1_r[b]
            h2_t, d_h2 = h2_r[b]
            y0_t, d_y0 = y_r[2 * b]
            y1_t, d_y1 = y_r[2 * b + 1]
            kb.dma("sp", h1_t[:], h1s[r0:r1, :], R=[d_h1s], W=[d_h1])
            for k, (y_t, d_y) in enumerate(((y0_t, d_y0), (y1_t, d_y1))):
                kb.idma(out=y_t[:], out_off=None, in_=ypad[:, :],
                        in_off=bass.IndirectOffsetOnAxis(ap=dest_i[:, t, k:k + 1], axis=0),
                        R=[d_ypad, d_dest], W=[d_y], bounds_check=breg, oob_is_err=False)
            kb.op("dve", lambda e: e.scalar_tensor_tensor(out=h2_t[:], in0=y0_t[:], scalar=gw[:, t, 0:1], in1=h1_t[:],
                                                           op0=ALU.mult, op1=ALU.add), R=[d_y0, d_gw, d_h1], W=[d_h2])
            kb.op("dve", lambda e: e.scalar_tensor_tensor(out=h2_t[:], in0=y1_t[:], scalar=gw[:, t, 1:2], in1=h2_t[:],
                                                           op0=ALU.mult, op1=ALU.add), R=[d_y1, d_gw, d_h2], W=[d_h2])
            kb.dma("sp", h2_out[r0:r1, :], h2_t[:], R=[d_h2])
            if stage != "C":
                continue
            sc_t, d_sc = scr_r[b]
            ss_t, d_ss = ss_r[b]
            xn_t, d_xn = xn_r[b]
            xT_t, d_xT = xT_r[b]
            ps_t, d_pst = pst_r[b]
            rms_rstd(kb, h2_t[:], d_h2, D, sc_t[:], d_sc, ss_t[:], d_ss)
            kb.op("dve", lambda e: e.scalar_tensor_tensor(out=xn_t[:], in0=h2_t[:], scalar=ss_t[:, 0:1], in1=hn_t[:],
                                                           op0=ALU.mult, op1=ALU.mult), R=[d_h2, d_ss, d_hn], W=[d_xn])
            transpose_chunks(kb, xn_t, d_xn, 8, ps_t, d_pst, xT_t, d_xT, ident, d_id)
            for grp, (dram_o, kind) in enumerate(((q1_out, "silu"), (lf1_out, "f"), (v1_out, "copy"), (gs_out, "silu"))):
                if kind == "f":
                    o_t, d_o = of_r[b]
                else:
                    o_t, d_o = ob_r[obi % 4]
                    obi += 1
                for half in range(2):
                    p_t, d_p = pmm_r[pi % 4]
                    pi += 1
                    c0 = grp * 1024 + half * 512
                    hs = slice(half * 512, (half + 1) * 512)
                    mm_acc(kb, p_t[:], d_p, xT_t, d_xT, hw_t, d_hw, c0, c0 + 512)
                    if kind == "silu":
                        kb.op("act", lambda e: e.activation(out=o_t[:, hs], in_=p_t[:], func=AF.Silu), R=[d_p], W=[d_o])
                    elif kind == "copy":
                        kb.op("act", lambda e: e.copy(out=o_t[:, hs], in_=p_t[:]), R=[d_p], W=[d_o])
                    else:
                        z_t, d_z = zf_r[half]
                        kb.op("dve", lambda e: e.tensor_tensor(out=z_t[:], in0=p_t[:], in1=hbf_t[:, hs], op=ALU.add),
                              R=[d_p, d_hbf], W=[d_z])
                        kb.op("act", lambda e: e.activation(out=z_t[:], in_=z_t[:], func=AF.Sigmoid), R=[d_z], W=[d_z])
                        kb.op("dve", lambda e: e.tensor_tensor(out=z_t[:], in0=z_t[:], in1=oml_t[:, hs], op=ALU.mult),
                              R=[d_z, d_oml], W=[d_z])
                        kb.op("dve", lambda e: e.tensor_tensor(out=z_t[:], in0=z_t[:], in1=lb_t[:, hs], op=ALU.add),
                              R=[d_z, d_lb], W=[d_z])
                        kb.op("act", lambda e: e.activation(out=o_t[:, hs], in_=z_t[:], func=AF.Ln), R=[d_z], W=[d_o])
                kb.dma("sp", dram_o[r0:r1, :], o_t[:], R=[d_o])
    kb.finish()
    return nc


def build_D(nb, nbh):
    L = nb * 128
    nc = bass.Bass("TRN2", target_bir_lowering=False)
    qT = nc.dram_tensor("qT", [nbh, 128, L], BF16, kind="ExternalInput").ap()
    lfT = nc.dram_tensor("lfT", [nbh, 128, L], F32, kind="ExternalInput").ap()
    vv = nc.dram_tensor("v", [nbh, L, 128], BF16, kind="ExternalInput").ap()
    go = nc.dram_tensor("go", [1, 128], F32, kind="ExternalInput").ap()
    on = nc.dram_tensor("on", [nbh, L, 128], F32, kind="ExternalOutput").ap()
    kb = KB(nc)
    identf, d_idf, ident, d_id = make_ident(kb)
    go_t, d_go = load_bc(kb, "go", go, 128)
    onesf = kb.sb("onesf", [128, 128], F32); d_onesf = Dep("onesf")
    mf = kb.sb("mf", [128, 128], F32); d_mf = Dep("mf")
    mku = kb.sb("mku", [128, 128], U32); d_mk = Dep("mku")
    kb.op("pool", lambda e: e.memset(onesf[:], 1.0), W=[d_onesf])
    kb.op("pool", lambda e: e.affine_select(out=mf[:], in_=onesf[:], pattern=[[1, 128]], compare_op=ALU.is_ge,
                                            fill=0.0, base=0, channel_multiplier=-1), R=[d_onesf], W=[d_mf])
    kb.op("dve", lambda e: e.tensor_copy(out=mku[:], in_=mf[:]), R=[d_mf], W=[d_mk])
    d_mk.ro = True
    d_onesf.ro = True
    q_t = kb.sb("q", [128, L], BF16); d_q = Dep("q")
    lf_t = kb.sb("lf", [128, L], F32); d_lf = Dep("lf")
    v_t = kb.sb("v", [128, nb, 128], BF16); d_v = Dep("v")
    S_t = kb.sb("S", [128, 128], F32); d_S = Dep("S")
    Sb_r = kb.ring("Sb", [128, 128], BF16, 2)
    b_r = kb.ring("b", [128, 128], F32, 2)
    col_r = kb.ring("col", [128, 8], F32, 2)
    e1_r = kb.ring("e1", [128, 128], F32, 2)
    e2_r = kb.ring("e2", [128, 128], F32, 2)
    kk_r = kb.ring("kk", [128, 128], F32, 2)
    qd_r = kb.ring("qd", [128, 128], BF16, 2)
    kd_r = kb.ring("kd", [128, 128], BF16, 2)
    qb_r = kb.ring("qb", [128, 128], BF16, 2)
    ke_r = kb.ring("ke", [128, 128], BF16, 2)
    keT_r = kb.ring("keT", [128, 128], BF16, 2)
    scm_r = kb.ring("scm", [128, 128], BF16, 2)
    osq_r = kb.ring("osq", [128, 128], F32, 2)
    oss_r = kb.ring("oss", [128, 2], F32, 2)
    on_r = kb.ring("on", [128, 128], F32, 2)
    psc_r = kb.ring("psc", [128, 512], F32, 2, psum=True)
    po_r = kb.ring("po", [128, 512], F32, 2, psum=True)
    pt_r = kb.ring("pt", [128, 8, 128], BF16, 2, psum=True)
    pu_r = kb.ring("pu", [128, 512], F32, 2, psum=True)
    for (t_, d_) in scm_r:
        kb.op("pool", lambda e: e.memset(t_[:], 0.0), W=[d_])
    for bh in range(nbh):
        kb.dma("sp", q_t[:], qT[bh], W=[d_q])
        for hlf in range(4):
            c0 = (L // 4) * hlf
            kb.dma("sp", lf_t[:, c0:c0 + L // 4], lfT[bh][:, c0:c0 + L // 4], W=[d_lf])
        kb.dma("sp", v_t[:], vv[bh].rearrange("(j p) d -> p j d", p=128), W=[d_v])
        kb.op("pool", lambda e: e.memset(S_t[:], 0.0), W=[d_S])
        kb.op("pool", lambda e: e.memset(Sb_r[0][0][:], 0.0), W=[Sb_r[0][1]])
        for c in range(nb):
            r = c % 2
            cs = slice(c * 128, (c + 1) * 128)
            b_t, d_b = b_r[r]
            col, d_col = col_r[r]
            e1, d_e1 = e1_r[r]
            e2, d_e2 = e2_r[r]
            kk, d_kk = kk_r[r]
            qd, d_qd = qd_r[r]
            kd, d_kd = kd_r[r]
            qb, d_qb = qb_r[r]
            ke, d_ke = ke_r[r]
            keT, d_keT = keT_r[r]
            scm, d_scm = scm_r[r]
            kb.op("dve", lambda e: e.tensor_tensor_scan(out=b_t[:], data0=onesf[:], data1=lf_t[:, cs], initial=0.0,
                                                         op0=ALU.mult, op1=ALU.add), R=[d_lf, d_onesf], W=[d_b])
            kb.op("dve", lambda e: e.tensor_copy(out=col[:, 0:1], in_=b_t[:, 63:64]), R=[d_b], W=[d_col])
            kb.op("dve", lambda e: e.tensor_tensor(out=col[:, 1:2], in0=b_t[:, 127:128], in1=b_t[:, 63:64],
                                                    op=ALU.subtract), R=[d_b], W=[d_col])
            kb.op("dve", lambda e: e.tensor_copy(out=col[:, 2:3], in_=b_t[:, 127:128]), R=[d_b], W=[d_col])
            kb.op("dve", lambda e: e.tensor_scalar(out=col[:, 3:4], in0=b_t[:, 63:64], scalar1=-1.0, scalar2=None,
                                                    op0=ALU.mult), R=[d_b], W=[d_col])
            kb.op("act", lambda e: e.activation(out=col[:, 4:7], in_=col[:, 0:3], func=AF.Exp), R=[d_col], W=[d_col])
            kb.op("act", lambda e: e.activation(out=e1[:], in_=b_t[:], func=AF.Exp, bias=col[:, 3:4], scale=1.0),
                  R=[d_b, d_col], W=[d_e1])
            kb.op("act", lambda e: e.activation(out=e2[:], in_=b_t[:], func=AF.Exp, bias=col[:, 0:1], scale=-1.0),
                  R=[d_b, d_col], W=[d_e2])
            kb.op("act", lambda e: e.activation(out=kk[:], in_=lf_t[:, cs], func=AF.Exp), R=[d_lf], W=[d_kk])
            kb.op("pool", lambda e: e.tensor_scalar(out=kk[:], in0=kk[:], scalar1=-1.0, scalar2=1.0, op0=ALU.mult,
                                                     op1=ALU.add), R=[d_kk], W=[d_kk])
            kb.op("dve", lambda e: e.tensor_tensor(out=qd[:], in0=q_t[:, cs], in1=e1[:], op=ALU.mult),
                  R=[d_q, d_e1], W=[d_qd])
            kb.op("dve", lambda e: e.tensor_tensor(out=kd[:], in0=kk[:], in1=e2[:], op=ALU.mult),
                  R=[d_kk, d_e2], W=[d_kd])
            kb.op("dve", lambda e: e.scalar_tensor_tensor(out=qb[:], in0=q_t[:, cs], scalar=col[:, 4:5], in1=e1[:],
                                                           op0=ALU.mult, op1=ALU.mult), R=[d_q, d_col, d_e1], W=[d_qb])
            kb.op("dve", lambda e: e.scalar_tensor_tensor(out=ke[:], in0=kk[:], scalar=col[:, 5:6], in1=e2[:],
                                                           op0=ALU.mult, op1=ALU.mult), R=[d_kk, d_col, d_e2], W=[d_ke])
            psc, d_psc = psc_r[r]
            kb.op("pe", lambda e: e.matmul(psc[:, 0:128], lhsT=kd[:], rhs=qd[:], start=True, stop=True),
                  R=[d_kd, d_qd], W=[d_psc])
            kb.op("dve", lambda e: e.copy_predicated(out=scm[:], mask=mku[:], data=psc[:, 0:128]),
                  R=[d_psc, d_mk], W=[d_scm])
            po, d_po = po_r[r]
            Sb, d_Sb = Sb_r[c % 2]
            kb.op("pe", lambda e: e.matmul(po[:, 0:128], lhsT=scm[:], rhs=v_t[:, c, :], start=True, stop=False),
                  R=[d_scm, d_v], W=[d_po])
            kb.op("pe", lambda e: e.matmul(po[:, 0:128], lhsT=qb[:], rhs=Sb[:], start=False, stop=True),
                  R=[d_qb, d_Sb], W=[d_po])
            pt, d_pt = pt_r[r]
            kb.op("pe", lambda e: e.transpose(out=pt[:, 0, :], in_=ke[:], identity=ident[:]), R=[d_ke, d_id], W=[d_pt])
            kb.op("act", lambda e: e.copy(out=keT[:], in_=pt[:, 0, :]), R=[d_pt], W=[d_keT])
            pu, d_pu = pu_r[r]
            kb.op("pe", lambda e: e.matmul(pu[:, 0:128], lhsT=keT[:], rhs=v_t[:, c, :], start=True, stop=True),
                  R=[d_keT, d_v], W=[d_pu])
            kb.op("dve", lambda e: e.scalar_tensor_tensor(out=S_t[:], in0=S_t[:], scalar=col[:, 6:7], in1=pu[:, 0:128],
                                                           op0=ALU.mult, op1=ALU.add), R=[d_S, d_col, d_pu], W=[d_S])
            Sb2, d_Sb2 = Sb_r[(c + 1) % 2]
            kb.op("pool", lambda e: e.tensor_copy(out=Sb2[:], in_=S_t[:]), R=[d_S], W=[d_Sb2])
            osq, d_osq = osq_r[r]
            oss, d_oss = oss_r[r]
            on_t, d_on = on_r[r]
            kb.op("act", lambda e: e.activation(out=osq[:], in_=po[:, 0:128], func=AF.Square, accum_out=oss[:, 0:1]),
                  R=[d_po], W=[d_osq, d_oss])
            kb.op("act", lambda e: e.activation(out=oss[:, 1:2], in_=oss[:, 0:1], func=AF.Ln, scale=1.0 / 128, bias=EPS),
                  R=[d_oss], W=[d_oss])
            kb.op("act", lambda e: e.activation(out=oss[:, 1:2], in_=oss[:, 1:2], func=AF.Exp, scale=-0.5),
                  R=[d_oss], W=[d_oss])
            kb.op("dve", lambda e: e.scalar_tensor_tensor(out=on_t[:], in0=po[:, 0:128], scalar=oss[:, 1:2], in1=go_t[:],
                                                           op0=ALU.mult, op1=ALU.mult), R=[d_po, d_oss, d_go], W=[d_on])
            kb.dma("sp", on[bh, c * 128:(c + 1) * 128, :], on_t[:], R=[d_on])
    kb.finish()
    return nc


_CACHE = {}


def _prog(key, fn):
    if key not in _CACHE:
        _CACHE[key] = fn()
    return _CACHE[key]


def _run(nc, in_maps):
    res = run_bass_kernel_spmd(nc, in_maps, core_ids=list(range(NCORES)))
    return res.results


def kernel_unfused(**inp):
    inp = {k: np.asarray(v) for k, v in inp.items()}
    x = inp["x"]
    meta = inp["meta_tokens"]
    f32 = np.float32
    metatile = np.zeros((128, D), f32)
    metatile[128 - NMETA:] = meta
    seg = SEQ // 4

    def core_rows(full_b, c, with_meta=True):
        s = 128 + (c % 4) * seg
        body = full_b[s:s + seg]
        return np.concatenate([full_b[0:128], body], 0) if with_meta else body

    def assemble(per_core):
        out = []
        for b in range(2):
            parts = [per_core[4 * b][0:128]] + [per_core[4 * b + j][128:] for j in range(4)]
            out.append(np.concatenate(parts, 0))
        return out

    hA = [np.concatenate([metatile, x[c // 4, (c % 4) * seg:(c % 4 + 1) * seg]], 0) for c in range(NCORES)]
    valid = np.ones((NT * 128, 1), f32)
    valid[0:128 - NMETA] = 0.0

    ncA = _prog("A", lambda: build_A(NT))
    cA = {"gain": inp["fox_norm"][0][None], "w_in": inp["fox_w_in"][0],
          "gq": np.tile(inp["fox_q_norm"][0], FH)[None], "gk": np.tile(inp["fox_k_norm"][0], FH)[None],
          "bf": inp["fox_b_f"][0][None]}
    rA = _run(ncA, [dict(cA, h=hA[c]) for c in range(NCORES)])
    qf = assemble([np.asarray(r["qo"]) for r in rA])
    kf = assemble([np.asarray(r["ko"]) for r in rA])
    vf = assemble([np.asarray(r["vo"]) for r in rA])
    lff = assemble([np.asarray(r["lfo"]) for r in rA])

    ncB = _prog("B", lambda: build_B(NB, 4))
    imB = []
    for c in range(NCORES):
        b = c // 4
        hs = [(4 * c + i) % FH for i in range(4)]
        imB.append({
            "qT": np.ascontiguousarray(np.stack([qf[b][:, h * FD:(h + 1) * FD].T for h in hs])),
            "kT": np.ascontiguousarray(np.stack([kf[b][:, h * FD:(h + 1) * FD].T for h in hs])),
            "v": np.ascontiguousarray(np.stack([vf[b][:, h * FD:(h + 1) * FD] for h in hs])),
            "lfr": np.ascontiguousarray(np.stack([lff[b][:, h] for h in hs])),
            "lfT": np.ascontiguousarray(np.stack([lff[b][:, h].reshape(NB, 128).T for h in hs])),
        })
    rB = _run(ncB, imB)
    oun = [np.zeros((LP, D), f32) for _ in range(2)]
    den = [np.zeros((LP, FH), f32) for _ in range(2)]
    for c in range(NCORES):
        b = c // 4
        o = np.asarray(rB[c]["oT"])
        for i in range(4):
            h = (4 * c + i) % FH
            oun[b][:, h * FD:(h + 1) * FD] = o[i, 0:FD].T
            den[b][:, h] = o[i, FD]

    ncC = _prog("C", lambda: build_CE("C", NT, True))
    cC = {"valid_in": valid, "w_out": inp["fox_w_out"][0], "mnorm": inp["moe_norm"][0][None],
          "w_r": np.ascontiguousarray(np.concatenate([inp["moe_w_grp"][0], inp["moe_w_rt"][0]], 1)),
          "b_r": np.concatenate([inp["moe_b_grp"][0], inp["moe_b_rt"][0]])[None],
          "w_up": inp["moe_w_up"][0], "w_dn": inp["moe_w_down"][0],
          "hnorm": inp["hg_norm"][0][None], "hw_in": inp["hg_w_in"][0], "hbf": inp["hg_b_f"][0][None],
          "lbl": inp["hg_lb_logits"]}
    rC = _run(ncC, [dict(cC, a_in=core_rows(oun[c // 4], c), den_in=core_rows(den[c // 4], c), hp_in=hA[c])
                    for c in range(NCORES)])
    h2 = [np.asarray(r["h2_out"]) for r in rC]
    q1 = assemble([np.asarray(r["q1_out"]) for r in rC])
    lf1 = assemble([np.asarray(r["lf1_out"]) for r in rC])
    v1 = assemble([np.asarray(r["v1_out"]) for r in rC])
    gs = assemble([np.asarray(r["gs_out"]) for r in rC])
    npad = 128 - NMETA
    for b in range(2):
        q1[b][0:npad] = 0
        lf1[b][0:npad] = 0
        v1[b][0:npad] = 0

    ncD = _prog("D", lambda: build_D(NB, 2))
    imD = []
    for c in range(NCORES):
        prs = [2 * c, 2 * c + 1]
        imD.append({
            "qT": np.ascontiguousarray(np.stack([q1[p // HH][:, (p % HH) * 128:(p % HH + 1) * 128].T for p in prs])),
            "lfT": np.ascontiguousarray(np.stack([lf1[p // HH][:, (p % HH) * 128:(p % HH + 1) * 128].T for p in prs])),
            "v": np.ascontiguousarray(np.stack([v1[p // HH][:, (p % HH) * 128:(p % HH + 1) * 128] for p in prs])),
            "go": inp["hg_o_norm"][0][None],
        })
    rD = _run(ncD, imD)
    onf = [np.zeros((LP, D), f32) for _ in range(2)]
    for c in range(NCORES):
        o = np.asarray(rD[c]["on"])
        for i in range(2):
            p = 2 * c + i
            onf[p // HH][:, (p % HH) * 128:(p % HH + 1) * 128] = o[i]

    ncE = _prog("E", lambda: build_CE("E", NTX, False))
    cE = {"valid_in": np.ones((NTX * 128, 1), f32), "w_out": inp["hg_w_out"][0], "mnorm": inp["moe_norm"][1][None],
          "w_r": np.ascontiguousarray(np.concatenate([inp["moe_w_grp"][1], inp["moe_w_rt"][1]], 1)),
          "b_r": np.concatenate([inp["moe_b_grp"][1], inp["moe_b_rt"][1]])[None],
          "w_up": inp["moe_w_up"][1], "w_dn": inp["moe_w_down"][1]}
    rE = _run(ncE, [dict(cE, a_in=core_rows(onf[c // 4], c, False), gs_in=core_rows(gs[c // 4], c, False),
                         hp_in=h2[c][128:]) for c in range(NCORES)])
    out = np.zeros((2, SEQ, D), f32)
    for c in range(NCORES):
        out[c // 4, (c % 4) * seg:(c % 4 + 1) * seg] = np.asarray(rE[c]["h2_out"])
    return out


GROUPS = [[0, 1, 2, 3], [4, 5, 6, 7]]


def build_fused(ntx=NTX, cap=CAP, stop=None):
    assert ntx % 4 == 0
    nt = ntx + 1
    nb = 4 * ntx + 1
    L = nb * 128
    nch = ntx // 4
    TX = ntx * 128
    nI = ntx + 1
    nslot = NE * cap
    nst = cap // 128
    nc = bass.Bass("TRN2", target_bir_lowering=False)

    def din(name, shape, dt=F32):
        return nc.dram_tensor(name, list(shape), dt, kind="ExternalInput").ap()

    def dint(name, shape, dt=BF16):
        return nc.dram_tensor(name, list(shape), dt).ap()

    x_in = din("x_in", [nt * 128, D])
    valid_in = din("valid_in", [nt * 128, 1])
    fnorm = din("fnorm", [1, D])
    wq_d, wk_d, wv_d = din("wq", [D, 256]), din("wk", [D, 256]), din("wv", [D, 256])
    wf_d = din("wf", [D, 4])
    gqc_d, gkc_d = din("gqc", [128, 1]), din("gkc", [128, 1])
    bf4r_d, bf4c_d = din("bf4r", [1, 4]), din("bf4c", [4, 1])
    wo1_d = din("wo1", [D, D])
    idx2_d = din("idx2", [128, 8], I32)
    idx3_d = din("idx3", [128, 8], I32)
    moe_d = []
    for l in range(2):
        if stop in ("T1", "H1p", "H1a") or (stop in ("T2", "H2") and l == 1):
            moe_d.append(None)
            continue
        moe_d.append(dict(mnorm=din("mnorm%d" % l, [1, D]), w_r=din("w_r%d" % l, [D, 36]), b_r=din("b_r%d" % l, [1, 36]),
                          w_up=din("w_up%d" % l, [NE, D, 2 * DE]), w_dn=din("w_dn%d" % l, [NE, DE, D])))
    hnorm = din("hnorm", [1, D])
    hwq_d, hwf_d, hwv_d = din("hwq", [2, D, 128]), din("hwf", [2, D, 128]), din("hwv", [2, D, 128])
    hwg_d = din("hwg", [D, D])
    hbfc_d, l0c_d, l1c_d = din("hbfc", [2, 128, 1]), din("l0c", [2, 128, 1]), din("l1c", [2, 128, 1])
    go_d = din("go", [1, 128])
    wo2_d = din("wo2", [D, D])
    out_d = nc.dram_tensor("out", [TX, D], F32, kind="ExternalOutput").ap()

    MA, MC = dint("MA", [128, 8 * 128]), dint("MC", [128, 8 * 128])
    SA, RA = dint("SA", [nch * 128, 8 * 512]), dint("RA", [nch * 512, 8 * 512])
    SC, RC = dint("SC", [nch * 128, 8 * 512]), dint("RC", [nch * 512, 8 * 512])
    qTs, kTs, vs = dint("qTs", [4, FD + 1, L]), dint("kTs", [4, FD, L]), dint("vs", [L, 256])
    SBb, RBb = dint("SBb", [8 * 128, TX]), dint("RBb", [8 * 512, TX])
    SBm, RBm = dint("SBm", [256, 128]), dint("RBm", [1024, 128])
    h1s, h2s = dint("h1s", [nt * 128, D], F32), dint("h2s", [nt * 128, D], F32)
    xpad, ypad = dint("xpad", [nslot, D]), dint("ypad", [nslot, D])
    GS = dint("GS", [nch * 128, 8 * 512])
    SD, RD = dint("SD", [8 * 128, TX]), dint("RD", [8 * 512, TX])
    d_MA, d_MC = (Dep(n) for n in ("MA", "MC"))
    d_SA = (Dep("SAw"), [Dep("SA%d" % j) for j in range(nch)])
    d_SC = (Dep("SCw"), [Dep("SC%d" % j) for j in range(nch)])
    d_SBc = [Dep("SBb%d" % j) for j in range(8)]
    d_SDc = [Dep("SD%d" % j) for j in range(8)]
    d_RA = [Dep("RA%d" % j) for j in range(nch)]
    d_RC = [Dep("RC%d" % j) for j in range(nch)]
    d_qTs, d_kTs, d_vs, d_SBb, d_RBb, d_SBm, d_RBm = (Dep(n) for n in ("qTs", "kTs", "vs", "SBb", "RBb", "SBm", "RBm"))
    d_h1s, d_h2s, d_xpad, d_ypad, d_GS, d_SD, d_RD = (Dep(n) for n in ("h1s", "h2s", "xpad", "ypad", "GS", "SD", "RD"))

    kb = KB(nc)
    identf, d_idf, ident, d_id = make_ident(kb)
    breg = nc.gpsimd.to_reg(nslot - 1)
    breg2 = nc.gpsimd.to_reg(8 * 512 - 1)

    def dump_and_finish(items):
        for name, ap, dep in items:
            o = nc.dram_tensor("dbg_" + name, list(ap.shape), ap.dtype, kind="ExternalOutput").ap()
            kb.dma("sp", o, ap, R=[dep])
        kb.finish()
        return nc

    def emit_hnT(src_t, d_src, gain_t, d_gain, scr, ssr, xnr, pstr, hcr, t, Mloc, d_Mloc, Ssend, d_S, Rrecv, d_R, hc_hook=None):
        b = t % 2
        sc_t, d_sc = scr[b]
        ss_t, d_ss = ssr[b]
        xn_t, d_xn = xnr[b]
        ps_t, d_pst = pstr[b]
        rms_rstd(kb, src_t[:], d_src, D, sc_t[:], d_sc, ss_t[:], d_ss)
        kb.op("dve", lambda e: e.scalar_tensor_tensor(out=xn_t[:], in0=src_t[:], scalar=ss_t[:, 0:1], in1=gain_t[:],
                                                       op0=ALU.mult, op1=ALU.mult), R=[d_src, d_ss, d_gain], W=[d_xn])
        for c in range(8):
            kb.op("pe", lambda e: e.transpose(out=ps_t[:, c, :], in_=xn_t[:, c * 128:(c + 1) * 128], identity=ident[:]),
                  R=[d_xn, d_id], W=[d_pst])
        if t == 0:
            hc_t, d_hc = hcr[0]
            kb.op("dve", lambda e: e.tensor_copy(out=hc_t[:, :, 0:128], in_=ps_t[:]), R=[d_pst], W=[d_hc])
            kb.dma("sp", Mloc.rearrange("p (c t) -> p c t", c=8), hc_t[:, :, 0:128], R=[d_hc], W=[d_Mloc])
            return
        j, s = (t - 1) // 4, (t - 1) % 4
        hc_t, d_hc = hcr[(j + 1) % 2]
        kb.op("dve", lambda e: e.tensor_copy(out=hc_t[:, :, s * 128:(s + 1) * 128], in_=ps_t[:]), R=[d_pst], W=[d_hc])
        if s == 3:
            kb.dma("sp", Ssend[j * 128:(j + 1) * 128, :], hc_t[:].rearrange("p c t -> p (c t)"), R=[d_hc],
                   W=[d_S[0], d_S[1][j]])
            kb.cc(Ssend[j * 128:(j + 1) * 128, :], Rrecv[j * 512:(j + 1) * 512, :], GROUPS, R=[d_S[1][j]], W=[d_R[j]])
            if hc_hook is not None:
                hc_hook(j, hc_t, d_hc)

    lfall = kb.sb("lfall", [128, nb, 4], F32)
    d_lfall = Dep("lfall")
    with kb.scope():
        g_t, d_g = load_bc(kb, "fnorm", fnorm, D)
        wq_t, d_wq = load_w(kb, "wq", wq_d, D, 256)
        wk_t, d_wk = load_w(kb, "wk", wk_d, D, 256)
        wv_t, d_wv = load_w(kb, "wv", wv_d, D, 256)
        wf_t, d_wf = load_w(kb, "wf", wf_d, D, 4)
        for d in (d_wq, d_wk, d_wv, d_wf):
            d.ro = True
        gqc = kb.sb("gqc", [128, 1], F32); d_gqc = Dep("gqc")
        gkc = kb.sb("gkc", [128, 1], F32); d_gkc = Dep("gkc")
        bf4c = kb.sb("bf4c", [4, 1], F32); d_bf4c = Dep("bf4c")
        kb.dma("sp", gqc[:], gqc_d, W=[d_gqc])
        kb.dma("sp", gkc[:], gkc_d, W=[d_gkc])
        kb.dma("sp", bf4c[:], bf4c_d, W=[d_bf4c])
        kb.op("dve", lambda e: e.tensor_scalar(out=bf4c[:], in0=bf4c[:], scalar1=-1.0, scalar2=None, op0=ALU.mult),
              R=[d_bf4c], W=[d_bf4c])
        kb.op("dve", lambda e: e.tensor_scalar(out=gqc[:], in0=gqc[:], scalar1=float(FD ** -0.5), scalar2=None,
                                                op0=ALU.mult), R=[d_gqc], W=[d_gqc])
        bf4r, d_bf4r = load_bc(kb, "bf4r", bf4r_d, 4)
        blkf = kb.sb("blkf", [128, 128], F32); d_blkf = Dep("blkf")
        blk = kb.sb("blk", [128, 128], BF16); d_blk = Dep("blk")
        ones1 = kb.sb("ones1", [128, 512], F32); d_ones1 = Dep("ones1")
        kb.op("pool", lambda e: e.memset(blkf[:], 0.0), W=[d_blkf])
        kb.op("pool", lambda e: e.memset(blkf[0:64, 0:64], 1.0), W=[d_blkf])
        kb.op("pool", lambda e: e.memset(blkf[64:128, 64:128], 1.0), W=[d_blkf])
        kb.op("dve", lambda e: e.tensor_copy(out=blk[:], in_=blkf[:]), R=[d_blkf], W=[d_blk])
        kb.op("pool", lambda e: e.memset(ones1[:], 1.0), W=[d_ones1])
        d_blk.ro = True
        d_ones1.ro = True
        xin = kb.ring("xin", [128, D], F32, 4)
        scr = kb.ring("scr", [128, D], F32, 2)
        ssr = kb.ring("ss", [128, 1], F32, 2)
        xnr = kb.ring("xn", [128, D], BF16, 2)
        hcr = kb.ring("hc", [128, 8, 512], BF16, 2)
        pstr = kb.ring("pst", [128, 8, 128], BF16, 2, psum=True)
        def t1_load(t):
            x_t, d_x = xin[t % 4]
            kb.dma("sp", x_t[:], x_in[t * 128:(t + 1) * 128, :], W=[d_x])
        for t in range(min(3, nt)):
            t1_load(t)
        for t in range(nt):
            if t + 3 < nt:
                t1_load(t + 3)
            x_t, d_x = xin[t % 4]
            emit_hnT(x_t, d_x, g_t, d_g, scr, ssr, xnr, pstr, hcr, t, MA, d_MA, SA, d_SA, RA, d_RA)

        if stop == "T1":
            kb.barrier()
            return dump_and_finish([("RA", RA, d_RA[nch - 1]), ("MA", MA, d_MA)])
        hgr = kb.ring("hg", [128, 8, 512], BF16, 2)
        sqr = kb.ring("sq", [128, 512], BF16, 2)
        rsr = kb.ring("rs", [128, 512], F32, 2)
        qnr = kb.ring("qn", [128, 512], BF16, 2)
        vsr = kb.ring("vsb", [128, 256], BF16, 2)
        lzr = kb.ring("lz", [128, 4], F32, 2)
        lrr = kb.ring("lr", [4, 512], F32, 2)
        drr = kb.ring("dr", [4, 512], BF16, 2)
        pqr = kb.ring("pq", [128, 512], F32, 2, psum=True)
        pssr = kb.ring("pss", [128, 512], F32, 1, psum=True)
        pvr = kb.ring("pv", [128, 512], F32, 2, psum=True)
        pfr = kb.ring("pf", [128, 512], F32, 1, psum=True)
        qi = 0
        vi = 0
        def h1_load(G):
            hg, d_hg = hgr[G % 2]
            if G == 0:
                kb.dma("sp", hg[:, :, 0:128], MA.rearrange("p (c t) -> p c t", c=8), R=[d_MA], W=[d_hg])
            else:
                j, rank = (G - 1) // 4, (G - 1) % 4
                r0 = j * 512 + rank * 128
                kb.dma("sp", hg[:].rearrange("p c t -> p (c t)"), RA[r0:r0 + 128, :], R=[d_RA[j]], W=[d_hg])
        h1_load(0)
        for G in range(1 + 4 * nch):
            hg, d_hg = hgr[G % 2]
            if G + 1 < 1 + 4 * nch:
                h1_load(G + 1)
            if G == 0:
                n, tok0 = 128, 0
            else:
                n, tok0 = 512, 128 + ((G - 1) % 4) * TX + ((G - 1) // 4) * 512
            for (w_t, d_w, gc, d_gc, dst, d_dst) in ((wq_t, d_wq, gqc, d_gqc, qTs, d_qTs), (wk_t, d_wk, gkc, d_gkc, kTs, d_kTs)):
                for pr in range(2):
                    pq, d_pq = pqr[qi % 2]
                    pss, d_pss = pssr[0]
                    sq, d_sq = sqr[qi % 2]
                    rs, d_rs = rsr[qi % 2]
                    qn, d_qn = qnr[qi % 2]
                    qi += 1
                    for c in range(8):
                        kb.op("pe", lambda e: e.matmul(pq[:, 0:n], lhsT=w_t[:, c, pr * 128:(pr + 1) * 128], rhs=hg[:, c, 0:n],
                                                       start=(c == 0), stop=(c == 7)), R=[d_w, d_hg], W=[d_pq])
                    kb.op("act", lambda e: e.activation(out=sq[:, 0:n], in_=pq[:, 0:n], func=AF.Square), R=[d_pq], W=[d_sq])
                    kb.op("pe", lambda e: e.matmul(pss[:, 0:n], lhsT=blk[:], rhs=sq[:, 0:n], start=True, stop=True),
                          R=[d_blk, d_sq], W=[d_pss])
                    kb.op("act", lambda e: e.activation(out=rs[:, 0:n], in_=pss[:, 0:n], func=AF.Ln, scale=1.0 / FD, bias=EPS),
                          R=[d_pss], W=[d_rs])
                    kb.op("act", lambda e: e.activation(out=rs[:, 0:n], in_=rs[:, 0:n], func=AF.Exp, scale=-0.5), R=[d_rs], W=[d_rs])
                    kb.op("dve", lambda e: e.scalar_tensor_tensor(out=qn[:, 0:n], in0=pq[:, 0:n], scalar=gc[:, 0:1], in1=rs[:, 0:n],
                                                                   op0=ALU.mult, op1=ALU.mult), R=[d_pq, d_gc, d_rs], W=[d_qn])
                    for hh in range(2):
                        kb.dma("sp", dst[2 * pr + hh, 0:FD, tok0:tok0 + n], qn[hh * 64:(hh + 1) * 64, 0:n], R=[d_qn], W=[d_dst])
            for s in range(n // 128):
                pv, d_pv = pvr[vi % 2]
                vsb, d_vsb = vsr[vi % 2]
                lz, d_lz = lzr[vi % 2]
                vi += 1
                for c in range(8):
                    kb.op("pe", lambda e: e.matmul(pv[:, 0:256], lhsT=hg[:, c, s * 128:(s + 1) * 128], rhs=wv_t[:, c, :],
                                                   start=(c == 0), stop=(c == 7)), R=[d_wv, d_hg], W=[d_pv])
                for c in range(8):
                    kb.op("pe", lambda e: e.matmul(pv[:, 256:260], lhsT=hg[:, c, s * 128:(s + 1) * 128], rhs=wf_t[:, c, :],
                                                   start=(c == 0), stop=(c == 7)), R=[d_wf, d_hg], W=[d_pv])
                kb.op("act", lambda e: e.copy(out=vsb[:], in_=pv[:, 0:256]), R=[d_pv], W=[d_vsb])
                kb.dma("sp", vs[tok0 + s * 128:tok0 + (s + 1) * 128, :], vsb[:], R=[d_vsb], W=[d_vs])
                blk_i = tok0 // 128 + s
                kb.op("dve", lambda e: e.tensor_tensor(out=lz[:], in0=pv[:, 256:260], in1=bf4r[:], op=ALU.add),
                      R=[d_pv, d_bf4r], W=[d_lz])
                kb.op("act", lambda e: e.activation(out=lz[:], in_=lz[:], func=AF.Exp, scale=-1.0), R=[d_lz], W=[d_lz])
                kb.op("act", lambda e: e.activation(out=lz[:], in_=lz[:], func=AF.Ln, bias=1.0), R=[d_lz], W=[d_lz])
                kb.op("dve", lambda e: e.tensor_scalar(out=lfall[:, blk_i, :], in0=lz[:], scalar1=-1.0, scalar2=None,
                                                        op0=ALU.mult), R=[d_lz], W=[d_lfall])
            pf, d_pf = pfr[0]
            lr, d_lr = lrr[G % 2]
            dr, d_dr = drr[G % 2]
            for c in range(8):
                kb.op("pe", lambda e: e.matmul(pf[0:4, 0:n], lhsT=wf_t[:, c, :], rhs=hg[:, c, 0:n], start=(c == 0), stop=(c == 7)),
                      R=[d_wf, d_hg], W=[d_pf])
            kb.op("act", lambda e: e.activation(out=lr[:, 0:n], in_=pf[0:4, 0:n], func=AF.Exp, scale=-1.0, bias=bf4c[:, 0:1]),
                  R=[d_pf, d_bf4c], W=[d_lr])
            kb.op("act", lambda e: e.activation(out=lr[:, 0:n], in_=lr[:, 0:n], func=AF.Ln, bias=1.0), R=[d_lr], W=[d_lr])
            kb.op("dve", lambda e: e.tensor_tensor_scan(out=lr[:, 0:n], data0=ones1[0:4, 0:n], data1=lr[:, 0:n], initial=0.0,
                                                         op0=ALU.mult, op1=ALU.subtract), R=[d_lr, d_ones1], W=[d_lr])
            kb.op("dve", lambda e: e.tensor_copy(out=dr[:, 0:n], in_=lr[:, 0:n]), R=[d_lr], W=[d_dr])
            kb.dma("sp", qTs[:, FD, tok0:tok0 + n], dr[:, 0:n], R=[d_dr], W=[d_qTs])

    if stop == "H1p":
        return dump_and_finish([("qTs", qTs, d_qTs), ("kTs", kTs, d_kTs), ("vs", vs, d_vs)])
    with kb.scope():
        trif = kb.sb("trif", [128, 128], F32); d_tri = Dep("trif")
        onesf = kb.sb("onesf", [128, 128], F32); d_ones = Dep("onesf")
        sel0 = kb.sb("sel0", [128, 128], F32); d_sel = Dep("sel0")
        maskb = kb.sb("maskb", [128, 128], BF16); d_mask = Dep("maskb")
        onerow = kb.sb("onerow", [128, nb], F32); d_or = Dep("onerow")
        kb.op("pool", lambda e: e.memset(onesf[:], 1.0), W=[d_ones])
        kb.op("pool", lambda e: e.memset(onerow[:], 1.0), W=[d_or])
        kb.op("pool", lambda e: e.affine_select(out=trif[:], in_=onesf[:], pattern=[[1, 128]], compare_op=ALU.is_ge,
                                                fill=0.0, base=0, channel_multiplier=-1), R=[d_ones], W=[d_tri])
        kb.op("pool", lambda e: e.affine_select(out=sel0[:], in_=onesf[:], pattern=[[0, 128]], compare_op=ALU.is_ge,
                                                fill=0.0, base=0, channel_multiplier=-1), R=[d_ones], W=[d_sel])
        kb.op("dve", lambda e: e.tensor_copy(out=maskb[:], in_=trif[:]), R=[d_tri], W=[d_mask])
        for d in (d_tri, d_ones, d_sel, d_mask, d_or):
            d.ro = True
        QA = kb.sb("QA", [FD + 1, L], BF16)
        tpc = ntx // 4
        qb_ = [0] + [128 + (c + 1) * tpc * 512 for c in range(3)] + [L]
        d_QAc = [Dep("QA%d" % c) for c in range(4)]

        def q_chunk(I):
            return 0 if I == 0 else (I - 1) // tpc

        def q_load(hx, c):
            kb.dma("sp", QA[:, qb_[c]:qb_[c + 1]], qTs[hx][:, qb_[c]:qb_[c + 1]], R=[d_qTs], W=[d_QAc[c]])
        KAr = kb.ring("KA", [FD + 1, L], BF16, 2)
        VAr = kb.ring("VA", [128, nb, 128], BF16, 2)
        lft = kb.sb("lft", [128, nb], F32); d_lft = Dep("lft")
        ct = kb.sb("ct", [128, nb], F32); d_ct = Dep("ct")
        tot = kb.sb("tot", [128, nb], F32); d_tot = Dep("tot")
        rall = kb.sb("rall", [128, nb], F32); d_rall = Dep("rall")
        biasr = kb.ring("bias", [128, nb], F32, 2)
        pr_ = kb.ring("P", [128, 512], BF16, 4)
        denr = kb.ring("den", [128, 512], F32, 2)
        numr = kb.ring("num", [64, 512], F32, 2)
        rdr = kb.ring("rden", [64, 512], F32, 2)
        onr = kb.ring("onb", [64, 512], BF16, 2)
        psS = kb.ring("psS", [128, 512], F32, 4, psum=True)
        psO = kb.ring("psO", [128, 512], F32, 2, psum=True)
        psM = kb.ring("psM", [128, 512], F32, 2, psum=True)
        for (KA_, d_KA_), (VA_, d_VA_) in zip(KAr, VAr):
            kb.op("pool", lambda e: e.memset(KA_[FD:FD + 1, :], 1.0), W=[d_KA_])
            kb.op("pool", lambda e: e.memset(VA_[:, :, FD:128], 1.0), W=[d_VA_])

        def kv_load(hx):
            KA_, d_KA_ = KAr[hx % 2]
            VA_, d_VA_ = VAr[hx % 2]
            kb.dma("sp", KA_[0:FD, :], kTs[hx], R=[d_kTs], W=[d_KA_])
            kb.dma("sp", VA_[:, :, 0:FD], vs[:, hx * FD:(hx + 1) * FD].rearrange("(j p) d -> p j d", p=128), R=[d_vs], W=[d_VA_])
            kb.op("pool", lambda e: e.memset(VA_[0:112, 0, :], 0.0), W=[d_VA_])

        for c in range(4):
            q_load(0, c)
        kv_load(0)
        for h4 in range(4):
            pr4, hh4 = h4 // 2, h4 % 2
            KA, d_KA = KAr[h4 % 2]
            VA, d_VA = VAr[h4 % 2]
            kb.op("dve", lambda e: e.tensor_copy(out=lft[:], in_=lfall[:, :, h4]), R=[d_lfall], W=[d_lft])
            pm0, d_pm0 = psM[0]
            pm1, d_pm1 = psM[1]
            kb.op("pe", lambda e: e.matmul(pm0[:, 0:nb], lhsT=trif[:], rhs=lft[:], start=True, stop=True),
                  R=[d_tri, d_lft], W=[d_pm0])
            kb.op("pe", lambda e: e.matmul(pm1[:, 0:nb], lhsT=onesf[:], rhs=lft[:], start=True, stop=True),
                  R=[d_ones, d_lft], W=[d_pm1])
            kb.op("dve", lambda e: e.tensor_copy(out=tot[:], in_=pm1[:, 0:nb]), R=[d_pm1], W=[d_tot])
            kb.op("dve", lambda e: e.tensor_tensor_scan(out=ct[:], data0=onerow[:], data1=tot[:], initial=0.0,
                                                         op0=ALU.mult, op1=ALU.add), R=[d_tot, d_or], W=[d_ct])
            kb.op("dve", lambda e: e.tensor_tensor(out=ct[:], in0=ct[:], in1=tot[:], op=ALU.subtract),
                  R=[d_ct, d_tot], W=[d_ct])
            kb.op("dve", lambda e: e.tensor_tensor(out=ct[:], in0=ct[:], in1=pm0[:, 0:nb], op=ALU.add),
                  R=[d_ct, d_pm0], W=[d_ct])
            kb.op("pe", lambda e: e.matmul(pm1[:, 0:nb], lhsT=sel0[:], rhs=ct[:], start=True, stop=True),
                  R=[d_sel, d_ct], W=[d_pm1])
            kb.op("dve", lambda e: e.tensor_copy(out=rall[:], in_=pm1[:, 0:nb]), R=[d_pm1], W=[d_rall])
            steps = []
            for I in range(nI):
                j0 = 0 if I == 0 else 4 * I - 3
                nblk = 1 if I == 0 else 4
                nJ = j0 + nblk
                for J in range(nJ):
                    steps.append((I, J, j0, nblk, nJ))
            LA = 3
            for idx in range(len(steps) + LA):
                if idx < len(steps):
                    I, J, j0, nblk, nJ = steps[idx]
                    q0 = j0 * 128
                    bias_t, d_bias = biasr[I % 2]
                    if J == 0:
                        kb.op("dve", lambda e: e.tensor_scalar(out=bias_t[:, 0:nJ], in0=ct[:, 0:nJ], scalar1=-1.0,
                                                                scalar2=rall[:, j0:j0 + 1], op0=ALU.mult, op1=ALU.add),
                              R=[d_ct, d_rall], W=[d_bias])
                    m = max(0, J - j0)
                    c0 = m * 128
                    c1 = nblk * 128
                    ps, d_ps = psS[idx % 4]
                    p_t, d_p = pr_[idx % 4]
                    kb.op("pe", lambda e: e.matmul(ps[:, c0:c1], lhsT=KA[:, J * 128:(J + 1) * 128],
                                                   rhs=QA[:, q0 + c0:q0 + c1], start=True, stop=True),
                          R=[d_KA, d_QAc[q_chunk(I)]], W=[d_ps])
                    kb.op("act", lambda e: e.activation(out=p_t[:, c0:c1], in_=ps[:, c0:c1], func=AF.Exp,
                                                        bias=bias_t[:, J:J + 1], scale=1.0),
                          R=[d_ps, d_bias], W=[d_p])
                    if J >= j0:
                        kb.op("dve", lambda e: e.tensor_tensor(out=p_t[:, c0:c0 + 128], in0=p_t[:, c0:c0 + 128],
                                                                in1=maskb[:], op=ALU.mult), R=[d_p, d_mask], W=[d_p])
                    if J == 0 and I == nI // 2 and h4 + 1 < 4:
                        kv_load(h4 + 1)
                    if J == nJ - 1 and I > 0 and I % tpc == 0 and h4 + 1 < 4:
                        q_load(h4 + 1, I // tpc - 1)
                if idx >= LA:
                    I, J, j0, nblk, nJ = steps[idx - LA]
                    m = max(0, J - j0)
                    c0 = m * 128
                    c1 = nblk * 128
                    p_t, d_p = pr_[(idx - LA) % 4]
                    po, d_po = psO[I % 2]
                    kb.op("pe", lambda e: e.matmul(po[:, c0:c1], lhsT=VA[:, J, :], rhs=p_t[:, c0:c1],
                                                   start=(J == 0), stop=(J == nJ - 1)), R=[d_VA, d_p], W=[d_po])
                    if J == nJ - 1:
                        ncol = nblk * 128
                        den, d_den = denr[I % 2]
                        rd, d_rd = rdr[I % 2]
                        onb, d_onb = onr[I % 2]
                        kb.op("dve", lambda e: e.tensor_scalar(out=den[64:128, 0:ncol], in0=po[64:128, 0:ncol], scalar1=1e-30,
                                                                scalar2=None, op0=ALU.max), R=[d_po], W=[d_den])
                        num, d_num = numr[I % 2]
                        kb.op("dve", lambda e: e.tensor_copy(out=num[:, 0:ncol], in_=po[0:64, 0:ncol]), R=[d_po], W=[d_num])
                        kb.dma("sp", rd[:, 0:ncol], den[64:128, 0:ncol], R=[d_den], W=[d_rd])
                        kb.op("dve", lambda e: e.reciprocal(out=rd[:, 0:ncol], in_=rd[:, 0:ncol]), R=[d_rd], W=[d_rd])
                        kb.op("dve", lambda e: e.tensor_tensor(out=onb[:, 0:ncol], in0=num[:, 0:ncol], in1=rd[:, 0:ncol],
                                                                op=ALU.mult), R=[d_num, d_rd], W=[d_onb])
                        if I == 0:
                            kb.dma("sp", SBm[h4 * 64:(h4 + 1) * 64, :], onb[:, 0:128], R=[d_onb], W=[d_SBm])
                        else:
                            off = (I - 1) * 512
                            qq, col = off // TX, off % TX
                            r0 = (pr4 * 4 + qq) * 128 + hh4 * 64
                            cidx = pr4 * 4 + qq
                            kb.dma("sp", SBb[r0:r0 + 64, col:col + 512], onb[:, 0:512], R=[d_onb], W=[d_SBb, d_SBc[cidx]])
                            if hh4 == 1 and (off + 512) % TX == 0:
                                kb.cc(SBb[cidx * 128:(cidx + 1) * 128, :], RBb[cidx * 512:(cidx + 1) * 512, :], GROUPS,
                                      R=[d_SBc[cidx]], W=[d_RBb])
        kb.cc(SBm[:, :], RBm[:, :], GROUPS, R=[d_SBm], W=[d_RBm])

    if stop == "H1a":
        return dump_and_finish([("RBb", RBb, d_RBb), ("RBm", RBm, d_RBm)])
    usb = kb.sb("usb", [128, 128], BF16); d_us = Dep("usb")
    onesb = kb.sb("onesb", [128, 128], BF16); d_onesb = Dep("onesb")
    onesf2 = kb.sb("onesf2", [128, 128], F32); d_onesf2 = Dep("onesf2")
    tmpf = kb.sb("tmpf", [128, 128], F32); d_tmpf = Dep("tmpf")
    kb.op("pool", lambda e: e.memset(onesf2[:], 1.0), W=[d_onesf2])
    kb.op("pool", lambda e: e.affine_select(out=tmpf[:], in_=onesf2[:], pattern=[[1, 128]], compare_op=ALU.is_gt,
                                            fill=0.0, base=0, channel_multiplier=-1), R=[d_onesf2], W=[d_tmpf])
    kb.op("dve", lambda e: e.tensor_copy(out=usb[:], in_=tmpf[:]), R=[d_tmpf], W=[d_us])
    kb.op("dve", lambda e: e.tensor_copy(out=onesb[:], in_=onesf2[:]), R=[d_onesf2], W=[d_onesb])
    for d in (d_us, d_onesb, d_onesf2):
        d.ro = True
    dest_i = kb.sb("dest_i", [128, nt, 2], I32); d_dest = Dep("dest_i")
    gw = kb.sb("gw", [128, nt, 2], F32); d_gw = Dep("gw")
    cntb = kb.sb("cntb", [128, NE], F32); d_cntb = Dep("cntb")

    def tok_stage(l, tiles, has_meta, setup_lhsT, hp_ap, wo_d, pass3_setup, pass3_tile):
        md = moe_d[l]
        kb.op("pool", lambda e: e.iota(cntb[:], pattern=[[cap, NE]], base=0, channel_multiplier=0,
                                       allow_small_or_imprecise_dtypes=True), W=[d_cntb])
        with kb.scope():
            mn_t, d_mn = load_bc(kb, "mnorm", md["mnorm"], D)
            br_t, d_br = load_bc(kb, "b_r", md["b_r"], 36)
            wr_t, d_wr = load_w(kb, "w_r", md["w_r"], D, 36, dt=F32)
            d_wr.ro = True
            with kb.scope():
                wo_t, d_wo = load_w(kb, "w_out", wo_d, D, D)
                d_wo.ro = True
                get_lhsT = setup_lhsT()
                hp_r = kb.ring("hp", [128, D], F32, 4)
                h1_r = kb.ring("h1", [128, D], F32, 2)
                scr_r = kb.ring("scr", [128, D], F32, 2)
                ss_r = kb.ring("ss", [128, 1], F32, 2)
                hmf_r = kb.ring("hmf", [128, D], F32, 2)
                hmb_r = kb.ring("hmb", [128, D], BF16, 6)
                hmT_r = kb.ring("hmT", [128, 8, 128], F32, 2)
                sm_r = kb.ring("sm", [128, 256], F32, 4)
                ab_r = kb.ring("ab", [128, NE], BF16, 4)
                val_r = kb.ring("val", [128, 2], F32, 4)
                pstf_r = kb.ring("pstf", [128, 8, 128], F32, 1, psum=True)
                pmm_r = kb.ring("pmm", [128, 512], F32, 2, psum=True)
                prt_r = kb.ring("prt", [128, 512], F32, 4, psum=True)
                def p1_load(ti):
                    hp_t, d_hp = hp_r[ti % 4]
                    kb.dma("sp", hp_t[:], hp_ap(tiles[ti]), R=[d_h2s], W=[d_hp])
                    if hasattr(get_lhsT, "prefetch"):
                        get_lhsT.prefetch(tiles[ti])

                def front(ti):
                    t = tiles[ti]
                    b = ti % 2
                    r0, r1 = t * 128, (t + 1) * 128
                    hp_t, d_hp = hp_r[ti % 4]
                    if ti + 3 < len(tiles):
                        p1_load(ti + 3)
                    lhsT, d_lhsT = get_lhsT(t)
                    h1_t, d_h1 = h1_r[b]
                    for half in range(2):
                        p_t, d_p = pmm_r[half]
                        for c in range(8):
                            kb.op("pe", lambda e: e.matmul(p_t[:], lhsT=lhsT(c), rhs=wo_t[:, c, half * 512:(half + 1) * 512],
                                                           start=(c == 0), stop=(c == 7)), R=d_lhsT + [d_wo], W=[d_p])
                        kb.op("dve", lambda e: e.tensor_tensor(out=h1_t[:, half * 512:(half + 1) * 512], in0=p_t[:],
                                                                in1=hp_t[:, half * 512:(half + 1) * 512], op=ALU.add),
                              R=[d_p, d_hp], W=[d_h1])
                    kb.dma("sp", h1s[r0:r1, :], h1_t[:], R=[d_h1], W=[d_h1s])
                    sc_t, d_sc = scr_r[b]
                    ss_t, d_ss = ss_r[b]
                    hmf_t, d_hmf = hmf_r[b]
                    hmb_t, d_hmb = hmb_r[ti % 6]
                    rms_rstd(kb, h1_t[:], d_h1, D, sc_t[:], d_sc, ss_t[:], d_ss)
                    kb.op("dve", lambda e: e.scalar_tensor_tensor(out=hmf_t[:], in0=h1_t[:], scalar=ss_t[:, 0:1], in1=mn_t[:],
                                                                   op0=ALU.mult, op1=ALU.mult),
                          R=[d_h1, d_ss, d_mn], W=[d_hmf])
                    kb.op("pool", lambda e: e.tensor_copy(out=hmb_t[:], in_=hmf_t[:]), R=[d_hmf], W=[d_hmb])

                def front2(ti):
                    t = tiles[ti]
                    b = ti % 2
                    r0, r1 = t * 128, (t + 1) * 128
                    hmf_t, d_hmf = hmf_r[b]
                    hmb_t, d_hmb = hmb_r[ti % 6]
                    pf_t, d_pf = pstf_r[0]
                    hmT_t, d_hmT = hmT_r[b]
                    for c in range(8):
                        kb.op("pe", lambda e: e.transpose(out=pf_t[:, c, :], in_=hmf_t[:, c * 128:(c + 1) * 128],
                                                          identity=identf[:]), R=[d_hmf, d_idf], W=[d_pf])
                    kb.op("act", lambda e: e.copy(out=hmT_t[:], in_=pf_t[:]), R=[d_pf], W=[d_hmT])
                    pr_t, d_pr = prt_r[ti % 4]
                    for c in range(8):
                        kb.op("pe", lambda e: e.matmul(pr_t[:, 0:36], lhsT=hmT_t[:, c, :], rhs=wr_t[:, c, :],
                                                       start=(c == 0), stop=(c == 7)), R=[d_hmT, d_wr], W=[d_pr])
                    return dict(t=t, b=ti % 4, r0=r0, r1=r1, hmb_t=hmb_t, d_hmb=d_hmb, pr_t=pr_t, d_pr=d_pr)

                def route_ops(cx):
                    t, b, r0, r1 = cx["t"], cx["b"], cx["r0"], cx["r1"]
                    hmb_t, d_hmb, pr_t, d_pr = cx["hmb_t"], cx["d_hmb"], cx["pr_t"], cx["d_pr"]
                    ops = []
                    add = ops.append
                    sm, d_sm = sm_r[b]
                    lg = sm[:, 0:36]
                    gmax = sm[:, 36:37]
                    ngmax = sm[:, 37:38]
                    gsum = sm[:, 38:39]
                    ggate = sm[:, 39:40]
                    eg = sm[:, 40:44]
                    ohg = sm[:, 44:48]
                    esel = sm[:, 48:56]
                    top8 = sm[:, 56:64]
                    oh0 = sm[:, 64:72]
                    oh1 = sm[:, 72:80]
                    A0 = sm[:, 80:112]
                    A1 = sm[:, 112:144]
                    sbt = sm[:, 144:176]
                    tmp = sm[:, 176:208]
                    destf = sm[:, 208:210]
                    diff = sm[:, 210:211]
                    sg = sm[:, 211:212]
                    S = [d_sm]
                    dv = lambda fn, R=(), W=(): kb.op("dve", fn, R=list(R) + S, W=list(W) + S)
                    add(lambda: dv(lambda e: e.tensor_tensor(out=lg, in0=pr_t[:, 0:36], in1=br_t[:], op=ALU.add), R=[d_pr, d_br]))
                    add(lambda: dv(lambda e: e.tensor_reduce(out=gmax, in_=lg[:, 0:4], axis=AX.X, op=ALU.max)))
                    add(lambda: dv(lambda e: e.tensor_scalar(out=ngmax, in0=gmax, scalar1=-1.0, scalar2=None, op0=ALU.mult)))
                    add(lambda: kb.op("act", lambda e: e.activation(out=eg, in_=lg[:, 0:4], func=AF.Exp, bias=ngmax, scale=1.0,
                                                                    accum_out=gsum), R=S, W=S))
                    add(lambda: dv(lambda e: e.reciprocal(out=ggate, in_=gsum)))
                    add(lambda: dv(lambda e: e.tensor_scalar(out=ohg, in0=lg[:, 0:4], scalar1=gmax, scalar2=None, op0=ALU.is_equal)))
                    add(lambda: dv(lambda e: e.tensor_scalar(out=esel, in0=lg[:, 4:12], scalar1=ohg[:, 0:1], scalar2=None, op0=ALU.mult)))
                    for g in range(1, 4):
                        add(lambda g=g: dv(lambda e: e.scalar_tensor_tensor(out=esel, in0=lg[:, 4 + 8 * g:12 + 8 * g],
                                                                            scalar=ohg[:, g:g + 1], in1=esel, op0=ALU.mult, op1=ALU.add)))
                    add(lambda: dv(lambda e: e.max(out=top8, in_=esel)))
                    add(lambda: dv(lambda e: e.tensor_scalar(out=oh0, in0=esel, scalar1=top8[:, 0:1], scalar2=None, op0=ALU.is_equal)))
                    add(lambda: dv(lambda e: e.tensor_scalar(out=oh1, in0=esel, scalar1=top8[:, 1:2], scalar2=None, op0=ALU.is_equal)))
                    for (Ak, ohk) in ((A0, oh0), (A1, oh1)):
                        add(lambda Ak=Ak, ohk=ohk: dv(lambda e: e.tensor_tensor(
                            out=Ak.rearrange("p (g j) -> p g j", j=8), in0=ohg.unsqueeze(2).to_broadcast([128, 4, 8]),
                            in1=ohk.unsqueeze(1).to_broadcast([128, 4, 8]), op=ALU.mult)))
                    use_valid = has_meta and t == 0
                    val_t, d_val = val_r[b]
                    if use_valid:
                        add(lambda: kb.dma("sp", val_t[:, 0:1], valid_in[r0:r1, :], W=[d_val]))
                        add(lambda: kb.op("dve", lambda e: e.tensor_scalar(out=val_t[:, 1:2], in0=val_t[:, 0:1], scalar1=-BIGIDX,
                                                                            scalar2=BIGIDX, op0=ALU.mult, op1=ALU.add),
                                          R=[d_val], W=[d_val]))
                        for Ak in (A0, A1):
                            add(lambda Ak=Ak: dv(lambda e: e.tensor_scalar(out=Ak, in0=Ak, scalar1=val_t[:, 0:1], scalar2=None,
                                                                           op0=ALU.mult), R=[d_val]))
                    ab_t, d_ab = ab_r[b]
                    add(lambda: kb.op("dve", lambda e: e.tensor_tensor(out=ab_t[:], in0=A0, in1=A1, op=ALU.add), R=S, W=[d_ab]))
                    add(lambda: kb.op("pe", lambda e: e.matmul(pr_t[:, 64:96], lhsT=usb[:], rhs=ab_t[:], start=True, stop=True),
                                      R=[d_us, d_ab], W=[d_pr]))
                    add(lambda: kb.op("pe", lambda e: e.matmul(pr_t[:, 96:128], lhsT=onesb[:], rhs=ab_t[:], start=True, stop=True),
                                      R=[d_onesb, d_ab], W=[d_pr]))

                    def cnt_ops():
                        dv(lambda e: e.tensor_tensor(out=sbt, in0=pr_t[:, 64:96], in1=cntb[:], op=ALU.add), R=[d_pr, d_cntb])
                        kb.op("dve", lambda e: e.tensor_tensor(out=cntb[:], in0=cntb[:], in1=pr_t[:, 96:128], op=ALU.add),
                              R=[d_pr, d_cntb] + S, W=[d_cntb])
                    add(cnt_ops)
                    for k, Ak in enumerate((A0, A1)):
                        add(lambda Ak=Ak: dv(lambda e: e.tensor_tensor(out=tmp, in0=Ak, in1=sbt, op=ALU.mult)))
                        add(lambda k=k: dv(lambda e: e.tensor_reduce(out=destf[:, k:k + 1], in_=tmp, axis=AX.X, op=ALU.add)))
                    if use_valid:
                        add(lambda: dv(lambda e: e.tensor_scalar(out=destf, in0=destf, scalar1=val_t[:, 0:1], scalar2=val_t[:, 1:2],
                                                                 op0=ALU.mult, op1=ALU.add), R=[d_val]))
                    add(lambda: kb.op("dve", lambda e: e.tensor_copy(out=dest_i[:, t, :], in_=destf), R=S, W=[d_dest]))
                    add(lambda: dv(lambda e: e.tensor_tensor(out=diff, in0=top8[:, 0:1], in1=top8[:, 1:2], op=ALU.subtract)))
                    add(lambda: kb.op("act", lambda e: e.activation(out=sg, in_=diff, func=AF.Sigmoid), R=S, W=S))
                    add(lambda: kb.op("dve", lambda e: e.tensor_tensor(out=gw[:, t, 0:1], in0=sg, in1=ggate, op=ALU.mult), R=S, W=[d_gw]))
                    add(lambda: kb.op("dve", lambda e: e.tensor_tensor(out=gw[:, t, 1:2], in0=ggate, in1=gw[:, t, 0:1], op=ALU.subtract),
                                      R=S + [d_gw], W=[d_gw]))
                    for k in range(2):
                        add(lambda k=k: kb.idma(out=xpad[:, :], out_off=bass.IndirectOffsetOnAxis(ap=dest_i[:, t, k:k + 1], axis=0),
                                                in_=hmb_t[:], in_off=None, R=[d_hmb, d_dest], W=[d_xpad],
                                                bounds_check=breg, oob_is_err=False))
                    return ops

                p1_load(0)

                def emit_routes(pair):
                    lists = [route_ops(cxs[i]) for i in pair]
                    for k in range(max(len(l) for l in lists)):
                        for l in lists:
                            if k < len(l):
                                l[k]()

                cxs = {}
                ntl = len(tiles)
                for ti in range(1, min(3, ntl)):
                    p1_load(ti)
                front(0)
                pending = None
                for ti in range(ntl):
                    if ti + 1 < ntl:
                        front(ti + 1)
                    cxs[ti] = front2(ti)
                    if ti % 2 == 1 or ti == ntl - 1:
                        pair = (ti - 1, ti) if ti % 2 == 1 else (ti,)
                        if pending is not None:
                            emit_routes(pending)
                        pending = pair
                emit_routes(pending)
            with kb.scope():
                wup_r = kb.ring("wup", [128, 8, 2 * DE], BF16, 2)
                wdn_r = kb.ring("wdn", [128, 4, D], BF16, 2)
                wst_r = kb.ring("wst", [128, D], F32, 12)
                wdeps = [[Dep("w%d_%d" % (bb, cc)) for cc in range(12)] for bb in range(2)]
                xs_r = kb.ring("xs", [128, D], BF16, 2 * nst)
                xT_r = kb.ring("xT", [128, 8, cap], BF16, 2)
                sa_r = kb.ring("sa", [128, cap], F32, 2)
                aT_r = kb.ring("aT", [128, 4, cap], BF16, 2)
                yb_r = kb.ring("yb", [128, D], BF16, 2)
                pst_r = kb.ring("pst", [128, 8, 128], BF16, 2, psum=True)
                pau_r = kb.ring("pau", [128, 512], F32, 4, psum=True)
                py_r = kb.ring("py", [128, 512], F32, 2, psum=True)
                CE = ("act", "dve", "act", "dve", "act", "dve", "act", "dve", "act", "dve", "act", "dve")

                def w_dma(ex):
                    wuv = md["w_up"][ex].rearrange("(c p) n -> p c n", p=128)
                    wdv = md["w_dn"][ex].rearrange("(c p) n -> p c n", p=128)
                    for c in range(12):
                        stg, d_stg = wst_r[c]
                        kb.dma("sp", stg[:], wuv[:, c, :] if c < 8 else wdv[:, c - 8, :], W=[d_stg])

                def w_cast(ex, cs):
                    bb = ex % 2
                    for c in cs:
                        stg, d_stg = wst_r[c]
                        dst_ap = wup_r[bb][0][:, c, :] if c < 8 else wdn_r[bb][0][:, c - 8, :]
                        if CE[c] == "act":
                            kb.op("act", lambda e: e.copy(out=dst_ap, in_=stg[:]), R=[d_stg], W=[wdeps[bb][c]])
                        else:
                            kb.op("dve", lambda e: e.tensor_copy(out=dst_ap, in_=stg[:]), R=[d_stg], W=[wdeps[bb][c]])

                def x_dma(ex):
                    for st in range(nst):
                        xs_t, d_xs = xs_r[(ex % 2) * nst + st]
                        s0 = ex * cap + st * 128
                        kb.dma("sp", xs_t[:], xpad[s0:s0 + 128, :], R=[d_xpad], W=[d_xs])

                w_dma(0)
                x_dma(0)
                w_cast(0, range(12))
                yi = 0
                for ex in range(NE):
                    b = ex % 2
                    wup_t = wup_r[b][0]
                    wdn_t = wdn_r[b][0]
                    if ex + 1 < NE:
                        w_dma(ex + 1)
                        x_dma(ex + 1)
                    xT_t, d_xT = xT_r[b]
                    for st in range(nst):
                        xs_t, d_xs = xs_r[b * nst + st]
                        ps_t, d_pst = pst_r[st % 2]
                        for c in range(8):
                            kb.op("pe", lambda e: e.transpose(out=ps_t[:, c, :], in_=xs_t[:, c * 128:(c + 1) * 128],
                                                              identity=ident[:]), R=[d_xs, d_id], W=[d_pst])
                        kb.op("act" if st % 2 else "dve",
                              (lambda e: e.copy(out=xT_t[:, :, st * 128:(st + 1) * 128], in_=ps_t[:])) if st % 2 else
                              (lambda e: e.tensor_copy(out=xT_t[:, :, st * 128:(st + 1) * 128], in_=ps_t[:])),
                              R=[d_pst], W=[d_xT])
                    aT_t, d_aT = aT_r[b]
                    for fc in range(4):
                        pa, d_pa = pau_r[(2 * fc) % 4]
                        pu, d_pu = pau_r[(2 * fc + 1) % 4]
                        for c in range(8):
                            kb.op("pe", lambda e: e.matmul(pa[:, 0:cap], lhsT=wup_t[:, c, fc * 128:(fc + 1) * 128],
                                                           rhs=xT_t[:, c, :], start=(c == 0), stop=(c == 7)),
                                  R=[wdeps[b][c], d_xT], W=[d_pa])
                        for c in range(8):
                            kb.op("pe", lambda e: e.matmul(pu[:, 0:cap], lhsT=wup_t[:, c, DE + fc * 128:DE + (fc + 1) * 128],
                                                           rhs=xT_t[:, c, :], start=(c == 0), stop=(c == 7)),
                                  R=[wdeps[b][c], d_xT], W=[d_pu])
                        sa_t, d_sa = sa_r[fc % 2]
                        kb.op("act", lambda e: e.activation(out=sa_t[:], in_=pa[:, 0:cap], func=AF.Silu), R=[d_pa], W=[d_sa])
                        kb.op("dve", lambda e: e.tensor_tensor(out=aT_t[:, fc, :], in0=sa_t[:], in1=pu[:, 0:cap], op=ALU.mult),
                              R=[d_sa, d_pu], W=[d_aT])
                    if ex + 1 < NE:
                        w_cast(ex + 1, range(0, 8))
                    for st in range(nst):
                        yb_t, d_yb = yb_r[yi % 2]
                        yi += 1
                        for half in range(2):
                            py, d_py = py_r[half]
                            for fc in range(4):
                                kb.op("pe", lambda e: e.matmul(py[:], lhsT=aT_t[:, fc, st * 128:(st + 1) * 128],
                                                               rhs=wdn_t[:, fc, half * 512:(half + 1) * 512],
                                                               start=(fc == 0), stop=(fc == 3)),
                                      R=[d_aT, wdeps[b][8 + fc]], W=[d_py])
                            if half == 0:
                                kb.op("act", lambda e: e.copy(out=yb_t[:, 0:512], in_=py[:]), R=[d_py], W=[d_yb])
                            else:
                                kb.op("dve", lambda e: e.tensor_copy(out=yb_t[:, 512:1024], in_=py[:]), R=[d_py], W=[d_yb])
                        s0 = ex * cap + st * 128
                        kb.dma("sp", ypad[s0:s0 + 128, :], yb_t[:], R=[d_yb], W=[d_ypad])
                    if ex + 1 < NE:
                        w_cast(ex + 1, range(8, 12))
            with kb.scope():
                h1_r = kb.ring("h1", [128, D], F32, 4)
                y_r = kb.ring("y", [128, D], BF16, 8)
                h2_r = kb.ring("h2", [128, D], F32, 2)
                for (y_t, d_y) in y_r:
                    kb.op("pool", lambda e: e.memset(y_t[:], 0.0), W=[d_y])
                p3 = pass3_setup()
                def p3_load(ti):
                    tt = tiles[ti]
                    bb = ti % 4
                    h1_t, d_h1 = h1_r[bb]
                    kb.dma("sp", h1_t[:], h1s[tt * 128:(tt + 1) * 128, :], R=[d_h1s], W=[d_h1])
                    for k in range(2):
                        y_t, d_y = y_r[2 * bb + k]
                        kb.idma(out=y_t[:], out_off=None, in_=ypad[:, :],
                                in_off=bass.IndirectOffsetOnAxis(ap=dest_i[:, tt, k:k + 1], axis=0),
                                R=[d_ypad, d_dest], W=[d_y], bounds_check=breg, oob_is_err=False)
                for ti in range(min(3, len(tiles))):
                    p3_load(ti)
                for ti, t in enumerate(tiles):
                    b = ti % 2
                    r0, r1 = t * 128, (t + 1) * 128
                    h1_t, d_h1 = h1_r[ti % 4]
                    h2_t, d_h2 = h2_r[b]
                    y0_t, d_y0 = y_r[2 * (ti % 4)]
                    y1_t, d_y1 = y_r[2 * (ti % 4) + 1]
                    if ti + 3 < len(tiles):
                        p3_load(ti + 3)
                    kb.op("dve", lambda e: e.scalar_tensor_tensor(out=h2_t[:], in0=y0_t[:], scalar=gw[:, t, 0:1], in1=h1_t[:],
                                                                   op0=ALU.mult, op1=ALU.add), R=[d_y0, d_gw, d_h1], W=[d_h2])
                    kb.op("dve", lambda e: e.scalar_tensor_tensor(out=h2_t[:], in0=y1_t[:], scalar=gw[:, t, 1:2], in1=h2_t[:],
                                                                   op0=ALU.mult, op1=ALU.add), R=[d_y1, d_gw, d_h2], W=[d_h2])
                    pass3_tile(p3, t, h2_t, d_h2)

    def t2_setup_lhsT():
        idx2 = kb.sb("idx2", [128, 8], I32); d_idx2 = Dep("idx2")
        kb.dma("sp", idx2[:], idx2_d, W=[d_idx2])
        oTall = kb.sb("oTall", [128, 8, TX], BF16); d_oTall = Dep("oTall")
        oTm = kb.sb("oTm", [128, 8, 128], BF16); d_oTm = Dep("oTm")
        for c8 in range(8):
            kb.idma(out=oTall[:, c8, :], out_off=None, in_=RBb[:, :],
                    in_off=bass.IndirectOffsetOnAxis(ap=idx2[:, c8:c8 + 1], axis=0),
                    R=[d_RBb, d_idx2], W=[d_oTall], bounds_check=breg2, oob_is_err=False)
        kb.dma("sp", oTm[:], RBm.rearrange("(c p) t -> p c t", p=128), R=[d_RBm], W=[d_oTm])

        def get(t):
            if t == 0:
                return (lambda c: oTm[:, c, :]), [d_oTm]
            return (lambda c: oTall[:, c, (t - 1) * 128:t * 128]), [d_oTall]
        return get

    def t2_pass3_setup():
        p = {}
        p["wg"], p["d_wg"] = load_w(kb, "hwg", hwg_d, D, D)
        p["d_wg"].ro = True
        p["hn"], p["d_hn"] = load_bc(kb, "hnorm", hnorm, D)
        p["scr"] = kb.ring("scr", [128, D], F32, 2)
        p["ss"] = kb.ring("ss", [128, 1], F32, 2)
        p["xn"] = kb.ring("xn", [128, D], BF16, 2)
        p["hc"] = kb.ring("hc", [128, 8, 512], BF16, 2)
        p["gsb"] = kb.ring("gsb", [128, 8, 512], BF16, 2)
        p["pst"] = kb.ring("pst", [128, 8, 128], BF16, 2, psum=True)
        p["pg"] = kb.ring("pg", [128, 512], F32, 2, psum=True)
        return p

    def t2_pass3_tile(p, t, h2_t, d_h2):
        kb.dma("sp", h2s[t * 128:(t + 1) * 128, :], h2_t[:], R=[d_h2], W=[d_h2s])

        def hook(j, hc_t, d_hc):
            gsb, d_gsb = p["gsb"][j % 2]
            for g in range(8):
                pg, d_pg = p["pg"][g % 2]
                for c in range(8):
                    kb.op("pe", lambda e: e.matmul(pg[:], lhsT=p["wg"][:, c, g * 128:(g + 1) * 128], rhs=hc_t[:, c, :],
                                                   start=(c == 0), stop=(c == 7)), R=[p["d_wg"], d_hc], W=[d_pg])
                kb.op("act", lambda e: e.activation(out=gsb[:, g, :], in_=pg[:], func=AF.Silu), R=[d_pg], W=[d_gsb])
            kb.dma("sp", GS[j * 128:(j + 1) * 128, :], gsb[:].rearrange("p g t -> p (g t)"), R=[d_gsb], W=[d_GS])
        emit_hnT(h2_t, d_h2, p["hn"], p["d_hn"], p["scr"], p["ss"], p["xn"], p["pst"], p["hc"], t,
                 MC, d_MC, SC, d_SC, RC, d_RC, hc_hook=hook)

    tok_stage(0, list(range(nt)), True, t2_setup_lhsT, lambda t: x_in[t * 128:(t + 1) * 128, :], wo1_d,
              t2_pass3_setup, t2_pass3_tile)

    if stop == "T2":
        return dump_and_finish([("h2s", h2s, d_h2s), ("RC", RC, d_RC[nch - 1]), ("GS", GS, d_GS)])
    for i2 in range(2):
        with kb.scope():
            q_t = kb.sb("q", [128, L], BF16); d_q = Dep("q")
            lf_t = kb.sb("lf", [128, L], F32); d_lf = Dep("lf")
            v_t = kb.sb("v", [128, nb, 128], BF16); d_v = Dep("v")
            go_t, d_go = load_bc(kb, "go", go_d, 128)
            with kb.scope():
                hwq_t, d_hwq = load_w(kb, "hwq", hwq_d[i2], D, 128)
                hwf_t, d_hwf = load_w(kb, "hwf", hwf_d[i2], D, 128)
                hwv_t, d_hwv = load_w(kb, "hwv", hwv_d[i2], D, 128)
                for d in (d_hwq, d_hwf, d_hwv):
                    d.ro = True
                cols = kb.sb("cols", [128, 8], F32); d_cols = Dep("cols")
                kb.dma("sp", cols[:, 0:1], hbfc_d[i2], W=[d_cols])
                kb.dma("sp", cols[:, 1:2], l0c_d[i2], W=[d_cols])
                kb.dma("sp", cols[:, 2:3], l1c_d[i2], W=[d_cols])
                kb.op("dve", lambda e: e.tensor_tensor(out=cols[:, 3:4], in0=cols[:, 2:3], in1=cols[:, 1:2], op=ALU.subtract),
                      R=[d_cols], W=[d_cols])
                kb.op("act", lambda e: e.activation(out=cols[:, 4:5], in_=cols[:, 3:4], func=AF.Sigmoid), R=[d_cols], W=[d_cols])
                kb.op("act", lambda e: e.activation(out=cols[:, 5:6], in_=cols[:, 3:4], func=AF.Sigmoid, scale=-1.0),
                      R=[d_cols], W=[d_cols])
                d_cols.ro = True
                hgr = kb.ring("hg", [128, 8, 512], BF16, 2)
                sgr = kb.ring("sg", [128, 512], F32, 2)
                pqr = kb.ring("pq", [128, 512], F32, 2, psum=True)
                pfr = kb.ring("pf", [128, 512], F32, 2, psum=True)
                pvr = kb.ring("pv", [128, 512], F32, 2, psum=True)
                vi = 0
                def h2_load(G):
                    hg, d_hg = hgr[G % 2]
                    if G == 0:
                        kb.dma("sp", hg[:, :, 0:128], MC.rearrange("p (c t) -> p c t", c=8), R=[d_MC], W=[d_hg])
                    else:
                        rank, j = (G - 1) // nch, (G - 1) % nch
                        r0 = j * 512 + rank * 128
                        kb.dma("sp", hg[:].rearrange("p c t -> p (c t)"), RC[r0:r0 + 128, :], R=[d_RC[j]], W=[d_hg])
                h2_load(0)
                for G in range(1 + 4 * nch):
                    hg, d_hg = hgr[G % 2]
                    if G + 1 < 1 + 4 * nch:
                        h2_load(G + 1)
                    if G == 0:
                        n, tok0 = 128, 0
                    else:
                        n, tok0 = 512, 128 + (G - 1) * 512
                    pq, d_pq = pqr[G % 2]
                    pf, d_pf = pfr[G % 2]
                    sg, d_sg = sgr[G % 2]
                    for c in range(8):
                        kb.op("pe", lambda e: e.matmul(pq[:, 0:n], lhsT=hwq_t[:, c, :], rhs=hg[:, c, 0:n], start=(c == 0), stop=(c == 7)),
                              R=[d_hwq, d_hg], W=[d_pq])
                    kb.op("act", lambda e: e.activation(out=q_t[:, tok0:tok0 + n], in_=pq[:, 0:n], func=AF.Silu), R=[d_pq], W=[d_q])
                    for c in range(8):
                        kb.op("pe", lambda e: e.matmul(pf[:, 0:n], lhsT=hwf_t[:, c, :], rhs=hg[:, c, 0:n], start=(c == 0), stop=(c == 7)),
                              R=[d_hwf, d_hg], W=[d_pf])
                    kb.op("act", lambda e: e.activation(out=sg[:, 0:n], in_=pf[:, 0:n], func=AF.Sigmoid, bias=cols[:, 0:1], scale=1.0),
                          R=[d_pf, d_cols], W=[d_sg])
                    kb.op("dve", lambda e: e.tensor_scalar(out=sg[:, 0:n], in0=sg[:, 0:n], scalar1=cols[:, 5:6], scalar2=cols[:, 4:5],
                                                            op0=ALU.mult, op1=ALU.add), R=[d_sg, d_cols], W=[d_sg])
                    kb.op("act", lambda e: e.activation(out=lf_t[:, tok0:tok0 + n], in_=sg[:, 0:n], func=AF.Ln), R=[d_sg], W=[d_lf])
                    for s_ in range(n // 128):
                        pv, d_pv = pvr[vi % 2]
                        vi += 1
                        for c in range(8):
                            kb.op("pe", lambda e: e.matmul(pv[:, 0:128], lhsT=hg[:, c, s_ * 128:(s_ + 1) * 128], rhs=hwv_t[:, c, :],
                                                           start=(c == 0), stop=(c == 7)), R=[d_hwv, d_hg], W=[d_pv])
                        kb.op("dve", lambda e: e.tensor_copy(out=v_t[:, tok0 // 128 + s_, :], in_=pv[:, 0:128]), R=[d_pv], W=[d_v])
                kb.op("pool", lambda e: e.memset(lf_t[:, 0:128 - NMETA], 0.0), W=[d_lf])
            with kb.scope():
                onesf3 = kb.sb("onesf3", [128, 128], F32); d_onesf3 = Dep("onesf3")
                mf = kb.sb("mf", [128, 128], F32); d_mf = Dep("mf")
                mku = kb.sb("mku", [128, 128], U32); d_mk = Dep("mku")
                kb.op("pool", lambda e: e.memset(onesf3[:], 1.0), W=[d_onesf3])
                kb.op("pool", lambda e: e.affine_select(out=mf[:], in_=onesf3[:], pattern=[[1, 128]], compare_op=ALU.is_ge,
                                                        fill=0.0, base=0, channel_multiplier=-1), R=[d_onesf3], W=[d_mf])
                kb.op("dve", lambda e: e.tensor_copy(out=mku[:], in_=mf[:]), R=[d_mf], W=[d_mk])
                d_mk.ro = True
                d_onesf3.ro = True
                S_t = kb.sb("S", [128, 128], F32); d_S = Dep("S")
                Sb_r = kb.ring("Sb", [128, 128], BF16, 2)
                b_r = kb.ring("b", [128, 128], F32, 3)
                col_r = kb.ring("col", [128, 8], F32, 3)
                e1_r = kb.ring("e1", [128, 128], F32, 3)
                e2_r = kb.ring("e2", [128, 128], F32, 3)
                kk_r = kb.ring("kk", [128, 128], F32, 3)
                qd_r = kb.ring("qd", [128, 128], BF16, 3)
                kd_r = kb.ring("kd", [128, 128], BF16, 3)
                qb_r = kb.ring("qb", [128, 128], BF16, 3)
                ke_r = kb.ring("ke", [128, 128], BF16, 3)
                keT_r = kb.ring("keT", [128, 128], BF16, 2)
                scm_r = kb.ring("scm", [128, 128], BF16, 3)
                osq_r = kb.ring("osq", [128, 128], F32, 2)
                oss_r = kb.ring("oss", [128, 2], F32, 3)
                on_r = kb.ring("on", [128, 128], BF16, 3)
                stg_r = kb.ring("stg", [128, TX], BF16, 2)
                psc_b = [kb.ps("psc%d" % k, [128, 512], F32) for k in range(2)]
                po_b = [kb.ps("po%d" % k, [128, 512], F32) for k in range(2)]
                pu_b = [kb.ps("pu%d" % k, [128, 512], F32) for k in range(2)]
                pt_b = [kb.ps("pt%d" % k, [128, 8, 128], BF16) for k in range(2)]
                psc_r = [(psc_b[k % 2][:, 0:128], Dep("psc%d" % k)) for k in range(2)] * 2
                po_r = [(po_b[k % 2][:, 0:128], Dep("po%d" % k)) for k in range(2)] * 2
                pu_r = [(pu_b[k % 2][:, 0:128], Dep("pu%d" % k)) for k in range(2)] * 2
                ptk_r = [(pt_b[k % 2][:, 0, :], Dep("ptk%d" % k)) for k in range(2)] * 2
                pto_r = [(pt_b[k % 2][:, 1, :], Dep("pto%d" % k)) for k in range(2)] * 2
                for (t_, d_) in scm_r:
                    kb.op("pool", lambda e: e.memset(t_[:], 0.0), W=[d_])
                kb.op("pool", lambda e: e.memset(S_t[:], 0.0), W=[d_S])
                kb.op("pool", lambda e: e.memset(Sb_r[0][0][:], 0.0), W=[Sb_r[0][1]])

                def st_a(c):
                    cs = slice(c * 128, (c + 1) * 128)
                    b_t, d_b = b_r[c % 3]
                    col, d_col = col_r[c % 3]
                    e1, d_e1 = e1_r[c % 3]
                    e2, d_e2 = e2_r[c % 3]
                    kk, d_kk = kk_r[c % 3]
                    qd, d_qd = qd_r[c % 3]
                    kd, d_kd = kd_r[c % 3]
                    qb, d_qb = qb_r[c % 3]
                    ke, d_ke = ke_r[c % 3]
                    kb.op("dve", lambda e: e.tensor_tensor_scan(out=b_t[:], data0=onesf3[:], data1=lf_t[:, cs], initial=0.0,
                                                                 op0=ALU.mult, op1=ALU.add), R=[d_lf, d_onesf3], W=[d_b])
                    kb.op("dve", lambda e: e.tensor_copy(out=col[:, 0:1], in_=b_t[:, 63:64]), R=[d_b], W=[d_col])
                    kb.op("dve", lambda e: e.tensor_tensor(out=col[:, 1:2], in0=b_t[:, 127:128], in1=b_t[:, 63:64],
                                                            op=ALU.subtract), R=[d_b], W=[d_col])
                    kb.op("dve", lambda e: e.tensor_copy(out=col[:, 2:3], in_=b_t[:, 127:128]), R=[d_b], W=[d_col])
                    kb.op("dve", lambda e: e.tensor_scalar(out=col[:, 3:4], in0=b_t[:, 63:64], scalar1=-1.0, scalar2=None,
                                                            op0=ALU.mult), R=[d_b], W=[d_col])
                    kb.op("act", lambda e: e.activation(out=col[:, 4:7], in_=col[:, 0:3], func=AF.Exp), R=[d_col], W=[d_col])
                    kb.op("act", lambda e: e.activation(out=e1[:], in_=b_t[:], func=AF.Exp, bias=col[:, 3:4], scale=1.0),
                          R=[d_b, d_col], W=[d_e1])
                    kb.op("act", lambda e: e.activation(out=e2[:], in_=b_t[:], func=AF.Exp, bias=col[:, 0:1], scale=-1.0),
                          R=[d_b, d_col], W=[d_e2])
                    kb.op("act", lambda e: e.activation(out=kk[:], in_=lf_t[:, cs], func=AF.Exp), R=[d_lf], W=[d_kk])
                    kb.op("pool", lambda e: e.tensor_scalar(out=kk[:], in0=kk[:], scalar1=-1.0, scalar2=1.0, op0=ALU.mult,
                                                             op1=ALU.add), R=[d_kk], W=[d_kk])
                    kb.op("dve", lambda e: e.tensor_tensor(out=qd[:], in0=q_t[:, cs], in1=e1[:], op=ALU.mult),
                          R=[d_q, d_e1], W=[d_qd])
                    kb.op("dve", lambda e: e.tensor_tensor(out=kd[:], in0=kk[:], in1=e2[:], op=ALU.mult),
                          R=[d_kk, d_e2], W=[d_kd])
                    kb.op("dve", lambda e: e.scalar_tensor_tensor(out=qb[:], in0=q_t[:, cs], scalar=col[:, 4:5], in1=e1[:],
                                                                   op0=ALU.mult, op1=ALU.mult), R=[d_q, d_col, d_e1], W=[d_qb])
                    kb.op("dve", lambda e: e.scalar_tensor_tensor(out=ke[:], in0=kk[:], scalar=col[:, 5:6], in1=e2[:],
                                                                   op0=ALU.mult, op1=ALU.mult), R=[d_kk, d_col, d_e2], W=[d_ke])

                def st_f(c):
                    qd, d_qd = qd_r[c % 3]
                    kd, d_kd = kd_r[c % 3]
                    ke, d_ke = ke_r[c % 3]
                    keT, d_keT = keT_r[c % 2]
                    scm, d_scm = scm_r[c % 3]
                    psc, d_psc = psc_r[c % 4]
                    ptk, d_ptk = ptk_r[c % 4]
                    pu, d_pu = pu_r[c % 4]
                    kb.op("pe", lambda e: e.matmul(psc, lhsT=kd[:], rhs=qd[:], start=True, stop=True), R=[d_kd, d_qd], W=[d_psc])
                    kb.op("dve", lambda e: e.copy_predicated(out=scm[:], mask=mku[:], data=psc), R=[d_psc, d_mk], W=[d_scm])
                    kb.op("pe", lambda e: e.transpose(out=ptk, in_=ke[:], identity=ident[:]), R=[d_ke, d_id], W=[d_ptk])
                    kb.op("act", lambda e: e.copy(out=keT[:], in_=ptk), R=[d_ptk], W=[d_keT])
                    kb.op("pe", lambda e: e.matmul(pu, lhsT=keT[:], rhs=v_t[:, c, :], start=True, stop=True), R=[d_keT, d_v], W=[d_pu])

                def st_k(c):
                    col, d_col = col_r[c % 3]
                    qb, d_qb = qb_r[c % 3]
                    scm, d_scm = scm_r[c % 3]
                    po, d_po = po_r[c % 4]
                    pu, d_pu = pu_r[c % 4]
                    Sb, d_Sb = Sb_r[c % 2]
                    kb.op("pe", lambda e: e.matmul(po, lhsT=scm[:], rhs=v_t[:, c, :], start=True, stop=False), R=[d_scm, d_v], W=[d_po])
                    kb.op("pe", lambda e: e.matmul(po, lhsT=qb[:], rhs=Sb[:], start=False, stop=True), R=[d_qb, d_Sb], W=[d_po])
                    kb.op("dve", lambda e: e.scalar_tensor_tensor(out=S_t[:], in0=S_t[:], scalar=col[:, 6:7], in1=pu,
                                                                   op0=ALU.mult, op1=ALU.add), R=[d_S, d_col, d_pu], W=[d_S])
                    Sb2, d_Sb2 = Sb_r[(c + 1) % 2]
                    kb.op("pool", lambda e: e.tensor_copy(out=Sb2[:], in_=S_t[:]), R=[d_S], W=[d_Sb2])

                def st_n(c):
                    po, d_po = po_r[c % 4]
                    osq, d_osq = osq_r[c % 2]
                    oss, d_oss = oss_r[c % 3]
                    on_t, d_on = on_r[c % 3]
                    kb.op("act", lambda e: e.activation(out=osq[:], in_=po, func=AF.Square, accum_out=oss[:, 0:1]),
                          R=[d_po], W=[d_osq, d_oss])
                    kb.op("act", lambda e: e.activation(out=oss[:, 1:2], in_=oss[:, 0:1], func=AF.Ln, scale=1.0 / 128, bias=EPS),
                          R=[d_oss], W=[d_oss])
                    kb.op("act", lambda e: e.activation(out=oss[:, 1:2], in_=oss[:, 1:2], func=AF.Exp, scale=-0.5),
                          R=[d_oss], W=[d_oss])
                    kb.op("dve", lambda e: e.scalar_tensor_tensor(out=on_t[:], in0=po, scalar=oss[:, 1:2], in1=go_t[:],
                                                                   op0=ALU.mult, op1=ALU.mult), R=[d_po, d_oss, d_go], W=[d_on])

                def st_t(c):
                    on_t, d_on = on_r[c % 3]
                    pto, d_pto = pto_r[c % 4]
                    xb = c - 1
                    qq, tb = xb // ntx, xb % ntx
                    stg, d_stg = stg_r[qq % 2]
                    kb.op("pe", lambda e: e.transpose(out=pto, in_=on_t[:], identity=ident[:]), R=[d_on, d_id], W=[d_pto])
                    kb.op("act", lambda e: e.copy(out=stg[:, tb * 128:(tb + 1) * 128], in_=pto), R=[d_pto], W=[d_stg])
                    if tb == ntx - 1:
                        cidx = i2 * 4 + qq
                        kb.dma("sp", SD[cidx * 128:(cidx + 1) * 128, :], stg[:], R=[d_stg], W=[d_SD, d_SDc[cidx]])
                        kb.cc(SD[cidx * 128:(cidx + 1) * 128, :], RD[cidx * 512:(cidx + 1) * 512, :], GROUPS, R=[d_SDc[cidx]], W=[d_RD])

                st_a(0)
                if nb > 1:
                    st_a(1)
                st_f(0)
                for i in range(nb + 2):
                    if i + 2 < nb:
                        st_a(i + 2)
                    if i + 1 < nb:
                        st_f(i + 1)
                    if i < nb:
                        st_k(i)
                    if 1 <= i - 1 < nb:
                        st_n(i - 1)
                    if 1 <= i - 2 < nb:
                        st_t(i - 2)

    if stop == "H2":
        return dump_and_finish([("RD", RD, d_RD)])
    def t3_setup_lhsT():
        idx3 = kb.sb("idx3", [128, 8], I32); d_idx3 = Dep("idx3")
        kb.dma("sp", idx3[:], idx3_d, W=[d_idx3])
        onall = kb.sb("onall", [128, 8, TX], BF16); d_onall = Dep("onall")
        for c8 in range(8):
            kb.idma(out=onall[:, c8, :], out_off=None, in_=RD[:, :],
                    in_off=bass.IndirectOffsetOnAxis(ap=idx3[:, c8:c8 + 1], axis=0),
                    R=[d_RD, d_idx3], W=[d_onall], bounds_check=breg2, oob_is_err=False)
        gsr = kb.ring("gsT", [128, 8, 512], BF16, 2)
        ogr = kb.ring("og", [128, 8, 128], BF16, 2)

        def prefetch(t):
            if (t - 1) % 4 == 0:
                jj = (t - 1) // 4
                gs_t, d_gs = gsr[jj % 2]
                kb.dma("sp", gs_t[:].rearrange("p g t -> p (g t)"), GS[jj * 128:(jj + 1) * 128, :], R=[d_GS], W=[d_gs])

        def get(t):
            jj, ss_ = (t - 1) // 4, (t - 1) % 4
            gs_t, d_gs = gsr[jj % 2]
            og_t, d_og = ogr[t % 2]
            kb.op("dve", lambda e: e.tensor_tensor(out=og_t[:], in0=onall[:, :, (t - 1) * 128:t * 128],
                                                    in1=gs_t[:, :, ss_ * 128:(ss_ + 1) * 128], op=ALU.mult),
                  R=[d_onall, d_gs], W=[d_og])
            return (lambda c: og_t[:, c, :]), [d_og]
        get.prefetch = prefetch
        return get

    def t3_pass3_tile(p, t, h2_t, d_h2):
        kb.dma("sp", out_d[(t - 1) * 128:t * 128, :], h2_t[:], R=[d_h2])

    tok_stage(1, list(range(1, nt)), False, t3_setup_lhsT, lambda t: h2s[t * 128:(t + 1) * 128, :], wo2_d,
              lambda: None, t3_pass3_tile)
    kb.finish()
    return nc


def fused_in_maps(inp, ntx):
    f32 = np.float32
    TX = ntx * 128
    nt = ntx + 1
    x = inp["x"]
    metatile = np.zeros((128, D), f32)
    metatile[128 - NMETA:] = inp["meta_tokens"]
    valid = np.ones((nt * 128, 1), f32)
    valid[0:128 - NMETA] = 0.0
    fw = inp["fox_w_in"][0]
    hw = inp["hg_w_in"][0]
    common = {
        "valid_in": valid, "fnorm": inp["fox_norm"][0][None],
        "gqc": np.tile(inp["fox_q_norm"][0], 2)[:, None].astype(f32), "gkc": np.tile(inp["fox_k_norm"][0], 2)[:, None].astype(f32),
        "wo1": inp["fox_w_out"][0], "wo2": inp["hg_w_out"][0], "hnorm": inp["hg_norm"][0][None],
        "hwg": np.ascontiguousarray(hw[:, 3 * D:4 * D]), "go": inp["hg_o_norm"][0][None],
    }
    for l in range(2):
        common["mnorm%d" % l] = inp["moe_norm"][l][None]
        common["w_r%d" % l] = np.ascontiguousarray(np.concatenate([inp["moe_w_grp"][l], inp["moe_w_rt"][l]], 1))
        common["b_r%d" % l] = np.concatenate([inp["moe_b_grp"][l], inp["moe_b_rt"][l]])[None]
        common["w_up%d" % l] = inp["moe_w_up"][l]
        common["w_dn%d" % l] = inp["moe_w_down"][l]
    maps = []
    p = np.arange(128)
    for c in range(NCORES):
        b, r = c // 4, c % 4
        m = dict(common)
        m["x_in"] = np.concatenate([metatile, x[b, r * TX:(r + 1) * TX]], 0)
        hc = slice(4 * r * FD, (4 * r + 4) * FD)
        m["wq"] = np.ascontiguousarray(fw[:, 0:D][:, hc])
        m["wk"] = np.ascontiguousarray(fw[:, D:2 * D][:, hc])
        m["wv"] = np.ascontiguousarray(fw[:, 2 * D:3 * D][:, hc])
        m["wf"] = np.ascontiguousarray(fw[:, 3 * D + 4 * r:3 * D + 4 * r + 4])
        bf4 = inp["fox_b_f"][0][4 * r:4 * r + 4]
        m["bf4r"] = bf4[None].astype(f32)
        m["bf4c"] = bf4[:, None].astype(f32)
        idx2 = np.zeros((128, 8), np.int32)
        idx3 = np.zeros((128, 8), np.int32)
        for c8 in range(8):
            rank, sub = c8 // 2, c8 % 2
            idx2[:, c8] = (sub * 4 + r) * 512 + rank * 128 + p
            idx3[:, c8] = (sub * 4 + r) * 512 + rank * 128 + p
        m["idx2"] = idx2
        m["idx3"] = idx3
        hs = [slice((2 * r + i) * 128, (2 * r + i + 1) * 128) for i in range(2)]
        m["hwq"] = np.ascontiguousarray(np.stack([hw[:, 0:D][:, s] for s in hs]))
        m["hwf"] = np.ascontiguousarray(np.stack([hw[:, D:2 * D][:, s] for s in hs]))
        m["hwv"] = np.ascontiguousarray(np.stack([hw[:, 2 * D:3 * D][:, s] for s in hs]))
        m["hbfc"] = np.stack([inp["hg_b_f"][0][s][:, None] for s in hs]).astype(f32)
        m["l0c"] = np.stack([inp["hg_lb_logits"][0][s][:, None] for s in hs]).astype(f32)
        m["l1c"] = np.stack([inp["hg_lb_logits"][1][s][:, None] for s in hs]).astype(f32)
        maps.append(m)
    return maps


def kernel_fused(inp, ntx=NTX, cap=CAP, stop=None):
    inp = {k: np.asarray(v) for k, v in inp.items()}
    nc = _prog(("F", ntx, cap, stop), lambda: build_fused(ntx, cap, stop))
    maps = fused_in_maps(inp, ntx)
    if stop is not None:
        drop = []
        for l in range(2):
            if stop in ("T1", "H1p", "H1a") or (stop in ("T2", "H2") and l == 1):
                drop += [k + str(l) for k in ("mnorm", "w_r", "b_r", "w_up", "w_dn")]
        maps = [{k: v for k, v in m.items() if k not in drop} for m in maps]
    res = _run(nc, maps)
    if stop is not None:
        return res
    TX = ntx * 128
    out = np.zeros((2, 4 * TX, D), np.float32)
    for c in range(NCORES):
        out[c // 4, (c % 4) * TX:(c % 4 + 1) * TX] = np.asarray(res[c]["out"])
    return out


def kernel(**inp):
    return kernel_fused(inp, NTX, CAP)
```
